# Optimizing a Trainium2 kernel written in Bass

```python
import math
import functools
import jax
import jax.numpy as jnp
from jax import lax
import numpy as np

D_MODEL = 1024
BATCH = 2
SEQ = 8192
DEPTH = 2

GRID_W = 64
CTX_LEN = 256

HEAD_DIM = 64
GROUP_HEADS = 4
GROUP_WIDTH = GROUP_HEADS * HEAD_DIM
N_GROUPS = 4
MIX_WIDTH = N_GROUPS * GROUP_WIDTH

DA_HEADS = 4
DA_QK_DIM = 32
DA_V_DIM = 64
DA_BLOCK = 128

SWA_HEADS = 4
SWA_KV_HEADS = 2
SWA_DIM = 64
SWA_WINDOW = 128
SWA_BLOCK = SWA_WINDOW

NA_HEADS = 4
NA_DIM = 64
NA_WIN_ROWS = 8
NA_WIN_COLS = 16
NA_COL_BLOCK = 16
NA_BAND_COLS = 2 * NA_WIN_COLS

RET_HEADS = 4
RET_QK_DIM = 64
RET_V_DIM = 64
RET_CHUNK = 128

DA_COLS = DA_HEADS * (4 * DA_QK_DIM + DA_V_DIM)
SWA_COLS = (SWA_HEADS + 2 * SWA_KV_HEADS) * SWA_DIM
NA_COLS = 3 * NA_HEADS * NA_DIM
RET_COLS = RET_HEADS * (2 * RET_QK_DIM + 2 * RET_V_DIM)
IN_WIDTH = DA_COLS + SWA_COLS + NA_COLS + RET_COLS
IN_CUTS = (DA_COLS, DA_COLS + SWA_COLS, DA_COLS + SWA_COLS + NA_COLS)

ROPE_BASE = 10000.0
FFN_DIM = 2816
N_EXPERTS = 8
TOP_K = 2
EXPERT_DIM = 3584
NORM_EPS = 1e-6
NEG_INF = -1e30

kernel_name = 'hybrid_dit_parallel_heads_block'


def _rmsnorm(x, g=None):
    xf = x.astype(jnp.float32)
    y = xf * lax.rsqrt(jnp.mean(xf * xf, axis=-1, keepdims=True) + NORM_EPS)
    if g is not None:
        y = y * g.astype(jnp.float32)
    return y.astype(x.dtype)


def _rope_axis(x, pos):
    dh = x.shape[-1]
    half = dh // 2
    inv = ROPE_BASE ** (-jnp.arange(half, dtype=jnp.float32) * 2.0 / dh)
    ang = pos[:, None] * inv[None, :]
    shape = (pos.shape[0],) + (1,) * (x.ndim - 3) + (half,)
    cos = jnp.cos(ang).reshape(shape).astype(x.dtype)
    sin = jnp.sin(ang).reshape(shape).astype(x.dtype)
    x1, x2 = x[..., :half], x[..., half:]
    return jnp.concatenate([x1 * cos - x2 * sin, x1 * sin + x2 * cos], axis=-1)


def _rope2d(x, row, col):
    h = x.shape[-1] // 2
    return jnp.concatenate([_rope_axis(x[..., :h], row), _rope_axis(x[..., h:], col)], axis=-1)


def _diff_attention(p, pc, row, col, lq1, lk1, lq2, lk2, subln_g, lambda_init, with_ctx):
    B, S, _ = p.shape
    nq = DA_HEADS * 2 * DA_QK_DIM

    def split(t):
        n = t.shape[1]
        q = t[..., :nq].reshape(B, n, DA_HEADS, 2, DA_QK_DIM)
        k = t[..., nq:2 * nq].reshape(B, n, DA_HEADS, 2, DA_QK_DIM)
        v = t[..., 2 * nq:].reshape(B, n, DA_HEADS, DA_V_DIM)
        return q, k, v

    q, k, v = split(p)
    q = _rope2d(q, row, col)
    k = _rope2d(k, row, col)
    qc, kc, vc = split(pc)
    lam = (jnp.exp(jnp.sum(lq1 * lk1)) - jnp.exp(jnp.sum(lq2 * lk2))).astype(jnp.float32) + lambda_init
    scale = DA_QK_DIM ** -0.5

    def attend(qb, keys, vals):
        s = jnp.einsum('bqhtd,bkhtd->bhtqk', qb, keys) * scale
        pr = jax.nn.softmax(s, axis=-1)
        a = pr[:, :, 0] - lam * pr[:, :, 1]
        return jnp.einsum('bhqk,bkhd->bqhd', a, vals)

    k_all = jnp.concatenate([kc, k], axis=1)
    v_all = jnp.concatenate([vc, v], axis=1)
    nb = S // DA_BLOCK
    q_blocks = jnp.swapaxes(q.reshape(B, nb, DA_BLOCK, DA_HEADS, 2, DA_QK_DIM), 0, 1)
    o = lax.map(lambda qb: attend(qb, k_all, v_all), q_blocks)
    o = jnp.swapaxes(o, 0, 1).reshape(B, S, DA_HEADS, DA_V_DIM)

    def head_norm(t):
        return (_rmsnorm(t, subln_g) * (1.0 - lambda_init)).reshape(t.shape[0], t.shape[1], DA_HEADS * DA_V_DIM)

    y = head_norm(o)
    yc = head_norm(attend(qc, kc, vc)) if with_ctx else None
    return y, yc


def _sink_softmax(s, sink):
    m = jnp.maximum(jnp.max(s, axis=-1, keepdims=True), sink)
    e = jnp.exp(s - m)
    return e / (jnp.sum(e, axis=-1, keepdims=True) + jnp.exp(sink - m))


def _window_gqa(p, pc, row, col, sink, with_ctx):
    B, S, _ = p.shape
    G = SWA_HEADS // SWA_KV_HEADS
    nq = SWA_HEADS * SWA_DIM
    nk = SWA_KV_HEADS * SWA_DIM

    def split(t):
        n = t.shape[1]
        q = t[..., :nq].reshape(B, n, SWA_KV_HEADS, G, SWA_DIM)
        k = t[..., nq:nq + nk].reshape(B, n, SWA_KV_HEADS, SWA_DIM)
        v = t[..., nq + nk:].reshape(B, n, SWA_KV_HEADS, SWA_DIM)
        return q, k, v

    q, k, v = split(p)
    q = _rope2d(q, row, col)
    k = _rope2d(k, row, col)
    qc, kc, vc = split(pc)
    sink = sink.astype(jnp.float32).reshape(SWA_KV_HEADS, G, 1, 1)
    scale = SWA_DIM ** -0.5
    nb = S // SWA_BLOCK
    kq = 3 * SWA_BLOCK

    def band(t):
        tp = jnp.pad(t, ((0, 0), (SWA_WINDOW, SWA_WINDOW), (0, 0), (0, 0)))
        tp = tp.reshape(B, nb + 2, SWA_BLOCK, SWA_KV_HEADS, SWA_DIM)
        return jnp.concatenate([tp[:, :-2], tp[:, 1:-1], tp[:, 2:]], axis=2)

    kb, vb = band(k), band(v)
    blk = jnp.arange(nb)[:, None]
    qpos = blk * SWA_BLOCK + jnp.arange(SWA_BLOCK)[None, :]
    kpos = blk * SWA_BLOCK - SWA_WINDOW + jnp.arange(kq)[None, :]
    valid = ((kpos[:, None, :] >= 0) & (kpos[:, None, :] < S)
             & (jnp.abs(qpos[:, :, None] - kpos[:, None, :]) <= SWA_WINDOW))
    qg = q.reshape(B, nb, SWA_BLOCK, SWA_KV_HEADS, G, SWA_DIM)
    s_loc = jnp.einsum('bnqhgd,bnkhd->bnhgqk', qg, kb) * scale
    s_loc = jnp.where(valid[None, :, None, None], s_loc, NEG_INF)
    s_ctx = jnp.einsum('bnqhgd,bkhd->bnhgqk', qg, kc) * scale
    pr = _sink_softmax(jnp.concatenate([s_loc, s_ctx], axis=-1), sink)
    o = (jnp.einsum('bnhgqk,bnkhd->bnqhgd', pr[..., :kq], vb)
         + jnp.einsum('bnhgqk,bkhd->bnqhgd', pr[..., kq:], vc))
    y = o.reshape(B, S, nq)
    yc = None
    if with_ctx:
        sc = jnp.einsum('bqhgd,bkhd->bhgqk', qc, kc) * scale
        yc = jnp.einsum('bhgqk,bkhd->bqhgd', _sink_softmax(sc, sink), vc).reshape(B, qc.shape[1], nq)
    return y, yc


def _neighborhood_attention(p, pc, rpb, with_ctx):
    B, S, _ = p.shape
    rows = S // GRID_W
    wr = min(NA_WIN_ROWS, rows)
    ncb = GRID_W // NA_COL_BLOCK
    nd = NA_HEADS * NA_DIM

    def split(t):
        n = t.shape[1]
        return [t[..., i * nd:(i + 1) * nd].reshape(B, n, NA_HEADS, NA_DIM) for i in range(3)]

    q, k, v = split(p)
    qc, kc, vc = split(pc)
    r = jnp.arange(rows)
    row_idx = jnp.clip(r - wr // 2, 0, rows - wr)[:, None] + jnp.arange(wr)[None, :]
    band_idx = (jnp.clip(jnp.arange(ncb) * NA_COL_BLOCK - NA_WIN_COLS // 2, 0, GRID_W - NA_BAND_COLS)[:, None]
                + jnp.arange(NA_BAND_COLS)[None, :])
    nkeys = wr * NA_BAND_COLS
    tok_idx = (row_idx[:, None, :, None] * GRID_W + band_idx[None, :, None, :]).reshape(rows, ncb, nkeys)
    kb = jnp.take(k, tok_idx, axis=1)
    vb = jnp.take(v, tok_idx, axis=1)
    q_col = jnp.arange(ncb)[:, None] * NA_COL_BLOCK + jnp.arange(NA_COL_BLOCK)[None, :]
    col_start = jnp.clip(q_col - NA_WIN_COLS // 2, 0, GRID_W - NA_WIN_COLS)
    col_ok = ((band_idx[:, None, :] >= col_start[:, :, None])
              & (band_idx[:, None, :] < col_start[:, :, None] + NA_WIN_COLS))
    mask = jnp.broadcast_to(col_ok[:, :, None, :], (ncb, NA_COL_BLOCK, wr, NA_BAND_COLS)).reshape(ncb, NA_COL_BLOCK, nkeys)
    d_row = row_idx - r[:, None] + (NA_WIN_ROWS - 1)
    d_col = jnp.clip(band_idx[:, None, :] - q_col[:, :, None], 1 - NA_WIN_COLS, NA_WIN_COLS - 1) + (NA_WIN_COLS - 1)
    bias = rpb[:, d_row[:, None, None, :, None], d_col[None, :, :, None, :]]
    bias = jnp.transpose(bias.reshape(NA_HEADS, rows, ncb, NA_COL_BLOCK, nkeys), (1, 2, 0, 3, 4)).astype(jnp.float32)
    scale = NA_DIM ** -0.5
    qg = q.reshape(B, rows, ncb, NA_COL_BLOCK, NA_HEADS, NA_DIM)
    s_loc = jnp.einsum('brjqhd,brjkhd->brjhqk', qg, kb) * scale + bias
    s_loc = jnp.where(mask[None, None, :, None], s_loc, NEG_INF)
    s_ctx = jnp.einsum('brjqhd,bkhd->brjhqk', qg, kc) * scale
    pr = jax.nn.softmax(jnp.concatenate([s_loc, s_ctx], axis=-1), axis=-1)
    o = (jnp.einsum('brjhqk,brjkhd->brjqhd', pr[..., :nkeys], vb)
         + jnp.einsum('brjhqk,bkhd->brjqhd', pr[..., nkeys:], vc))
    y = o.reshape(B, S, nd)
    yc = None
    if with_ctx:
        sc = jax.nn.softmax(jnp.einsum('bqhd,bkhd->bhqk', qc, kc) * scale, axis=-1)
        yc = jnp.einsum('bhqk,bkhd->bqhd', sc, vc).reshape(B, qc.shape[1], nd)
    return y, yc


def _ret_scan(q, k, v, log_g, s0, diag):
    B, N, H, dk = q.shape
    dv = v.shape[-1]
    C = RET_CHUNK
    nc = N // C
    qc = q.reshape(B, nc, C, H, dk)
    kc = k.reshape(B, nc, C, H, dk)
    vc = v.reshape(B, nc, C, H, dv)
    i = jnp.arange(C, dtype=jnp.float32)
    dist = i[:, None] - i[None, :]
    keep = (dist >= 0) if diag else (dist > 0)
    decay = jnp.where(keep[None], jnp.exp(jnp.where(keep, dist, 0.0)[None] * log_g[:, None, None]), 0.0)
    att = jnp.einsum('bnihd,bnjhd->bnhij', qc, kc) * decay
    intra = jnp.einsum('bnhij,bnjhe->bnihe', att, vc)
    zeta = jnp.exp((C - 1 - i)[None, :] * log_g[:, None])
    u = jnp.einsum('bnjhd,bnjhe,hj->nbhde', kc, vc, zeta)
    g_chunk = jnp.exp(C * log_g)[None, :, None, None]

    def step(s, u_n):
        return g_chunk * s + u_n, s

    s_last, s_prev = lax.scan(step, s0, u)
    xi = jnp.exp((i + 1)[None, :] * log_g[:, None])
    cross = jnp.einsum('bnihd,hi,nbhde->bnihe', qc, xi, s_prev)
    return (intra + cross).reshape(B, N, H, dv), s_last


def _ret_state(k, v, log_g):
    n = k.shape[1]
    w = jnp.exp((n - 1 - jnp.arange(n, dtype=jnp.float32))[None, :] * log_g[:, None])
    return jnp.einsum('bjhd,bjhe,hj->bhde', k, v, w)


def _retention(p, pc, row, col, gam_f, gam_b, with_ctx):
    B, S, _ = p.shape
    nqk = RET_HEADS * RET_QK_DIM
    nv = RET_HEADS * RET_V_DIM

    def split(t):
        n = t.shape[1]
        q = t[..., :nqk].reshape(B, n, RET_HEADS, RET_QK_DIM)
        k = t[..., nqk:2 * nqk].reshape(B, n, RET_HEADS, RET_QK_DIM) * (RET_QK_DIM ** -0.5)
        v = t[..., 2 * nqk:2 * nqk + nv].reshape(B, n, RET_HEADS, RET_V_DIM)
        g = t[..., 2 * nqk + nv:]
        return q, k, v, g

    q, k, v, g = split(p)
    q = _rope2d(q, row, col)
    k = _rope2d(k, row, col)
    qc, kc, vc, gc = split(pc)
    log_f = jax.nn.log_sigmoid(gam_f.astype(jnp.float32))
    log_b = jax.nn.log_sigmoid(gam_b.astype(jnp.float32))
    flip = lambda t: jnp.flip(t, axis=1)
    if with_ctx:
        s0 = jnp.zeros((B, RET_HEADS, RET_QK_DIM, RET_V_DIM), jnp.float32)
        oc_f, s_f = _ret_scan(qc, kc, vc, log_f, s0, True)
        oc_b, s_b = _ret_scan(flip(qc), flip(kc), flip(vc), log_b, s0, False)
    else:
        s_f = _ret_state(kc, vc, log_f)
        s_b = _ret_state(flip(kc), flip(vc), log_b)
    o_f, _ = _ret_scan(q, k, v, log_f, s_f, True)
    o_b, _ = _ret_scan(flip(q), flip(k), flip(v), log_b, s_b, False)

    def gate_out(o, gate):
        return (_rmsnorm(o) * jax.nn.silu(gate).reshape(o.shape)).reshape(o.shape[0], o.shape[1], nv)

    y = gate_out(o_f + flip(o_b), g)
    yc = gate_out(oc_f + flip(oc_b), gc) if with_ctx else None
    return y, yc


def _swiglu_ffn(h, w_gate, w_up, w_down):
    return (jax.nn.silu(h @ w_gate) * (h @ w_up)) @ w_down


def _moe_ffn(h, router, w_gate, w_up, w_down):
    logits = jnp.einsum('bsd,de->bse', h, router).astype(jnp.float32)
    top_v, top_i = lax.top_k(logits, TOP_K)
    w = jax.nn.softmax(top_v, axis=-1)
    gates = jnp.sum(jax.nn.one_hot(top_i, N_EXPERTS, dtype=jnp.float32) * w[..., None], axis=-2)
    y = jnp.zeros_like(h)
    for e in range(N_EXPERTS):
        y = y + gates[..., e:e + 1].astype(h.dtype) * _swiglu_ffn(h, w_gate[e], w_up[e], w_down[e])
    return y


def _layer(x, xc, c, c_ctx, row, col, w_mod, b_mod, g_attn_pre, g_attn_post, g_ffn_pre, g_ffn_post,
           w_in, w_out, lq1, lk1, lq2, lk2, subln_g, sink, rpb, gam_f, gam_b, ffn, lambda_init, with_ctx):
    mod = jax.nn.silu(c) @ w_mod + b_mod
    mod_c = jax.nn.silu(c_ctx) @ w_mod + b_mod
    sh1, sc1, gt1, sh2, sc2, gt2 = jnp.split(mod[:, None, :], 6, axis=-1)
    csh1, csc1, cgt1, csh2, csc2, cgt2 = jnp.split(mod_c, 6, axis=-1)

    h = _rmsnorm(x, g_attn_pre) * (1 + sc1) + sh1
    hc = _rmsnorm(xc, g_attn_pre) * (1 + csc1) + csh1
    p = (h @ w_in).astype(jnp.float32)
    pc = (hc @ w_in).astype(jnp.float32)
    pa, pb, pn, pr = jnp.split(p, IN_CUTS, axis=-1)
    pca, pcb, pcn, pcr = jnp.split(pc, IN_CUTS, axis=-1)
    ya, yca = _diff_attention(pa, pca, row, col, lq1, lk1, lq2, lk2, subln_g, lambda_init, with_ctx)
    yb, ycb = _window_gqa(pb, pcb, row, col, sink, with_ctx)
    yn, ycn = _neighborhood_attention(pn, pcn, rpb, with_ctx)
    yr, ycr = _retention(pr, pcr, row, col, gam_f, gam_b, with_ctx)
    y = jnp.concatenate([ya, yb, yn, yr], axis=-1).astype(x.dtype) @ w_out
    x = x + gt1 * _rmsnorm(y, g_attn_post)

    h = _rmsnorm(x, g_ffn_pre) * (1 + sc2) + sh2
    x = x + gt2 * _rmsnorm(ffn(h), g_ffn_post)

    if with_ctx:
        yc = jnp.concatenate([yca, ycb, ycn, ycr], axis=-1).astype(xc.dtype) @ w_out
        xc = xc + cgt1 * _rmsnorm(yc, g_attn_post)
        hc = _rmsnorm(xc, g_ffn_pre) * (1 + csc2) + csh2
        xc = xc + cgt2 * _rmsnorm(ffn(hc), g_ffn_post)
    return x, xc


def setup_inputs(seed: int = 0) -> dict:
    key = jax.random.key(seed)
    ks = jax.random.split(key, 32)
    D = D_MODEL
    n_dense = (DEPTH + 1) // 2
    n_moe = DEPTH // 2

    def nrm(k, shape, s):
        return s * jax.random.normal(k, shape, jnp.float32)

    gam = 1.0 - 2.0 ** (-5.0 - jnp.arange(RET_HEADS, dtype=jnp.float32))
    gam_logit = jnp.log(gam) - jnp.log1p(-gam)
    return {
        'x': nrm(ks[0], (BATCH, SEQ, D), 1.0),
        'c': nrm(ks[1], (BATCH, D), 1.0),
        'ctx': nrm(ks[2], (BATCH, CTX_LEN, D), 1.0),
        'c_ctx': nrm(ks[3], (D,), 1.0),
        'w_mod': nrm(ks[4], (DEPTH, D, 6 * D), 0.5 * D ** -0.5),
        'b_mod': nrm(ks[5], (DEPTH, 6 * D), 0.02),
        'g_attn_pre': 1.0 + nrm(ks[6], (DEPTH, D), 0.02),
        'g_attn_post': 1.0 + nrm(ks[7], (DEPTH, D), 0.02),
        'g_ffn_pre': 1.0 + nrm(ks[8], (DEPTH, D), 0.02),
        'g_ffn_post': 1.0 + nrm(ks[9], (DEPTH, D), 0.02),
        'w_in': nrm(ks[10], (DEPTH, D, IN_WIDTH), D ** -0.5),
        'w_out': nrm(ks[11], (DEPTH, MIX_WIDTH, D), MIX_WIDTH ** -0.5),
        'da_lambda_q1': nrm(ks[12], (DEPTH, DA_QK_DIM), 0.1),
        'da_lambda_k1': nrm(ks[13], (DEPTH, DA_QK_DIM), 0.1),
        'da_lambda_q2': nrm(ks[14], (DEPTH, DA_QK_DIM), 0.1),
        'da_lambda_k2': nrm(ks[15], (DEPTH, DA_QK_DIM), 0.1),
        'da_subln': 1.0 + nrm(ks[16], (DEPTH, DA_V_DIM), 0.02),
        'swa_sink': nrm(ks[17], (DEPTH, SWA_HEADS), 0.5),
        'na_rpb': nrm(ks[18], (DEPTH, NA_HEADS, 2 * NA_WIN_ROWS - 1, 2 * NA_WIN_COLS - 1), 0.1),
        'ret_gamma_fwd': gam_logit[None, :] + nrm(ks[19], (DEPTH, RET_HEADS), 0.05),
        'ret_gamma_bwd': gam_logit[None, :] + nrm(ks[20], (DEPTH, RET_HEADS), 0.05),
        'ffn_w_gate': nrm(ks[21], (n_dense, D, FFN_DIM), D ** -0.5),
        'ffn_w_up': nrm(ks[22], (n_dense, D, FFN_DIM), D ** -0.5),
        'ffn_w_down': nrm(ks[23], (n_dense, FFN_DIM, D), FFN_DIM ** -0.5),
        'moe_router': nrm(ks[24], (n_moe, D, N_EXPERTS), D ** -0.5),
        'moe_w_gate': nrm(ks[25], (n_moe, N_EXPERTS, D, EXPERT_DIM), D ** -0.5),
        'moe_w_up': nrm(ks[26], (n_moe, N_EXPERTS, D, EXPERT_DIM), D ** -0.5),
        'moe_w_down': nrm(ks[27], (n_moe, N_EXPERTS, EXPERT_DIM, D), EXPERT_DIM ** -0.5),
    }


def reference(x, c, ctx, c_ctx, w_mod, b_mod, g_attn_pre, g_attn_post, g_ffn_pre, g_ffn_post, w_in, w_out,
              da_lambda_q1, da_lambda_k1, da_lambda_q2, da_lambda_k2, da_subln, swa_sink, na_rpb,
              ret_gamma_fwd, ret_gamma_bwd, ffn_w_gate, ffn_w_up, ffn_w_down,
              moe_router, moe_w_gate, moe_w_up, moe_w_down):
    S = x.shape[1]
    t = jnp.arange(S)
    row = (t // GRID_W).astype(jnp.float32)
    col = (t % GRID_W).astype(jnp.float32)
    xc = ctx
    for l in range(DEPTH):
        if l % 2 == 0:
            ffn = functools.partial(_swiglu_ffn, w_gate=ffn_w_gate[l // 2], w_up=ffn_w_up[l // 2],
                                    w_down=ffn_w_down[l // 2])
        else:
            ffn = functools.partial(_moe_ffn, router=moe_router[l // 2], w_gate=moe_w_gate[l // 2],
                                    w_up=moe_w_up[l // 2], w_down=moe_w_down[l // 2])
        lambda_init = 0.8 - 0.6 * math.exp(-0.3 * l)
        x, xc = _layer(x, xc, c, c_ctx, row, col, w_mod[l], b_mod[l], g_attn_pre[l], g_attn_post[l],
                       g_ffn_pre[l], g_ffn_post[l], w_in[l], w_out[l],
                       da_lambda_q1[l], da_lambda_k1[l], da_lambda_q2[l], da_lambda_k2[l], da_subln[l],
                       swa_sink[l], na_rpb[l], ret_gamma_fwd[l], ret_gamma_bwd[l], ffn, lambda_init,
                       l < DEPTH - 1)
    return x
```

```python
import contextlib
import os
import math
import numpy as np
import concourse.bass as bass
import concourse.mybir as mybir
from concourse.bass_utils import run_bass_kernel_spmd

F32 = mybir.dt.float32
BF16 = mybir.dt.bfloat16
AF = mybir.ActivationFunctionType
ALU = mybir.AluOpType
AX = mybir.AxisListType
ENGS = ("pe", "act", "dve", "pool", "sp")
EPS = 1e-6
NT = 16
NTC = 18
TOK = 2048
TOKC = 2304
DEBUG = []
STOP_AFTER = None


class Op:
    __slots__ = ("eng", "fn", "tl", "deps", "awaited", "count", "inc", "idx")


class Prog:
    def __init__(self):
        self.ops = []
        self.last_w = {}
        self.readers = {}
        self.tl_last = {}
        self.bar = set()
        self.bar_done = set(ENGS)

    def op(self, eng, fn, reads=(), writes=(), tl=None, inc=1):
        o = Op()
        o.eng = eng
        o.fn = fn
        o.tl = tl if tl is not None else eng
        o.inc = inc
        o.awaited = o.tl not in ENGS
        o.count = None
        o.idx = len(self.ops)
        deps = set()
        for r in reads:
            w = self.last_w.get(r)
            if w is not None:
                deps.add(w)
        for w_ in writes:
            w = self.last_w.get(w_)
            if w is not None:
                deps.add(w)
            rl = self.readers.get(w_)
            if rl:
                deps.update(rl)
        if eng not in self.bar_done:
            deps |= self.bar
            self.bar_done.add(eng)
        o.deps = deps
        self.ops.append(o)
        for r in reads:
            self.readers.setdefault(r, []).append(o.idx)
        for w_ in writes:
            self.last_w[w_] = o.idx
            self.readers[w_] = []
        self.tl_last[o.tl] = o.idx
        return o

    def barrier(self):
        self.bar = set(self.tl_last.values())
        self.bar_done = set()

    def finalize(self):
        ops = self.ops
        for i in self.tl_last.values():
            ops[i].awaited = True
        for o in ops:
            for d in o.deps:
                od = ops[d]
                if od.tl == "pe" and o.tl == "pe":
                    continue
                od.awaited = True
        cnt = {}
        for o in ops:
            if o.awaited:
                cnt[o.tl] = cnt.get(o.tl, 0) + o.inc
                o.count = cnt[o.tl]
        self.totals = cnt
        run_latest = {}
        self.need = [None] * len(ops)
        for o in ops:
            need = {}
            for d in o.deps:
                od = ops[d]
                if od.tl == "pe" and o.tl == "pe":
                    continue
                v = od.count if od.tl in ENGS else run_latest[od.tl]
                if need.get(od.tl, 0) < v:
                    need[od.tl] = v
            self.need[o.idx] = need
            if o.awaited:
                run_latest[o.tl] = o.count
        return sorted(cnt.keys(), key=str)

    def engine_body(self, ename, sems, final=False):
        mine = [o for o in self.ops if o.eng == ename]

        def body(e):
            waited = {}
            for o in mine:
                for tl, v in self.need[o.idx].items():
                    if waited.get(tl, 0) < v:
                        e.wait_ge(sems[tl], v)
                        waited[tl] = v
                ins = o.fn(e)
                if o.awaited:
                    ins.then_inc(sems[o.tl], o.inc)
            if final:
                for tl, v in self.totals.items():
                    if waited.get(tl, 0) < v:
                        e.wait_ge(sems[tl], v)
        return body


class Arena:
    def __init__(self, ap, nbytes, prog=None):
        self.prog = prog
        self.ap = ap
        self.cap = nbytes
        self.off = 0
        self.stack = []
        self.peak = 0

    def alloc(self, shape, dt):
        shape = list(shape)
        n = int(np.prod(shape))
        nb = n * (4 if dt == F32 else 2)
        nb = (nb + 63) // 64 * 64
        assert self.off + nb <= self.cap, f"SBUF arena overflow {self.off}+{nb}>{self.cap}"
        v = self.ap[:, self.off // 2:(self.off + nb) // 2]
        if dt == F32:
            v = v.bitcast(F32)
        v = v[:, 0:n]
        self.off += nb
        self.peak = max(self.peak, self.off)
        if len(shape) == 2:
            v = v.rearrange("p (a b) -> p a b", b=shape[1])
        elif len(shape) == 3:
            v = v.rearrange("p (a b c) -> p a b c", b=shape[1], c=shape[2])
        elif len(shape) == 4:
            v = v.rearrange("p (a b c d) -> p a b c d", b=shape[1], c=shape[2], d=shape[3])
        return v

    def push(self):
        self.stack.append(self.off)

    def pop(self):
        self.off = self.stack.pop()
        if self.prog is not None:
            self.prog.barrier()


def _swap_idx(dh):
    q = dh // 4
    return np.concatenate([np.arange(q, 2 * q), np.arange(0, q), np.arange(3 * q, 4 * q), np.arange(2 * q, 3 * q)])


def _win_perm():
    cols = []
    base = 0
    q = np.arange(base, base + 256)
    k = np.arange(base + 256, base + 512)
    v = np.arange(base + 512, base + 768)
    sw32 = np.concatenate([_swap_idx(32) + 32 * i for i in range(8)])
    cols += [q, k, q[sw32], k[sw32], v]
    base = 768
    qn = np.arange(base, base + 256).reshape(2, 2, 64)
    qperm = np.transpose(qn, (1, 0, 2)).reshape(256)
    kk = np.arange(base + 256, base + 384)
    vv = np.arange(base + 384, base + 512)
    sw64_4 = np.concatenate([_swap_idx(64) + 64 * i for i in range(4)])
    sw64_2 = np.concatenate([_swap_idx(64) + 64 * i for i in range(2)])
    cols += [qperm, kk, qperm[sw64_4], kk[sw64_2], vv]
    base = 1280
    cols += [np.arange(base, base + 768)]
    base = 2048
    q = np.arange(base, base + 256)
    k = np.arange(base + 256, base + 512)
    vg = np.arange(base + 512, base + 1024)
    cols += [q, k, q[sw64_4], k[sw64_4], vg]
    return np.concatenate(cols)


WIN_PERM = _win_perm()
NWIN = len(WIN_PERM)
DA0, SW0, NA0, RT0 = 0, 1280, 2176, 2944


def _rope_tables(j, dh):
    t = 2048 * j + np.arange(2048)
    row = (t // 64).astype(np.float64)
    col = (t % 64).astype(np.float64)
    half = dh // 2
    qd = dh // 4
    inv = 10000.0 ** (-np.arange(qd, dtype=np.float64) * 2.0 / half)
    C = np.zeros((128, 2048), np.float32)
    S = np.zeros((128, 2048), np.float32)
    for p in range(128):
        d = p % dh
        pos = row if d < half else col
        dd = d % half
        i = dd % qd
        ang = pos * inv[i]
        C[p] = np.cos(ang)
        S[p] = -np.sin(ang) if dd < qd else np.sin(ang)
    return C, S


def _swa_masks(j):
    kk = np.arange(128)[:, None]
    qq = np.arange(128)[None, :]
    mprev = (qq <= kk).astype(np.float32)
    mnext = (kk <= qq).astype(np.float32)
    m = np.zeros((10, 128, 128), np.float32)
    m[0] = mprev
    m[1] = mnext
    for r in range(4):
        if r == j - 1:
            m[2 + r] = mprev
        if r == j + 1:
            m[6 + r] = mnext
    return m


def _na_mask(Tq, Tk, flag=True):
    if (not flag) or Tk < 0 or Tk > 63:
        return np.zeros((128, 128), np.float32)
    p = np.arange(128)
    Rk = (2 * Tk + p // 64)[:, None]
    kc = (p % 64)[:, None]
    Rq = (2 * Tq + p // 64)[None, :]
    qc = (p % 64)[None, :]
    start = np.clip(Rq - 4, 0, 120)
    cs = np.clip(qc - 8, 0, 48)
    ok = (Rk >= start) & (Rk < start + 8) & (kc >= cs) & (kc < cs + 16)
    return ok.astype(np.float32)


def _na_masks(j):
    m = np.zeros((45, 128, 128), np.float32)
    for d in range(-2, 3):
        m[d + 2] = _na_mask(10, 10 + d)
    T0 = 16 * j
    idx = 5
    for d in (0, 1, 2, 3):
        m[idx] = _na_mask(T0, T0 + d); idx += 1
    for r in range(4):
        m[idx] = _na_mask(T0, T0 - 2, r == j - 1); idx += 1
    for r in range(4):
        m[idx] = _na_mask(T0, T0 - 1, r == j - 1); idx += 1
    for d in (-1, 0, 1, 2):
        m[idx] = _na_mask(T0 + 1, T0 + 1 + d); idx += 1
    for r in range(4):
        m[idx] = _na_mask(T0 + 1, T0 - 1, r == j - 1); idx += 1
    for d in (-2, -1, 0, 1):
        m[idx] = _na_mask(T0 + 14, T0 + 14 + d); idx += 1
    for r in range(4):
        m[idx] = _na_mask(T0 + 14, T0 + 16, r == j + 1); idx += 1
    for d in (-3, -2, -1, 0):
        m[idx] = _na_mask(T0 + 15, T0 + 15 + d); idx += 1
    for r in range(4):
        m[idx] = _na_mask(T0 + 15, T0 + 16, r == j + 1); idx += 1
    for r in range(4):
        m[idx] = _na_mask(T0 + 15, T0 + 17, r == j + 1); idx += 1
    assert idx == 45
    return m


def _na_bias_layout(rpb):
    p = np.arange(128)
    kr = (p // 64)[:, None]; kc = (p % 64)[:, None]
    qr = (p // 64)[None, :]; qc = (p % 64)[None, :]
    out = np.empty((7, 4, 128, 128), np.float32)
    dc = np.clip(kc - qc, -15, 15) + 15
    for di, d in enumerate(range(-3, 4)):
        dr = np.clip(2 * d + kr - qr, -7, 7) + 7
        out[di] = rpb[:, dr, dc]
    return out


def _ret_consts(j):
    c = np.zeros((128, 700), np.float32)
    i = np.arange(128)
    o = 0
    dif = i[None, :] - i[:, None]
    c[:, 0:128] = np.maximum(dif, 0)
    c[:, 128:256] = (dif >= 0) * 0.125
    c[:, 256:384] = np.maximum(-dif, 0)
    c[:, 384:512] = (dif < 0) * 0.125
    c[:, 512:640] = (i + 1)[None, :]
    c[:, 640] = 127 - i
    c[:, 641] = i
    c[:, 642:660] = (128.0 * np.arange(18))[None, :]
    c[:, 660:678] = (128.0 * (15 - np.arange(18)))[None, :]
    for r in range(4):
        if r < j:
            c[:, 678 + r] = 2048.0 * (j - 1 - r); c[:, 688 + r] = 1.0
        if r > j:
            c[:, 683 + r] = 2048.0 * (r - j - 1); c[:, 693 + r] = 1.0
    c[:, 682] = 2048.0 * j; c[:, 692] = 1.0
    c[:, 687] = 2048.0 * (3 - j); c[:, 697] = 1.0
    return c


def _idxb_table():
    i = np.arange(128)
    return np.broadcast_to((128 - i)[None, :], (128, 128)).astype(np.float32).copy()


def build_program():
    nc = bass.Bass("TRN2", target_bir_lowering=False)
    P = Prog()
    es = contextlib.ExitStack()

    def din(name, shape, dt=F32):
        return nc.dram_tensor(name, list(shape), dt, kind="ExternalInput").ap()

    def dint(name, shape, dt):
        return nc.dram_tensor(name, list(shape), dt)

    SHAPES = {"xin": [TOK, 1024], "xcin": [256, 1024], "cvec": [128, 16], "fwg": [1024, 2816], "fwu": [1024, 2816], "fwd": [2816, 1024],
              "router": [1, 8 * 1024], "mwg": [8, 1024, 3584], "mwu": [8, 1024, 3584], "mwd": [8, 3584, 1024],
              "rope64": [128, 2, 2048], "rope32": [128, 2, 2048], "swamask": [10, 128, 128], "namask": [45, 128, 128],
              "retc": [128, 700], "idxb": [128, 128]}
    for l_ in range(2):
        SHAPES.update({f"wmod{l_}": [1024, 6144], f"bmod{l_}": [1, 6144], f"gvec{l_}": [1, 4096], f"win{l_}": [1024, NWIN],
                       f"wout{l_}": [1024, 1024], f"dal{l_}": [1, 128], f"subln{l_}": [1, 64], f"sink{l_}": [1, 4],
                       f"gam{l_}": [1, 8], f"nabias{l_}": [7, 4, 128, 128]})

    class LazyIn(dict):
        def __missing__(self, k):
            self[k] = din(k, SHAPES[k])
            return self[k]
    I = LazyIn()
    USED_INPUTS = I
    out = nc.dram_tensor("out", [TOK, 1024], F32, kind="ExternalOutput").ap()
    dbg = {}
    for name, shape, dt in (("dbg_ot", [NTC, 128, 8, 128], BF16), ("dbg_x", [TOK, 1024], F32), ("dbg_xc", [256, 1024], F32),
                            ("dbg_misc", [128, 4096], F32)):
        if name in DEBUG:
            dbg[name] = nc.dram_tensor(name, shape, dt, kind="ExternalOutput").ap()

    xs = dint("xs", [TOK, 1024], F32).ap(); xcs = dint("xcs", [256, 1024], F32).ap()
    GROUPS = [[0, 1, 2, 3], [4, 5, 6, 7]]

    arena_t = es.enter_context(nc.sbuf_tensor("arena", [128, 94 * 1024], BF16))
    A = Arena(arena_t, 188 * 1024, P)
    psum = es.enter_context(nc.psum_tensor("psum", [128, 4096], F32))

    def bank(i, n=512, off=0):
        return psum[:, 512 * i + off:512 * i + off + n]

    def bank_bf(i):
        return psum[:, 512 * i:512 * (i + 1)].bitcast(BF16)

    def MM(o, lhsT, rhs, st, sp_, r, w, tp=None, sgc=False):
        kw = {}
        if tp is not None:
            kw["tile_position"] = tp
        if sgc:
            kw["skip_group_check"] = True
        P.op("pe", lambda e: e.matmul(o, lhsT=lhsT, rhs=rhs, start=st, stop=sp_, **kw), r, w)

    def MM64(o, lhsT, rhs, base, st, sp_, r, w):
        if base == 0:
            MM(o, lhsT[0:64], rhs[0:64], st, sp_, r, w, sgc=True)
        else:
            MM(o, lhsT[64:96], rhs[64:96], st, False, r, w, tp=(64, 0), sgc=True)
            MM(o, lhsT[96:128], rhs[96:128], False, sp_, r, w, tp=(96, 0), sgc=True)

    def TR(o, i, ident, r, w):
        P.op("pe", lambda e: e.transpose(o, i, ident), r, w)

    def ACTV(o, i, func, r, w, bias=None, scale=None, accum=None):
        kw = {}
        if bias is not None:
            kw["bias"] = bias
        if scale is not None:
            kw["scale"] = scale
        if accum is not None:
            kw["accum_out"] = accum
        P.op("act", lambda e: e.activation(out=o, in_=i, func=func, **kw), r, w)

    def TT(eng, o, a, b, op, r, w):
        P.op(eng, lambda e: e.tensor_tensor(out=o, in0=a, in1=b, op=op), r, w)

    def TS(eng, o, a, s1, op0, r, w, s2=None, op1=None):
        if op1 is None:
            P.op(eng, lambda e: e.tensor_scalar(out=o, in0=a, scalar1=s1, scalar2=None, op0=op0), r, w)
        else:
            P.op(eng, lambda e: e.tensor_scalar(out=o, in0=a, scalar1=s1, scalar2=s2, op0=op0, op1=op1), r, w)

    def STT(eng, o, a, s, b, op0, op1, r, w):
        P.op(eng, lambda e: e.scalar_tensor_tensor(out=o, in0=a, scalar=s, in1=b, op0=op0, op1=op1), r, w)

    def CP(eng, o, i, r, w):
        if eng == "act":
            P.op("act", lambda e: e.copy(out=o, in_=i), r, w)
        else:
            P.op(eng, lambda e: e.tensor_copy(out=o, in_=i), r, w)

    def MSET(eng, o, val, w):
        P.op(eng, lambda e: e.memset(o, val), (), w)

    def RED(eng, o, i, r, w, mx=False):
        if mx:
            P.op(eng, lambda e: e.reduce_max(out=o, in_=i, axis=AX.X), r, w)
        else:
            P.op(eng, lambda e: e.reduce_sum(out=o, in_=i, axis=AX.X), r, w)

    def RECIP(o, i, r, w):
        P.op("dve", lambda e: e.reciprocal(out=o, in_=i), r, w)

    def DMA(q, o, i, r, w, tl):
        P.op(q, lambda e: e.dma_start(out=o, in_=i), r, w, tl=tl, inc=16)

    def AG(src, dst, r, w, tl):
        P.op("pool", lambda e: e.collective_compute("AllGather", ALU.bypass, replica_groups=GROUPS,
                                                    ins=[src.ap().opt()], outs=[dst.ap().opt()]), r, w, tl=tl, inc=1)

    def rstd_from_ss(ss, n, rstd, r, w):
        ACTV(rstd, ss, AF.Sqrt, r, w, bias=epsc[:, 0:1], scale=1.0 / n)
        RECIP(rstd, rstd, w, w)

    ident_bf = A.alloc([128], BF16); ident_f = A.alloc([128], F32); zeros = A.alloc([128], BF16)
    epsc = A.alloc([1], F32)
    junk = A.alloc([1024], BF16)
    MOD = A.alloc([2, 6, 1024], BF16)
    MSET("pool", ident_f, 0.0, ["ident_f"])
    P.op("pool", lambda e: e.affine_select(out=ident_f, in_=ident_f, pattern=[[-1, 128]], compare_op=ALU.not_equal,
                                           fill=1.0, base=0, channel_multiplier=1), ["ident_f"], ["ident_f"])
    CP("pool", ident_bf, ident_f, ["ident_f"], ["ident_bf"])
    HM = A.alloc([2], F32)
    RED("dve", HM[:, 0:1], ident_f[:, 0:64], ["ident_f"], ["HM"])
    RED("dve", HM[:, 1:2], ident_f[:, 64:128], ["ident_f"], ["HM"])
    MSET("pool", zeros, 0.0, ["zeros"])
    MSET("pool", epsc, EPS, ["epsc"])

    def pbc(ap):
        b = ap.partition_broadcast(128)
        if len(b.shape) == 3 and b.shape[1] == 1:
            b = b[:, 0]
        return b

    stop = [False]

    def tap(name, ap, reads, flat):
        if name in DEBUG:
            shp = [128, int(np.prod(ap.shape[1:]))]
            d = nc.dram_tensor(name, shp, ap.dtype, kind="ExternalOutput").ap()
            DMA("sp", d, ap.rearrange(flat) if flat else ap, reads, [name], "dbg")

    def check_stop(name):
        if STOP_AFTER == name:
            stop[0] = True
        return stop[0]

    for l in range(2):
        if stop[0]:
            break
        with_ctx = (l == 0)
        lam_init = 0.8 - 0.6 * math.exp(-0.3 * l)
        ntl = NTC
        nto = NTC if with_ctx else NT
        xsrc = (I["xin"], I["xcin"]) if l == 0 else (xs, xcs)
        L = f"L{l}"

        def xtile_ap(src2, i):
            return src2[0][i * 128:(i + 1) * 128, :] if i < NT else src2[1][(i - NT) * 128:(i - NT + 1) * 128, :]

        P.barrier()
        A.push()
        cv = A.alloc([16], F32); sil = A.alloc([16], F32); sbc = A.alloc([2, 8, 128], BF16)
        gv = A.alloc([4, 1024], F32); tmpm = A.alloc([1024], F32)
        wm = [A.alloc([8, 1024], BF16) for _ in range(2)]
        bs = [A.alloc([1024], F32) for _ in range(2)]
        DMA("sp", cv, I["cvec"], [], [L + "cv"], "m0")
        DMA("sp", gv, pbc(I[f"gvec{l}"]).rearrange("p (a b) -> p a b", b=1024), [], [L + "gv"], "m0")
        ACTV(sil, cv, AF.Silu, [L + "cv"], [L + "sil"])
        for v in range(2):
            for kc in range(8):
                ACTV(sbc[:, v, kc, :], zeros, AF.Identity, [L + "sil", "zeros"], [L + "sbc"], bias=sil[:, v * 8 + kc:v * 8 + kc + 1])
        wmv = I[f"wmod{l}"].rearrange("(kc p) n -> p kc n", p=128)
        for s in range(6):
            sl = s % 2
            DMA("pool", wm[sl], wmv[:, :, s * 1024:(s + 1) * 1024], [], [L + f"wm{sl}"], f"wm{sl}")
            DMA("sp", bs[sl], pbc(I[f"bmod{l}"][0:1, s * 1024:(s + 1) * 1024]), [], [L + f"bs{sl}"], f"bs{sl}")
            for v in range(2):
                pb = 4 * (s % 2) + 2 * v
                for hf in range(2):
                    for kc in range(8):
                        MM(bank(pb + hf), sbc[:, v, kc, :], wm[sl][:, kc, hf * 512:(hf + 1) * 512], kc == 0, kc == 7,
                           [L + "sbc", L + f"wm{sl}"], [f"ps{pb + hf}"])
                TT("dve", tmpm, psum[:, 512 * pb:512 * pb + 1024], bs[sl], ALU.add, [f"ps{pb}", f"ps{pb + 1}", L + f"bs{sl}"], [L + "tmpm"])
                dst = MOD[:, v, s, :]
                if s in (0, 3):
                    CP("dve", dst, tmpm, [L + "tmpm"], [f"MOD{v}{s}"])
                elif s in (1, 4):
                    STT("dve", dst, tmpm, 1.0, gv[:, 0 if s == 1 else 2, :], ALU.add, ALU.mult, [L + "tmpm", L + "gv"], [f"MOD{v}{s}"])
                else:
                    TT("dve", dst, tmpm, gv[:, 1 if s == 2 else 3, :], ALU.mult, [L + "tmpm", L + "gv"], [f"MOD{v}{s}"])
        if l == 0:
            tap("t_mod", MOD, [f"MOD{v}{s}" for v in range(2) for s in range(6)], "p a b c -> p (a b c)")
        A.pop()
        if check_stop(f"p0_{l}"):
            break

        P.barrier()
        A.push()
        moe = (l == 1)
        if moe:
            LOGI = A.alloc([NT, 8], F32); GATES = A.alloc([NT, 8], F32)
        hT = A.alloc([8, TOKC], BF16)
        otd = dint(L + "otd", [NTC, 128, 8, 128], BF16).ap()
        A.push()
        otst = [A.alloc([2, 128], BF16) for _ in range(2)]

        A.push()
        xt = [A.alloc([1024], F32) for _ in range(2)]
        t1 = [A.alloc([1024], F32) for _ in range(2)]
        hb = [A.alloc([1024], BF16) for _ in range(2)]
        ssb = A.alloc([NTC], F32); rsb = A.alloc([NTC], F32)
        for i in range(ntl):
            s2 = i % 2
            v = 0 if i < NT else 1
            DMA("sp", xt[s2], xtile_ap(xsrc, i), [], [L + f"xt{s2}"], f"xt{s2}")
            ACTV(junk, xt[s2], AF.Square, [L + f"xt{s2}"], ["junk", L + f"ss{i}"], accum=ssb[:, i:i + 1])
            rstd_from_ss(ssb[:, i:i + 1], 1024, rsb[:, i:i + 1], [L + f"ss{i}", "epsc"], [L + f"rs{i}"])
            STT("dve", t1[s2], xt[s2], rsb[:, i:i + 1], MOD[:, v, 1, :], ALU.mult, ALU.mult, [L + f"xt{s2}", L + f"rs{i}", f"MOD{v}1"], [L + f"t1{s2}"])
            TT("pool", hb[s2], t1[s2], MOD[:, v, 0, :], ALU.add, [L + f"t1{s2}", f"MOD{v}0"], [L + f"hb{s2}"])
            for kc in range(8):
                TR(bank_bf(s2)[:, kc * 128:(kc + 1) * 128], hb[s2][:, kc * 128:(kc + 1) * 128], ident_bf, [L + f"hb{s2}", "ident_bf"], [f"ps{s2}"])
            CP("act", hT[:, :, i * 128:(i + 1) * 128], bank_bf(s2).rearrange("p (k t) -> p k t", t=128), [f"ps{s2}"], [L + f"hT{i}"])
        A.pop()
        HT_ALL = [L + f"hT{i}" for i in range(ntl)]
        if l == 0:
            tap("t_hT", hT, HT_ALL, "p a b -> p (a b)")
        if check_stop(f"p1a_{l}"):
            A.pop(); A.pop(); break

        winv = I[f"win{l}"].rearrange("(kc p) n -> p kc n", p=128)

        def proj_rope(wq, wqs, Ctab, Stab, dests, tag):
            tA = [A.alloc([512], F32) for _ in range(2)]
            tB = [A.alloc([512], F32) for _ in range(2)]
            n = 0
            for ci in range(len(wq)):
                for tb in range(4):
                    s2 = n % 2; n += 1
                    ts = slice(tb * 512, (tb + 1) * 512)
                    for kc in range(8):
                        MM(bank(s2), wq[ci][:, kc, :], hT[:, kc, ts], kc == 0, kc == 7, HT_ALL[4 * tb:4 * tb + 4] + [tag + "w"], [f"ps{s2}"])
                    for kc in range(8):
                        MM(bank(2 + s2), wqs[ci][:, kc, :], hT[:, kc, ts], kc == 0, kc == 7, HT_ALL[4 * tb:4 * tb + 4] + [tag + "w"], [f"ps{2 + s2}"])
                    TT("dve", tA[s2], bank(s2), Ctab[:, ts], ALU.mult, [f"ps{s2}", L + "rope"], [tag + f"tA{s2}"])
                    TT("dve", tB[s2], bank(2 + s2), Stab[:, ts], ALU.mult, [f"ps{2 + s2}", L + "rope"], [tag + f"tB{s2}"])
                    TT("pool", dests[ci][:, ts], tA[s2], tB[s2], ALU.add, [tag + f"tA{s2}", tag + f"tB{s2}"], [tag + f"d{ci}"])

        def proj_feat_plain(wq, dest, t0, nt, tag, ci, pb):
            for kc in range(8):
                MM(bank(pb, nt), wq[:, kc, :], hT[:, kc, t0:t0 + nt], kc == 0, kc == 7, HT_ALL + [tag + "w"], [f"ps{pb}"])
            CP("act", dest, bank(pb, nt), [f"ps{pb}"], [tag + f"d{ci}"])

        def proj_tok(wv, ncols, dest_fn, tiles, tag, post=None, view=None):
            for n, i in enumerate(tiles):
                pb = 4 + n % 2
                for kc in range(8):
                    MM(bank(pb, ncols), hT[:, kc, i * 128:(i + 1) * 128], wv[:, kc, :], kc == 0, kc == 7, [L + f"hT{i}", tag + "w"], [f"ps{pb}"])
                if post is None:
                    src_ = bank(pb, ncols)
                    if view is not None:
                        src_ = view(src_)
                    CP("act", dest_fn(i), src_, [f"ps{pb}"], [tag + f"v{i}"])
                else:
                    post(i, pb)

        def out_transposes(ytok, chunk0, i, tag, rd):
            pb = 6 + (i % 2)
            st = otst[i % 2]
            for cc in range(2):
                TR(bank_bf(pb)[:, cc * 128:(cc + 1) * 128], ytok[:, cc * 128:(cc + 1) * 128], ident_bf, rd + ["ident_bf"], [f"ps{pb}"])
            CP("act", st, bank_bf(pb)[:, 0:256].rearrange("p (c t) -> p c t", t=128), [f"ps{pb}"], [L + f"otst{i % 2}"])
            DMA("sp", otd[i, :, chunk0:chunk0 + 2, :], st, [L + f"otst{i % 2}"], [L + f"OT{chunk0}_{i}"], f"ot{i % 2}")

        def attn_pipeline(tiles_slots, S_fn, E_fn, AV_fn, FIN_fn):
            units = []
            for (i, slots) in tiles_slots:
                ngr = (len(slots) + 1) // 2
                for gi in range(ngr):
                    units.append((i, gi, ngr, slots[2 * gi:2 * gi + 2]))
            pending = None
            for k, u in enumerate(units):
                if k == 0:
                    S_fn(0, u)
                E_fn(k, u)
                if k + 1 < len(units):
                    S_fn(k + 1, units[k + 1])
                AV_fn(k, u)
                if pending is not None:
                    FIN_fn(pending); pending = None
                if u[1] == u[2] - 1:
                    pending = u[0]
            if pending is not None:
                FIN_fn(pending)

        rope64 = A.alloc([2, 2048], BF16); rope32 = A.alloc([2, 2048], BF16)
        DMA("pool", rope64, I["rope64"], [], [L + "rope"], "rp")
        DMA("pool", rope32, I["rope32"], [], [L + "rope"], "rp")

        T = L + "da"
        A.push()
        QT = A.alloc([2, TOKC], BF16); KTc = A.alloc([2, 256], BF16)
        Vc = A.alloc([2, 256], BF16)
        nlam = A.alloc([1], F32); gsub = A.alloc([64], F32)
        A.push()
        dl = A.alloc([4, 32], F32); pr = A.alloc([2, 32], F32); s12 = A.alloc([2], F32)
        DMA("sp", dl, pbc(I[f"dal{l}"]).rearrange("p (a b) -> p a b", b=32), [], [T + "dl"], "m0")
        DMA("sp", gsub, pbc(I[f"subln{l}"]), [], [T + "gsub"], "m0")
        TT("dve", pr[:, 0, :], dl[:, 0, :], dl[:, 1, :], ALU.mult, [T + "dl"], [T + "pr"])
        TT("dve", pr[:, 1, :], dl[:, 2, :], dl[:, 3, :], ALU.mult, [T + "dl"], [T + "pr"])
        RED("dve", s12, pr, [T + "pr"], [T + "s12"])
        ACTV(s12, s12, AF.Exp, [T + "s12"], [T + "s12"])
        TT("dve", nlam, s12[:, 1:2], s12[:, 0:1], ALU.subtract, [T + "s12"], [T + "nlam"])
        TS("dve", nlam, nlam, -lam_init, ALU.add, [T + "nlam"], [T + "nlam"])
        TS("dve", gsub, gsub, 1.0 - lam_init, ALU.mult, [T + "gsub"], [T + "gsub"])
        A.pop()
        A.push()
        wda = A.alloc([8, 1280], BF16)
        KTo = A.alloc([2, TOK], BF16); Vo = A.alloc([NT, 256], BF16)
        DMA("pool", wda, winv[:, :, DA0:DA0 + 1280], [], [T + "w"], "wA")
        qk_dest = [QT[:, 0, 0:TOK], QT[:, 1, 0:TOK], KTo[:, 0, :], KTo[:, 1, :]]
        proj_rope([wda[:, :, c * 128:(c + 1) * 128] for c in range(4)], [wda[:, :, 512 + c * 128:512 + (c + 1) * 128] for c in range(4)],
                  rope32[:, 0, :], rope32[:, 1, :], qk_dest, T)
        for ci in range(4):
            dest = QT[:, ci, TOK:TOKC] if ci < 2 else KTc[:, ci - 2, :]
            proj_feat_plain(wda[:, :, ci * 128:(ci + 1) * 128], dest, TOK, 256, T, f"c{ci}", ci % 2)
        proj_tok(wda[:, :, 1024:1280], 256, lambda i: Vo[:, i, :] if i < NT else Vc[:, i - NT, :], range(NTC), T)
        QK_R = [T + f"d{ci}" for ci in range(4)] + [T + f"dc{ci}" for ci in range(4)]
        V_R = [T + f"v{i}" for i in range(NTC)]
        if check_stop(f"daproj_{l}"):
            tap("t_qt", QT, QK_R, "p a b -> p (a b)")
            tap("t_kto", KTo, QK_R, "p a b -> p (a b)")
            tap("t_vo", Vo, V_R, "p a b -> p (a b)")
            tap("t_ktc", KTc, QK_R, "p a b -> p (a b)")
            tap("t_vc", Vc, V_R, "p a b -> p (a b)")
            A.pop(); A.pop(); A.pop(); A.pop(); break
        e_k = dint(T + "ek", [256, TOK], BF16); e_v = dint(T + "ev", [TOK, 256], BF16)
        g_k = dint(T + "gk", [1024, TOK], BF16); g_v = dint(T + "gv", [4 * TOK, 256], BF16)
        DMA("sp", e_k.ap().rearrange("(c p) t -> p c t", p=128), KTo, QK_R, [T + "ek"], "ex")
        DMA("sp", e_v.ap().rearrange("(i p) f -> p i f", p=128), Vo, V_R, [T + "ev"], "ex")
        AG(e_k, g_k, [T + "ek"], [T + "gk"], "cc")
        AG(e_v, g_v, [T + "ev"], [T + "gv"], "cc")
        A.pop()
        P.barrier()
        if check_stop(f"daag_{l}"):
            A.pop(); A.pop(); A.pop(); break
        KT = A.alloc([8448], BF16); V1 = A.alloc([66, 2, 65], BF16)
        ptH = [[A.alloc([2, 512], BF16) for _ in range(2)] for _ in range(2)]
        o_f = A.alloc([2, 64], F32); o1 = A.alloc([64], F32)
        rec = A.alloc([4], F32); rn = A.alloc([2], F32); ssd = A.alloc([2], F32); rsd = A.alloc([2], F32)
        yda = [A.alloc([2, 64], BF16) for _ in range(2)]
        MSET("pool", V1[:, :, :, 64:65], 1.0, [T + "V1ones"])
        g_kv = g_k.ap().rearrange("(r c p) t -> p r c t", r=4, c=2)
        g_vv = g_v.ap().rearrange("(r i p) f -> p r i f", r=4, i=NT)
        nblk = 0
        for c in range(2):
            CP("pool", KT[:, 0:256], KTc[:, c, :], QK_R, [T + "KT"])
            for r in range(4):
                DMA("sp", KT[:, 256 + r * TOK:256 + (r + 1) * TOK], g_kv[:, r, c, :], [T + "gk"], [T + "KT"], "kt")
                for hh in range(2):
                    DMA("sp", V1[:, 2 + r * NT:2 + (r + 1) * NT, hh, 0:64], g_vv[:, r, :, c * 128 + hh * 64:c * 128 + hh * 64 + 64],
                        [T + "gv"], [T + "V1"], "kt")
            CP("pool", V1[:, 0:2, :, 0:64], Vc[:, :, c * 128:(c + 1) * 128].rearrange("p i (h d) -> p i h d", d=64), V_R, [T + "V1"])
            if STOP_AFTER == f"daload_{l}":
                continue
            qblocks = [(qb * 512, 512, 0, 66) for qb in range(4)]
            if STOP_AFTER == f"daq1_{l}":
                qblocks = [(0, 512, 0, 66)] if c == 0 else []
            if STOP_AFTER == f"daq1nf_{l}":
                qblocks = [(0, 512, 0, 66)] if c == 0 else []
            if with_ctx:
                qblocks.append((TOK, 256, 0, 2))
            for (q0, nq, k0, k1) in qblocks:
                sc_ = 1.0 / math.sqrt(32.0)

                def S_half(kt, hf):
                    for g in (2 * hf, 2 * hf + 1):
                        MM(bank(g, nq), KT[32 * g:32 * g + 32, kt * 128:(kt + 1) * 128], QT[32 * g:32 * g + 32, c, q0:q0 + nq], True, True,
                           [T + "KT"] + QK_R, [f"psS{hf}"], tp=(32 * g, 0))

                def E_half(kt, hf):
                    s2 = (kt - k0) % 2
                    ACTV(ptH[hf][s2][:, :, 0:nq], psum[:, 1024 * hf:1024 * hf + 1024].rearrange("p (g n) -> p g n", n=512)[:, :, 0:nq], AF.Exp,
                         [f"psS{hf}"], [T + f"pt{hf}{s2}"], scale=sc_)

                def AV_half(kt, hf):
                    s2 = (kt - k0) % 2
                    for sb in range(nq // 128):
                        for gg in range(2):
                            g = 2 * hf + gg
                            MM(bank(4 + sb, 65, g * 65), ptH[hf][s2][:, gg, sb * 128:(sb + 1) * 128], V1[:, kt, hf, :], kt == k0 and g == 0, kt == k1 - 1,
                               [T + f"pt{hf}{s2}", T + "V1", T + "V1ones"], [f"psO{sb}"], sgc=True)

                S_half(k0, 0); S_half(k0, 1)
                for kt in range(k0, k1):
                    E_half(kt, 0); E_half(kt, 1)
                    AV_half(kt, 0)
                    if kt + 1 < k1:
                        S_half(kt + 1, 0)
                    AV_half(kt, 1)
                    if kt + 1 < k1:
                        S_half(kt + 1, 1)
                if STOP_AFTER == f"daq1nf_{l}":
                    continue
                for sb in range(nq // 128):
                    tile_i = (q0 + sb * 128) // 128
                    yb = yda[nblk % 2]; ybn = T + f"yda{nblk % 2}"; nblk += 1
                    bk = 4 + sb
                    pr_ = f"psO{sb}"
                    Tv = bank(bk, 260).rearrange("p (g e) -> p g e", e=65)
                    RECIP(rec, Tv[:, :, 64], [pr_], [T + "rec"])
                    TS("dve", rn, rec.rearrange("p (h m) -> p h m", m=2)[:, :, 1], nlam[:, 0:1], ALU.mult, [T + "rec", T + "nlam"], [T + "rn"])
                    for hh in range(2):
                        TS("dve", o1, Tv[:, 2 * hh, 0:64], rec[:, 2 * hh:2 * hh + 1], ALU.mult, [pr_, T + "rec"], [T + "o1"])
                        STT("dve", o_f[:, hh, :], Tv[:, 2 * hh + 1, 0:64], rn[:, hh:hh + 1], o1, ALU.mult, ALU.add, [pr_, T + "rn", T + "o1"], [T + "o_f"])
                        ACTV(junk[:, 0:64], o_f[:, hh, :], AF.Square, [T + "o_f"], ["junk", T + "ssd"], accum=ssd[:, hh:hh + 1])
                    rstd_from_ss(ssd, 64, rsd, [T + "ssd", "epsc"], [T + "rsd"])
                    for hh in range(2):
                        STT("dve", yb[:, hh, :], o_f[:, hh, :], rsd[:, hh:hh + 1], gsub, ALU.mult, ALU.mult, [T + "o_f", T + "rsd", T + "gsub"], [ybn])
                    TR(bank_bf(bk)[:, 640:768], yb.rearrange("p h d -> p (h d)"), ident_bf, [ybn, "ident_bf"], [pr_])
                    CP("act", otst[sb % 2][:, 0, :], bank_bf(bk)[:, 640:768], [pr_], [L + f"otst{sb % 2}"])
                    DMA("sp", otd[tile_i, :, c, :], otst[sb % 2][:, 0, :], [L + f"otst{sb % 2}"], [L + f"OT{c}_{tile_i}"], f"ot{sb % 2}")
        A.pop()
        if check_stop(f"da_{l}") or STOP_AFTER in (f"daload_{l}", f"daq1_{l}", f"daq1nf_{l}"):
            P.barrier()
            tap("t_nlam", nlam, [], None)
            tap("t_gsub", gsub, [], None)
            if "dbg_ot" in dbg:
                P.barrier()
                DMA("sp", dbg["dbg_ot"], otd, [], ["dbgot"], "dbg")
            A.pop(); A.pop(); break

        P.barrier()
        T = L + "sw"
        A.push()
        QT = A.alloc([2, TOKC], BF16)
        KT = A.alloc([3328], BF16)
        V1 = A.alloc([26, 2, 65], BF16)
        MSW = A.alloc([10, 128], BF16)
        esink = A.alloc([4], F32)
        DMA("pool", MSW, I["swamask"].rearrange("m k q -> k m q"), [], [T + "msw"], "wB")
        DMA("sp", esink, pbc(I[f"sink{l}"]), [], [T + "esink"], "m0")
        ACTV(esink, esink, AF.Exp, [T + "esink"], [T + "esink"])
        MSET("pool", V1[:, :, :, 64:65], 1.0, [T + "V1ones"])
        A.push()
        wsw = A.alloc([8, 896], BF16)
        DMA("pool", wsw, winv[:, :, SW0:SW0 + 896], [], [T + "w"], "wA")
        dests = [QT[:, 0, 0:TOK], QT[:, 1, 0:TOK], KT[:, 0:TOK]]
        proj_rope([wsw[:, :, c * 128:(c + 1) * 128] for c in range(3)], [wsw[:, :, 384 + c * 128:384 + (c + 1) * 128] for c in range(3)],
                  rope64[:, 0, :], rope64[:, 1, :], dests, T)
        for ci in range(3):
            dest = QT[:, ci, TOK:TOKC] if ci < 2 else KT[:, TOK:TOKC]
            proj_feat_plain(wsw[:, :, ci * 128:(ci + 1) * 128], dest, TOK, 256, T, f"c{ci}", ci % 2)
        proj_tok(wsw[:, :, 768:896], 128, lambda i: V1[:, i, :, 0:64], range(NTC), T, view=lambda a: a.rearrange("p (h d) -> p h d", d=64))
        A.pop()
        QK_R = [T + f"d{ci}" for ci in range(3)] + [T + f"dc{ci}" for ci in range(3)]
        V_R = [T + f"v{i}" for i in range(NTC)]
        if check_stop(f"swproj_{l}"):
            A.pop(); A.pop(); A.pop(); break
        e_s = dint(T + "e", [128, 512], BF16); g_s = dint(T + "g", [512, 512], BF16)
        DMA("sp", e_s.ap()[:, 0:128], KT[:, 0:128], QK_R, [T + "e"], "ex")
        DMA("sp", e_s.ap()[:, 128:256], KT[:, TOK - 128:TOK], QK_R, [T + "e"], "ex")
        DMA("sp", e_s.ap()[:, 256:384].rearrange("p (h d) -> p h d", d=64), V1[:, 0, :, 0:64], V_R, [T + "e"], "ex")
        DMA("sp", e_s.ap()[:, 384:512].rearrange("p (h d) -> p h d", d=64), V1[:, 15, :, 0:64], V_R, [T + "e"], "ex")
        AG(e_s, g_s, [T + "e"], [T + "g"], "cc")
        g_sv = g_s.ap().rearrange("(r p) f -> p r f", p=128)
        DMA("sp", KT[:, TOKC:TOKC + 512].rearrange("p (r t) -> p r t", t=128), g_sv[:, :, 128:256], [T + "g"], [T + "halo"], "kt")
        DMA("sp", KT[:, TOKC + 512:TOKC + 1024].rearrange("p (r t) -> p r t", t=128), g_sv[:, :, 0:128], [T + "g"], [T + "halo"], "kt")
        for kv in range(2):
            DMA("sp", V1[:, 18:22, kv, 0:64], g_sv[:, :, 384 + kv * 64:448 + kv * 64], [T + "g"], [T + "halo"], "kt")
            DMA("sp", V1[:, 22:26, kv, 0:64], g_sv[:, :, 256 + kv * 64:320 + kv * 64], [T + "g"], [T + "halo"], "kt")
        if check_stop(f"swag_{l}"):
            A.pop(); A.pop(); A.pop(); break
        ptw = [A.alloc([2, 4, 128], BF16) for _ in range(2)]
        qz = [A.alloc([2, 2, 128], BF16) for _ in range(2)]
        den = [A.alloc([4], F32) for _ in range(2)]; ysw = [A.alloc([4, 64], BF16) for _ in range(2)]
        ALLR = QK_R + V_R + [T + "halo", T + "V1ones"]
        tiles_slots = []
        for i in range(nto):
            if i < NT:
                slots = []
                if i > 0:
                    slots.append((128 * (i - 1), i - 1, 0))
                slots.append((128 * i, i, None))
                if i < NT - 1:
                    slots.append((128 * (i + 1), i + 1, 1))
                slots += [(TOK, 16, None), (TOK + 128, 17, None)]
                if i == 0:
                    slots += [(TOKC + 128 * r, 18 + r, 2 + r) for r in range(4)]
                if i == NT - 1:
                    slots += [(TOKC + 512 + 128 * r, 22 + r, 6 + r) for r in range(4)]
            else:
                slots = [(TOK, 16, None), (TOK + 128, 17, None)]
            tiles_slots.append((i, slots))

        def sw_S(k, u):
            i, gi, ngr, grp = u
            s2 = k % 2
            ts = slice(i * 128, (i + 1) * 128)
            if gi == 0:
                for kv in range(2):
                    TS("pool" if kv else "dve", qz[i % 2][:, kv], QT[:, :, ts], HM[:, kv:kv + 1], ALU.mult, QK_R + ["HM"], [T + f"qz{i % 2}"])
            for si, (kc0, vt, mi) in enumerate(grp):
                for kv in range(2):
                    for g in range(2):
                        MM(bank(2 * s2 + si, 128, (kv * 2 + g) * 128), KT[:, kc0:kc0 + 128], qz[i % 2][:, kv, g, :], True, True,
                           ALLR + [T + f"qz{i % 2}"], [f"psS{s2}"], sgc=True)

        def sw_E(k, u):
            i, gi, ngr, grp = u
            s2 = k % 2
            ns = len(grp)
            ACTV(ptw[s2][:, 0:ns].rearrange("p s h q -> p (s h q)"), psum[:, 1024 * s2:1024 * s2 + 512 * ns], AF.Exp, [f"psS{s2}"], [T + f"pt{s2}"], scale=0.125)
            for si, (kc0, vt, mi) in enumerate(grp):
                if mi is not None:
                    TT("dve", ptw[s2][:, si], ptw[s2][:, si], MSW[:, mi, :].unsqueeze(1).to_broadcast([128, 4, 128]), ALU.mult, [T + f"pt{s2}", T + "msw"], [T + f"pt{s2}"])

        def sw_AV(k, u):
            i, gi, ngr, grp = u
            s2 = k % 2
            ns = len(grp)
            for si, (kc0, vt, mi) in enumerate(grp):
                first = (gi == 0 and si == 0); last = (gi == ngr - 1 and si == ns - 1)
                for kv in range(2):
                    for g in range(2):
                        h = kv * 2 + g
                        MM(bank(4 + i % 2, 65, h * 65), ptw[s2][:, si, h, :], V1[:, vt, kv, :], first and h == 0, last, [T + f"pt{s2}"] + ALLR, [f"psO{i % 2}"], sgc=True)

        def sw_FIN(i):
            Ov = bank(4 + i % 2, 260).rearrange("p (h e) -> p h e", e=65)
            dn = den[i % 2]
            TT("dve", dn, Ov[:, :, 64], esink, ALU.add, [f"psO{i % 2}", T + "esink"], [T + f"den{i % 2}"])
            RECIP(dn, dn, [T + f"den{i % 2}"], [T + f"den{i % 2}"])
            yb = ysw[i % 2]
            TT("dve", yb, Ov[:, :, 0:64], dn.unsqueeze(2).to_broadcast([128, 4, 64]), ALU.mult, [f"psO{i % 2}", T + f"den{i % 2}"], [T + f"y{i % 2}"])
            out_transposes(yb.rearrange("p h d -> p (h d)"), 2, i, T, [T + f"y{i % 2}"])

        attn_pipeline(tiles_slots, sw_S, sw_E, sw_AV, sw_FIN)
        A.pop()
        if check_stop(f"sw_{l}") or (STOP_AFTER or "").startswith("swq1"):
            if "dbg_ot" in dbg:
                P.barrier()
                DMA("sp", dbg["dbg_ot"], otd, [], ["dbgot"], "dbg")
            A.pop(); A.pop(); break

        P.barrier()
        T = L + "na"
        A.push()
        QT = A.alloc([2, TOKC], BF16)
        KT = A.alloc([2, 4352], BF16)
        V1 = A.alloc([34, 4, 65], BF16)
        MBK = A.alloc([45, 128], BF16)
        BEX = A.alloc([7, 4, 128], BF16)
        EIN = A.alloc([5, 4, 128], BF16)
        for m0 in range(0, 45, 9):
            DMA("pool", MBK[:, m0:m0 + 9, :], I["namask"][m0:m0 + 9].rearrange("m k q -> k m q"), [], [T + "mbk"], "wB")
        A.push()
        bfl = A.alloc([7, 4, 128], F32)
        for d7 in range(7):
            DMA("sp", bfl[:, d7], I[f"nabias{l}"][d7].rearrange("h k q -> k h q"), [], [T + "bfl"], "m0")
        ACTV(BEX, bfl, AF.Exp, [T + "bfl"], [T + "bex"])
        A.pop()
        for d in range(5):
            TT("dve", EIN[:, d], BEX[:, d + 1], MBK[:, d, :].unsqueeze(1).to_broadcast([128, 4, 128]), ALU.mult, [T + "bex", T + "mbk"], [T + "ein"])
        MSET("pool", V1[:, :, :, 64:65], 1.0, [T + "V1ones"])
        A.push()
        wna = A.alloc([8, 768], BF16)
        DMA("pool", wna, winv[:, :, NA0:NA0 + 768], [], [T + "w"], "wA")
        n = 0
        for ci in range(4):
            for (t0, nt_) in ((0, 512), (512, 512), (1024, 512), (1536, 512), (TOK, 256)):
                dest = QT[:, ci, t0:t0 + nt_] if ci < 2 else KT[:, ci - 2, t0:t0 + nt_]
                proj_feat_plain(wna[:, :, ci * 128:(ci + 1) * 128], dest, t0, nt_, T, f"c{ci}", n % 4); n += 1
        proj_tok(wna[:, :, 512:768], 256, lambda i: V1[:, i, :, 0:64], range(NTC), T, view=lambda a: a.rearrange("p (h d) -> p h d", d=64))
        A.pop()
        P.barrier()
        QK_R = [T + f"dc{ci}" for ci in range(4)]
        V_R = [T + f"v{i}" for i in range(NTC)]
        e_n = dint(T + "e", [128, 2048], BF16); g_n = dint(T + "g", [512, 2048], BF16)
        env = e_n.ap()
        for c in range(2):
            DMA("sp", env[:, c * 512:c * 512 + 256], KT[:, c, 0:256], QK_R, [T + "e"], "ex")
            DMA("sp", env[:, c * 512 + 256:c * 512 + 512], KT[:, c, TOK - 256:TOK], QK_R, [T + "e"], "ex")
        DMA("sp", env[:, 1024:1536].rearrange("p (i h d) -> p i h d", h=4, d=64), V1[:, 0:2, :, 0:64], V_R, [T + "e"], "ex")
        DMA("sp", env[:, 1536:2048].rearrange("p (i h d) -> p i h d", h=4, d=64), V1[:, 14:16, :, 0:64], V_R, [T + "e"], "ex")
        AG(e_n, g_n, [T + "e"], [T + "g"], "cc")
        g_nv = g_n.ap().rearrange("(r p) f -> p r f", p=128)
        for c in range(2):
            DMA("sp", KT[:, c, TOKC:TOKC + 1024].rearrange("p (r t) -> p r t", t=256), g_nv[:, :, c * 512 + 256:c * 512 + 512], [T + "g"], [T + "halo"], "kt")
            DMA("sp", KT[:, c, TOKC + 1024:TOKC + 2048].rearrange("p (r t) -> p r t", t=256), g_nv[:, :, c * 512:c * 512 + 256], [T + "g"], [T + "halo"], "kt")
        for r in range(4):
            DMA("sp", V1[:, 18 + 2 * r:20 + 2 * r, :, 0:64], g_nv[:, r, 1536:2048].rearrange("p (i h d) -> p i h d", h=4, d=64), [T + "g"], [T + "halo"], "kt")
            DMA("sp", V1[:, 26 + 2 * r:28 + 2 * r, :, 0:64], g_nv[:, r, 1024:1536].rearrange("p (i h d) -> p i h d", h=4, d=64), [T + "g"], [T + "halo"], "kt")
        ptn = [A.alloc([2, 4, 128], BF16) for _ in range(2)]
        qz = [A.alloc([2, 2, 128], BF16) for _ in range(2)]
        den = [A.alloc([4], F32) for _ in range(2)]; yna = [A.alloc([4, 64], BF16) for _ in range(2)]
        ALLR = QK_R + V_R + [T + "halo", T + "V1ones"]
        PC0 = TOKC; NC0 = TOKC + 1024
        tiles_slots = []
        for i in range(nto):
            if i >= NT:
                slots = [(TOK, 16, 0, 0, 0), (TOK + 128, 17, 0, 0, 0)]
            elif 2 <= i <= 13:
                slots = [(128 * (i + d), i + d, 1, d + 2, 0) for d in range(-2, 3)]
            elif i == 0:
                slots = [(128 * d, d, 2, d + 3, 5 + d) for d in (0, 1, 2, 3)]
                slots += [(PC0 + 256 * r, 18 + 2 * r, 2, 1, 9 + r) for r in range(4)]
                slots += [(PC0 + 256 * r + 128, 19 + 2 * r, 2, 2, 13 + r) for r in range(4)]
            elif i == 1:
                slots = [(128 * (1 + d), 1 + d, 2, d + 3, 17 + (d + 1)) for d in (-1, 0, 1, 2)]
                slots += [(PC0 + 256 * r + 128, 19 + 2 * r, 2, 1, 21 + r) for r in range(4)]
            elif i == 14:
                slots = [(128 * (14 + d), 14 + d, 2, d + 3, 25 + (d + 2)) for d in (-2, -1, 0, 1)]
                slots += [(NC0 + 256 * r, 26 + 2 * r, 2, 5, 29 + r) for r in range(4)]
            else:
                slots = [(128 * (15 + d), 15 + d, 2, d + 3, 33 + (d + 3)) for d in (-3, -2, -1, 0)]
                slots += [(NC0 + 256 * r, 26 + 2 * r, 2, 4, 37 + r) for r in range(4)]
                slots += [(NC0 + 256 * r + 128, 27 + 2 * r, 2, 5, 41 + r) for r in range(4)]
            if i < NT:
                slots += [(TOK, 16, 0, 0, 0), (TOK + 128, 17, 0, 0, 0)]
            tiles_slots.append((i, slots))

        def na_S(k, u):
            i, gi, ngr, grp = u
            s2 = k % 2
            ts = slice(i * 128, (i + 1) * 128)
            if gi == 0:
                for hb_ in range(2):
                    TS("pool" if hb_ else "dve", qz[i % 2][:, hb_], QT[:, :, ts], HM[:, hb_:hb_ + 1], ALU.mult, QK_R + ["HM"], [T + f"qz{i % 2}"])
            for si, (kc0, vt, kind, bd, mi) in enumerate(grp):
                for h in range(4):
                    c, hb_ = h // 2, h % 2
                    MM(bank(2 * s2 + si, 128, h * 128), KT[:, c, kc0:kc0 + 128], qz[i % 2][:, hb_, c, :], True, True, ALLR + [T + f"qz{i % 2}"], [f"psS{s2}"], sgc=True)

        def na_E(k, u):
            i, gi, ngr, grp = u
            s2 = k % 2
            ns = len(grp)
            ACTV(ptn[s2][:, 0:ns].rearrange("p s h q -> p (s h q)"), psum[:, 1024 * s2:1024 * s2 + 512 * ns], AF.Exp, [f"psS{s2}"], [T + f"pt{s2}"], scale=0.125)
            for si, (kc0, vt, kind, bd, mi) in enumerate(grp):
                if kind == 1:
                    TT("dve", ptn[s2][:, si], ptn[s2][:, si], EIN[:, bd], ALU.mult, [T + f"pt{s2}", T + "ein"], [T + f"pt{s2}"])
                elif kind == 2:
                    TT("dve", ptn[s2][:, si], ptn[s2][:, si], BEX[:, bd], ALU.mult, [T + f"pt{s2}", T + "bex"], [T + f"pt{s2}"])
                    TT("pool", ptn[s2][:, si], ptn[s2][:, si], MBK[:, mi, :].unsqueeze(1).to_broadcast([128, 4, 128]), ALU.mult, [T + f"pt{s2}", T + "mbk"], [T + f"pt{s2}"])

        def na_AV(k, u):
            i, gi, ngr, grp = u
            s2 = k % 2
            ns = len(grp)
            for si, (kc0, vt, kind, bd, mi) in enumerate(grp):
                first = (gi == 0 and si == 0); last = (gi == ngr - 1 and si == ns - 1)
                for h in range(4):
                    MM(bank(4 + i % 2, 65, h * 65), ptn[s2][:, si, h, :], V1[:, vt, h, :], first and h == 0, last, [T + f"pt{s2}"] + ALLR, [f"psO{i % 2}"], sgc=True)

        def na_FIN(i):
            Ov = bank(4 + i % 2, 260).rearrange("p (h e) -> p h e", e=65)
            dn = den[i % 2]
            RECIP(dn, Ov[:, :, 64], [f"psO{i % 2}"], [T + f"den{i % 2}"])
            yb = yna[i % 2]
            TT("dve", yb, Ov[:, :, 0:64], dn.unsqueeze(2).to_broadcast([128, 4, 64]), ALU.mult, [f"psO{i % 2}", T + f"den{i % 2}"], [T + f"y{i % 2}"])
            out_transposes(yb.rearrange("p h d -> p (h d)"), 4, i, T, [T + f"y{i % 2}"])

        attn_pipeline(tiles_slots, na_S, na_E, na_AV, na_FIN)
        A.pop()
        if check_stop(f"na_{l}"):
            if "dbg_ot" in dbg:
                P.barrier()
                DMA("sp", dbg["dbg_ot"], otd, [], ["dbgot"], "dbg")
            A.pop(); A.pop(); break

        P.barrier()
        T = L + "rt"
        A.push()
        QT = A.alloc([2, TOKC], BF16); KT = A.alloc([2, TOKC], BF16)
        VR = A.alloc([NTC, 256], BF16); GT = A.alloc([NTC, 256], BF16)
        RC = A.alloc([700], F32); IDXB = A.alloc([128], F32)
        LG = A.alloc([8], F32); LGS = A.alloc([2, 2], F32)
        DEC = A.alloc([4, 128], BF16); XI = A.alloc([2, 2, 128], BF16)
        ZZ = A.alloc([2, 4], F32); GC = A.alloc([2, 2], F32); GPW = A.alloc([2, 2, 18], F32); CFC = A.alloc([2, 2, 5], F32)
        DMA("sp", RC, I["retc"], [], [T + "rc"], "m0")
        DMA("sp", IDXB, I["idxb"], [], [T + "rc"], "m0")
        DMA("sp", LG, pbc(I[f"gam{l}"]), [], [T + "lg"], "m0")
        ACTV(LG, LG, AF.Exp, [T + "lg"], [T + "lg"], scale=-1.0)
        TS("dve", LG, LG, 1.0, ALU.add, [T + "lg"], [T + "lg"])
        ACTV(LG, LG, AF.Ln, [T + "lg"], [T + "lg"])
        TS("dve", LG, LG, -1.0, ALU.mult, [T + "lg"], [T + "lg"])
        for d_ in range(2):
            for c in range(2):
                k0_ = 4 * d_ + 2 * c
                TS("dve", LGS[:, d_, c:c + 1], LG[:, k0_:k0_ + 1], HM[:, 0:1], ALU.mult, [T + "lg", "HM"], [T + "lgs"])
                STT("dve", LGS[:, d_, c:c + 1], LG[:, k0_ + 1:k0_ + 2], HM[:, 1:2], LGS[:, d_, c:c + 1], ALU.mult, ALU.add, [T + "lg", "HM", T + "lgs"], [T + "lgs"])
        A.push()
        tf = A.alloc([128], F32); tb_ = A.alloc([128], F32)
        for h in range(4):
            ACTV(tf, RC[:, 0:128], AF.Exp, [T + "rc", T + "lg"], [T + "tf"], scale=LG[:, h:h + 1])
            TT("dve", tf, tf, RC[:, 128:256], ALU.mult, [T + "tf", T + "rc"], [T + "tf"])
            ACTV(tb_, RC[:, 256:384], AF.Exp, [T + "rc", T + "lg"], [T + "tb"], scale=LG[:, 4 + h:5 + h])
            TT("dve", tb_, tb_, RC[:, 384:512], ALU.mult, [T + "tb", T + "rc"], [T + "tb"])
            TT("dve", DEC[:, h, :], tf, tb_, ALU.add, [T + "tf", T + "tb"], [T + "dec"])
        A.pop()
        for c in range(2):
            ACTV(XI[:, 0, c, :], RC[:, 512:640], AF.Exp, [T + "rc", T + "lgs"], [T + "xi"], scale=LGS[:, 0, c:c + 1])
            ACTV(XI[:, 1, c, :], IDXB, AF.Exp, [T + "rc", T + "lgs"], [T + "xi"], scale=LGS[:, 1, c:c + 1])
            for d_ in range(2):
                ACTV(GC[:, d_, c:c + 1], LGS[:, d_, c:c + 1], AF.Exp, [T + "lgs"], [T + "gc"], scale=128.0)
                ACTV(GPW[:, d_, c, :], RC[:, 642 + 18 * d_:660 + 18 * d_], AF.Exp, [T + "rc", T + "lgs"], [T + "gpw"], scale=LGS[:, d_, c:c + 1])
                ACTV(CFC[:, d_, c, :], RC[:, 678 + 5 * d_:683 + 5 * d_], AF.Exp, [T + "rc", T + "lgs"], [T + "cfc"], scale=LGS[:, d_, c:c + 1])
                TT("dve", CFC[:, d_, c, :], CFC[:, d_, c, :], RC[:, 688 + 5 * d_:693 + 5 * d_], ALU.mult, [T + "cfc", T + "rc"], [T + "cfc"])
        ACTV(ZZ[:, 0, :], LG[:, 0:4], AF.Exp, [T + "lg", T + "rc"], [T + "zz"], scale=RC[:, 640:641])
        ACTV(ZZ[:, 1, :], LG[:, 4:8], AF.Exp, [T + "lg", T + "rc"], [T + "zz"], scale=RC[:, 641:642])
        TS("dve", ZZ, ZZ, 0.125, ALU.mult, [T + "zz"], [T + "zz"])
        if check_stop(f"rtparam_{l}"):
            A.pop(); A.pop(); A.pop(); break
        A.push()
        wrt = A.alloc([8, 1536], BF16)
        DMA("pool", wrt, winv[:, :, RT0:RT0 + 1536], [], [T + "w"], "wA")
        dests = [QT[:, 0, 0:TOK], QT[:, 1, 0:TOK], KT[:, 0, 0:TOK], KT[:, 1, 0:TOK]]
        proj_rope([wrt[:, :, c * 128:(c + 1) * 128] for c in range(4)], [wrt[:, :, 512 + c * 128:512 + (c + 1) * 128] for c in range(4)],
                  rope64[:, 0, :], rope64[:, 1, :], dests, T)
        for ci in range(4):
            dest = QT[:, ci, TOK:TOKC] if ci < 2 else KT[:, ci - 2, TOK:TOKC]
            proj_feat_plain(wrt[:, :, ci * 128:(ci + 1) * 128], dest, TOK, 256, T, f"c{ci}", ci % 2)

        def vg_post(i, pb):
            CP("act", VR[:, i, :], bank(pb, 256), [f"ps{pb}"], [T + f"v{i}"])
            ACTV(GT[:, i, :], bank(pb, 256, 256), AF.Silu, [f"ps{pb}"], [T + f"g{i}"])
        proj_tok(wrt[:, :, 1024:1536], 512, None, range(NTC), T, post=vg_post)
        A.pop()
        P.barrier()
        QK_R = [T + f"d{ci}" for ci in range(4)] + [T + f"dc{ci}" for ci in range(4)]
        if check_stop(f"rtproj_{l}"):
            A.pop(); A.pop(); A.pop(); break
        KTOK = A.alloc([NTC, 256], BF16)
        SZ = A.alloc([2, 18, 2, 64], F32)
        UCX = A.alloc([4, 2, 64], F32)
        SCX = A.alloc([2, 2, 64], F32)
        S0 = A.alloc([2, 2, 64], F32)
        SB = A.alloc([2, NTC, 2, 64], BF16)
        GR = A.alloc([4, 256], F32); EXPB = A.alloc([2, 2, 64], F32)
        for i in range(NTC):
            pb = i % 2
            for c in range(2):
                TR(bank_bf(pb)[:, c * 128:(c + 1) * 128], KT[:, c, i * 128:(i + 1) * 128], ident_bf, QK_R + ["ident_bf"], [f"ps{pb}"])
            CP("act", KTOK[:, i, :], bank_bf(pb)[:, 0:256], [f"ps{pb}"], [T + f"kt{i}"])
        vz = [A.alloc([2, 256], BF16) for _ in range(2)]
        MSET("pool", SZ[:, 0, 0], 0.0, [T + "sz"])
        MSET("pool", SZ[:, 1, 16], 0.0, [T + "sz"])

        def chunk_U(n, s2):
            for d_ in range(2):
                TT("dve" if d_ == 0 else "pool", vz[s2][:, d_].rearrange("p (h e) -> p h e", e=64), VR[:, n, :].rearrange("p (h e) -> p h e", e=64),
                   ZZ[:, d_, :].unsqueeze(2).to_broadcast([128, 4, 64]), ALU.mult, [T + f"v{n}", T + "zz"], [T + f"vz{s2}"])
            for d_ in range(2):
                for c in range(2):
                    MM(bank(2 + s2, 128, (d_ * 2 + c) * 128), KTOK[:, n, c * 128:(c + 1) * 128], vz[s2][:, d_, c * 128:(c + 1) * 128], True, True,
                       [T + f"kt{n}", T + f"vz{s2}"], [f"ps{2 + s2}"])

        udg = A.alloc([64], F32); udt = A.alloc([64], F32)

        def udiag(s2, d_, c, dst=None, dreg=None):
            blk = bank(2 + s2, 128, (d_ * 2 + c) * 128)
            o_ = udg if dst is None else dst
            TS("dve", udt, blk[:, 0:64], HM[:, 0:1], ALU.mult, [f"ps{2 + s2}", "HM"], [T + "udt"])
            STT("dve", o_, blk[:, 64:128], HM[:, 1:2], udt, ALU.mult, ALU.add, [f"ps{2 + s2}", "HM", T + "udt"], [T + "udg" if dreg is None else dreg])
            return o_

        for n in range(NT):
            chunk_U(n, n % 2)
            for c in range(2):
                u_ = udiag(n % 2, 0, c)
                STT("dve", SZ[:, 0, n + 1, c, :], SZ[:, 0, n, c, :], GC[:, 0, c:c + 1], u_, ALU.mult, ALU.add, [T + "sz", T + "gc", T + "udg"], [T + "sz"])
                udiag(n % 2, 1, c, dst=SZ[:, 1, n, c, :], dreg=T + "szb")
        for n in range(NT - 1, -1, -1):
            for c in range(2):
                STT("dve", SZ[:, 1, n, c, :], SZ[:, 1, n + 1, c, :], GC[:, 1, c:c + 1], SZ[:, 1, n, c, :], ALU.mult, ALU.add, [T + "sz", T + "szb", T + "gc"], [T + "sz", T + "szb"])
        for k_, n in enumerate((16, 17)):
            chunk_U(n, k_)
            for d_ in range(2):
                for c in range(2):
                    u_ = udiag(k_, d_, c)
                    CP("dve", UCX[:, 2 * d_ + k_, c, :], u_, [T + "udg"], [T + "ucx"])
        for c in range(2):
            STT("dve", SCX[:, 0, c, :], UCX[:, 0, c, :], GC[:, 0, c:c + 1], UCX[:, 1, c, :], ALU.mult, ALU.add, [T + "ucx", T + "gc"], [T + "scx"])
            STT("dve", SCX[:, 1, c, :], UCX[:, 3, c, :], GC[:, 1, c:c + 1], UCX[:, 2, c, :], ALU.mult, ALU.add, [T + "ucx", T + "gc"], [T + "scx"])
        CP("dve", EXPB[:, 0], SZ[:, 0, 16], [T + "sz"], [T + "expb"])
        CP("dve", EXPB[:, 1], SZ[:, 1, 0], [T + "sz"], [T + "expb"])
        e_r = dint(T + "e", [128, 256], F32); g_r = dint(T + "g", [512, 256], F32)
        DMA("sp", e_r.ap(), EXPB.rearrange("p d c e -> p (d c e)"), [T + "expb"], [T + "e"], "ex")
        AG(e_r, g_r, [T + "e"], [T + "g"], "cc")
        DMA("sp", GR, g_r.ap().rearrange("(r p) f -> p r f", p=128), [T + "g"], [T + "gr"], "kt")
        GRv = GR.rearrange("p r (d c e) -> p r d c e", d=2, c=2)
        for d_ in range(2):
            for c in range(2):
                TS("dve", S0[:, d_, c, :], SCX[:, d_, c, :], CFC[:, d_, c, 4:5], ALU.mult, [T + "scx", T + "cfc"], [T + "s0"])
                for r in range(4):
                    STT("dve", S0[:, d_, c, :], GRv[:, r, d_, c, :], CFC[:, d_, c, r:r + 1], S0[:, d_, c, :], ALU.mult, ALU.add, [T + "gr", T + "cfc", T + "s0"], [T + "s0"])
        for n in range(NT):
            for c in range(2):
                STT("dve", SB[:, 0, n, c, :], S0[:, 0, c, :], GPW[:, 0, c, n:n + 1], SZ[:, 0, n, c, :], ALU.mult, ALU.add, [T + "s0", T + "gpw", T + "sz"], [T + "sb"])
                STT("dve", SB[:, 1, n, c, :], S0[:, 1, c, :], GPW[:, 1, c, n:n + 1], SZ[:, 1, n + 1, c, :], ALU.mult, ALU.add, [T + "s0", T + "gpw", T + "sz"], [T + "sb"])
        MSET("pool", SB[:, 0, 16], 0.0, [T + "sb"])
        MSET("pool", SB[:, 1, 17], 0.0, [T + "sb"])
        CP("dve", SB[:, 0, 17], UCX[:, 0], [T + "ucx"], [T + "sb"])
        CP("dve", SB[:, 1, 16], UCX[:, 3], [T + "ucx"], [T + "sb"])
        if check_stop(f"rtA_{l}"):
            A.pop(); A.pop(); A.pop(); break
        AD = [A.alloc([4, 128], BF16) for _ in range(2)]
        QX = [A.alloc([2, 2, 2, 128], BF16) for _ in range(2)]
        qz = [A.alloc([2, 2, 128], BF16) for _ in range(2)]
        of_ = [A.alloc([4, 64], F32) for _ in range(2)]
        sq = A.alloc([4, 64], F32); ssr = A.alloc([4], F32); rsr = A.alloc([4], F32)
        yr_ = [A.alloc([4, 64], BF16) for _ in range(2)]
        def rt_A(i):
            s2 = i % 2
            ts = slice(i * 128, (i + 1) * 128)
            for hb_ in range(2):
                TS("pool" if hb_ else "dve", qz[s2][:, hb_], QT[:, :, ts], HM[:, hb_:hb_ + 1], ALU.mult, QK_R + ["HM"], [T + f"qz{s2}"])
            for h in range(4):
                c, hb_ = h // 2, h % 2
                MM(bank(s2, 128, h * 128), KT[:, c, ts], qz[s2][:, hb_, c, :], True, True, QK_R + [T + f"qz{s2}"], [f"ps{s2}"], sgc=True)

        def rt_mid(i):
            s2 = i % 2
            TT("dve", AD[s2], bank(s2).rearrange("p (h i) -> p h i", i=128), DEC, ALU.mult, [f"ps{s2}", T + "dec"], [T + f"ad{s2}"])
            for d_ in range(2):
                for hb_ in range(2):
                    TT("dve", QX[s2][:, d_, hb_], qz[s2][:, hb_], XI[:, d_], ALU.mult, [T + f"qz{s2}", T + "xi"], [T + f"qx{s2}"])

        def rt_out(i):
            s2 = i % 2
            pO = 4 + s2
            for h in range(4):
                c, hb_ = h // 2, h % 2
                o_ = bank(pO, 64, h * 64)
                MM(o_, AD[s2][:, h, :], VR[:, i, h * 64:(h + 1) * 64], h == 0, False, [T + f"ad{s2}", T + f"v{i}"], [f"ps{pO}"], sgc=True)
                MM(o_, QX[s2][:, 0, hb_, c, :], SB[:, 0, i, c, :], False, False, [T + f"qx{s2}", T + "sb"], [f"ps{pO}"], sgc=True)
                MM(o_, QX[s2][:, 1, hb_, c, :], SB[:, 1, i, c, :], False, True, [T + f"qx{s2}", T + "sb"], [f"ps{pO}"], sgc=True)

        def rt_fin(i):
            s2 = i % 2
            pO = 4 + s2
            ov = of_[s2]
            CP("act", ov, bank(pO, 256).rearrange("p (h e) -> p h e", e=64), [f"ps{pO}"], [T + f"of{s2}"])
            TT("dve", sq, ov, ov, ALU.mult, [T + f"of{s2}"], [T + "sq"])
            RED("dve", ssr, sq, [T + "sq"], [T + "ssr"])
            rstd_from_ss(ssr, 64, rsr, [T + "ssr", "epsc"], [T + "rsr"])
            TT("dve", sq, ov, rsr.unsqueeze(2).to_broadcast([128, 4, 64]), ALU.mult, [T + f"of{s2}", T + "rsr"], [T + "sq"])
            TT("pool", yr_[s2], sq, GT[:, i, :].rearrange("p (h e) -> p h e", e=64), ALU.mult, [T + "sq", T + f"g{i}"], [T + f"y{s2}"])

        def rt_tr(i):
            out_transposes(yr_[i % 2].rearrange("p h d -> p (h d)"), 6, i, T, [T + f"y{i % 2}"])

        rt_A(0)
        for i in range(nto):
            rt_mid(i)
            if i + 1 < nto:
                rt_A(i + 1)
            rt_out(i)
            rt_fin(i)
            if i >= 1:
                rt_tr(i - 1)
        rt_tr(nto - 1)
        A.pop()
        A.pop()
        if "dbg_ot" in dbg and l == 0:
            DMA("sp", dbg["dbg_ot"], otd, [L + f"OT{c0}_{i}" for c0 in (0, 1, 2, 4, 6) for i in range(nto)], ["dbgot"], "dbg")
        if check_stop(f"rt_{l}") or (STOP_AFTER or "").startswith("rtB"):
            A.pop(); break

        P.barrier()
        A.push()
        h2T = hT
        wo = A.alloc([8, 1024], BF16)
        DMA("pool", wo, I[f"wout{l}"].rearrange("(kc p) n -> p kc n", p=128), [], [L + "wo"], "wA")
        if moe:
            RB = A.alloc([8, 1024], F32)
            DMA("sp", RB, pbc(I["router"]).rearrange("p (e d) -> p e d", d=1024), [], [L + "rb"], "m0")
            rj = A.alloc([1024], F32); sm = A.alloc([8, 8], F32)
        xt = [A.alloc([1024], F32) for _ in range(2)]
        t1 = [A.alloc([1024], F32) for _ in range(2)]
        xm = [A.alloc([1024], F32) for _ in range(2)]
        hb = [A.alloc([1024], BF16) for _ in range(2)]
        ott = [A.alloc([8, 128], BF16) for _ in range(2)]
        ss3 = A.alloc([NTC, 2], F32); rs3 = A.alloc([NTC, 2], F32)
        xdst = (xs, xcs)

        def p3_mm(i):
            s2 = i % 2
            pb = 2 * s2
            ot_r = [L + f"OT{c0}_{i}" for c0 in (0, 1, 2, 4, 6)]
            DMA("sp", ott[s2], otd[i], ot_r, [L + f"ott{s2}"], f"ott{s2}")
            for hf in range(2):
                for kc in range(8):
                    MM(bank(pb + hf), ott[s2][:, kc, :], wo[:, kc, hf * 512:(hf + 1) * 512], kc == 0, kc == 7, [L + f"ott{s2}", L + "wo"], [f"ps{pb + hf}"])

        def p3_chain(i):
            s2 = i % 2
            v = 0 if i < NT else 1
            pb = 2 * s2
            yps = psum[:, 512 * pb:512 * pb + 1024]
            ACTV(junk, yps, AF.Square, [f"ps{pb}", f"ps{pb + 1}"], ["junk", L + f"s3_{i}"], accum=ss3[:, i, 0:1])
            rstd_from_ss(ss3[:, i, 0:1], 1024, rs3[:, i, 0:1], [L + f"s3_{i}", "epsc"], [L + f"r3_{i}"])
            DMA("sp", xt[s2], xtile_ap(xsrc, i), [], [L + f"p3xt{s2}"], f"xt{s2}")
            STT("dve", t1[s2], yps, rs3[:, i, 0:1], MOD[:, v, 2, :], ALU.mult, ALU.mult, [f"ps{pb}", f"ps{pb + 1}", L + f"r3_{i}", f"MOD{v}2"], [L + f"p3t1{s2}"])
            TT("pool", xm[s2], t1[s2], xt[s2], ALU.add, [L + f"p3t1{s2}", L + f"p3xt{s2}"], [L + f"xm{s2}"])
            DMA("sp", xtile_ap(xdst, i), xm[s2], [L + f"xm{s2}"], [L + f"xs{i}"], f"xst{s2}")
            ACTV(junk, xm[s2], AF.Square, [L + f"xm{s2}"], ["junk", L + f"s4_{i}"], accum=ss3[:, i, 1:2])
            rstd_from_ss(ss3[:, i, 1:2], 1024, rs3[:, i, 1:2], [L + f"s4_{i}", "epsc"], [L + f"r4_{i}"])
            STT("dve", t1[s2], xm[s2], rs3[:, i, 1:2], MOD[:, v, 4, :], ALU.mult, ALU.mult, [L + f"xm{s2}", L + f"r4_{i}", f"MOD{v}4"], [L + f"p3t1{s2}"])
            if moe:
                TT("pool", xt[s2], t1[s2], MOD[:, v, 3, :], ALU.add, [L + f"p3t1{s2}", f"MOD{v}3"], [L + f"p3xt{s2}"])
                CP("act", hb[s2], xt[s2], [L + f"p3xt{s2}"], [L + f"p3hb{s2}"])
                for e_ in range(8):
                    TT("dve", rj, xt[s2], RB[:, e_, :], ALU.mult, [L + f"p3xt{s2}", L + "rb"], [L + "rj"])
                    RED("dve", LOGI[:, i, e_:e_ + 1], rj, [L + "rj"], [L + f"logi{i}"])
                lg_ = LOGI[:, i, :]
                RED("dve", sm[:, 0, 0:1], lg_, [L + f"logi{i}"], [L + "sm"], mx=True)
                TS("dve", sm[:, 1, :], lg_, sm[:, 0, 0:1], ALU.is_equal, [L + f"logi{i}", L + "sm"], [L + "sm"])
                STT("dve", sm[:, 2, :], sm[:, 1, :], -1e30, lg_, ALU.mult, ALU.add, [L + "sm", L + f"logi{i}"], [L + "sm"])
                RED("dve", sm[:, 0, 1:2], sm[:, 2, :], [L + "sm"], [L + "sm"], mx=True)
                TS("dve", sm[:, 3, :], lg_, sm[:, 0, 1:2], ALU.is_ge, [L + f"logi{i}", L + "sm"], [L + "sm"])
                TS("dve", sm[:, 0, 2:3], sm[:, 0, 0:1], -1.0, ALU.mult, [L + "sm"], [L + "sm"])
                ACTV(sm[:, 4, :], lg_, AF.Exp, [L + f"logi{i}", L + "sm"], [L + "sm"], bias=sm[:, 0, 2:3])
                TT("dve", sm[:, 4, :], sm[:, 4, :], sm[:, 3, :], ALU.mult, [L + "sm"], [L + "sm"])
                RED("dve", sm[:, 0, 3:4], sm[:, 4, :], [L + "sm"], [L + "sm"])
                RECIP(sm[:, 0, 3:4], sm[:, 0, 3:4], [L + "sm"], [L + "sm"])
                TS("dve", GATES[:, i, :], sm[:, 4, :], sm[:, 0, 3:4], ALU.mult, [L + "sm"], [L + f"gates{i}"])
            else:
                TT("pool", hb[s2], t1[s2], MOD[:, v, 3, :], ALU.add, [L + f"p3t1{s2}", f"MOD{v}3"], [L + f"p3hb{s2}"])

        def p3_tr(i):
            s2 = i % 2
            ts = slice(i * 128, (i + 1) * 128)
            pt_ = 4 + s2
            for kc in range(8):
                TR(bank_bf(pt_)[:, kc * 128:(kc + 1) * 128], hb[s2][:, kc * 128:(kc + 1) * 128], ident_bf, [L + f"p3hb{s2}", "ident_bf"], [f"ps{pt_}"])
            CP("act", h2T[:, :, ts], bank_bf(pt_).rearrange("p (k t) -> p k t", t=128), [f"ps{pt_}"], [L + f"h2T{i}"])

        p3_mm(0)
        for i in range(nto):
            p3_chain(i)
            if i + 1 < nto:
                p3_mm(i + 1)
            p3_tr(i)
        H2_ALL = [L + f"h2T{i}" for i in range(nto)]
        A.pop()
        if check_stop(f"p3_{l}"):
            A.pop(); break

        P.barrier()
        Y = A.alloc([nto, 1024], F32)
        A.push()
        wg = [A.alloc([8, 256], BF16) for _ in range(2)]
        wu = [A.alloc([8, 256], BF16) for _ in range(2)]
        wd = [A.alloc([2, 1024], BF16) for _ in range(2)]
        sg = [A.alloc([512], BF16) for _ in range(2)]
        AT = [A.alloc([2, 512], BF16) for _ in range(2)]
        ntok = nto * 128
        tblocks = [(t0, min(512, ntok - t0)) for t0 in range(0, ntok, 512)]
        if moe:
            slabs = [(e_, s_) for e_ in range(8) for s_ in range(14)]
        else:
            slabs = [(None, s_) for s_ in range(11)]
        nmm = 0
        for si, (e_, s_) in enumerate(slabs):
            sl = si % 2
            if moe:
                gsrc = I["mwg"][e_].rearrange("(kc p) f -> p kc f", p=128)[:, :, s_ * 256:(s_ + 1) * 256]
                usrc = I["mwu"][e_].rearrange("(kc p) f -> p kc f", p=128)[:, :, s_ * 256:(s_ + 1) * 256]
                dsrc = I["mwd"][e_][s_ * 256:(s_ + 1) * 256, :].rearrange("(c p) n -> p c n", p=128)
            else:
                gsrc = I["fwg"].rearrange("(kc p) f -> p kc f", p=128)[:, :, s_ * 256:(s_ + 1) * 256]
                usrc = I["fwu"].rearrange("(kc p) f -> p kc f", p=128)[:, :, s_ * 256:(s_ + 1) * 256]
                dsrc = I["fwd"][s_ * 256:(s_ + 1) * 256, :].rearrange("(c p) n -> p c n", p=128)
            DMA("pool", wg[sl], gsrc, [], [L + f"wg{sl}"], f"fw{sl}")
            DMA("pool", wu[sl], usrc, [], [L + f"wu{sl}"], f"fw{sl}")
            DMA("pool", wd[sl], dsrc, [], [L + f"wd{sl}"], f"fw{sl}")
            for bi, (t0, nt_) in enumerate(tblocks):
                a2 = bi % 2
                for fcl in range(2):
                    pg = nmm % 2; nmm += 1
                    for kc in range(8):
                        MM(bank(pg, nt_), wg[sl][:, kc, fcl * 128:(fcl + 1) * 128], h2T[:, kc, t0:t0 + nt_], kc == 0, kc == 7, H2_ALL + [L + f"wg{sl}"], [f"ps{pg}"])
                    for kc in range(8):
                        MM(bank(2 + pg, nt_), wu[sl][:, kc, fcl * 128:(fcl + 1) * 128], h2T[:, kc, t0:t0 + nt_], kc == 0, kc == 7, H2_ALL + [L + f"wu{sl}"], [f"ps{2 + pg}"])
                    ACTV(sg[pg][:, 0:nt_], bank(pg, nt_), AF.Silu, [f"ps{pg}"], [L + f"sg{pg}"])
                    TT("dve", AT[a2][:, fcl, 0:nt_], sg[pg][:, 0:nt_], bank(2 + pg, nt_), ALU.mult, [L + f"sg{pg}", f"ps{2 + pg}"], [L + f"at{a2}_{fcl}"])
                for tt in range(nt_ // 128):
                    ti = t0 // 128 + tt
                    py = 4 + 2 * (ti % 2)
                    for hf in range(2):
                        for fcl in range(2):
                            MM(bank(py + hf), AT[a2][:, fcl, tt * 128:(tt + 1) * 128], wd[sl][:, fcl, hf * 512:(hf + 1) * 512], fcl == 0, fcl == 1,
                               [L + f"at{a2}_0", L + f"at{a2}_1", L + f"wd{sl}"], [f"ps{py + hf}"])
                    yps = psum[:, 512 * py:512 * py + 1024]
                    rr = [f"ps{py}", f"ps{py + 1}"]
                    if moe:
                        gsc = GATES[:, ti, e_:e_ + 1]
                        if si == 0:
                            TS("dve", Y[:, ti, :], yps, gsc, ALU.mult, rr + [L + f"gates{ti}"], [L + f"Y{ti}"])
                        else:
                            STT("dve", Y[:, ti, :], yps, gsc, Y[:, ti, :], ALU.mult, ALU.add, rr + [L + f"gates{ti}", L + f"Y{ti}"], [L + f"Y{ti}"])
                    else:
                        if si == 0:
                            CP("dve", Y[:, ti, :], yps, rr, [L + f"Y{ti}"])
                        else:
                            TT("dve", Y[:, ti, :], yps, Y[:, ti, :], ALU.add, rr + [L + f"Y{ti}"], [L + f"Y{ti}"])
        A.pop()
        if check_stop(f"p4_{l}"):
            A.pop(); break

        A.push()
        ss5 = A.alloc([NTC], F32); rs5 = A.alloc([NTC], F32)
        xt = [A.alloc([1024], F32) for _ in range(2)]
        t1 = [A.alloc([1024], F32) for _ in range(2)]
        xo = [A.alloc([1024], F32) for _ in range(2)]
        for i in range(nto):
            s2 = i % 2
            v = 0 if i < NT else 1
            ACTV(junk, Y[:, i, :], AF.Square, [L + f"Y{i}"], ["junk", L + f"s5_{i}"], accum=ss5[:, i:i + 1])
            rstd_from_ss(ss5[:, i:i + 1], 1024, rs5[:, i:i + 1], [L + f"s5_{i}", "epsc"], [L + f"r5_{i}"])
            DMA("sp", xt[s2], xtile_ap(xdst, i), [L + f"xs{i}"], [L + f"p5xt{s2}"], f"xt{s2}")
            STT("dve", t1[s2], Y[:, i, :], rs5[:, i:i + 1], MOD[:, v, 5, :], ALU.mult, ALU.mult, [L + f"Y{i}", L + f"r5_{i}", f"MOD{v}5"], [L + f"p5t1{s2}"])
            TT("pool", xo[s2], t1[s2], xt[s2], ALU.add, [L + f"p5t1{s2}", L + f"p5xt{s2}"], [L + f"xo{s2}"])
            if l == 0:
                DMA("sp", xtile_ap(xdst, i), xo[s2], [L + f"xo{s2}"], [L + f"xs{i}"], f"xst{s2}")
                if "dbg_x" in dbg:
                    dd = dbg["dbg_x"][i * 128:(i + 1) * 128, :] if i < NT else dbg["dbg_xc"][(i - NT) * 128:(i - NT + 1) * 128, :]
                    DMA("sp", dd, xo[s2], [L + f"xo{s2}"], [L + f"dbgx{i}"], "dbg")
            else:
                DMA("sp", out[i * 128:(i + 1) * 128, :], xo[s2], [L + f"xo{s2}"], [f"out{i}"], f"xst{s2}")
        A.pop()
        A.pop()
        if check_stop(f"l{l}"):
            break
    return nc, P, es, A, I


def _emit(nc, P, es):
    tls = P.finalize()
    sems = {tl: es.enter_context(nc.semaphore("s_" + str(tl))) for tl in tls}
    with nc.Block() as block:
        block.sync(P.engine_body("sp", sems, final=True))
        block.tensor(P.engine_body("pe", sems))
        block.vector(P.engine_body("dve", sems))
        block.scalar(P.engine_body("act", sems))
        block.gpsimd(P.engine_body("pool", sems))


_CACHE = {}


def _get_program():
    if "nc" not in _CACHE:
        nc, P, es, A, I = build_program()
        _CACHE["inputs"] = list(I.keys())
        with es:
            _emit(nc, P, es)
        _CACHE["nc"] = nc
        _CACHE["peak"] = A.peak
        _CACHE["nops"] = len(P.ops)
    return _CACHE["nc"]


def _host_inputs(inp):
    f = lambda a: np.ascontiguousarray(np.asarray(a, dtype=np.float32))
    shared = {}
    for l in range(2):
        shared[f"wmod{l}"] = f(inp["w_mod"][l])
        shared[f"bmod{l}"] = f(inp["b_mod"][l]).reshape(1, 6144)
        shared[f"gvec{l}"] = f(np.concatenate([inp["g_attn_pre"][l], inp["g_attn_post"][l], inp["g_ffn_pre"][l], inp["g_ffn_post"][l]])).reshape(1, 4096)
        shared[f"win{l}"] = f(np.asarray(inp["w_in"][l])[:, WIN_PERM])
        shared[f"wout{l}"] = f(inp["w_out"][l])
        shared[f"dal{l}"] = f(np.concatenate([inp["da_lambda_q1"][l], inp["da_lambda_k1"][l], inp["da_lambda_q2"][l], inp["da_lambda_k2"][l]])).reshape(1, 128)
        shared[f"subln{l}"] = f(inp["da_subln"][l]).reshape(1, 64)
        shared[f"sink{l}"] = f(inp["swa_sink"][l]).reshape(1, 4)
        shared[f"gam{l}"] = f(np.concatenate([inp["ret_gamma_fwd"][l], inp["ret_gamma_bwd"][l]])).reshape(1, 8)
        shared[f"nabias{l}"] = _na_bias_layout(np.asarray(inp["na_rpb"][l], dtype=np.float32))
    shared["fwg"] = f(inp["ffn_w_gate"][0]); shared["fwu"] = f(inp["ffn_w_up"][0]); shared["fwd"] = f(inp["ffn_w_down"][0])
    shared["router"] = f(np.asarray(inp["moe_router"][0]).T).reshape(1, 8 * 1024)
    shared["mwg"] = f(inp["moe_w_gate"][0]); shared["mwu"] = f(inp["moe_w_up"][0]); shared["mwd"] = f(inp["moe_w_down"][0])
    shared["idxb"] = _idxb_table()
    x = np.asarray(inp["x"], dtype=np.float32); ctx = np.asarray(inp["ctx"], dtype=np.float32)
    c = np.asarray(inp["c"], dtype=np.float32); c_ctx = np.asarray(inp["c_ctx"], dtype=np.float32)
    maps = []
    for core in range(8):
        b, j = core // 4, core % 4
        m = dict(shared)
        m["xin"] = np.ascontiguousarray(x[b, TOK * j:TOK * (j + 1)])
        m["xcin"] = np.ascontiguousarray(ctx[b])
        m["cvec"] = np.ascontiguousarray(np.concatenate([c[b].reshape(8, 128).T, c_ctx.reshape(8, 128).T], axis=1))
        C64, S64 = _rope_tables(j, 64)
        C32, S32 = _rope_tables(j, 32)
        m["rope64"] = np.ascontiguousarray(np.stack([C64, S64], axis=1))
        m["rope32"] = np.ascontiguousarray(np.stack([C32, S32], axis=1))
        m["swamask"] = _swa_masks(j)
        m["namask"] = _na_masks(j)
        m["retc"] = _ret_consts(j)
        if "inputs" in _CACHE:
            m = {k: v for k, v in m.items() if k in _CACHE["inputs"]}
        maps.append(m)
    return maps


def kernel(**inputs):
    nc = _get_program()
    maps = _host_inputs(inputs)
    res = run_bass_kernel_spmd(nc, maps, core_ids=list(range(8)))
    _CACHE["last"] = res
    outp = np.empty((2, 8192, 1024), np.float32)
    for core in range(8):
        b, j = core // 4, core % 4
        outp[b, TOK * j:TOK * (j + 1)] = res.results[core]["out"]
    return outp
```

```python
import contextlib
import os
import math
import numpy as np
import concourse.bass as bass
import concourse.mybir as mybir
from concourse.bass_utils import run_bass_kernel_spmd

F32 = mybir.dt.float32
BF16 = mybir.dt.bfloat16
AF = mybir.ActivationFunctionType
ALU = mybir.AluOpType
AX = mybir.AxisListType
ENGS = ("pe", "act", "dve", "pool", "sp")
EPS = 1e-6
NT = 16
NTC = 18
TOK = 2048
TOKC = 2304
DEBUG = []
STOP_AFTER = None


class Op:
    __slots__ = ("eng", "fn", "tl", "deps", "awaited", "count", "inc", "idx")


class Prog:
    def __init__(self):
        self.ops = []
        self.last_w = {}
        self.readers = {}
        self.tl_last = {}
        self.bar = set()
        self.bar_done = set(ENGS)

    def op(self, eng, fn, reads=(), writes=(), tl=None, inc=1):
        o = Op()
        o.eng = eng
        o.fn = fn
        o.tl = tl if tl is not None else eng
        o.inc = inc
        o.awaited = o.tl not in ENGS
        o.count = None
        o.idx = len(self.ops)
        deps = set()
        for r in reads:
            w = self.last_w.get(r)
            if w is not None:
                deps.add(w)
        for w_ in writes:
            w = self.last_w.get(w_)
            if w is not None:
                deps.add(w)
            rl = self.readers.get(w_)
            if rl:
                deps.update(rl)
        if eng not in self.bar_done:
            deps |= self.bar
            self.bar_done.add(eng)
        o.deps = deps
        self.ops.append(o)
        for r in reads:
            self.readers.setdefault(r, []).append(o.idx)
        for w_ in writes:
            self.last_w[w_] = o.idx
            self.readers[w_] = []
        self.tl_last[o.tl] = o.idx
        return o

    def barrier(self):
        self.bar = set(self.tl_last.values())
        self.bar_done = set()

    def finalize(self):
        ops = self.ops
        for i in self.tl_last.values():
            ops[i].awaited = True
        for o in ops:
            for d in o.deps:
                od = ops[d]
                if od.tl == "pe" and o.tl == "pe":
                    continue
                od.awaited = True
        cnt = {}
        for o in ops:
            if o.awaited:
                cnt[o.tl] = cnt.get(o.tl, 0) + o.inc
                o.count = cnt[o.tl]
        self.totals = cnt
        run_latest = {}
        self.need = [None] * len(ops)
        for o in ops:
            need = {}
            for d in o.deps:
                od = ops[d]
                if od.tl == "pe" and o.tl == "pe":
                    continue
                v = od.count if od.tl in ENGS else run_latest[od.tl]
                if need.get(od.tl, 0) < v:
                    need[od.tl] = v
            self.need[o.idx] = need
            if o.awaited:
                run_latest[o.tl] = o.count
        return sorted(cnt.keys(), key=str)

    def engine_body(self, ename, sems, final=False):
        mine = [o for o in self.ops if o.eng == ename]

        def body(e):
            waited = {}
            for o in mine:
                for tl, v in self.need[o.idx].items():
                    if waited.get(tl, 0) < v:
                        e.wait_ge(sems[tl], v)
                        waited[tl] = v
                ins = o.fn(e)
                if o.awaited:
                    ins.then_inc(sems[o.tl], o.inc)
            if final:
                for tl, v in self.totals.items():
                    if waited.get(tl, 0) < v:
                        e.wait_ge(sems[tl], v)
        return body


class Arena:
    def __init__(self, ap, nbytes, prog=None):
        self.prog = prog
        self.ap = ap
        self.cap = nbytes
        self.off = 0
        self.stack = []
        self.peak = 0

    def alloc(self, shape, dt):
        shape = list(shape)
        n = int(np.prod(shape))
        nb = n * (4 if dt == F32 else 2)
        nb = (nb + 63) // 64 * 64
        assert self.off + nb <= self.cap, f"SBUF arena overflow {self.off}+{nb}>{self.cap}"
        v = self.ap[:, self.off // 2:(self.off + nb) // 2]
        if dt == F32:
            v = v.bitcast(F32)
        v = v[:, 0:n]
        self.off += nb
        self.peak = max(self.peak, self.off)
        if len(shape) == 2:
            v = v.rearrange("p (a b) -> p a b", b=shape[1])
        elif len(shape) == 3:
            v = v.rearrange("p (a b c) -> p a b c", b=shape[1], c=shape[2])
        elif len(shape) == 4:
            v = v.rearrange("p (a b c d) -> p a b c d", b=shape[1], c=shape[2], d=shape[3])
        return v

    def push(self):
        self.stack.append(self.off)

    def pop(self):
        self.off = self.stack.pop()
        if self.prog is not None:
            self.prog.barrier()


def _swap_idx(dh):
    q = dh // 4
    return np.concatenate([np.arange(q, 2 * q), np.arange(0, q), np.arange(3 * q, 4 * q), np.arange(2 * q, 3 * q)])


def _win_perm():
    cols = []
    base = 0
    q = np.arange(base, base + 256)
    k = np.arange(base + 256, base + 512)
    v = np.arange(base + 512, base + 768)
    sw32 = np.concatenate([_swap_idx(32) + 32 * i for i in range(8)])
    cols += [q, k, q[sw32], k[sw32], v]
    base = 768
    qn = np.arange(base, base + 256).reshape(2, 2, 64)
    qperm = np.transpose(qn, (1, 0, 2)).reshape(256)
    kk = np.arange(base + 256, base + 384)
    vv = np.arange(base + 384, base + 512)
    sw64_4 = np.concatenate([_swap_idx(64) + 64 * i for i in range(4)])
    sw64_2 = np.concatenate([_swap_idx(64) + 64 * i for i in range(2)])
    cols += [qperm, kk, qperm[sw64_4], kk[sw64_2], vv]
    base = 1280
    cols += [np.arange(base, base + 768)]
    base = 2048
    q = np.arange(base, base + 256)
    k = np.arange(base + 256, base + 512)
    vg = np.arange(base + 512, base + 1024)
    cols += [q, k, q[sw64_4], k[sw64_4], vg]
    return np.concatenate(cols)


WIN_PERM = _win_perm()
NWIN = len(WIN_PERM)
DA0, SW0, NA0, RT0 = 0, 1280, 2176, 2944


def _rope_tables(j, dh):
    t = 2048 * j + np.arange(2048)
    row = (t // 64).astype(np.float64)
    col = (t % 64).astype(np.float64)
    half = dh // 2
    qd = dh // 4
    inv = 10000.0 ** (-np.arange(qd, dtype=np.float64) * 2.0 / half)
    C = np.zeros((128, 2048), np.float32)
    S = np.zeros((128, 2048), np.float32)
    for p in range(128):
        d = p % dh
        pos = row if d < half else col
        dd = d % half
        i = dd % qd
        ang = pos * inv[i]
        C[p] = np.cos(ang)
        S[p] = -np.sin(ang) if dd < qd else np.sin(ang)
    return C, S


def _swa_masks(j):
    kk = np.arange(128)[:, None]
    qq = np.arange(128)[None, :]
    mprev = (qq <= kk).astype(np.float32)
    mnext = (kk <= qq).astype(np.float32)
    m = np.zeros((10, 128, 128), np.float32)
    m[0] = mprev
    m[1] = mnext
    for r in range(4):
        if r == j - 1:
            m[2 + r] = mprev
        if r == j + 1:
            m[6 + r] = mnext
    return m


def _na_mask(Tq, Tk, flag=True):
    if (not flag) or Tk < 0 or Tk > 63:
        return np.zeros((128, 128), np.float32)
    p = np.arange(128)
    Rk = (2 * Tk + p // 64)[:, None]
    kc = (p % 64)[:, None]
    Rq = (2 * Tq + p // 64)[None, :]
    qc = (p % 64)[None, :]
    start = np.clip(Rq - 4, 0, 120)
    cs = np.clip(qc - 8, 0, 48)
    ok = (Rk >= start) & (Rk < start + 8) & (kc >= cs) & (kc < cs + 16)
    return ok.astype(np.float32)


def _na_masks(j):
    m = np.zeros((45, 128, 128), np.float32)
    for d in range(-2, 3):
        m[d + 2] = _na_mask(10, 10 + d)
    T0 = 16 * j
    idx = 5
    for d in (0, 1, 2, 3):
        m[idx] = _na_mask(T0, T0 + d); idx += 1
    for r in range(4):
        m[idx] = _na_mask(T0, T0 - 2, r == j - 1); idx += 1
    for r in range(4):
        m[idx] = _na_mask(T0, T0 - 1, r == j - 1); idx += 1
    for d in (-1, 0, 1, 2):
        m[idx] = _na_mask(T0 + 1, T0 + 1 + d); idx += 1
    for r in range(4):
        m[idx] = _na_mask(T0 + 1, T0 - 1, r == j - 1); idx += 1
    for d in (-2, -1, 0, 1):
        m[idx] = _na_mask(T0 + 14, T0 + 14 + d); idx += 1
    for r in range(4):
        m[idx] = _na_mask(T0 + 14, T0 + 16, r == j + 1); idx += 1
    for d in (-3, -2, -1, 0):
        m[idx] = _na_mask(T0 + 15, T0 + 15 + d); idx += 1
    for r in range(4):
        m[idx] = _na_mask(T0 + 15, T0 + 16, r == j + 1); idx += 1
    for r in range(4):
        m[idx] = _na_mask(T0 + 15, T0 + 17, r == j + 1); idx += 1
    assert idx == 45
    return m


def _na_bias_layout(rpb):
    p = np.arange(128)
    kr = (p // 64)[:, None]; kc = (p % 64)[:, None]
    qr = (p // 64)[None, :]; qc = (p % 64)[None, :]
    out = np.empty((7, 4, 128, 128), np.float32)
    dc = np.clip(kc - qc, -15, 15) + 15
    for di, d in enumerate(range(-3, 4)):
        dr = np.clip(2 * d + kr - qr, -7, 7) + 7
        out[di] = rpb[:, dr, dc]
    return out


def _ret_consts(j):
    c = np.zeros((128, 700), np.float32)
    i = np.arange(128)
    o = 0
    dif = i[None, :] - i[:, None]
    c[:, 0:128] = np.maximum(dif, 0)
    c[:, 128:256] = (dif >= 0) * 0.125
    c[:, 256:384] = np.maximum(-dif, 0)
    c[:, 384:512] = (dif < 0) * 0.125
    c[:, 512:640] = (i + 1)[None, :]
    c[:, 640] = 127 - i
    c[:, 641] = i
    c[:, 642:660] = (128.0 * np.arange(18))[None, :]
    c[:, 660:678] = (128.0 * (15 - np.arange(18)))[None, :]
    for r in range(4):
        if r < j:
            c[:, 678 + r] = 2048.0 * (j - 1 - r); c[:, 688 + r] = 1.0
        if r > j:
            c[:, 683 + r] = 2048.0 * (r - j - 1); c[:, 693 + r] = 1.0
    c[:, 682] = 2048.0 * j; c[:, 692] = 1.0
    c[:, 687] = 2048.0 * (3 - j); c[:, 697] = 1.0
    return c


def _idxb_table():
    i = np.arange(128)
    return np.broadcast_to((128 - i)[None, :], (128, 128)).astype(np.float32).copy()


def build_program():
    nc = bass.Bass("TRN2", target_bir_lowering=False)
    P = Prog()
    es = contextlib.ExitStack()

    def din(name, shape, dt=F32):
        return nc.dram_tensor(name, list(shape), dt, kind="ExternalInput").ap()

    def dint(name, shape, dt):
        return nc.dram_tensor(name, list(shape), dt)

    SHAPES = {"xin": [TOK, 1024], "xcin": [256, 1024], "cvec": [128, 16], "fwg": [1024, 2816], "fwu": [1024, 2816], "fwd": [2816, 1024],
              "router": [1, 8 * 1024], "mwg": [8, 1024, 3584], "mwu": [8, 1024, 3584], "mwd": [8, 3584, 1024],
              "rope64": [128, 2, 2048], "rope32": [128, 2, 2048], "swamask": [10, 128, 128], "namask": [45, 128, 128],
              "retc": [128, 700], "idxb": [128, 128]}
    for l_ in range(2):
        SHAPES.update({f"wmod{l_}": [1024, 6144], f"bmod{l_}": [1, 6144], f"gvec{l_}": [1, 4096], f"win{l_}": [1024, NWIN],
                       f"wout{l_}": [1024, 1024], f"dal{l_}": [1, 128], f"subln{l_}": [1, 64], f"sink{l_}": [1, 4],
                       f"gam{l_}": [1, 8], f"nabias{l_}": [7, 4, 128, 128]})

    class LazyIn(dict):
        def __missing__(self, k):
            self[k] = din(k, SHAPES[k])
            return self[k]
    I = LazyIn()
    USED_INPUTS = I
    out = nc.dram_tensor("out", [TOK, 1024], F32, kind="ExternalOutput").ap()
    dbg = {}
    for name, shape, dt in (("dbg_ot", [NTC, 128, 8, 128], BF16), ("dbg_x", [TOK, 1024], F32), ("dbg_xc", [256, 1024], F32),
                            ("dbg_misc", [128, 4096], F32)):
        if name in DEBUG:
            dbg[name] = nc.dram_tensor(name, shape, dt, kind="ExternalOutput").ap()

    xs = dint("xs", [TOK, 1024], F32).ap(); xcs = dint("xcs", [256, 1024], F32).ap()
    GROUPS = [[0, 1, 2, 3], [4, 5, 6, 7]]

    arena_t = es.enter_context(nc.sbuf_tensor("arena", [128, 94 * 1024], BF16))
    A = Arena(arena_t, 188 * 1024, P)
    psum = es.enter_context(nc.psum_tensor("psum", [128, 4096], F32))

    def bank(i, n=512, off=0):
        return psum[:, 512 * i + off:512 * i + off + n]

    def bank_bf(i):
        return psum[:, 512 * i:512 * (i + 1)].bitcast(BF16)

    def MM(o, lhsT, rhs, st, sp_, r, w, tp=None, sgc=False):
        kw = {}
        if tp is not None:
            kw["tile_position"] = tp
        if sgc:
            kw["skip_group_check"] = True
        P.op("pe", lambda e: e.matmul(o, lhsT=lhsT, rhs=rhs, start=st, stop=sp_, **kw), r, w)

    def MM64(o, lhsT, rhs, base, st, sp_, r, w):
        if base == 0:
            MM(o, lhsT[0:64], rhs[0:64], st, sp_, r, w, sgc=True)
        else:
            MM(o, lhsT[64:96], rhs[64:96], st, False, r, w, tp=(64, 0), sgc=True)
            MM(o, lhsT[96:128], rhs[96:128], False, sp_, r, w, tp=(96, 0), sgc=True)

    def TR(o, i, ident, r, w):
        P.op("pe", lambda e: e.transpose(o, i, ident), r, w)

    def ACTV(o, i, func, r, w, bias=None, scale=None, accum=None):
        kw = {}
        if bias is not None:
            kw["bias"] = bias
        if scale is not None:
            kw["scale"] = scale
        if accum is not None:
            kw["accum_out"] = accum
        P.op("act", lambda e: e.activation(out=o, in_=i, func=func, **kw), r, w)

    def TT(eng, o, a, b, op, r, w):
        P.op(eng, lambda e: e.tensor_tensor(out=o, in0=a, in1=b, op=op), r, w)

    def TS(eng, o, a, s1, op0, r, w, s2=None, op1=None):
        if op1 is None:
            P.op(eng, lambda e: e.tensor_scalar(out=o, in0=a, scalar1=s1, scalar2=None, op0=op0), r, w)
        else:
            P.op(eng, lambda e: e.tensor_scalar(out=o, in0=a, scalar1=s1, scalar2=s2, op0=op0, op1=op1), r, w)

    def STT(eng, o, a, s, b, op0, op1, r, w):
        P.op(eng, lambda e: e.scalar_tensor_tensor(out=o, in0=a, scalar=s, in1=b, op0=op0, op1=op1), r, w)

    def CP(eng, o, i, r, w):
        if eng == "act":
            P.op("act", lambda e: e.copy(out=o, in_=i), r, w)
        else:
            P.op(eng, lambda e: e.tensor_copy(out=o, in_=i), r, w)

    def MSET(eng, o, val, w):
        P.op(eng, lambda e: e.memset(o, val), (), w)

    def RED(eng, o, i, r, w, mx=False):
        if mx:
            P.op(eng, lambda e: e.reduce_max(out=o, in_=i, axis=AX.X), r, w)
        else:
            P.op(eng, lambda e: e.reduce_sum(out=o, in_=i, axis=AX.X), r, w)

    def RECIP(o, i, r, w):
        P.op("dve", lambda e: e.reciprocal(out=o, in_=i), r, w)

    def DMA(q, o, i, r, w, tl):
        P.op(q, lambda e: e.dma_start(out=o, in_=i), r, w, tl=tl, inc=16)

    def AG(src, dst, r, w, tl):
        P.op("pool", lambda e: e.collective_compute("AllGather", ALU.bypass, replica_groups=GROUPS,
                                                    ins=[src.ap().opt()], outs=[dst.ap().opt()]), r, w, tl=tl, inc=1)

    def rstd_from_ss(ss, n, rstd, r, w):
        ACTV(rstd, ss, AF.Sqrt, r, w, bias=epsc[:, 0:1], scale=1.0 / n)
        RECIP(rstd, rstd, w, w)

    ident_bf = A.alloc([128], BF16); ident_f = A.alloc([128], F32); zeros = A.alloc([128], BF16)
    epsc = A.alloc([1], F32)
    junk = A.alloc([1024], BF16)
    MOD = A.alloc([2, 6, 1024], BF16)
    MSET("pool", ident_f, 0.0, ["ident_f"])
    P.op("pool", lambda e: e.affine_select(out=ident_f, in_=ident_f, pattern=[[-1, 128]], compare_op=ALU.not_equal,
                                           fill=1.0, base=0, channel_multiplier=1), ["ident_f"], ["ident_f"])
    CP("pool", ident_bf, ident_f, ["ident_f"], ["ident_bf"])
    HM = A.alloc([2], F32)
    RED("dve", HM[:, 0:1], ident_f[:, 0:64], ["ident_f"], ["HM"])
    RED("dve", HM[:, 1:2], ident_f[:, 64:128], ["ident_f"], ["HM"])
    MSET("pool", zeros, 0.0, ["zeros"])
    MSET("pool", epsc, EPS, ["epsc"])

    def pbc(ap):
        b = ap.partition_broadcast(128)
        if len(b.shape) == 3 and b.shape[1] == 1:
            b = b[:, 0]
        return b

    stop = [False]

    def tap(name, ap, reads, flat):
        if name in DEBUG:
            shp = [128, int(np.prod(ap.shape[1:]))]
            d = nc.dram_tensor(name, shp, ap.dtype, kind="ExternalOutput").ap()
            DMA("sp", d, ap.rearrange(flat) if flat else ap, reads, [name], "dbg")

    def check_stop(name):
        if STOP_AFTER == name:
            stop[0] = True
        return stop[0]

    for l in range(2):
        if stop[0]:
            break
        with_ctx = (l == 0)
        lam_init = 0.8 - 0.6 * math.exp(-0.3 * l)
        ntl = NTC
        nto = NTC if with_ctx else NT
        xsrc = (I["xin"], I["xcin"]) if l == 0 else (xs, xcs)
        L = f"L{l}"

        def xtile_ap(src2, i):
            return src2[0][i * 128:(i + 1) * 128, :] if i < NT else src2[1][(i - NT) * 128:(i - NT + 1) * 128, :]

        P.barrier()
        A.push()
        cv = A.alloc([16], F32); sil = A.alloc([16], F32); sbc = A.alloc([2, 8, 128], BF16)
        gv = A.alloc([4, 1024], F32); tmpm = A.alloc([1024], F32)
        wm = [A.alloc([8, 1024], BF16) for _ in range(2)]
        bs = [A.alloc([1024], F32) for _ in range(2)]
        DMA("sp", cv, I["cvec"], [], [L + "cv"], "m0")
        DMA("sp", gv, pbc(I[f"gvec{l}"]).rearrange("p (a b) -> p a b", b=1024), [], [L + "gv"], "m0")
        ACTV(sil, cv, AF.Silu, [L + "cv"], [L + "sil"])
        for v in range(2):
            for kc in range(8):
                ACTV(sbc[:, v, kc, :], zeros, AF.Identity, [L + "sil", "zeros"], [L + "sbc"], bias=sil[:, v * 8 + kc:v * 8 + kc + 1])
        wmv = I[f"wmod{l}"].rearrange("(kc p) n -> p kc n", p=128)
        for s in range(6):
            sl = s % 2
            DMA("pool", wm[sl], wmv[:, :, s * 1024:(s + 1) * 1024], [], [L + f"wm{sl}"], f"wm{sl}")
            DMA("sp", bs[sl], pbc(I[f"bmod{l}"][0:1, s * 1024:(s + 1) * 1024]), [], [L + f"bs{sl}"], f"bs{sl}")
            for v in range(2):
                pb = 4 * (s % 2) + 2 * v
                for hf in range(2):
                    for kc in range(8):
                        MM(bank(pb + hf), sbc[:, v, kc, :], wm[sl][:, kc, hf * 512:(hf + 1) * 512], kc == 0, kc == 7,
                           [L + "sbc", L + f"wm{sl}"], [f"ps{pb + hf}"])
                TT("dve", tmpm, psum[:, 512 * pb:512 * pb + 1024], bs[sl], ALU.add, [f"ps{pb}", f"ps{pb + 1}", L + f"bs{sl}"], [L + "tmpm"])
                dst = MOD[:, v, s, :]
                if s in (0, 3):
                    CP("dve", dst, tmpm, [L + "tmpm"], [f"MOD{v}{s}"])
                elif s in (1, 4):
                    STT("dve", dst, tmpm, 1.0, gv[:, 0 if s == 1 else 2, :], ALU.add, ALU.mult, [L + "tmpm", L + "gv"], [f"MOD{v}{s}"])
                else:
                    TT("dve", dst, tmpm, gv[:, 1 if s == 2 else 3, :], ALU.mult, [L + "tmpm", L + "gv"], [f"MOD{v}{s}"])
        if l == 0:
            tap("t_mod", MOD, [f"MOD{v}{s}" for v in range(2) for s in range(6)], "p a b c -> p (a b c)")
        A.pop()
        if check_stop(f"p0_{l}"):
            break

        P.barrier()
        A.push()
        moe = (l == 1)
        if moe:
            LOGI = A.alloc([NT, 8], F32); GATES = A.alloc([NT, 8], F32)
        hT = A.alloc([8, TOKC], BF16)
        otd = dint(L + "otd", [NTC, 128, 8, 128], BF16).ap()
        A.push()
        otst = [A.alloc([2, 128], BF16) for _ in range(2)]

        A.push()
        xt = [A.alloc([1024], F32) for _ in range(2)]
        t1 = [A.alloc([1024], F32) for _ in range(2)]
        hb = [A.alloc([1024], BF16) for _ in range(2)]
        ssb = A.alloc([NTC], F32); rsb = A.alloc([NTC], F32)
        for i in range(ntl):
            s2 = i % 2
            v = 0 if i < NT else 1
            DMA("sp", xt[s2], xtile_ap(xsrc, i), [], [L + f"xt{s2}"], f"xt{s2}")
            ACTV(junk, xt[s2], AF.Square, [L + f"xt{s2}"], ["junk", L + f"ss{i}"], accum=ssb[:, i:i + 1])
            rstd_from_ss(ssb[:, i:i + 1], 1024, rsb[:, i:i + 1], [L + f"ss{i}", "epsc"], [L + f"rs{i}"])
            STT("dve", t1[s2], xt[s2], rsb[:, i:i + 1], MOD[:, v, 1, :], ALU.mult, ALU.mult, [L + f"xt{s2}", L + f"rs{i}", f"MOD{v}1"], [L + f"t1{s2}"])
            TT("dve", hb[s2], t1[s2], MOD[:, v, 0, :], ALU.add, [L + f"t1{s2}", f"MOD{v}0"], [L + f"hb{s2}"])
            for kc in range(8):
                TR(bank_bf(s2)[:, kc * 128:(kc + 1) * 128], hb[s2][:, kc * 128:(kc + 1) * 128], ident_bf, [L + f"hb{s2}", "ident_bf"], [f"ps{s2}"])
            CP("act", hT[:, :, i * 128:(i + 1) * 128], bank_bf(s2).rearrange("p (k t) -> p k t", t=128), [f"ps{s2}"], [L + f"hT{i}"])
        A.pop()
        HT_ALL = [L + f"hT{i}" for i in range(ntl)]
        if l == 0:
            tap("t_hT", hT, HT_ALL, "p a b -> p (a b)")
        if check_stop(f"p1a_{l}"):
            A.pop(); A.pop(); break

        winv = I[f"win{l}"].rearrange("(kc p) n -> p kc n", p=128)

        def proj_rope(wq, wqs, Ctab, Stab, dests, tag):
            tA = [A.alloc([512], F32) for _ in range(2)]
            tB = [A.alloc([512], F32) for _ in range(2)]
            n = 0
            for ci in range(len(wq)):
                for tb in range(4):
                    s2 = n % 2; n += 1
                    ts = slice(tb * 512, (tb + 1) * 512)
                    for kc in range(8):
                        MM(bank(s2), wq[ci][:, kc, :], hT[:, kc, ts], kc == 0, kc == 7, HT_ALL[4 * tb:4 * tb + 4] + [tag + "w"], [f"ps{s2}"])
                    for kc in range(8):
                        MM(bank(2 + s2), wqs[ci][:, kc, :], hT[:, kc, ts], kc == 0, kc == 7, HT_ALL[4 * tb:4 * tb + 4] + [tag + "w"], [f"ps{2 + s2}"])
                    TT("dve", tA[s2], bank(s2), Ctab[:, ts], ALU.mult, [f"ps{s2}", L + "rope"], [tag + f"tA{s2}"])
                    TT("dve", tB[s2], bank(2 + s2), Stab[:, ts], ALU.mult, [f"ps{2 + s2}", L + "rope"], [tag + f"tB{s2}"])
                    TT("dve", dests[ci][:, ts], tA[s2], tB[s2], ALU.add, [tag + f"tA{s2}", tag + f"tB{s2}"], [tag + f"d{ci}"])

        def proj_feat_plain(wq, dest, t0, nt, tag, ci, pb):
            for kc in range(8):
                MM(bank(pb, nt), wq[:, kc, :], hT[:, kc, t0:t0 + nt], kc == 0, kc == 7, HT_ALL + [tag + "w"], [f"ps{pb}"])
            CP("act", dest, bank(pb, nt), [f"ps{pb}"], [tag + f"d{ci}"])

        def proj_tok(wv, ncols, dest_fn, tiles, tag, post=None, view=None):
            for n, i in enumerate(tiles):
                pb = 4 + n % 2
                for kc in range(8):
                    MM(bank(pb, ncols), hT[:, kc, i * 128:(i + 1) * 128], wv[:, kc, :], kc == 0, kc == 7, [L + f"hT{i}", tag + "w"], [f"ps{pb}"])
                if post is None:
                    src_ = bank(pb, ncols)
                    if view is not None:
                        src_ = view(src_)
                    CP("act", dest_fn(i), src_, [f"ps{pb}"], [tag + f"v{i}"])
                else:
                    post(i, pb)

        def out_transposes(ytok, chunk0, i, tag, rd):
            pb = 6 + (i % 2)
            st = otst[i % 2]
            for cc in range(2):
                TR(bank_bf(pb)[:, cc * 128:(cc + 1) * 128], ytok[:, cc * 128:(cc + 1) * 128], ident_bf, rd + ["ident_bf"], [f"ps{pb}"])
            CP("act", st, bank_bf(pb)[:, 0:256].rearrange("p (c t) -> p c t", t=128), [f"ps{pb}"], [L + f"otst{i % 2}"])
            DMA("sp", otd[i, :, chunk0:chunk0 + 2, :], st, [L + f"otst{i % 2}"], [L + f"OT{chunk0}_{i}"], f"ot{i % 2}")

        def attn_pipeline(tiles_slots, S_fn, E_fn, AV_fn, FIN_fn):
            units = []
            for (i, slots) in tiles_slots:
                ngr = (len(slots) + 1) // 2
                for gi in range(ngr):
                    units.append((i, gi, ngr, slots[2 * gi:2 * gi + 2]))
            pending = None
            for k, u in enumerate(units):
                if k == 0:
                    S_fn(0, u)
                E_fn(k, u)
                if k + 1 < len(units):
                    S_fn(k + 1, units[k + 1])
                AV_fn(k, u)
                if pending is not None:
                    FIN_fn(pending); pending = None
                if u[1] == u[2] - 1:
                    pending = u[0]
            if pending is not None:
                FIN_fn(pending)

        rope64 = A.alloc([2, 2048], BF16); rope32 = A.alloc([2, 2048], BF16)
        DMA("pool", rope64, I["rope64"], [], [L + "rope"], "rp")
        DMA("pool", rope32, I["rope32"], [], [L + "rope"], "rp")

        T = L + "da"
        A.push()
        QT = A.alloc([2, TOKC], BF16); KTc = A.alloc([2, 256], BF16)
        Vc = A.alloc([2, 256], BF16)
        nlam = A.alloc([1], F32); gsub = A.alloc([64], F32)
        A.push()
        dl = A.alloc([4, 32], F32); pr = A.alloc([2, 32], F32); s12 = A.alloc([2], F32)
        DMA("sp", dl, pbc(I[f"dal{l}"]).rearrange("p (a b) -> p a b", b=32), [], [T + "dl"], "m0")
        DMA("sp", gsub, pbc(I[f"subln{l}"]), [], [T + "gsub"], "m0")
        TT("dve", pr[:, 0, :], dl[:, 0, :], dl[:, 1, :], ALU.mult, [T + "dl"], [T + "pr"])
        TT("dve", pr[:, 1, :], dl[:, 2, :], dl[:, 3, :], ALU.mult, [T + "dl"], [T + "pr"])
        RED("dve", s12, pr, [T + "pr"], [T + "s12"])
        ACTV(s12, s12, AF.Exp, [T + "s12"], [T + "s12"])
        TT("dve", nlam, s12[:, 1:2], s12[:, 0:1], ALU.subtract, [T + "s12"], [T + "nlam"])
        TS("dve", nlam, nlam, -lam_init, ALU.add, [T + "nlam"], [T + "nlam"])
        TS("dve", gsub, gsub, 1.0 - lam_init, ALU.mult, [T + "gsub"], [T + "gsub"])
        A.pop()
        A.push()
        wda = A.alloc([8, 1280], BF16)
        KTo = A.alloc([2, TOK], BF16); Vo = A.alloc([NT, 256], BF16)
        DMA("pool", wda, winv[:, :, DA0:DA0 + 1280], [], [T + "w"], "wA")
        qk_dest = [QT[:, 0, 0:TOK], QT[:, 1, 0:TOK], KTo[:, 0, :], KTo[:, 1, :]]
        proj_rope([wda[:, :, c * 128:(c + 1) * 128] for c in range(4)], [wda[:, :, 512 + c * 128:512 + (c + 1) * 128] for c in range(4)],
                  rope32[:, 0, :], rope32[:, 1, :], qk_dest, T)
        for ci in range(4):
            dest = QT[:, ci, TOK:TOKC] if ci < 2 else KTc[:, ci - 2, :]
            proj_feat_plain(wda[:, :, ci * 128:(ci + 1) * 128], dest, TOK, 256, T, f"c{ci}", ci % 2)
        proj_tok(wda[:, :, 1024:1280], 256, lambda i: Vo[:, i, :] if i < NT else Vc[:, i - NT, :], range(NTC), T)
        QK_R = [T + f"d{ci}" for ci in range(4)] + [T + f"dc{ci}" for ci in range(4)]
        V_R = [T + f"v{i}" for i in range(NTC)]
        if check_stop(f"daproj_{l}"):
            tap("t_qt", QT, QK_R, "p a b -> p (a b)")
            tap("t_kto", KTo, QK_R, "p a b -> p (a b)")
            tap("t_vo", Vo, V_R, "p a b -> p (a b)")
            tap("t_ktc", KTc, QK_R, "p a b -> p (a b)")
            tap("t_vc", Vc, V_R, "p a b -> p (a b)")
            A.pop(); A.pop(); A.pop(); A.pop(); break
        e_k = dint(T + "ek", [256, TOK], BF16); e_v = dint(T + "ev", [TOK, 256], BF16)
        g_k = dint(T + "gk", [1024, TOK], BF16); g_v = dint(T + "gv", [4 * TOK, 256], BF16)
        DMA("sp", e_k.ap().rearrange("(c p) t -> p c t", p=128), KTo, QK_R, [T + "ek"], "ex")
        DMA("sp", e_v.ap().rearrange("(i p) f -> p i f", p=128), Vo, V_R, [T + "ev"], "ex")
        AG(e_k, g_k, [T + "ek"], [T + "gk"], "cc")
        AG(e_v, g_v, [T + "ev"], [T + "gv"], "cc")
        A.pop()
        P.barrier()
        if check_stop(f"daag_{l}"):
            A.pop(); A.pop(); A.pop(); break
        KT = A.alloc([8448], BF16); V1 = A.alloc([66, 2, 65], BF16)
        ptH = [[A.alloc([2, 512], BF16) for _ in range(2)] for _ in range(2)]
        o_f = A.alloc([2, 64], F32); o1 = A.alloc([64], F32)
        rec = A.alloc([4], F32); rn = A.alloc([2], F32); ssd = A.alloc([2], F32); rsd = A.alloc([2], F32)
        yda = [A.alloc([2, 64], BF16) for _ in range(2)]
        MSET("pool", V1[:, :, :, 64:65], 1.0, [T + "V1ones"])
        g_kv = g_k.ap().rearrange("(r c p) t -> p r c t", r=4, c=2)
        g_vv = g_v.ap().rearrange("(r i p) f -> p r i f", r=4, i=NT)
        nblk = 0
        for c in range(2):
            CP("pool", KT[:, 0:256], KTc[:, c, :], QK_R, [T + "KT"])
            for r in range(4):
                DMA("sp", KT[:, 256 + r * TOK:256 + (r + 1) * TOK], g_kv[:, r, c, :], [T + "gk"], [T + "KT"], "kt")
                for hh in range(2):
                    DMA("sp", V1[:, 2 + r * NT:2 + (r + 1) * NT, hh, 0:64], g_vv[:, r, :, c * 128 + hh * 64:c * 128 + hh * 64 + 64],
                        [T + "gv"], [T + "V1"], "kt")
            CP("pool", V1[:, 0:2, :, 0:64], Vc[:, :, c * 128:(c + 1) * 128].rearrange("p i (h d) -> p i h d", d=64), V_R, [T + "V1"])
            if STOP_AFTER == f"daload_{l}":
                continue
            qblocks = [(qb * 512, 512, 0, 66) for qb in range(4)]
            if STOP_AFTER == f"daq1_{l}":
                qblocks = [(0, 512, 0, 66)] if c == 0 else []
            if STOP_AFTER == f"daq1nf_{l}":
                qblocks = [(0, 512, 0, 66)] if c == 0 else []
            if with_ctx:
                qblocks.append((TOK, 256, 0, 2))
            for (q0, nq, k0, k1) in qblocks:
                sc_ = 1.0 / math.sqrt(32.0)

                def S_half(kt, hf):
                    for g in (2 * hf, 2 * hf + 1):
                        MM(bank(g, nq), KT[32 * g:32 * g + 32, kt * 128:(kt + 1) * 128], QT[32 * g:32 * g + 32, c, q0:q0 + nq], True, True,
                           [T + "KT"] + QK_R, [f"psS{hf}"], tp=(32 * g, 0))

                def E_half(kt, hf):
                    s2 = (kt - k0) % 2
                    ACTV(ptH[hf][s2][:, :, 0:nq], psum[:, 1024 * hf:1024 * hf + 1024].rearrange("p (g n) -> p g n", n=512)[:, :, 0:nq], AF.Exp,
                         [f"psS{hf}"], [T + f"pt{hf}{s2}"], scale=sc_)

                def AV_half(kt, hf):
                    s2 = (kt - k0) % 2
                    for sb in range(nq // 128):
                        for gg in range(2):
                            g = 2 * hf + gg
                            MM(bank(4 + sb, 65, g * 65), ptH[hf][s2][:, gg, sb * 128:(sb + 1) * 128], V1[:, kt, hf, :], kt == k0 and g == 0, kt == k1 - 1,
                               [T + f"pt{hf}{s2}", T + "V1", T + "V1ones"], [f"psO{sb}"], sgc=True)

                S_half(k0, 0); S_half(k0, 1)
                for kt in range(k0, k1):
                    E_half(kt, 0); E_half(kt, 1)
                    AV_half(kt, 0)
                    if kt + 1 < k1:
                        S_half(kt + 1, 0)
                    AV_half(kt, 1)
                    if kt + 1 < k1:
                        S_half(kt + 1, 1)
                if STOP_AFTER == f"daq1nf_{l}":
                    continue
                for sb in range(nq // 128):
                    tile_i = (q0 + sb * 128) // 128
                    yb = yda[nblk % 2]; ybn = T + f"yda{nblk % 2}"; nblk += 1
                    bk = 4 + sb
                    pr_ = f"psO{sb}"
                    Tv = bank(bk, 260).rearrange("p (g e) -> p g e", e=65)
                    RECIP(rec, Tv[:, :, 64], [pr_], [T + "rec"])
                    TS("dve", rn, rec.rearrange("p (h m) -> p h m", m=2)[:, :, 1], nlam[:, 0:1], ALU.mult, [T + "rec", T + "nlam"], [T + "rn"])
                    for hh in range(2):
                        TS("dve", o1, Tv[:, 2 * hh, 0:64], rec[:, 2 * hh:2 * hh + 1], ALU.mult, [pr_, T + "rec"], [T + "o1"])
                        STT("dve", o_f[:, hh, :], Tv[:, 2 * hh + 1, 0:64], rn[:, hh:hh + 1], o1, ALU.mult, ALU.add, [pr_, T + "rn", T + "o1"], [T + "o_f"])
                        ACTV(junk[:, 0:64], o_f[:, hh, :], AF.Square, [T + "o_f"], ["junk", T + "ssd"], accum=ssd[:, hh:hh + 1])
                    rstd_from_ss(ssd, 64, rsd, [T + "ssd", "epsc"], [T + "rsd"])
                    for hh in range(2):
                        STT("dve", yb[:, hh, :], o_f[:, hh, :], rsd[:, hh:hh + 1], gsub, ALU.mult, ALU.mult, [T + "o_f", T + "rsd", T + "gsub"], [ybn])
                    TR(bank_bf(bk)[:, 640:768], yb.rearrange("p h d -> p (h d)"), ident_bf, [ybn, "ident_bf"], [pr_])
                    CP("act", otst[sb % 2][:, 0, :], bank_bf(bk)[:, 640:768], [pr_], [L + f"otst{sb % 2}"])
                    DMA("sp", otd[tile_i, :, c, :], otst[sb % 2][:, 0, :], [L + f"otst{sb % 2}"], [L + f"OT{c}_{tile_i}"], f"ot{sb % 2}")
        A.pop()
        if check_stop(f"da_{l}") or STOP_AFTER in (f"daload_{l}", f"daq1_{l}", f"daq1nf_{l}"):
            P.barrier()
            tap("t_nlam", nlam, [], None)
            tap("t_gsub", gsub, [], None)
            if "dbg_ot" in dbg:
                P.barrier()
                DMA("sp", dbg["dbg_ot"], otd, [], ["dbgot"], "dbg")
            A.pop(); A.pop(); break

        P.barrier()
        T = L + "sw"
        A.push()
        QT = A.alloc([2, TOKC], BF16)
        KT = A.alloc([3328], BF16)
        V1 = A.alloc([26, 2, 65], BF16)
        MSW = A.alloc([10, 128], BF16)
        esink = A.alloc([4], F32)
        DMA("pool", MSW, I["swamask"].rearrange("m k q -> k m q"), [], [T + "msw"], "wB")
        DMA("sp", esink, pbc(I[f"sink{l}"]), [], [T + "esink"], "m0")
        ACTV(esink, esink, AF.Exp, [T + "esink"], [T + "esink"])
        MSET("pool", V1[:, :, :, 64:65], 1.0, [T + "V1ones"])
        A.push()
        wsw = A.alloc([8, 896], BF16)
        DMA("pool", wsw, winv[:, :, SW0:SW0 + 896], [], [T + "w"], "wA")
        dests = [QT[:, 0, 0:TOK], QT[:, 1, 0:TOK], KT[:, 0:TOK]]
        proj_rope([wsw[:, :, c * 128:(c + 1) * 128] for c in range(3)], [wsw[:, :, 384 + c * 128:384 + (c + 1) * 128] for c in range(3)],
                  rope64[:, 0, :], rope64[:, 1, :], dests, T)
        for ci in range(3):
            dest = QT[:, ci, TOK:TOKC] if ci < 2 else KT[:, TOK:TOKC]
            proj_feat_plain(wsw[:, :, ci * 128:(ci + 1) * 128], dest, TOK, 256, T, f"c{ci}", ci % 2)
        proj_tok(wsw[:, :, 768:896], 128, lambda i: V1[:, i, :, 0:64], range(NTC), T, view=lambda a: a.rearrange("p (h d) -> p h d", d=64))
        A.pop()
        QK_R = [T + f"d{ci}" for ci in range(3)] + [T + f"dc{ci}" for ci in range(3)]
        V_R = [T + f"v{i}" for i in range(NTC)]
        if check_stop(f"swproj_{l}"):
            A.pop(); A.pop(); A.pop(); break
        e_s = dint(T + "e", [128, 512], BF16); g_s = dint(T + "g", [512, 512], BF16)
        DMA("sp", e_s.ap()[:, 0:128], KT[:, 0:128], QK_R, [T + "e"], "ex")
        DMA("sp", e_s.ap()[:, 128:256], KT[:, TOK - 128:TOK], QK_R, [T + "e"], "ex")
        DMA("sp", e_s.ap()[:, 256:384].rearrange("p (h d) -> p h d", d=64), V1[:, 0, :, 0:64], V_R, [T + "e"], "ex")
        DMA("sp", e_s.ap()[:, 384:512].rearrange("p (h d) -> p h d", d=64), V1[:, 15, :, 0:64], V_R, [T + "e"], "ex")
        AG(e_s, g_s, [T + "e"], [T + "g"], "cc")
        g_sv = g_s.ap().rearrange("(r p) f -> p r f", p=128)
        DMA("sp", KT[:, TOKC:TOKC + 512].rearrange("p (r t) -> p r t", t=128), g_sv[:, :, 128:256], [T + "g"], [T + "halo"], "kt")
        DMA("sp", KT[:, TOKC + 512:TOKC + 1024].rearrange("p (r t) -> p r t", t=128), g_sv[:, :, 0:128], [T + "g"], [T + "halo"], "kt")
        for kv in range(2):
            DMA("sp", V1[:, 18:22, kv, 0:64], g_sv[:, :, 384 + kv * 64:448 + kv * 64], [T + "g"], [T + "halo"], "kt")
            DMA("sp", V1[:, 22:26, kv, 0:64], g_sv[:, :, 256 + kv * 64:320 + kv * 64], [T + "g"], [T + "halo"], "kt")
        if check_stop(f"swag_{l}"):
            A.pop(); A.pop(); A.pop(); break
        ptw = [A.alloc([2, 4, 128], BF16) for _ in range(2)]
        qz = [A.alloc([2, 2, 128], BF16) for _ in range(2)]
        den = [A.alloc([4], F32) for _ in range(2)]; ysw = [A.alloc([4, 64], BF16) for _ in range(2)]
        ALLR = QK_R + V_R + [T + "halo", T + "V1ones"]
        tiles_slots = []
        for i in range(nto):
            if i < NT:
                slots = []
                if i > 0:
                    slots.append((128 * (i - 1), i - 1, 0))
                slots.append((128 * i, i, None))
                if i < NT - 1:
                    slots.append((128 * (i + 1), i + 1, 1))
                slots += [(TOK, 16, None), (TOK + 128, 17, None)]
                if i == 0:
                    slots += [(TOKC + 128 * r, 18 + r, 2 + r) for r in range(4)]
                if i == NT - 1:
                    slots += [(TOKC + 512 + 128 * r, 22 + r, 6 + r) for r in range(4)]
            else:
                slots = [(TOK, 16, None), (TOK + 128, 17, None)]
            tiles_slots.append((i, slots))

        def sw_S(k, u):
            i, gi, ngr, grp = u
            s2 = k % 2
            ts = slice(i * 128, (i + 1) * 128)
            if gi == 0:
                for kv in range(2):
                    TS("pool" if kv else "dve", qz[i % 2][:, kv], QT[:, :, ts], HM[:, kv:kv + 1], ALU.mult, QK_R + ["HM"], [T + f"qz{i % 2}"])
            for si, (kc0, vt, mi) in enumerate(grp):
                for kv in range(2):
                    for g in range(2):
                        MM(bank(2 * s2 + si, 128, (kv * 2 + g) * 128), KT[:, kc0:kc0 + 128], qz[i % 2][:, kv, g, :], True, True,
                           ALLR + [T + f"qz{i % 2}"], [f"psS{s2}"], sgc=True)

        def sw_E(k, u):
            i, gi, ngr, grp = u
            s2 = k % 2
            ns = len(grp)
            ACTV(ptw[s2][:, 0:ns].rearrange("p s h q -> p (s h q)"), psum[:, 1024 * s2:1024 * s2 + 512 * ns], AF.Exp, [f"psS{s2}"], [T + f"pt{s2}"], scale=0.125)
            for si, (kc0, vt, mi) in enumerate(grp):
                if mi is not None:
                    TT("dve", ptw[s2][:, si], ptw[s2][:, si], MSW[:, mi, :].unsqueeze(1).to_broadcast([128, 4, 128]), ALU.mult, [T + f"pt{s2}", T + "msw"], [T + f"pt{s2}"])

        def sw_AV(k, u):
            i, gi, ngr, grp = u
            s2 = k % 2
            ns = len(grp)
            for si, (kc0, vt, mi) in enumerate(grp):
                first = (gi == 0 and si == 0); last = (gi == ngr - 1 and si == ns - 1)
                for kv in range(2):
                    for g in range(2):
                        h = kv * 2 + g
                        MM(bank(4 + i % 2, 65, h * 65), ptw[s2][:, si, h, :], V1[:, vt, kv, :], first and h == 0, last, [T + f"pt{s2}"] + ALLR, [f"psO{i % 2}"], sgc=True)

        def sw_FIN(i):
            Ov = bank(4 + i % 2, 260).rearrange("p (h e) -> p h e", e=65)
            dn = den[i % 2]
            TT("dve", dn, Ov[:, :, 64], esink, ALU.add, [f"psO{i % 2}", T + "esink"], [T + f"den{i % 2}"])
            RECIP(dn, dn, [T + f"den{i % 2}"], [T + f"den{i % 2}"])
            yb = ysw[i % 2]
            TT("dve", yb, Ov[:, :, 0:64], dn.unsqueeze(2).to_broadcast([128, 4, 64]), ALU.mult, [f"psO{i % 2}", T + f"den{i % 2}"], [T + f"y{i % 2}"])
            out_transposes(yb.rearrange("p h d -> p (h d)"), 2, i, T, [T + f"y{i % 2}"])

        attn_pipeline(tiles_slots, sw_S, sw_E, sw_AV, sw_FIN)
        A.pop()
        if check_stop(f"sw_{l}") or (STOP_AFTER or "").startswith("swq1"):
            if "dbg_ot" in dbg:
                P.barrier()
                DMA("sp", dbg["dbg_ot"], otd, [], ["dbgot"], "dbg")
            A.pop(); A.pop(); break

        P.barrier()
        T = L + "na"
        A.push()
        QT = A.alloc([2, TOKC], BF16)
        KT = A.alloc([2, 4352], BF16)
        V1 = A.alloc([34, 4, 65], BF16)
        MBK = A.alloc([45, 128], BF16)
        BEX = A.alloc([7, 4, 128], BF16)
        EIN = A.alloc([5, 4, 128], BF16)
        for m0 in range(0, 45, 9):
            DMA("pool", MBK[:, m0:m0 + 9, :], I["namask"][m0:m0 + 9].rearrange("m k q -> k m q"), [], [T + "mbk"], "wB")
        A.push()
        bfl = A.alloc([7, 4, 128], F32)
        for d7 in range(7):
            DMA("sp", bfl[:, d7], I[f"nabias{l}"][d7].rearrange("h k q -> k h q"), [], [T + "bfl"], "m0")
        ACTV(BEX, bfl, AF.Exp, [T + "bfl"], [T + "bex"])
        A.pop()
        for d in range(5):
            TT("dve", EIN[:, d], BEX[:, d + 1], MBK[:, d, :].unsqueeze(1).to_broadcast([128, 4, 128]), ALU.mult, [T + "bex", T + "mbk"], [T + "ein"])
        MSET("pool", V1[:, :, :, 64:65], 1.0, [T + "V1ones"])
        A.push()
        wna = A.alloc([8, 768], BF16)
        DMA("pool", wna, winv[:, :, NA0:NA0 + 768], [], [T + "w"], "wA")
        n = 0
        for ci in range(4):
            for (t0, nt_) in ((0, 512), (512, 512), (1024, 512), (1536, 512), (TOK, 256)):
                dest = QT[:, ci, t0:t0 + nt_] if ci < 2 else KT[:, ci - 2, t0:t0 + nt_]
                proj_feat_plain(wna[:, :, ci * 128:(ci + 1) * 128], dest, t0, nt_, T, f"c{ci}", n % 4); n += 1
        proj_tok(wna[:, :, 512:768], 256, lambda i: V1[:, i, :, 0:64], range(NTC), T, view=lambda a: a.rearrange("p (h d) -> p h d", d=64))
        A.pop()
        P.barrier()
        QK_R = [T + f"dc{ci}" for ci in range(4)]
        V_R = [T + f"v{i}" for i in range(NTC)]
        e_n = dint(T + "e", [128, 2048], BF16); g_n = dint(T + "g", [512, 2048], BF16)
        env = e_n.ap()
        for c in range(2):
            DMA("sp", env[:, c * 512:c * 512 + 256], KT[:, c, 0:256], QK_R, [T + "e"], "ex")
            DMA("sp", env[:, c * 512 + 256:c * 512 + 512], KT[:, c, TOK - 256:TOK], QK_R, [T + "e"], "ex")
        DMA("sp", env[:, 1024:1536].rearrange("p (i h d) -> p i h d", h=4, d=64), V1[:, 0:2, :, 0:64], V_R, [T + "e"], "ex")
        DMA("sp", env[:, 1536:2048].rearrange("p (i h d) -> p i h d", h=4, d=64), V1[:, 14:16, :, 0:64], V_R, [T + "e"], "ex")
        AG(e_n, g_n, [T + "e"], [T + "g"], "cc")
        g_nv = g_n.ap().rearrange("(r p) f -> p r f", p=128)
        for c in range(2):
            DMA("sp", KT[:, c, TOKC:TOKC + 1024].rearrange("p (r t) -> p r t", t=256), g_nv[:, :, c * 512 + 256:c * 512 + 512], [T + "g"], [T + "halo"], "kt")
            DMA("sp", KT[:, c, TOKC + 1024:TOKC + 2048].rearrange("p (r t) -> p r t", t=256), g_nv[:, :, c * 512:c * 512 + 256], [T + "g"], [T + "halo"], "kt")
        for r in range(4):
            DMA("sp", V1[:, 18 + 2 * r:20 + 2 * r, :, 0:64], g_nv[:, r, 1536:2048].rearrange("p (i h d) -> p i h d", h=4, d=64), [T + "g"], [T + "halo"], "kt")
            DMA("sp", V1[:, 26 + 2 * r:28 + 2 * r, :, 0:64], g_nv[:, r, 1024:1536].rearrange("p (i h d) -> p i h d", h=4, d=64), [T + "g"], [T + "halo"], "kt")
        ptn = [A.alloc([2, 4, 128], BF16) for _ in range(2)]
        qz = [A.alloc([2, 2, 128], BF16) for _ in range(2)]
        den = [A.alloc([4], F32) for _ in range(2)]; yna = [A.alloc([4, 64], BF16) for _ in range(2)]
        ALLR = QK_R + V_R + [T + "halo", T + "V1ones"]
        PC0 = TOKC; NC0 = TOKC + 1024
        tiles_slots = []
        for i in range(nto):
            if i >= NT:
                slots = [(TOK, 16, 0, 0, 0), (TOK + 128, 17, 0, 0, 0)]
            elif 2 <= i <= 13:
                slots = [(128 * (i + d), i + d, 1, d + 2, 0) for d in range(-2, 3)]
            elif i == 0:
                slots = [(128 * d, d, 2, d + 3, 5 + d) for d in (0, 1, 2, 3)]
                slots += [(PC0 + 256 * r, 18 + 2 * r, 2, 1, 9 + r) for r in range(4)]
                slots += [(PC0 + 256 * r + 128, 19 + 2 * r, 2, 2, 13 + r) for r in range(4)]
            elif i == 1:
                slots = [(128 * (1 + d), 1 + d, 2, d + 3, 17 + (d + 1)) for d in (-1, 0, 1, 2)]
                slots += [(PC0 + 256 * r + 128, 19 + 2 * r, 2, 1, 21 + r) for r in range(4)]
            elif i == 14:
                slots = [(128 * (14 + d), 14 + d, 2, d + 3, 25 + (d + 2)) for d in (-2, -1, 0, 1)]
                slots += [(NC0 + 256 * r, 26 + 2 * r, 2, 5, 29 + r) for r in range(4)]
            else:
                slots = [(128 * (15 + d), 15 + d, 2, d + 3, 33 + (d + 3)) for d in (-3, -2, -1, 0)]
                slots += [(NC0 + 256 * r, 26 + 2 * r, 2, 4, 37 + r) for r in range(4)]
                slots += [(NC0 + 256 * r + 128, 27 + 2 * r, 2, 5, 41 + r) for r in range(4)]
            if i < NT:
                slots += [(TOK, 16, 0, 0, 0), (TOK + 128, 17, 0, 0, 0)]
            tiles_slots.append((i, slots))

        def na_S(k, u):
            i, gi, ngr, grp = u
            s2 = k % 2
            ts = slice(i * 128, (i + 1) * 128)
            if gi == 0:
                for hb_ in range(2):
                    TS("pool" if hb_ else "dve", qz[i % 2][:, hb_], QT[:, :, ts], HM[:, hb_:hb_ + 1], ALU.mult, QK_R + ["HM"], [T + f"qz{i % 2}"])
            for si, (kc0, vt, kind, bd, mi) in enumerate(grp):
                for h in range(4):
                    c, hb_ = h // 2, h % 2
                    MM(bank(2 * s2 + si, 128, h * 128), KT[:, c, kc0:kc0 + 128], qz[i % 2][:, hb_, c, :], True, True, ALLR + [T + f"qz{i % 2}"], [f"psS{s2}"], sgc=True)

        def na_E(k, u):
            i, gi, ngr, grp = u
            s2 = k % 2
            ns = len(grp)
            ACTV(ptn[s2][:, 0:ns].rearrange("p s h q -> p (s h q)"), psum[:, 1024 * s2:1024 * s2 + 512 * ns], AF.Exp, [f"psS{s2}"], [T + f"pt{s2}"], scale=0.125)
            for si, (kc0, vt, kind, bd, mi) in enumerate(grp):
                if kind == 1:
                    TT("dve", ptn[s2][:, si], ptn[s2][:, si], EIN[:, bd], ALU.mult, [T + f"pt{s2}", T + "ein"], [T + f"pt{s2}"])
                elif kind == 2:
                    TT("dve", ptn[s2][:, si], ptn[s2][:, si], BEX[:, bd], ALU.mult, [T + f"pt{s2}", T + "bex"], [T + f"pt{s2}"])
                    TT("pool", ptn[s2][:, si], ptn[s2][:, si], MBK[:, mi, :].unsqueeze(1).to_broadcast([128, 4, 128]), ALU.mult, [T + f"pt{s2}", T + "mbk"], [T + f"pt{s2}"])

        def na_AV(k, u):
            i, gi, ngr, grp = u
            s2 = k % 2
            ns = len(grp)
            for si, (kc0, vt, kind, bd, mi) in enumerate(grp):
                first = (gi == 0 and si == 0); last = (gi == ngr - 1 and si == ns - 1)
                for h in range(4):
                    MM(bank(4 + i % 2, 65, h * 65), ptn[s2][:, si, h, :], V1[:, vt, h, :], first and h == 0, last, [T + f"pt{s2}"] + ALLR, [f"psO{i % 2}"], sgc=True)

        def na_FIN(i):
            Ov = bank(4 + i % 2, 260).rearrange("p (h e) -> p h e", e=65)
            dn = den[i % 2]
            RECIP(dn, Ov[:, :, 64], [f"psO{i % 2}"], [T + f"den{i % 2}"])
            yb = yna[i % 2]
            TT("dve", yb, Ov[:, :, 0:64], dn.unsqueeze(2).to_broadcast([128, 4, 64]), ALU.mult, [f"psO{i % 2}", T + f"den{i % 2}"], [T + f"y{i % 2}"])
            out_transposes(yb.rearrange("p h d -> p (h d)"), 4, i, T, [T + f"y{i % 2}"])

        attn_pipeline(tiles_slots, na_S, na_E, na_AV, na_FIN)
        A.pop()
        if check_stop(f"na_{l}"):
            if "dbg_ot" in dbg:
                P.barrier()
                DMA("sp", dbg["dbg_ot"], otd, [], ["dbgot"], "dbg")
            A.pop(); A.pop(); break

        P.barrier()
        T = L + "rt"
        A.push()
        QT = A.alloc([2, TOKC], BF16); KT = A.alloc([2, TOKC], BF16)
        VR = A.alloc([NTC, 256], BF16); GT = A.alloc([NTC, 256], BF16)
        RC = A.alloc([700], F32); IDXB = A.alloc([128], F32)
        LG = A.alloc([8], F32); LGS = A.alloc([2, 2], F32)
        DEC = A.alloc([4, 128], BF16); XI = A.alloc([2, 2, 128], BF16)
        ZZ = A.alloc([2, 4], F32); GC = A.alloc([2, 2], F32); GPW = A.alloc([2, 2, 18], F32); CFC = A.alloc([2, 2, 5], F32)
        DMA("sp", RC, I["retc"], [], [T + "rc"], "m0")
        DMA("sp", IDXB, I["idxb"], [], [T + "rc"], "m0")
        DMA("sp", LG, pbc(I[f"gam{l}"]), [], [T + "lg"], "m0")
        ACTV(LG, LG, AF.Exp, [T + "lg"], [T + "lg"], scale=-1.0)
        TS("dve", LG, LG, 1.0, ALU.add, [T + "lg"], [T + "lg"])
        ACTV(LG, LG, AF.Ln, [T + "lg"], [T + "lg"])
        TS("dve", LG, LG, -1.0, ALU.mult, [T + "lg"], [T + "lg"])
        for d_ in range(2):
            for c in range(2):
                k0_ = 4 * d_ + 2 * c
                TS("dve", LGS[:, d_, c:c + 1], LG[:, k0_:k0_ + 1], HM[:, 0:1], ALU.mult, [T + "lg", "HM"], [T + "lgs"])
                STT("dve", LGS[:, d_, c:c + 1], LG[:, k0_ + 1:k0_ + 2], HM[:, 1:2], LGS[:, d_, c:c + 1], ALU.mult, ALU.add, [T + "lg", "HM", T + "lgs"], [T + "lgs"])
        A.push()
        tf = A.alloc([128], F32); tb_ = A.alloc([128], F32)
        for h in range(4):
            ACTV(tf, RC[:, 0:128], AF.Exp, [T + "rc", T + "lg"], [T + "tf"], scale=LG[:, h:h + 1])
            TT("dve", tf, tf, RC[:, 128:256], ALU.mult, [T + "tf", T + "rc"], [T + "tf"])
            ACTV(tb_, RC[:, 256:384], AF.Exp, [T + "rc", T + "lg"], [T + "tb"], scale=LG[:, 4 + h:5 + h])
            TT("dve", tb_, tb_, RC[:, 384:512], ALU.mult, [T + "tb", T + "rc"], [T + "tb"])
            TT("dve", DEC[:, h, :], tf, tb_, ALU.add, [T + "tf", T + "tb"], [T + "dec"])
        A.pop()
        for c in range(2):
            ACTV(XI[:, 0, c, :], RC[:, 512:640], AF.Exp, [T + "rc", T + "lgs"], [T + "xi"], scale=LGS[:, 0, c:c + 1])
            ACTV(XI[:, 1, c, :], IDXB, AF.Exp, [T + "rc", T + "lgs"], [T + "xi"], scale=LGS[:, 1, c:c + 1])
            for d_ in range(2):
                ACTV(GC[:, d_, c:c + 1], LGS[:, d_, c:c + 1], AF.Exp, [T + "lgs"], [T + "gc"], scale=128.0)
                ACTV(GPW[:, d_, c, :], RC[:, 642 + 18 * d_:660 + 18 * d_], AF.Exp, [T + "rc", T + "lgs"], [T + "gpw"], scale=LGS[:, d_, c:c + 1])
                ACTV(CFC[:, d_, c, :], RC[:, 678 + 5 * d_:683 + 5 * d_], AF.Exp, [T + "rc", T + "lgs"], [T + "cfc"], scale=LGS[:, d_, c:c + 1])
                TT("dve", CFC[:, d_, c, :], CFC[:, d_, c, :], RC[:, 688 + 5 * d_:693 + 5 * d_], ALU.mult, [T + "cfc", T + "rc"], [T + "cfc"])
        ACTV(ZZ[:, 0, :], LG[:, 0:4], AF.Exp, [T + "lg", T + "rc"], [T + "zz"], scale=RC[:, 640:641])
        ACTV(ZZ[:, 1, :], LG[:, 4:8], AF.Exp, [T + "lg", T + "rc"], [T + "zz"], scale=RC[:, 641:642])
        TS("dve", ZZ, ZZ, 0.125, ALU.mult, [T + "zz"], [T + "zz"])
        if check_stop(f"rtparam_{l}"):
            A.pop(); A.pop(); A.pop(); break
        A.push()
        wrt = A.alloc([8, 1536], BF16)
        DMA("pool", wrt, winv[:, :, RT0:RT0 + 1536], [], [T + "w"], "wA")
        dests = [QT[:, 0, 0:TOK], QT[:, 1, 0:TOK], KT[:, 0, 0:TOK], KT[:, 1, 0:TOK]]
        proj_rope([wrt[:, :, c * 128:(c + 1) * 128] for c in range(4)], [wrt[:, :, 512 + c * 128:512 + (c + 1) * 128] for c in range(4)],
                  rope64[:, 0, :], rope64[:, 1, :], dests, T)
        for ci in range(4):
            dest = QT[:, ci, TOK:TOKC] if ci < 2 else KT[:, ci - 2, TOK:TOKC]
            proj_feat_plain(wrt[:, :, ci * 128:(ci + 1) * 128], dest, TOK, 256, T, f"c{ci}", ci % 2)

        def vg_post(i, pb):
            CP("act", VR[:, i, :], bank(pb, 256), [f"ps{pb}"], [T + f"v{i}"])
            ACTV(GT[:, i, :], bank(pb, 256, 256), AF.Silu, [f"ps{pb}"], [T + f"g{i}"])
        proj_tok(wrt[:, :, 1024:1536], 512, None, range(NTC), T, post=vg_post)
        A.pop()
        P.barrier()
        QK_R = [T + f"d{ci}" for ci in range(4)] + [T + f"dc{ci}" for ci in range(4)]
        if check_stop(f"rtproj_{l}"):
            A.pop(); A.pop(); A.pop(); break
        KTOK = A.alloc([NTC, 256], BF16)
        SZ = A.alloc([2, 18, 2, 64], F32)
        UCX = A.alloc([4, 2, 64], F32)
        SCX = A.alloc([2, 2, 64], F32)
        S0 = A.alloc([2, 2, 64], F32)
        SB = A.alloc([2, NTC, 2, 64], BF16)
        GR = A.alloc([4, 256], F32); EXPB = A.alloc([2, 2, 64], F32)
        for i in range(NTC):
            pb = i % 2
            for c in range(2):
                TR(bank_bf(pb)[:, c * 128:(c + 1) * 128], KT[:, c, i * 128:(i + 1) * 128], ident_bf, QK_R + ["ident_bf"], [f"ps{pb}"])
            CP("act", KTOK[:, i, :], bank_bf(pb)[:, 0:256], [f"ps{pb}"], [T + f"kt{i}"])
        vz = [A.alloc([2, 256], BF16) for _ in range(2)]
        MSET("pool", SZ[:, 0, 0], 0.0, [T + "sz"])
        MSET("pool", SZ[:, 1, 16], 0.0, [T + "sz"])

        def chunk_U(n, s2):
            for d_ in range(2):
                TT("dve" if d_ == 0 else "pool", vz[s2][:, d_].rearrange("p (h e) -> p h e", e=64), VR[:, n, :].rearrange("p (h e) -> p h e", e=64),
                   ZZ[:, d_, :].unsqueeze(2).to_broadcast([128, 4, 64]), ALU.mult, [T + f"v{n}", T + "zz"], [T + f"vz{s2}"])
            for d_ in range(2):
                for c in range(2):
                    MM(bank(2 + s2, 128, (d_ * 2 + c) * 128), KTOK[:, n, c * 128:(c + 1) * 128], vz[s2][:, d_, c * 128:(c + 1) * 128], True, True,
                       [T + f"kt{n}", T + f"vz{s2}"], [f"ps{2 + s2}"])

        udg = A.alloc([64], F32); udt = A.alloc([64], F32)

        def udiag(s2, d_, c, dst=None, dreg=None):
            blk = bank(2 + s2, 128, (d_ * 2 + c) * 128)
            o_ = udg if dst is None else dst
            TS("dve", udt, blk[:, 0:64], HM[:, 0:1], ALU.mult, [f"ps{2 + s2}", "HM"], [T + "udt"])
            STT("dve", o_, blk[:, 64:128], HM[:, 1:2], udt, ALU.mult, ALU.add, [f"ps{2 + s2}", "HM", T + "udt"], [T + "udg" if dreg is None else dreg])
            return o_

        for n in range(NT):
            chunk_U(n, n % 2)
            for c in range(2):
                u_ = udiag(n % 2, 0, c)
                STT("dve", SZ[:, 0, n + 1, c, :], SZ[:, 0, n, c, :], GC[:, 0, c:c + 1], u_, ALU.mult, ALU.add, [T + "sz", T + "gc", T + "udg"], [T + "sz"])
                udiag(n % 2, 1, c, dst=SZ[:, 1, n, c, :], dreg=T + "szb")
        for n in range(NT - 1, -1, -1):
            for c in range(2):
                STT("dve", SZ[:, 1, n, c, :], SZ[:, 1, n + 1, c, :], GC[:, 1, c:c + 1], SZ[:, 1, n, c, :], ALU.mult, ALU.add, [T + "sz", T + "szb", T + "gc"], [T + "sz", T + "szb"])
        for k_, n in enumerate((16, 17)):
            chunk_U(n, k_)
            for d_ in range(2):
                for c in range(2):
                    u_ = udiag(k_, d_, c)
                    CP("dve", UCX[:, 2 * d_ + k_, c, :], u_, [T + "udg"], [T + "ucx"])
        for c in range(2):
            STT("dve", SCX[:, 0, c, :], UCX[:, 0, c, :], GC[:, 0, c:c + 1], UCX[:, 1, c, :], ALU.mult, ALU.add, [T + "ucx", T + "gc"], [T + "scx"])
            STT("dve", SCX[:, 1, c, :], UCX[:, 3, c, :], GC[:, 1, c:c + 1], UCX[:, 2, c, :], ALU.mult, ALU.add, [T + "ucx", T + "gc"], [T + "scx"])
        CP("dve", EXPB[:, 0], SZ[:, 0, 16], [T + "sz"], [T + "expb"])
        CP("dve", EXPB[:, 1], SZ[:, 1, 0], [T + "sz"], [T + "expb"])
        e_r = dint(T + "e", [128, 256], F32); g_r = dint(T + "g", [512, 256], F32)
        DMA("sp", e_r.ap(), EXPB.rearrange("p d c e -> p (d c e)"), [T + "expb"], [T + "e"], "ex")
        AG(e_r, g_r, [T + "e"], [T + "g"], "cc")
        DMA("sp", GR, g_r.ap().rearrange("(r p) f -> p r f", p=128), [T + "g"], [T + "gr"], "kt")
        GRv = GR.rearrange("p r (d c e) -> p r d c e", d=2, c=2)
        for d_ in range(2):
            for c in range(2):
                TS("dve", S0[:, d_, c, :], SCX[:, d_, c, :], CFC[:, d_, c, 4:5], ALU.mult, [T + "scx", T + "cfc"], [T + "s0"])
                for r in range(4):
                    STT("dve", S0[:, d_, c, :], GRv[:, r, d_, c, :], CFC[:, d_, c, r:r + 1], S0[:, d_, c, :], ALU.mult, ALU.add, [T + "gr", T + "cfc", T + "s0"], [T + "s0"])
        for n in range(NT):
            for c in range(2):
                STT("dve", SB[:, 0, n, c, :], S0[:, 0, c, :], GPW[:, 0, c, n:n + 1], SZ[:, 0, n, c, :], ALU.mult, ALU.add, [T + "s0", T + "gpw", T + "sz"], [T + "sb"])
                STT("dve", SB[:, 1, n, c, :], S0[:, 1, c, :], GPW[:, 1, c, n:n + 1], SZ[:, 1, n + 1, c, :], ALU.mult, ALU.add, [T + "s0", T + "gpw", T + "sz"], [T + "sb"])
        MSET("pool", SB[:, 0, 16], 0.0, [T + "sb"])
        MSET("pool", SB[:, 1, 17], 0.0, [T + "sb"])
        CP("dve", SB[:, 0, 17], UCX[:, 0], [T + "ucx"], [T + "sb"])
        CP("dve", SB[:, 1, 16], UCX[:, 3], [T + "ucx"], [T + "sb"])
        if check_stop(f"rtA_{l}"):
            A.pop(); A.pop(); A.pop(); break
        AD = [A.alloc([4, 128], BF16) for _ in range(2)]
        QX = [A.alloc([2, 2, 2, 128], BF16) for _ in range(2)]
        qz = [A.alloc([2, 2, 128], BF16) for _ in range(2)]
        of_ = [A.alloc([4, 64], F32) for _ in range(2)]
        sq = A.alloc([4, 64], F32); ssr = A.alloc([4], F32); rsr = A.alloc([4], F32)
        yr_ = [A.alloc([4, 64], BF16) for _ in range(2)]
        def rt_A(i):
            s2 = i % 2
            ts = slice(i * 128, (i + 1) * 128)
            for hb_ in range(2):
                TS("pool" if hb_ else "dve", qz[s2][:, hb_], QT[:, :, ts], HM[:, hb_:hb_ + 1], ALU.mult, QK_R + ["HM"], [T + f"qz{s2}"])
            for h in range(4):
                c, hb_ = h // 2, h % 2
                MM(bank(s2, 128, h * 128), KT[:, c, ts], qz[s2][:, hb_, c, :], True, True, QK_R + [T + f"qz{s2}"], [f"ps{s2}"], sgc=True)

        def rt_mid(i):
            s2 = i % 2
            TT("dve", AD[s2], bank(s2).rearrange("p (h i) -> p h i", i=128), DEC, ALU.mult, [f"ps{s2}", T + "dec"], [T + f"ad{s2}"])
            for d_ in range(2):
                for hb_ in range(2):
                    TT("dve", QX[s2][:, d_, hb_], qz[s2][:, hb_], XI[:, d_], ALU.mult, [T + f"qz{s2}", T + "xi"], [T + f"qx{s2}"])

        def rt_out(i):
            s2 = i % 2
            pO = 4 + s2
            for h in range(4):
                c, hb_ = h // 2, h % 2
                o_ = bank(pO, 64, h * 64)
                MM(o_, AD[s2][:, h, :], VR[:, i, h * 64:(h + 1) * 64], h == 0, False, [T + f"ad{s2}", T + f"v{i}"], [f"ps{pO}"], sgc=True)
                MM(o_, QX[s2][:, 0, hb_, c, :], SB[:, 0, i, c, :], False, False, [T + f"qx{s2}", T + "sb"], [f"ps{pO}"], sgc=True)
                MM(o_, QX[s2][:, 1, hb_, c, :], SB[:, 1, i, c, :], False, True, [T + f"qx{s2}", T + "sb"], [f"ps{pO}"], sgc=True)

        def rt_fin(i):
            s2 = i % 2
            pO = 4 + s2
            ov = of_[s2]
            CP("act", ov, bank(pO, 256).rearrange("p (h e) -> p h e", e=64), [f"ps{pO}"], [T + f"of{s2}"])
            TT("dve", sq, ov, ov, ALU.mult, [T + f"of{s2}"], [T + "sq"])
            RED("dve", ssr, sq, [T + "sq"], [T + "ssr"])
            rstd_from_ss(ssr, 64, rsr, [T + "ssr", "epsc"], [T + "rsr"])
            TT("dve", sq, ov, rsr.unsqueeze(2).to_broadcast([128, 4, 64]), ALU.mult, [T + f"of{s2}", T + "rsr"], [T + "sq"])
            TT("pool", yr_[s2], sq, GT[:, i, :].rearrange("p (h e) -> p h e", e=64), ALU.mult, [T + "sq", T + f"g{i}"], [T + f"y{s2}"])

        def rt_tr(i):
            out_transposes(yr_[i % 2].rearrange("p h d -> p (h d)"), 6, i, T, [T + f"y{i % 2}"])

        rt_A(0)
        for i in range(nto):
            rt_mid(i)
            if i + 1 < nto:
                rt_A(i + 1)
            rt_out(i)
            rt_fin(i)
            if i >= 1:
                rt_tr(i - 1)
        rt_tr(nto - 1)
        A.pop()
        A.pop()
        if "dbg_ot" in dbg and l == 0:
            DMA("sp", dbg["dbg_ot"], otd, [L + f"OT{c0}_{i}" for c0 in (0, 1, 2, 4, 6) for i in range(nto)], ["dbgot"], "dbg")
        if check_stop(f"rt_{l}") or (STOP_AFTER or "").startswith("rtB"):
            A.pop(); break

        P.barrier()
        A.push()
        h2T = hT
        wo = A.alloc([8, 1024], BF16)
        DMA("pool", wo, I[f"wout{l}"].rearrange("(kc p) n -> p kc n", p=128), [], [L + "wo"], "wA")
        if moe:
            RB = A.alloc([8, 1024], F32)
            DMA("sp", RB, pbc(I["router"]).rearrange("p (e d) -> p e d", d=1024), [], [L + "rb"], "m0")
            rj = A.alloc([1024], F32); sm = A.alloc([8, 8], F32)
        xt = [A.alloc([1024], F32) for _ in range(2)]
        t1 = [A.alloc([1024], F32) for _ in range(2)]
        xm = [A.alloc([1024], F32) for _ in range(2)]
        hb = [A.alloc([1024], BF16) for _ in range(2)]
        ott = [A.alloc([8, 128], BF16) for _ in range(2)]
        ss3 = A.alloc([NTC, 2], F32); rs3 = A.alloc([NTC, 2], F32)
        xdst = (xs, xcs)

        def p3_mm(i):
            s2 = i % 2
            pb = 2 * s2
            ot_r = [L + f"OT{c0}_{i}" for c0 in (0, 1, 2, 4, 6)]
            DMA("sp", ott[s2], otd[i], ot_r, [L + f"ott{s2}"], f"ott{s2}")
            for hf in range(2):
                for kc in range(8):
                    MM(bank(pb + hf), ott[s2][:, kc, :], wo[:, kc, hf * 512:(hf + 1) * 512], kc == 0, kc == 7, [L + f"ott{s2}", L + "wo"], [f"ps{pb + hf}"])

        def p3_chain(i):
            s2 = i % 2
            v = 0 if i < NT else 1
            pb = 2 * s2
            yps = psum[:, 512 * pb:512 * pb + 1024]
            ACTV(junk, yps, AF.Square, [f"ps{pb}", f"ps{pb + 1}"], ["junk", L + f"s3_{i}"], accum=ss3[:, i, 0:1])
            rstd_from_ss(ss3[:, i, 0:1], 1024, rs3[:, i, 0:1], [L + f"s3_{i}", "epsc"], [L + f"r3_{i}"])
            DMA("sp", xt[s2], xtile_ap(xsrc, i), [], [L + f"p3xt{s2}"], f"xt{s2}")
            STT("dve", t1[s2], yps, rs3[:, i, 0:1], MOD[:, v, 2, :], ALU.mult, ALU.mult, [f"ps{pb}", f"ps{pb + 1}", L + f"r3_{i}", f"MOD{v}2"], [L + f"p3t1{s2}"])
            TT("pool" if moe else "dve", xm[s2], t1[s2], xt[s2], ALU.add, [L + f"p3t1{s2}", L + f"p3xt{s2}"], [L + f"xm{s2}"])
            DMA("sp", xtile_ap(xdst, i), xm[s2], [L + f"xm{s2}"], [L + f"xs{i}"], f"xst{s2}")
            ACTV(junk, xm[s2], AF.Square, [L + f"xm{s2}"], ["junk", L + f"s4_{i}"], accum=ss3[:, i, 1:2])
            rstd_from_ss(ss3[:, i, 1:2], 1024, rs3[:, i, 1:2], [L + f"s4_{i}", "epsc"], [L + f"r4_{i}"])
            STT("dve", t1[s2], xm[s2], rs3[:, i, 1:2], MOD[:, v, 4, :], ALU.mult, ALU.mult, [L + f"xm{s2}", L + f"r4_{i}", f"MOD{v}4"], [L + f"p3t1{s2}"])
            if moe:
                TT("pool", xt[s2], t1[s2], MOD[:, v, 3, :], ALU.add, [L + f"p3t1{s2}", f"MOD{v}3"], [L + f"p3xt{s2}"])
                CP("act", hb[s2], xt[s2], [L + f"p3xt{s2}"], [L + f"p3hb{s2}"])
                for e_ in range(8):
                    TT("dve", rj, xt[s2], RB[:, e_, :], ALU.mult, [L + f"p3xt{s2}", L + "rb"], [L + "rj"])
                    RED("dve", LOGI[:, i, e_:e_ + 1], rj, [L + "rj"], [L + f"logi{i}"])
                lg_ = LOGI[:, i, :]
                RED("dve", sm[:, 0, 0:1], lg_, [L + f"logi{i}"], [L + "sm"], mx=True)
                TS("dve", sm[:, 1, :], lg_, sm[:, 0, 0:1], ALU.is_equal, [L + f"logi{i}", L + "sm"], [L + "sm"])
                STT("dve", sm[:, 2, :], sm[:, 1, :], -1e30, lg_, ALU.mult, ALU.add, [L + "sm", L + f"logi{i}"], [L + "sm"])
                RED("dve", sm[:, 0, 1:2], sm[:, 2, :], [L + "sm"], [L + "sm"], mx=True)
                TS("dve", sm[:, 3, :], lg_, sm[:, 0, 1:2], ALU.is_ge, [L + f"logi{i}", L + "sm"], [L + "sm"])
                TS("dve", sm[:, 0, 2:3], sm[:, 0, 0:1], -1.0, ALU.mult, [L + "sm"], [L + "sm"])
                ACTV(sm[:, 4, :], lg_, AF.Exp, [L + f"logi{i}", L + "sm"], [L + "sm"], bias=sm[:, 0, 2:3])
                TT("dve", sm[:, 4, :], sm[:, 4, :], sm[:, 3, :], ALU.mult, [L + "sm"], [L + "sm"])
                RED("dve", sm[:, 0, 3:4], sm[:, 4, :], [L + "sm"], [L + "sm"])
                RECIP(sm[:, 0, 3:4], sm[:, 0, 3:4], [L + "sm"], [L + "sm"])
                TS("dve", GATES[:, i, :], sm[:, 4, :], sm[:, 0, 3:4], ALU.mult, [L + "sm"], [L + f"gates{i}"])
            else:
                TT("dve", hb[s2], t1[s2], MOD[:, v, 3, :], ALU.add, [L + f"p3t1{s2}", f"MOD{v}3"], [L + f"p3hb{s2}"])

        def p3_tr(i):
            s2 = i % 2
            ts = slice(i * 128, (i + 1) * 128)
            pt_ = 4 + s2
            for kc in range(8):
                TR(bank_bf(pt_)[:, kc * 128:(kc + 1) * 128], hb[s2][:, kc * 128:(kc + 1) * 128], ident_bf, [L + f"p3hb{s2}", "ident_bf"], [f"ps{pt_}"])
            CP("act", h2T[:, :, ts], bank_bf(pt_).rearrange("p (k t) -> p k t", t=128), [f"ps{pt_}"], [L + f"h2T{i}"])

        p3_mm(0)
        for i in range(nto):
            p3_chain(i)
            if i + 1 < nto:
                p3_mm(i + 1)
            p3_tr(i)
        H2_ALL = [L + f"h2T{i}" for i in range(nto)]
        A.pop()
        if check_stop(f"p3_{l}"):
            A.pop(); break

        P.barrier()
        Y = A.alloc([nto, 1024], F32)
        A.push()
        wg = [A.alloc([8, 256], BF16) for _ in range(2)]
        wu = [A.alloc([8, 256], BF16) for _ in range(2)]
        wd = [A.alloc([2, 1024], BF16) for _ in range(2)]
        sg = [A.alloc([512], BF16) for _ in range(2)]
        AT = [A.alloc([2, 512], BF16) for _ in range(2)]
        ntok = nto * 128
        tblocks = [(t0, min(512, ntok - t0)) for t0 in range(0, ntok, 512)]
        if moe:
            slabs = [(e_, s_) for e_ in range(8) for s_ in range(14)]
        else:
            slabs = [(None, s_) for s_ in range(11)]
        nmm = 0
        for si, (e_, s_) in enumerate(slabs):
            sl = si % 2
            if moe:
                gsrc = I["mwg"][e_].rearrange("(kc p) f -> p kc f", p=128)[:, :, s_ * 256:(s_ + 1) * 256]
                usrc = I["mwu"][e_].rearrange("(kc p) f -> p kc f", p=128)[:, :, s_ * 256:(s_ + 1) * 256]
                dsrc = I["mwd"][e_][s_ * 256:(s_ + 1) * 256, :].rearrange("(c p) n -> p c n", p=128)
            else:
                gsrc = I["fwg"].rearrange("(kc p) f -> p kc f", p=128)[:, :, s_ * 256:(s_ + 1) * 256]
                usrc = I["fwu"].rearrange("(kc p) f -> p kc f", p=128)[:, :, s_ * 256:(s_ + 1) * 256]
                dsrc = I["fwd"][s_ * 256:(s_ + 1) * 256, :].rearrange("(c p) n -> p c n", p=128)
            DMA("pool", wg[sl], gsrc, [], [L + f"wg{sl}"], f"fw{sl}")
            DMA("pool", wu[sl], usrc, [], [L + f"wu{sl}"], f"fw{sl}")
            DMA("pool", wd[sl], dsrc, [], [L + f"wd{sl}"], f"fw{sl}")
            for bi, (t0, nt_) in enumerate(tblocks):
                a2 = bi % 2
                for fcl in range(2):
                    pg = nmm % 2; nmm += 1
                    for kc in range(8):
                        MM(bank(pg, nt_), wg[sl][:, kc, fcl * 128:(fcl + 1) * 128], h2T[:, kc, t0:t0 + nt_], kc == 0, kc == 7, H2_ALL + [L + f"wg{sl}"], [f"ps{pg}"])
                    for kc in range(8):
                        MM(bank(2 + pg, nt_), wu[sl][:, kc, fcl * 128:(fcl + 1) * 128], h2T[:, kc, t0:t0 + nt_], kc == 0, kc == 7, H2_ALL + [L + f"wu{sl}"], [f"ps{2 + pg}"])
                    ACTV(sg[pg][:, 0:nt_], bank(pg, nt_), AF.Silu, [f"ps{pg}"], [L + f"sg{pg}"])
                    TT("dve", AT[a2][:, fcl, 0:nt_], sg[pg][:, 0:nt_], bank(2 + pg, nt_), ALU.mult, [L + f"sg{pg}", f"ps{2 + pg}"], [L + f"at{a2}_{fcl}"])
                for tt in range(nt_ // 128):
                    ti = t0 // 128 + tt
                    py = 4 + 2 * (ti % 2)
                    for hf in range(2):
                        for fcl in range(2):
                            MM(bank(py + hf), AT[a2][:, fcl, tt * 128:(tt + 1) * 128], wd[sl][:, fcl, hf * 512:(hf + 1) * 512], fcl == 0, fcl == 1,
                               [L + f"at{a2}_0", L + f"at{a2}_1", L + f"wd{sl}"], [f"ps{py + hf}"])
                    yps = psum[:, 512 * py:512 * py + 1024]
                    rr = [f"ps{py}", f"ps{py + 1}"]
                    if moe:
                        gsc = GATES[:, ti, e_:e_ + 1]
                        if si == 0:
                            TS("dve", Y[:, ti, :], yps, gsc, ALU.mult, rr + [L + f"gates{ti}"], [L + f"Y{ti}"])
                        else:
                            STT("dve", Y[:, ti, :], yps, gsc, Y[:, ti, :], ALU.mult, ALU.add, rr + [L + f"gates{ti}", L + f"Y{ti}"], [L + f"Y{ti}"])
                    else:
                        if si == 0:
                            CP("dve", Y[:, ti, :], yps, rr, [L + f"Y{ti}"])
                        else:
                            TT("dve", Y[:, ti, :], yps, Y[:, ti, :], ALU.add, rr + [L + f"Y{ti}"], [L + f"Y{ti}"])
        A.pop()
        if check_stop(f"p4_{l}"):
            A.pop(); break

        A.push()
        ss5 = A.alloc([NTC], F32); rs5 = A.alloc([NTC], F32)
        xt = [A.alloc([1024], F32) for _ in range(2)]
        t1 = [A.alloc([1024], F32) for _ in range(2)]
        xo = [A.alloc([1024], F32) for _ in range(2)]
        for i in range(nto):
            s2 = i % 2
            v = 0 if i < NT else 1
            ACTV(junk, Y[:, i, :], AF.Square, [L + f"Y{i}"], ["junk", L + f"s5_{i}"], accum=ss5[:, i:i + 1])
            rstd_from_ss(ss5[:, i:i + 1], 1024, rs5[:, i:i + 1], [L + f"s5_{i}", "epsc"], [L + f"r5_{i}"])
            DMA("sp", xt[s2], xtile_ap(xdst, i), [L + f"xs{i}"], [L + f"p5xt{s2}"], f"xt{s2}")
            STT("dve", t1[s2], Y[:, i, :], rs5[:, i:i + 1], MOD[:, v, 5, :], ALU.mult, ALU.mult, [L + f"Y{i}", L + f"r5_{i}", f"MOD{v}5"], [L + f"p5t1{s2}"])
            TT("dve", xo[s2], t1[s2], xt[s2], ALU.add, [L + f"p5t1{s2}", L + f"p5xt{s2}"], [L + f"xo{s2}"])
            if l == 0:
                DMA("sp", xtile_ap(xdst, i), xo[s2], [L + f"xo{s2}"], [L + f"xs{i}"], f"xst{s2}")
                if "dbg_x" in dbg:
                    dd = dbg["dbg_x"][i * 128:(i + 1) * 128, :] if i < NT else dbg["dbg_xc"][(i - NT) * 128:(i - NT + 1) * 128, :]
                    DMA("sp", dd, xo[s2], [L + f"xo{s2}"], [L + f"dbgx{i}"], "dbg")
            else:
                DMA("sp", out[i * 128:(i + 1) * 128, :], xo[s2], [L + f"xo{s2}"], [f"out{i}"], f"xst{s2}")
        A.pop()
        A.pop()
        if check_stop(f"l{l}"):
            break
    return nc, P, es, A, I


def _emit(nc, P, es):
    tls = P.finalize()
    sems = {tl: es.enter_context(nc.semaphore("s_" + str(tl))) for tl in tls}
    with nc.Block() as block:
        block.sync(P.engine_body("sp", sems, final=True))
        block.tensor(P.engine_body("pe", sems))
        block.vector(P.engine_body("dve", sems))
        block.scalar(P.engine_body("act", sems))
        block.gpsimd(P.engine_body("pool", sems))


_CACHE = {}


def _get_program():
    if "nc" not in _CACHE:
        nc, P, es, A, I = build_program()
        _CACHE["inputs"] = list(I.keys())
        with es:
            _emit(nc, P, es)
        _CACHE["nc"] = nc
        _CACHE["peak"] = A.peak
        _CACHE["nops"] = len(P.ops)
    return _CACHE["nc"]


def _host_inputs(inp):
    f = lambda a: np.ascontiguousarray(np.asarray(a, dtype=np.float32))
    shared = {}
    for l in range(2):
        shared[f"wmod{l}"] = f(inp["w_mod"][l])
        shared[f"bmod{l}"] = f(inp["b_mod"][l]).reshape(1, 6144)
        shared[f"gvec{l}"] = f(np.concatenate([inp["g_attn_pre"][l], inp["g_attn_post"][l], inp["g_ffn_pre"][l], inp["g_ffn_post"][l]])).reshape(1, 4096)
        shared[f"win{l}"] = f(np.asarray(inp["w_in"][l])[:, WIN_PERM])
        shared[f"wout{l}"] = f(inp["w_out"][l])
        shared[f"dal{l}"] = f(np.concatenate([inp["da_lambda_q1"][l], inp["da_lambda_k1"][l], inp["da_lambda_q2"][l], inp["da_lambda_k2"][l]])).reshape(1, 128)
        shared[f"subln{l}"] = f(inp["da_subln"][l]).reshape(1, 64)
        shared[f"sink{l}"] = f(inp["swa_sink"][l]).reshape(1, 4)
        shared[f"gam{l}"] = f(np.concatenate([inp["ret_gamma_fwd"][l], inp["ret_gamma_bwd"][l]])).reshape(1, 8)
        shared[f"nabias{l}"] = _na_bias_layout(np.asarray(inp["na_rpb"][l], dtype=np.float32))
    shared["fwg"] = f(inp["ffn_w_gate"][0]); shared["fwu"] = f(inp["ffn_w_up"][0]); shared["fwd"] = f(inp["ffn_w_down"][0])
    shared["router"] = f(np.asarray(inp["moe_router"][0]).T).reshape(1, 8 * 1024)
    shared["mwg"] = f(inp["moe_w_gate"][0]); shared["mwu"] = f(inp["moe_w_up"][0]); shared["mwd"] = f(inp["moe_w_down"][0])
    shared["idxb"] = _idxb_table()
    x = np.asarray(inp["x"], dtype=np.float32); ctx = np.asarray(inp["ctx"], dtype=np.float32)
    c = np.asarray(inp["c"], dtype=np.float32); c_ctx = np.asarray(inp["c_ctx"], dtype=np.float32)
    maps = []
    for core in range(8):
        b, j = core // 4, core % 4
        m = dict(shared)
        m["xin"] = np.ascontiguousarray(x[b, TOK * j:TOK * (j + 1)])
        m["xcin"] = np.ascontiguousarray(ctx[b])
        m["cvec"] = np.ascontiguousarray(np.concatenate([c[b].reshape(8, 128).T, c_ctx.reshape(8, 128).T], axis=1))
        C64, S64 = _rope_tables(j, 64)
        C32, S32 = _rope_tables(j, 32)
        m["rope64"] = np.ascontiguousarray(np.stack([C64, S64], axis=1))
        m["rope32"] = np.ascontiguousarray(np.stack([C32, S32], axis=1))
        m["swamask"] = _swa_masks(j)
        m["namask"] = _na_masks(j)
        m["retc"] = _ret_consts(j)
        if "inputs" in _CACHE:
            m = {k: v for k, v in m.items() if k in _CACHE["inputs"]}
        maps.append(m)
    return maps


def kernel(**inputs):
    nc = _get_program()
    maps = _host_inputs(inputs)
    res = run_bass_kernel_spmd(nc, maps, core_ids=list(range(8)))
    _CACHE["last"] = res
    outp = np.empty((2, 8192, 1024), np.float32)
    for core in range(8):
        b, j = core // 4, core % 4
        outp[b, TOK * j:TOK * (j + 1)] = res.results[core]["out"]
    return outp
```

```python
import contextlib
import os
import math
import numpy as np
import concourse.bass as bass
import concourse.mybir as mybir
from concourse.bass_utils import run_bass_kernel_spmd

F32 = mybir.dt.float32
BF16 = mybir.dt.bfloat16
AF = mybir.ActivationFunctionType
ALU = mybir.AluOpType
AX = mybir.AxisListType
ENGS = ("pe", "act", "dve", "pool", "sp")
EPS = 1e-6
NT = 16
NTC = 18
TOK = 2048
TOKC = 2304
DEBUG = []
STOP_AFTER = None


class Op:
    __slots__ = ("eng", "fn", "tl", "deps", "awaited", "count", "inc", "idx")


class Prog:
    def __init__(self):
        self.ops = []
        self.last_w = {}
        self.readers = {}
        self.tl_last = {}
        self.bar = set()
        self.bar_done = set(ENGS)

    def op(self, eng, fn, reads=(), writes=(), tl=None, inc=1):
        o = Op()
        o.eng = eng
        o.fn = fn
        o.tl = tl if tl is not None else eng
        o.inc = inc
        o.awaited = o.tl not in ENGS
        o.count = None
        o.idx = len(self.ops)
        deps = set()
        for r in reads:
            w = self.last_w.get(r)
            if w is not None:
                deps.add(w)
        for w_ in writes:
            w = self.last_w.get(w_)
            if w is not None:
                deps.add(w)
            rl = self.readers.get(w_)
            if rl:
                deps.update(rl)
        if eng not in self.bar_done:
            deps |= self.bar
            self.bar_done.add(eng)
        o.deps = deps
        self.ops.append(o)
        for r in reads:
            self.readers.setdefault(r, []).append(o.idx)
        for w_ in writes:
            self.last_w[w_] = o.idx
            self.readers[w_] = []
        self.tl_last[o.tl] = o.idx
        return o

    def barrier(self):
        self.bar = set(self.tl_last.values())
        self.bar_done = set()

    def finalize(self):
        ops = self.ops
        for i in self.tl_last.values():
            ops[i].awaited = True
        for o in ops:
            for d in o.deps:
                od = ops[d]
                if od.tl == "pe" and o.tl == "pe":
                    continue
                od.awaited = True
        cnt = {}
        for o in ops:
            if o.awaited:
                cnt[o.tl] = cnt.get(o.tl, 0) + o.inc
                o.count = cnt[o.tl]
        self.totals = cnt
        run_latest = {}
        self.need = [None] * len(ops)
        for o in ops:
            need = {}
            for d in o.deps:
                od = ops[d]
                if od.tl == "pe" and o.tl == "pe":
                    continue
                v = od.count if od.tl in ENGS else run_latest[od.tl]
                if need.get(od.tl, 0) < v:
                    need[od.tl] = v
            self.need[o.idx] = need
            if o.awaited:
                run_latest[o.tl] = o.count
        return sorted(cnt.keys(), key=str)

    def engine_body(self, ename, sems, final=False):
        mine = [o for o in self.ops if o.eng == ename]

        def body(e):
            waited = {}
            for o in mine:
                for tl, v in self.need[o.idx].items():
                    if waited.get(tl, 0) < v:
                        e.wait_ge(sems[tl], v)
                        waited[tl] = v
                ins = o.fn(e)
                if o.awaited:
                    ins.then_inc(sems[o.tl], o.inc)
            if final:
                for tl, v in self.totals.items():
                    if waited.get(tl, 0) < v:
                        e.wait_ge(sems[tl], v)
        return body


class Arena:
    def __init__(self, ap, nbytes, prog=None):
        self.prog = prog
        self.ap = ap
        self.cap = nbytes
        self.off = 0
        self.stack = []
        self.peak = 0

    def alloc(self, shape, dt):
        shape = list(shape)
        n = int(np.prod(shape))
        nb = n * (4 if dt == F32 else 2)
        nb = (nb + 63) // 64 * 64
        assert self.off + nb <= self.cap, f"SBUF arena overflow {self.off}+{nb}>{self.cap}"
        v = self.ap[:, self.off // 2:(self.off + nb) // 2]
        if dt == F32:
            v = v.bitcast(F32)
        v = v[:, 0:n]
        self.off += nb
        self.peak = max(self.peak, self.off)
        if len(shape) == 2:
            v = v.rearrange("p (a b) -> p a b", b=shape[1])
        elif len(shape) == 3:
            v = v.rearrange("p (a b c) -> p a b c", b=shape[1], c=shape[2])
        elif len(shape) == 4:
            v = v.rearrange("p (a b c d) -> p a b c d", b=shape[1], c=shape[2], d=shape[3])
        return v

    def push(self):
        self.stack.append(self.off)

    def pop(self):
        self.off = self.stack.pop()
        if self.prog is not None:
            self.prog.barrier()


def _swap_idx(dh):
    q = dh // 4
    return np.concatenate([np.arange(q, 2 * q), np.arange(0, q), np.arange(3 * q, 4 * q), np.arange(2 * q, 3 * q)])


def _win_perm():
    cols = []
    base = 0
    q = np.arange(base, base + 256)
    k = np.arange(base + 256, base + 512)
    v = np.arange(base + 512, base + 768)
    sw32 = np.concatenate([_swap_idx(32) + 32 * i for i in range(8)])
    cols += [q, k, q[sw32], k[sw32], v]
    base = 768
    qn = np.arange(base, base + 256).reshape(2, 2, 64)
    qperm = np.transpose(qn, (1, 0, 2)).reshape(256)
    kk = np.arange(base + 256, base + 384)
    vv = np.arange(base + 384, base + 512)
    sw64_4 = np.concatenate([_swap_idx(64) + 64 * i for i in range(4)])
    sw64_2 = np.concatenate([_swap_idx(64) + 64 * i for i in range(2)])
    cols += [qperm, kk, qperm[sw64_4], kk[sw64_2], vv]
    base = 1280
    cols += [np.arange(base, base + 768)]
    base = 2048
    q = np.arange(base, base + 256)
    k = np.arange(base + 256, base + 512)
    vg = np.arange(base + 512, base + 1024)
    cols += [q, k, q[sw64_4], k[sw64_4], vg]
    return np.concatenate(cols)


WIN_PERM = _win_perm()
NWIN = len(WIN_PERM)
DA0, SW0, NA0, RT0 = 0, 1280, 2176, 2944


def _rope_tables(j, dh):
    t = 2048 * j + np.arange(2048)
    row = (t // 64).astype(np.float64)
    col = (t % 64).astype(np.float64)
    half = dh // 2
    qd = dh // 4
    inv = 10000.0 ** (-np.arange(qd, dtype=np.float64) * 2.0 / half)
    C = np.zeros((128, 2048), np.float32)
    S = np.zeros((128, 2048), np.float32)
    for p in range(128):
        d = p % dh
        pos = row if d < half else col
        dd = d % half
        i = dd % qd
        ang = pos * inv[i]
        C[p] = np.cos(ang)
        S[p] = -np.sin(ang) if dd < qd else np.sin(ang)
    return C, S


def _swa_masks(j):
    kk = np.arange(128)[:, None]
    qq = np.arange(128)[None, :]
    mprev = (qq <= kk).astype(np.float32)
    mnext = (kk <= qq).astype(np.float32)
    m = np.zeros((10, 128, 128), np.float32)
    m[0] = mprev
    m[1] = mnext
    for r in range(4):
        if r == j - 1:
            m[2 + r] = mprev
        if r == j + 1:
            m[6 + r] = mnext
    return m


def _na_mask(Tq, Tk, flag=True):
    if (not flag) or Tk < 0 or Tk > 63:
        return np.zeros((128, 128), np.float32)
    p = np.arange(128)
    Rk = (2 * Tk + p // 64)[:, None]
    kc = (p % 64)[:, None]
    Rq = (2 * Tq + p // 64)[None, :]
    qc = (p % 64)[None, :]
    start = np.clip(Rq - 4, 0, 120)
    cs = np.clip(qc - 8, 0, 48)
    ok = (Rk >= start) & (Rk < start + 8) & (kc >= cs) & (kc < cs + 16)
    return ok.astype(np.float32)


def _na_masks(j):
    m = np.zeros((45, 128, 128), np.float32)
    for d in range(-2, 3):
        m[d + 2] = _na_mask(10, 10 + d)
    T0 = 16 * j
    idx = 5
    for d in (0, 1, 2, 3):
        m[idx] = _na_mask(T0, T0 + d); idx += 1
    for r in range(4):
        m[idx] = _na_mask(T0, T0 - 2, r == j - 1); idx += 1
    for r in range(4):
        m[idx] = _na_mask(T0, T0 - 1, r == j - 1); idx += 1
    for d in (-1, 0, 1, 2):
        m[idx] = _na_mask(T0 + 1, T0 + 1 + d); idx += 1
    for r in range(4):
        m[idx] = _na_mask(T0 + 1, T0 - 1, r == j - 1); idx += 1
    for d in (-2, -1, 0, 1):
        m[idx] = _na_mask(T0 + 14, T0 + 14 + d); idx += 1
    for r in range(4):
        m[idx] = _na_mask(T0 + 14, T0 + 16, r == j + 1); idx += 1
    for d in (-3, -2, -1, 0):
        m[idx] = _na_mask(T0 + 15, T0 + 15 + d); idx += 1
    for r in range(4):
        m[idx] = _na_mask(T0 + 15, T0 + 16, r == j + 1); idx += 1
    for r in range(4):
        m[idx] = _na_mask(T0 + 15, T0 + 17, r == j + 1); idx += 1
    assert idx == 45
    return m


def _na_bias_layout(rpb):
    p = np.arange(128)
    kr = (p // 64)[:, None]; kc = (p % 64)[:, None]
    qr = (p // 64)[None, :]; qc = (p % 64)[None, :]
    out = np.empty((7, 4, 128, 128), np.float32)
    dc = np.clip(kc - qc, -15, 15) + 15
    for di, d in enumerate(range(-3, 4)):
        dr = np.clip(2 * d + kr - qr, -7, 7) + 7
        out[di] = rpb[:, dr, dc]
    return out


def _ret_consts(j):
    c = np.zeros((128, 700), np.float32)
    i = np.arange(128)
    o = 0
    dif = i[None, :] - i[:, None]
    c[:, 0:128] = np.maximum(dif, 0)
    c[:, 128:256] = (dif >= 0) * 0.125
    c[:, 256:384] = np.maximum(-dif, 0)
    c[:, 384:512] = (dif < 0) * 0.125
    c[:, 512:640] = (i + 1)[None, :]
    c[:, 640] = 127 - i
    c[:, 641] = i
    c[:, 642:660] = (128.0 * np.arange(18))[None, :]
    c[:, 660:678] = (128.0 * (15 - np.arange(18)))[None, :]
    for r in range(4):
        if r < j:
            c[:, 678 + r] = 2048.0 * (j - 1 - r); c[:, 688 + r] = 1.0
        if r > j:
            c[:, 683 + r] = 2048.0 * (r - j - 1); c[:, 693 + r] = 1.0
    c[:, 682] = 2048.0 * j; c[:, 692] = 1.0
    c[:, 687] = 2048.0 * (3 - j); c[:, 697] = 1.0
    return c


def _idxb_table():
    i = np.arange(128)
    return np.broadcast_to((128 - i)[None, :], (128, 128)).astype(np.float32).copy()


def build_program():
    nc = bass.Bass("TRN2", target_bir_lowering=False)
    P = Prog()
    es = contextlib.ExitStack()

    def din(name, shape, dt=F32):
        return nc.dram_tensor(name, list(shape), dt, kind="ExternalInput").ap()

    def dint(name, shape, dt):
        return nc.dram_tensor(name, list(shape), dt)

    SHAPES = {"xin": [TOK, 1024], "xcin": [256, 1024], "cvec": [128, 16], "fwg": [1024, 2816], "fwu": [1024, 2816], "fwd": [2816, 1024],
              "router": [1, 8 * 1024], "mwg": [8, 1024, 3584], "mwu": [8, 1024, 3584], "mwd": [8, 3584, 1024],
              "rope64": [128, 2, 2048], "rope32": [128, 2, 2048], "swamask": [10, 128, 128], "namask": [45, 128, 128],
              "retc": [128, 700], "idxb": [128, 128]}
    for l_ in range(2):
        SHAPES.update({f"wmod{l_}": [1024, 6144], f"bmod{l_}": [1, 6144], f"gvec{l_}": [1, 4096], f"win{l_}": [1024, NWIN],
                       f"wout{l_}": [1024, 1024], f"dal{l_}": [1, 128], f"subln{l_}": [1, 64], f"sink{l_}": [1, 4],
                       f"gam{l_}": [1, 8], f"nabias{l_}": [7, 4, 128, 128]})

    class LazyIn(dict):
        def __missing__(self, k):
            self[k] = din(k, SHAPES[k])
            return self[k]
    I = LazyIn()
    USED_INPUTS = I
    out = nc.dram_tensor("out", [TOK, 1024], F32, kind="ExternalOutput").ap()
    dbg = {}
    for name, shape, dt in (("dbg_ot", [NTC, 128, 8, 128], BF16), ("dbg_x", [TOK, 1024], F32), ("dbg_xc", [256, 1024], F32),
                            ("dbg_misc", [128, 4096], F32)):
        if name in DEBUG:
            dbg[name] = nc.dram_tensor(name, shape, dt, kind="ExternalOutput").ap()

    xs = dint("xs", [TOK, 1024], F32).ap(); xcs = dint("xcs", [256, 1024], F32).ap()
    GROUPS = [[0, 1, 2, 3], [4, 5, 6, 7]]

    arena_t = es.enter_context(nc.sbuf_tensor("arena", [128, 94 * 1024], BF16))
    A = Arena(arena_t, 188 * 1024, P)
    psum = es.enter_context(nc.psum_tensor("psum", [128, 4096], F32))

    def bank(i, n=512, off=0):
        return psum[:, 512 * i + off:512 * i + off + n]

    def bank_bf(i):
        return psum[:, 512 * i:512 * (i + 1)].bitcast(BF16)

    def MM(o, lhsT, rhs, st, sp_, r, w, tp=None, sgc=False):
        kw = {}
        if tp is not None:
            kw["tile_position"] = tp
        if sgc:
            kw["skip_group_check"] = True
        P.op("pe", lambda e: e.matmul(o, lhsT=lhsT, rhs=rhs, start=st, stop=sp_, **kw), r, w)

    def MM64(o, lhsT, rhs, base, st, sp_, r, w):
        if base == 0:
            MM(o, lhsT[0:64], rhs[0:64], st, sp_, r, w, sgc=True)
        else:
            MM(o, lhsT[64:96], rhs[64:96], st, False, r, w, tp=(64, 0), sgc=True)
            MM(o, lhsT[96:128], rhs[96:128], False, sp_, r, w, tp=(96, 0), sgc=True)

    def TR(o, i, ident, r, w):
        P.op("pe", lambda e: e.transpose(o, i, ident), r, w)

    def ACTV(o, i, func, r, w, bias=None, scale=None, accum=None):
        kw = {}
        if bias is not None:
            kw["bias"] = bias
        if scale is not None:
            kw["scale"] = scale
        if accum is not None:
            kw["accum_out"] = accum
        P.op("act", lambda e: e.activation(out=o, in_=i, func=func, **kw), r, w)

    def TT(eng, o, a, b, op, r, w):
        P.op(eng, lambda e: e.tensor_tensor(out=o, in0=a, in1=b, op=op), r, w)

    def TS(eng, o, a, s1, op0, r, w, s2=None, op1=None):
        if op1 is None:
            P.op(eng, lambda e: e.tensor_scalar(out=o, in0=a, scalar1=s1, scalar2=None, op0=op0), r, w)
        else:
            P.op(eng, lambda e: e.tensor_scalar(out=o, in0=a, scalar1=s1, scalar2=s2, op0=op0, op1=op1), r, w)

    def STT(eng, o, a, s, b, op0, op1, r, w):
        P.op(eng, lambda e: e.scalar_tensor_tensor(out=o, in0=a, scalar=s, in1=b, op0=op0, op1=op1), r, w)

    def CP(eng, o, i, r, w):
        if eng == "act":
            P.op("act", lambda e: e.copy(out=o, in_=i), r, w)
        else:
            P.op(eng, lambda e: e.tensor_copy(out=o, in_=i), r, w)

    def MSET(eng, o, val, w):
        P.op(eng, lambda e: e.memset(o, val), (), w)

    def RED(eng, o, i, r, w, mx=False):
        if mx:
            P.op(eng, lambda e: e.reduce_max(out=o, in_=i, axis=AX.X), r, w)
        else:
            P.op(eng, lambda e: e.reduce_sum(out=o, in_=i, axis=AX.X), r, w)

    def RECIP(o, i, r, w):
        P.op("dve", lambda e: e.reciprocal(out=o, in_=i), r, w)

    def DMA(q, o, i, r, w, tl):
        P.op(q, lambda e: e.dma_start(out=o, in_=i), r, w, tl=tl, inc=16)

    def AG(src, dst, r, w, tl):
        P.op("pool", lambda e: e.collective_compute("AllGather", ALU.bypass, replica_groups=GROUPS,
                                                    ins=[src.ap().opt()], outs=[dst.ap().opt()]), r, w, tl=tl, inc=1)

    def rstd_from_ss(ss, n, rstd, r, w):
        ACTV(rstd, ss, AF.Sqrt, r, w, bias=epsc[:, 0:1], scale=1.0 / n)
        RECIP(rstd, rstd, w, w)

    ident_bf = A.alloc([128], BF16); ident_f = A.alloc([128], F32); zeros = A.alloc([128], BF16)
    epsc = A.alloc([1], F32)
    junk = A.alloc([1024], BF16)
    MOD = A.alloc([2, 6, 1024], BF16)
    MSET("pool", ident_f, 0.0, ["ident_f"])
    P.op("pool", lambda e: e.affine_select(out=ident_f, in_=ident_f, pattern=[[-1, 128]], compare_op=ALU.not_equal,
                                           fill=1.0, base=0, channel_multiplier=1), ["ident_f"], ["ident_f"])
    CP("pool", ident_bf, ident_f, ["ident_f"], ["ident_bf"])
    HM = A.alloc([2], F32)
    RED("dve", HM[:, 0:1], ident_f[:, 0:64], ["ident_f"], ["HM"])
    RED("dve", HM[:, 1:2], ident_f[:, 64:128], ["ident_f"], ["HM"])
    MSET("pool", zeros, 0.0, ["zeros"])
    MSET("pool", epsc, EPS, ["epsc"])

    def pbc(ap):
        b = ap.partition_broadcast(128)
        if len(b.shape) == 3 and b.shape[1] == 1:
            b = b[:, 0]
        return b

    stop = [False]

    def tap(name, ap, reads, flat):
        if name in DEBUG:
            shp = [128, int(np.prod(ap.shape[1:]))]
            d = nc.dram_tensor(name, shp, ap.dtype, kind="ExternalOutput").ap()
            DMA("sp", d, ap.rearrange(flat) if flat else ap, reads, [name], "dbg")

    def check_stop(name):
        if STOP_AFTER == name:
            stop[0] = True
        return stop[0]

    for l in range(2):
        if stop[0]:
            break
        with_ctx = (l == 0)
        lam_init = 0.8 - 0.6 * math.exp(-0.3 * l)
        ntl = NTC
        nto = NTC if with_ctx else NT
        xsrc = (I["xin"], I["xcin"]) if l == 0 else (xs, xcs)
        L = f"L{l}"

        def xtile_ap(src2, i):
            return src2[0][i * 128:(i + 1) * 128, :] if i < NT else src2[1][(i - NT) * 128:(i - NT + 1) * 128, :]

        P.barrier()
        A.push()
        cv = A.alloc([16], F32); sil = A.alloc([16], F32); sbc = A.alloc([2, 8, 128], BF16)
        gv = A.alloc([4, 1024], F32); tmpm = A.alloc([1024], F32)
        wm = [A.alloc([8, 1024], BF16) for _ in range(2)]
        bs = [A.alloc([1024], F32) for _ in range(2)]
        DMA("sp", cv, I["cvec"], [], [L + "cv"], "m0")
        DMA("sp", gv, pbc(I[f"gvec{l}"]).rearrange("p (a b) -> p a b", b=1024), [], [L + "gv"], "m0")
        ACTV(sil, cv, AF.Silu, [L + "cv"], [L + "sil"])
        for v in range(2):
            for kc in range(8):
                ACTV(sbc[:, v, kc, :], zeros, AF.Identity, [L + "sil", "zeros"], [L + "sbc"], bias=sil[:, v * 8 + kc:v * 8 + kc + 1])
        wmv = I[f"wmod{l}"].rearrange("(kc p) n -> p kc n", p=128)
        for s in range(6):
            sl = s % 2
            DMA("pool", wm[sl], wmv[:, :, s * 1024:(s + 1) * 1024], [], [L + f"wm{sl}"], f"wm{sl}")
            DMA("sp", bs[sl], pbc(I[f"bmod{l}"][0:1, s * 1024:(s + 1) * 1024]), [], [L + f"bs{sl}"], f"bs{sl}")
            for v in range(2):
                pb = 4 * (s % 2) + 2 * v
                for hf in range(2):
                    for kc in range(8):
                        MM(bank(pb + hf), sbc[:, v, kc, :], wm[sl][:, kc, hf * 512:(hf + 1) * 512], kc == 0, kc == 7,
                           [L + "sbc", L + f"wm{sl}"], [f"ps{pb + hf}"])
                TT("dve", tmpm, psum[:, 512 * pb:512 * pb + 1024], bs[sl], ALU.add, [f"ps{pb}", f"ps{pb + 1}", L + f"bs{sl}"], [L + "tmpm"])
                dst = MOD[:, v, s, :]
                if s in (0, 3):
                    CP("dve", dst, tmpm, [L + "tmpm"], [f"MOD{v}{s}"])
                elif s in (1, 4):
                    STT("dve", dst, tmpm, 1.0, gv[:, 0 if s == 1 else 2, :], ALU.add, ALU.mult, [L + "tmpm", L + "gv"], [f"MOD{v}{s}"])
                else:
                    TT("dve", dst, tmpm, gv[:, 1 if s == 2 else 3, :], ALU.mult, [L + "tmpm", L + "gv"], [f"MOD{v}{s}"])
        if l == 0:
            tap("t_mod", MOD, [f"MOD{v}{s}" for v in range(2) for s in range(6)], "p a b c -> p (a b c)")
        A.pop()
        if check_stop(f"p0_{l}"):
            break

        P.barrier()
        A.push()
        moe = (l == 1)
        if moe:
            LOGI = A.alloc([NT, 8], F32); GATES = A.alloc([NT, 8], F32)
        hT = A.alloc([8, TOKC], BF16)
        otd = dint(L + "otd", [NTC, 128, 8, 128], BF16).ap()
        A.push()
        otst = [A.alloc([2, 128], BF16) for _ in range(2)]

        A.push()
        xt = [A.alloc([1024], F32) for _ in range(2)]
        t1 = [A.alloc([1024], F32) for _ in range(2)]
        hb = [A.alloc([1024], BF16) for _ in range(2)]
        ssb = A.alloc([NTC], F32); rsb = A.alloc([NTC], F32)
        for i in range(ntl):
            s2 = i % 2
            v = 0 if i < NT else 1
            DMA("sp", xt[s2], xtile_ap(xsrc, i), [], [L + f"xt{s2}"], f"xt{s2}")
            ACTV(junk, xt[s2], AF.Square, [L + f"xt{s2}"], ["junk", L + f"ss{i}"], accum=ssb[:, i:i + 1])
            rstd_from_ss(ssb[:, i:i + 1], 1024, rsb[:, i:i + 1], [L + f"ss{i}", "epsc"], [L + f"rs{i}"])
            STT("dve", t1[s2], xt[s2], rsb[:, i:i + 1], MOD[:, v, 1, :], ALU.mult, ALU.mult, [L + f"xt{s2}", L + f"rs{i}", f"MOD{v}1"], [L + f"t1{s2}"])
            TT("dve", hb[s2], t1[s2], MOD[:, v, 0, :], ALU.add, [L + f"t1{s2}", f"MOD{v}0"], [L + f"hb{s2}"])
            for kc in range(8):
                TR(bank_bf(s2)[:, kc * 128:(kc + 1) * 128], hb[s2][:, kc * 128:(kc + 1) * 128], ident_bf, [L + f"hb{s2}", "ident_bf"], [f"ps{s2}"])
            CP("act", hT[:, :, i * 128:(i + 1) * 128], bank_bf(s2).rearrange("p (k t) -> p k t", t=128), [f"ps{s2}"], [L + f"hT{i}"])
        A.pop()
        HT_ALL = [L + f"hT{i}" for i in range(ntl)]
        if l == 0:
            tap("t_hT", hT, HT_ALL, "p a b -> p (a b)")
        if check_stop(f"p1a_{l}"):
            A.pop(); A.pop(); break

        winv = I[f"win{l}"].rearrange("(kc p) n -> p kc n", p=128)

        def proj_rope(wq, wqs, Ctab, Stab, dests, tag):
            tA = [A.alloc([512], F32) for _ in range(2)]
            tB = [A.alloc([512], F32) for _ in range(2)]
            n = 0
            for ci in range(len(wq)):
                for tb in range(4):
                    s2 = n % 2; n += 1
                    ts = slice(tb * 512, (tb + 1) * 512)
                    for kc in range(8):
                        MM(bank(s2), wq[ci][:, kc, :], hT[:, kc, ts], kc == 0, kc == 7, HT_ALL[4 * tb:4 * tb + 4] + [tag + "w"], [f"ps{s2}"])
                    for kc in range(8):
                        MM(bank(2 + s2), wqs[ci][:, kc, :], hT[:, kc, ts], kc == 0, kc == 7, HT_ALL[4 * tb:4 * tb + 4] + [tag + "w"], [f"ps{2 + s2}"])
                    TT("dve", tA[s2], bank(s2), Ctab[:, ts], ALU.mult, [f"ps{s2}", L + "rope"], [tag + f"tA{s2}"])
                    TT("dve", tB[s2], bank(2 + s2), Stab[:, ts], ALU.mult, [f"ps{2 + s2}", L + "rope"], [tag + f"tB{s2}"])
                    TT("dve", dests[ci][:, ts], tA[s2], tB[s2], ALU.add, [tag + f"tA{s2}", tag + f"tB{s2}"], [tag + f"d{ci}"])

        def proj_feat_plain(wq, dest, t0, nt, tag, ci, pb):
            for kc in range(8):
                MM(bank(pb, nt), wq[:, kc, :], hT[:, kc, t0:t0 + nt], kc == 0, kc == 7, HT_ALL + [tag + "w"], [f"ps{pb}"])
            CP("act", dest, bank(pb, nt), [f"ps{pb}"], [tag + f"d{ci}"])

        def proj_tok(wv, ncols, dest_fn, tiles, tag, post=None, view=None):
            for n, i in enumerate(tiles):
                pb = 4 + n % 2
                for kc in range(8):
                    MM(bank(pb, ncols), hT[:, kc, i * 128:(i + 1) * 128], wv[:, kc, :], kc == 0, kc == 7, [L + f"hT{i}", tag + "w"], [f"ps{pb}"])
                if post is None:
                    src_ = bank(pb, ncols)
                    if view is not None:
                        src_ = view(src_)
                    CP("act", dest_fn(i), src_, [f"ps{pb}"], [tag + f"v{i}"])
                else:
                    post(i, pb)

        def out_transposes(ytok, chunk0, i, tag, rd):
            pb = 6 + (i % 2)
            st = otst[i % 2]
            for cc in range(2):
                TR(bank_bf(pb)[:, cc * 128:(cc + 1) * 128], ytok[:, cc * 128:(cc + 1) * 128], ident_bf, rd + ["ident_bf"], [f"ps{pb}"])
            CP("act", st, bank_bf(pb)[:, 0:256].rearrange("p (c t) -> p c t", t=128), [f"ps{pb}"], [L + f"otst{i % 2}"])
            DMA("sp", otd[i, :, chunk0:chunk0 + 2, :], st, [L + f"otst{i % 2}"], [L + f"OT{chunk0}_{i}"], f"ot{i % 2}")

        def attn_pipeline(tiles_slots, S_fn, E_fn, AV_fn, FIN_fn):
            units = []
            for (i, slots) in tiles_slots:
                ngr = (len(slots) + 1) // 2
                for gi in range(ngr):
                    units.append((i, gi, ngr, slots[2 * gi:2 * gi + 2]))
            pending = None
            for k, u in enumerate(units):
                if k == 0:
                    S_fn(0, u)
                E_fn(k, u)
                if k + 1 < len(units):
                    S_fn(k + 1, units[k + 1])
                AV_fn(k, u)
                if pending is not None:
                    FIN_fn(pending); pending = None
                if u[1] == u[2] - 1:
                    pending = u[0]
            if pending is not None:
                FIN_fn(pending)

        rope64 = A.alloc([2, 2048], BF16); rope32 = A.alloc([2, 2048], BF16)
        DMA("pool", rope64, I["rope64"], [], [L + "rope"], "rp")
        DMA("pool", rope32, I["rope32"], [], [L + "rope"], "rp")

        T = L + "da"
        A.push()
        QT = A.alloc([2, TOKC], BF16); KTc = A.alloc([2, 256], BF16)
        Vc = A.alloc([2, 256], BF16)
        nlam = A.alloc([1], F32); gsub = A.alloc([64], F32)
        A.push()
        dl = A.alloc([4, 32], F32); pr = A.alloc([2, 32], F32); s12 = A.alloc([2], F32)
        DMA("sp", dl, pbc(I[f"dal{l}"]).rearrange("p (a b) -> p a b", b=32), [], [T + "dl"], "m0")
        DMA("sp", gsub, pbc(I[f"subln{l}"]), [], [T + "gsub"], "m0")
        TT("dve", pr[:, 0, :], dl[:, 0, :], dl[:, 1, :], ALU.mult, [T + "dl"], [T + "pr"])
        TT("dve", pr[:, 1, :], dl[:, 2, :], dl[:, 3, :], ALU.mult, [T + "dl"], [T + "pr"])
        RED("dve", s12, pr, [T + "pr"], [T + "s12"])
        ACTV(s12, s12, AF.Exp, [T + "s12"], [T + "s12"])
        TT("dve", nlam, s12[:, 1:2], s12[:, 0:1], ALU.subtract, [T + "s12"], [T + "nlam"])
        TS("dve", nlam, nlam, -lam_init, ALU.add, [T + "nlam"], [T + "nlam"])
        TS("dve", gsub, gsub, 1.0 - lam_init, ALU.mult, [T + "gsub"], [T + "gsub"])
        A.pop()
        A.push()
        wda = A.alloc([8, 1280], BF16)
        KTo = A.alloc([2, TOK], BF16); Vo = A.alloc([NT, 256], BF16)
        DMA("pool", wda, winv[:, :, DA0:DA0 + 1280], [], [T + "w"], "wA")
        qk_dest = [QT[:, 0, 0:TOK], QT[:, 1, 0:TOK], KTo[:, 0, :], KTo[:, 1, :]]
        proj_rope([wda[:, :, c * 128:(c + 1) * 128] for c in range(4)], [wda[:, :, 512 + c * 128:512 + (c + 1) * 128] for c in range(4)],
                  rope32[:, 0, :], rope32[:, 1, :], qk_dest, T)
        for ci in range(4):
            dest = QT[:, ci, TOK:TOKC] if ci < 2 else KTc[:, ci - 2, :]
            proj_feat_plain(wda[:, :, ci * 128:(ci + 1) * 128], dest, TOK, 256, T, f"c{ci}", ci % 2)
        proj_tok(wda[:, :, 1024:1280], 256, lambda i: Vo[:, i, :] if i < NT else Vc[:, i - NT, :], range(NTC), T)
        QK_R = [T + f"d{ci}" for ci in range(4)] + [T + f"dc{ci}" for ci in range(4)]
        V_R = [T + f"v{i}" for i in range(NTC)]
        if check_stop(f"daproj_{l}"):
            tap("t_qt", QT, QK_R, "p a b -> p (a b)")
            tap("t_kto", KTo, QK_R, "p a b -> p (a b)")
            tap("t_vo", Vo, V_R, "p a b -> p (a b)")
            tap("t_ktc", KTc, QK_R, "p a b -> p (a b)")
            tap("t_vc", Vc, V_R, "p a b -> p (a b)")
            A.pop(); A.pop(); A.pop(); A.pop(); break
        e_k = dint(T + "ek", [256, TOK], BF16); e_v = dint(T + "ev", [TOK, 256], BF16)
        g_k = dint(T + "gk", [1024, TOK], BF16); g_v = dint(T + "gv", [4 * TOK, 256], BF16)
        DMA("sp", e_k.ap().rearrange("(c p) t -> p c t", p=128), KTo, QK_R, [T + "ek"], "ex")
        DMA("sp", e_v.ap().rearrange("(i p) f -> p i f", p=128), Vo, V_R, [T + "ev"], "ex")
        AG(e_k, g_k, [T + "ek"], [T + "gk"], "cc")
        AG(e_v, g_v, [T + "ev"], [T + "gv"], "cc")
        A.pop()
        P.barrier()
        if check_stop(f"daag_{l}"):
            A.pop(); A.pop(); A.pop(); break
        KT = A.alloc([8448], BF16); V1 = A.alloc([66, 2, 65], BF16)
        ptH = [[A.alloc([2, 512], BF16) for _ in range(2)] for _ in range(2)]
        o_f = A.alloc([2, 64], F32); o1 = A.alloc([64], F32)
        rec = A.alloc([4], F32); rn = A.alloc([2], F32); ssd = A.alloc([2], F32); rsd = A.alloc([2], F32)
        yda = [A.alloc([2, 64], BF16) for _ in range(2)]
        MSET("pool", V1[:, :, :, 64:65], 1.0, [T + "V1ones"])
        g_kv = g_k.ap().rearrange("(r c p) t -> p r c t", r=4, c=2)
        g_vv = g_v.ap().rearrange("(r i p) f -> p r i f", r=4, i=NT)
        nblk = 0
        for c in range(2):
            CP("pool", KT[:, 0:256], KTc[:, c, :], QK_R, [T + "KT"])
            for r in range(4):
                DMA("sp", KT[:, 256 + r * TOK:256 + (r + 1) * TOK], g_kv[:, r, c, :], [T + "gk"], [T + "KT"], "kt")
                for hh in range(2):
                    DMA("sp", V1[:, 2 + r * NT:2 + (r + 1) * NT, hh, 0:64], g_vv[:, r, :, c * 128 + hh * 64:c * 128 + hh * 64 + 64],
                        [T + "gv"], [T + "V1"], "kt")
            CP("pool", V1[:, 0:2, :, 0:64], Vc[:, :, c * 128:(c + 1) * 128].rearrange("p i (h d) -> p i h d", d=64), V_R, [T + "V1"])
            if STOP_AFTER == f"daload_{l}":
                continue
            qblocks = [(qb * 512, 512, 0, 66) for qb in range(4)]
            if STOP_AFTER == f"daq1_{l}":
                qblocks = [(0, 512, 0, 66)] if c == 0 else []
            if STOP_AFTER == f"daq1nf_{l}":
                qblocks = [(0, 512, 0, 66)] if c == 0 else []
            if with_ctx:
                qblocks.append((TOK, 256, 0, 2))
            for (q0, nq, k0, k1) in qblocks:
                sc_ = 1.0 / math.sqrt(32.0)

                def S_half(kt, hf):
                    for g in (2 * hf, 2 * hf + 1):
                        MM(bank(g, nq), KT[32 * g:32 * g + 32, kt * 128:(kt + 1) * 128], QT[32 * g:32 * g + 32, c, q0:q0 + nq], True, True,
                           [T + "KT"] + QK_R, [f"psS{hf}"], tp=(32 * g, 0))

                def E_half(kt, hf):
                    s2 = (kt - k0) % 2
                    ACTV(ptH[hf][s2][:, :, 0:nq], psum[:, 1024 * hf:1024 * hf + 1024].rearrange("p (g n) -> p g n", n=512)[:, :, 0:nq], AF.Exp,
                         [f"psS{hf}"], [T + f"pt{hf}{s2}"], scale=sc_)

                def AV_half(kt, hf):
                    s2 = (kt - k0) % 2
                    for sb in range(nq // 128):
                        for gg in range(2):
                            g = 2 * hf + gg
                            MM(bank(4 + sb, 65, g * 65), ptH[hf][s2][:, gg, sb * 128:(sb + 1) * 128], V1[:, kt, hf, :], kt == k0 and g == 0, kt == k1 - 1,
                               [T + f"pt{hf}{s2}", T + "V1", T + "V1ones"], [f"psO{sb}"], sgc=True)

                S_half(k0, 0); S_half(k0, 1)
                for kt in range(k0, k1):
                    E_half(kt, 0); E_half(kt, 1)
                    AV_half(kt, 0)
                    if kt + 1 < k1:
                        S_half(kt + 1, 0)
                    AV_half(kt, 1)
                    if kt + 1 < k1:
                        S_half(kt + 1, 1)
                if STOP_AFTER == f"daq1nf_{l}":
                    continue
                for sb in range(nq // 128):
                    tile_i = (q0 + sb * 128) // 128
                    yb = yda[nblk % 2]; ybn = T + f"yda{nblk % 2}"; nblk += 1
                    bk = 4 + sb
                    pr_ = f"psO{sb}"
                    Tv = bank(bk, 260).rearrange("p (g e) -> p g e", e=65)
                    RECIP(rec, Tv[:, :, 64], [pr_], [T + "rec"])
                    TS("dve", rn, rec.rearrange("p (h m) -> p h m", m=2)[:, :, 1], nlam[:, 0:1], ALU.mult, [T + "rec", T + "nlam"], [T + "rn"])
                    for hh in range(2):
                        TS("dve", o1, Tv[:, 2 * hh, 0:64], rec[:, 2 * hh:2 * hh + 1], ALU.mult, [pr_, T + "rec"], [T + "o1"])
                        STT("dve", o_f[:, hh, :], Tv[:, 2 * hh + 1, 0:64], rn[:, hh:hh + 1], o1, ALU.mult, ALU.add, [pr_, T + "rn", T + "o1"], [T + "o_f"])
                        ACTV(junk[:, 0:64], o_f[:, hh, :], AF.Square, [T + "o_f"], ["junk", T + "ssd"], accum=ssd[:, hh:hh + 1])
                    rstd_from_ss(ssd, 64, rsd, [T + "ssd", "epsc"], [T + "rsd"])
                    for hh in range(2):
                        STT("dve", yb[:, hh, :], o_f[:, hh, :], rsd[:, hh:hh + 1], gsub, ALU.mult, ALU.mult, [T + "o_f", T + "rsd", T + "gsub"], [ybn])
                    TR(bank_bf(bk)[:, 640:768], yb.rearrange("p h d -> p (h d)"), ident_bf, [ybn, "ident_bf"], [pr_])
                    CP("act", otst[sb % 2][:, 0, :], bank_bf(bk)[:, 640:768], [pr_], [L + f"otst{sb % 2}"])
                    DMA("sp", otd[tile_i, :, c, :], otst[sb % 2][:, 0, :], [L + f"otst{sb % 2}"], [L + f"OT{c}_{tile_i}"], f"ot{sb % 2}")
        A.pop()
        if check_stop(f"da_{l}") or STOP_AFTER in (f"daload_{l}", f"daq1_{l}", f"daq1nf_{l}"):
            P.barrier()
            tap("t_nlam", nlam, [], None)
            tap("t_gsub", gsub, [], None)
            if "dbg_ot" in dbg:
                P.barrier()
                DMA("sp", dbg["dbg_ot"], otd, [], ["dbgot"], "dbg")
            A.pop(); A.pop(); break

        P.barrier()
        T = L + "sw"
        A.push()
        QT = A.alloc([2, TOKC], BF16)
        KT = A.alloc([3328], BF16)
        V1 = A.alloc([26, 2, 65], BF16)
        MSW = A.alloc([10, 128], BF16)
        esink = A.alloc([4], F32)
        DMA("pool", MSW, I["swamask"].rearrange("m k q -> k m q"), [], [T + "msw"], "wB")
        DMA("sp", esink, pbc(I[f"sink{l}"]), [], [T + "esink"], "m0")
        ACTV(esink, esink, AF.Exp, [T + "esink"], [T + "esink"])
        MSET("pool", V1[:, :, :, 64:65], 1.0, [T + "V1ones"])
        A.push()
        wsw = A.alloc([8, 896], BF16)
        DMA("pool", wsw, winv[:, :, SW0:SW0 + 896], [], [T + "w"], "wA")
        dests = [QT[:, 0, 0:TOK], QT[:, 1, 0:TOK], KT[:, 0:TOK]]
        proj_rope([wsw[:, :, c * 128:(c + 1) * 128] for c in range(3)], [wsw[:, :, 384 + c * 128:384 + (c + 1) * 128] for c in range(3)],
                  rope64[:, 0, :], rope64[:, 1, :], dests, T)
        for ci in range(3):
            dest = QT[:, ci, TOK:TOKC] if ci < 2 else KT[:, TOK:TOKC]
            proj_feat_plain(wsw[:, :, ci * 128:(ci + 1) * 128], dest, TOK, 256, T, f"c{ci}", ci % 2)
        proj_tok(wsw[:, :, 768:896], 128, lambda i: V1[:, i, :, 0:64], range(NTC), T, view=lambda a: a.rearrange("p (h d) -> p h d", d=64))
        A.pop()
        QK_R = [T + f"d{ci}" for ci in range(3)] + [T + f"dc{ci}" for ci in range(3)]
        V_R = [T + f"v{i}" for i in range(NTC)]
        if check_stop(f"swproj_{l}"):
            A.pop(); A.pop(); A.pop(); break
        e_s = dint(T + "e", [128, 512], BF16); g_s = dint(T + "g", [512, 512], BF16)
        DMA("sp", e_s.ap()[:, 0:128], KT[:, 0:128], QK_R, [T + "e"], "ex")
        DMA("sp", e_s.ap()[:, 128:256], KT[:, TOK - 128:TOK], QK_R, [T + "e"], "ex")
        DMA("sp", e_s.ap()[:, 256:384].rearrange("p (h d) -> p h d", d=64), V1[:, 0, :, 0:64], V_R, [T + "e"], "ex")
        DMA("sp", e_s.ap()[:, 384:512].rearrange("p (h d) -> p h d", d=64), V1[:, 15, :, 0:64], V_R, [T + "e"], "ex")
        AG(e_s, g_s, [T + "e"], [T + "g"], "cc")
        g_sv = g_s.ap().rearrange("(r p) f -> p r f", p=128)
        DMA("sp", KT[:, TOKC:TOKC + 512].rearrange("p (r t) -> p r t", t=128), g_sv[:, :, 128:256], [T + "g"], [T + "halo"], "kt")
        DMA("sp", KT[:, TOKC + 512:TOKC + 1024].rearrange("p (r t) -> p r t", t=128), g_sv[:, :, 0:128], [T + "g"], [T + "halo"], "kt")
        for kv in range(2):
            DMA("sp", V1[:, 18:22, kv, 0:64], g_sv[:, :, 384 + kv * 64:448 + kv * 64], [T + "g"], [T + "halo"], "kt")
            DMA("sp", V1[:, 22:26, kv, 0:64], g_sv[:, :, 256 + kv * 64:320 + kv * 64], [T + "g"], [T + "halo"], "kt")
        if check_stop(f"swag_{l}"):
            A.pop(); A.pop(); A.pop(); break
        ptw = [A.alloc([2, 4, 128], BF16) for _ in range(2)]
        qz = [A.alloc([2, 2, 128], BF16) for _ in range(2)]
        den = [A.alloc([4], F32) for _ in range(2)]; ysw = [A.alloc([4, 64], BF16) for _ in range(2)]
        ALLR = QK_R + V_R + [T + "halo", T + "V1ones"]
        tiles_slots = []
        for i in range(nto):
            if i < NT:
                slots = []
                if i > 0:
                    slots.append((128 * (i - 1), i - 1, 0))
                slots.append((128 * i, i, None))
                if i < NT - 1:
                    slots.append((128 * (i + 1), i + 1, 1))
                slots += [(TOK, 16, None), (TOK + 128, 17, None)]
                if i == 0:
                    slots += [(TOKC + 128 * r, 18 + r, 2 + r) for r in range(4)]
                if i == NT - 1:
                    slots += [(TOKC + 512 + 128 * r, 22 + r, 6 + r) for r in range(4)]
            else:
                slots = [(TOK, 16, None), (TOK + 128, 17, None)]
            tiles_slots.append((i, slots))

        def sw_S(k, u):
            i, gi, ngr, grp = u
            s2 = k % 2
            ts = slice(i * 128, (i + 1) * 128)
            if gi == 0:
                for kv in range(2):
                    TS("pool" if kv else "dve", qz[i % 2][:, kv], QT[:, :, ts], HM[:, kv:kv + 1], ALU.mult, QK_R + ["HM"], [T + f"qz{i % 2}"])
            for si, (kc0, vt, mi) in enumerate(grp):
                for kv in range(2):
                    for g in range(2):
                        MM(bank(2 * s2 + si, 128, (kv * 2 + g) * 128), KT[:, kc0:kc0 + 128], qz[i % 2][:, kv, g, :], True, True,
                           ALLR + [T + f"qz{i % 2}"], [f"psS{s2}"], sgc=True)

        def sw_E(k, u):
            i, gi, ngr, grp = u
            s2 = k % 2
            ns = len(grp)
            ACTV(ptw[s2][:, 0:ns].rearrange("p s h q -> p (s h q)"), psum[:, 1024 * s2:1024 * s2 + 512 * ns], AF.Exp, [f"psS{s2}"], [T + f"pt{s2}"], scale=0.125)
            for si, (kc0, vt, mi) in enumerate(grp):
                if mi is not None:
                    TT("dve", ptw[s2][:, si], ptw[s2][:, si], MSW[:, mi, :].unsqueeze(1).to_broadcast([128, 4, 128]), ALU.mult, [T + f"pt{s2}", T + "msw"], [T + f"pt{s2}"])

        def sw_AV(k, u):
            i, gi, ngr, grp = u
            s2 = k % 2
            ns = len(grp)
            for si, (kc0, vt, mi) in enumerate(grp):
                first = (gi == 0 and si == 0); last = (gi == ngr - 1 and si == ns - 1)
                for kv in range(2):
                    for g in range(2):
                        h = kv * 2 + g
                        MM(bank(4 + i % 2, 65, h * 65), ptw[s2][:, si, h, :], V1[:, vt, kv, :], first and h == 0, last, [T + f"pt{s2}"] + ALLR, [f"psO{i % 2}"], sgc=True)

        def sw_FIN(i):
            Ov = bank(4 + i % 2, 260).rearrange("p (h e) -> p h e", e=65)
            dn = den[i % 2]
            TT("dve", dn, Ov[:, :, 64], esink, ALU.add, [f"psO{i % 2}", T + "esink"], [T + f"den{i % 2}"])
            RECIP(dn, dn, [T + f"den{i % 2}"], [T + f"den{i % 2}"])
            yb = ysw[i % 2]
            TT("dve", yb, Ov[:, :, 0:64], dn.unsqueeze(2).to_broadcast([128, 4, 64]), ALU.mult, [f"psO{i % 2}", T + f"den{i % 2}"], [T + f"y{i % 2}"])
            out_transposes(yb.rearrange("p h d -> p (h d)"), 2, i, T, [T + f"y{i % 2}"])

        attn_pipeline(tiles_slots, sw_S, sw_E, sw_AV, sw_FIN)
        A.pop()
        if check_stop(f"sw_{l}") or (STOP_AFTER or "").startswith("swq1"):
            if "dbg_ot" in dbg:
                P.barrier()
                DMA("sp", dbg["dbg_ot"], otd, [], ["dbgot"], "dbg")
            A.pop(); A.pop(); break

        P.barrier()
        T = L + "na"
        A.push()
        QT = A.alloc([2, TOKC], BF16)
        KT = A.alloc([2, 4352], BF16)
        V1 = A.alloc([34, 4, 65], BF16)
        MBK = A.alloc([45, 128], BF16)
        BEX = A.alloc([7, 4, 128], BF16)
        EIN = A.alloc([5, 4, 128], BF16)
        for m0 in range(0, 45, 9):
            DMA("pool", MBK[:, m0:m0 + 9, :], I["namask"][m0:m0 + 9].rearrange("m k q -> k m q"), [], [T + "mbk"], "wB")
        A.push()
        bfl = A.alloc([7, 4, 128], F32)
        for d7 in range(7):
            DMA("sp", bfl[:, d7], I[f"nabias{l}"][d7].rearrange("h k q -> k h q"), [], [T + "bfl"], "m0")
        ACTV(BEX, bfl, AF.Exp, [T + "bfl"], [T + "bex"])
        A.pop()
        for d in range(5):
            TT("dve", EIN[:, d], BEX[:, d + 1], MBK[:, d, :].unsqueeze(1).to_broadcast([128, 4, 128]), ALU.mult, [T + "bex", T + "mbk"], [T + "ein"])
        MSET("pool", V1[:, :, :, 64:65], 1.0, [T + "V1ones"])
        A.push()
        wna = A.alloc([8, 768], BF16)
        DMA("pool", wna, winv[:, :, NA0:NA0 + 768], [], [T + "w"], "wA")
        n = 0
        for ci in range(4):
            for (t0, nt_) in ((0, 512), (512, 512), (1024, 512), (1536, 512), (TOK, 256)):
                dest = QT[:, ci, t0:t0 + nt_] if ci < 2 else KT[:, ci - 2, t0:t0 + nt_]
                proj_feat_plain(wna[:, :, ci * 128:(ci + 1) * 128], dest, t0, nt_, T, f"c{ci}", n % 4); n += 1
        proj_tok(wna[:, :, 512:768], 256, lambda i: V1[:, i, :, 0:64], range(NTC), T, view=lambda a: a.rearrange("p (h d) -> p h d", d=64))
        A.pop()
        P.barrier()
        QK_R = [T + f"dc{ci}" for ci in range(4)]
        V_R = [T + f"v{i}" for i in range(NTC)]
        e_n = dint(T + "e", [128, 2048], BF16); g_n = dint(T + "g", [512, 2048], BF16)
        env = e_n.ap()
        for c in range(2):
            DMA("sp", env[:, c * 512:c * 512 + 256], KT[:, c, 0:256], QK_R, [T + "e"], "ex")
            DMA("sp", env[:, c * 512 + 256:c * 512 + 512], KT[:, c, TOK - 256:TOK], QK_R, [T + "e"], "ex")
        DMA("sp", env[:, 1024:1536].rearrange("p (i h d) -> p i h d", h=4, d=64), V1[:, 0:2, :, 0:64], V_R, [T + "e"], "ex")
        DMA("sp", env[:, 1536:2048].rearrange("p (i h d) -> p i h d", h=4, d=64), V1[:, 14:16, :, 0:64], V_R, [T + "e"], "ex")
        AG(e_n, g_n, [T + "e"], [T + "g"], "cc")
        g_nv = g_n.ap().rearrange("(r p) f -> p r f", p=128)
        for c in range(2):
            DMA("sp", KT[:, c, TOKC:TOKC + 1024].rearrange("p (r t) -> p r t", t=256), g_nv[:, :, c * 512 + 256:c * 512 + 512], [T + "g"], [T + "halo"], "kt")
            DMA("sp", KT[:, c, TOKC + 1024:TOKC + 2048].rearrange("p (r t) -> p r t", t=256), g_nv[:, :, c * 512:c * 512 + 256], [T + "g"], [T + "halo"], "kt")
        for r in range(4):
            DMA("sp", V1[:, 18 + 2 * r:20 + 2 * r, :, 0:64], g_nv[:, r, 1536:2048].rearrange("p (i h d) -> p i h d", h=4, d=64), [T + "g"], [T + "halo"], "kt")
            DMA("sp", V1[:, 26 + 2 * r:28 + 2 * r, :, 0:64], g_nv[:, r, 1024:1536].rearrange("p (i h d) -> p i h d", h=4, d=64), [T + "g"], [T + "halo"], "kt")
        ptn = [A.alloc([2, 4, 128], BF16) for _ in range(2)]
        qz = [A.alloc([2, 2, 128], BF16) for _ in range(2)]
        den = [A.alloc([4], F32) for _ in range(2)]; yna = [A.alloc([4, 64], BF16) for _ in range(2)]
        ALLR = QK_R + V_R + [T + "halo", T + "V1ones"]
        PC0 = TOKC; NC0 = TOKC + 1024
        tiles_slots = []
        for i in range(nto):
            if i >= NT:
                slots = [(TOK, 16, 0, 0, 0), (TOK + 128, 17, 0, 0, 0)]
            elif 2 <= i <= 13:
                slots = [(128 * (i + d), i + d, 1, d + 2, 0) for d in range(-2, 3)]
            elif i == 0:
                slots = [(128 * d, d, 2, d + 3, 5 + d) for d in (0, 1, 2, 3)]
                slots += [(PC0 + 256 * r, 18 + 2 * r, 2, 1, 9 + r) for r in range(4)]
                slots += [(PC0 + 256 * r + 128, 19 + 2 * r, 2, 2, 13 + r) for r in range(4)]
            elif i == 1:
                slots = [(128 * (1 + d), 1 + d, 2, d + 3, 17 + (d + 1)) for d in (-1, 0, 1, 2)]
                slots += [(PC0 + 256 * r + 128, 19 + 2 * r, 2, 1, 21 + r) for r in range(4)]
            elif i == 14:
                slots = [(128 * (14 + d), 14 + d, 2, d + 3, 25 + (d + 2)) for d in (-2, -1, 0, 1)]
                slots += [(NC0 + 256 * r, 26 + 2 * r, 2, 5, 29 + r) for r in range(4)]
            else:
                slots = [(128 * (15 + d), 15 + d, 2, d + 3, 33 + (d + 3)) for d in (-3, -2, -1, 0)]
                slots += [(NC0 + 256 * r, 26 + 2 * r, 2, 4, 37 + r) for r in range(4)]
                slots += [(NC0 + 256 * r + 128, 27 + 2 * r, 2, 5, 41 + r) for r in range(4)]
            if i < NT:
                slots += [(TOK, 16, 0, 0, 0), (TOK + 128, 17, 0, 0, 0)]
            tiles_slots.append((i, slots))

        def na_S(k, u):
            i, gi, ngr, grp = u
            s2 = k % 2
            ts = slice(i * 128, (i + 1) * 128)
            if gi == 0:
                for hb_ in range(2):
                    TS("pool" if hb_ else "dve", qz[i % 2][:, hb_], QT[:, :, ts], HM[:, hb_:hb_ + 1], ALU.mult, QK_R + ["HM"], [T + f"qz{i % 2}"])
            for si, (kc0, vt, kind, bd, mi) in enumerate(grp):
                for h in range(4):
                    c, hb_ = h // 2, h % 2
                    MM(bank(2 * s2 + si, 128, h * 128), KT[:, c, kc0:kc0 + 128], qz[i % 2][:, hb_, c, :], True, True, ALLR + [T + f"qz{i % 2}"], [f"psS{s2}"], sgc=True)

        def na_E(k, u):
            i, gi, ngr, grp = u
            s2 = k % 2
            ns = len(grp)
            ACTV(ptn[s2][:, 0:ns].rearrange("p s h q -> p (s h q)"), psum[:, 1024 * s2:1024 * s2 + 512 * ns], AF.Exp, [f"psS{s2}"], [T + f"pt{s2}"], scale=0.125)
            for si, (kc0, vt, kind, bd, mi) in enumerate(grp):
                if kind == 1:
                    TT("dve", ptn[s2][:, si], ptn[s2][:, si], EIN[:, bd], ALU.mult, [T + f"pt{s2}", T + "ein"], [T + f"pt{s2}"])
                elif kind == 2:
                    TT("dve", ptn[s2][:, si], ptn[s2][:, si], BEX[:, bd], ALU.mult, [T + f"pt{s2}", T + "bex"], [T + f"pt{s2}"])
                    TT("pool", ptn[s2][:, si], ptn[s2][:, si], MBK[:, mi, :].unsqueeze(1).to_broadcast([128, 4, 128]), ALU.mult, [T + f"pt{s2}", T + "mbk"], [T + f"pt{s2}"])

        def na_AV(k, u):
            i, gi, ngr, grp = u
            s2 = k % 2
            ns = len(grp)
            for si, (kc0, vt, kind, bd, mi) in enumerate(grp):
                first = (gi == 0 and si == 0); last = (gi == ngr - 1 and si == ns - 1)
                for h in range(4):
                    MM(bank(4 + i % 2, 65, h * 65), ptn[s2][:, si, h, :], V1[:, vt, h, :], first and h == 0, last, [T + f"pt{s2}"] + ALLR, [f"psO{i % 2}"], sgc=True)

        def na_FIN(i):
            Ov = bank(4 + i % 2, 260).rearrange("p (h e) -> p h e", e=65)
            dn = den[i % 2]
            RECIP(dn, Ov[:, :, 64], [f"psO{i % 2}"], [T + f"den{i % 2}"])
            yb = yna[i % 2]
            TT("dve", yb, Ov[:, :, 0:64], dn.unsqueeze(2).to_broadcast([128, 4, 64]), ALU.mult, [f"psO{i % 2}", T + f"den{i % 2}"], [T + f"y{i % 2}"])
            out_transposes(yb.rearrange("p h d -> p (h d)"), 4, i, T, [T + f"y{i % 2}"])

        attn_pipeline(tiles_slots, na_S, na_E, na_AV, na_FIN)
        A.pop()
        if check_stop(f"na_{l}"):
            if "dbg_ot" in dbg:
                P.barrier()
                DMA("sp", dbg["dbg_ot"], otd, [], ["dbgot"], "dbg")
            A.pop(); A.pop(); break

        P.barrier()
        T = L + "rt"
        A.push()
        QT = A.alloc([2, TOKC], BF16); KT = A.alloc([2, TOKC], BF16)
        VR = A.alloc([NTC, 256], BF16); GT = A.alloc([NTC, 256], BF16)
        RC = A.alloc([700], F32); IDXB = A.alloc([128], F32)
        LG = A.alloc([8], F32); LGS = A.alloc([2, 2], F32)
        DEC = A.alloc([4, 128], BF16); XI = A.alloc([2, 2, 128], BF16)
        ZZ = A.alloc([2, 4], F32); GC = A.alloc([2, 2], F32); GPW = A.alloc([2, 2, 18], F32); CFC = A.alloc([2, 2, 5], F32)
        DMA("sp", RC, I["retc"], [], [T + "rc"], "m0")
        DMA("sp", IDXB, I["idxb"], [], [T + "rc"], "m0")
        DMA("sp", LG, pbc(I[f"gam{l}"]), [], [T + "lg"], "m0")
        ACTV(LG, LG, AF.Exp, [T + "lg"], [T + "lg"], scale=-1.0)
        TS("dve", LG, LG, 1.0, ALU.add, [T + "lg"], [T + "lg"])
        ACTV(LG, LG, AF.Ln, [T + "lg"], [T + "lg"])
        TS("dve", LG, LG, -1.0, ALU.mult, [T + "lg"], [T + "lg"])
        for d_ in range(2):
            for c in range(2):
                k0_ = 4 * d_ + 2 * c
                TS("dve", LGS[:, d_, c:c + 1], LG[:, k0_:k0_ + 1], HM[:, 0:1], ALU.mult, [T + "lg", "HM"], [T + "lgs"])
                STT("dve", LGS[:, d_, c:c + 1], LG[:, k0_ + 1:k0_ + 2], HM[:, 1:2], LGS[:, d_, c:c + 1], ALU.mult, ALU.add, [T + "lg", "HM", T + "lgs"], [T + "lgs"])
        A.push()
        tf = A.alloc([128], F32); tb_ = A.alloc([128], F32)
        for h in range(4):
            ACTV(tf, RC[:, 0:128], AF.Exp, [T + "rc", T + "lg"], [T + "tf"], scale=LG[:, h:h + 1])
            TT("dve", tf, tf, RC[:, 128:256], ALU.mult, [T + "tf", T + "rc"], [T + "tf"])
            ACTV(tb_, RC[:, 256:384], AF.Exp, [T + "rc", T + "lg"], [T + "tb"], scale=LG[:, 4 + h:5 + h])
            TT("dve", tb_, tb_, RC[:, 384:512], ALU.mult, [T + "tb", T + "rc"], [T + "tb"])
            TT("dve", DEC[:, h, :], tf, tb_, ALU.add, [T + "tf", T + "tb"], [T + "dec"])
        A.pop()
        for c in range(2):
            ACTV(XI[:, 0, c, :], RC[:, 512:640], AF.Exp, [T + "rc", T + "lgs"], [T + "xi"], scale=LGS[:, 0, c:c + 1])
            ACTV(XI[:, 1, c, :], IDXB, AF.Exp, [T + "rc", T + "lgs"], [T + "xi"], scale=LGS[:, 1, c:c + 1])
            for d_ in range(2):
                ACTV(GC[:, d_, c:c + 1], LGS[:, d_, c:c + 1], AF.Exp, [T + "lgs"], [T + "gc"], scale=128.0)
                ACTV(GPW[:, d_, c, :], RC[:, 642 + 18 * d_:660 + 18 * d_], AF.Exp, [T + "rc", T + "lgs"], [T + "gpw"], scale=LGS[:, d_, c:c + 1])
                ACTV(CFC[:, d_, c, :], RC[:, 678 + 5 * d_:683 + 5 * d_], AF.Exp, [T + "rc", T + "lgs"], [T + "cfc"], scale=LGS[:, d_, c:c + 1])
                TT("dve", CFC[:, d_, c, :], CFC[:, d_, c, :], RC[:, 688 + 5 * d_:693 + 5 * d_], ALU.mult, [T + "cfc", T + "rc"], [T + "cfc"])
        ACTV(ZZ[:, 0, :], LG[:, 0:4], AF.Exp, [T + "lg", T + "rc"], [T + "zz"], scale=RC[:, 640:641])
        ACTV(ZZ[:, 1, :], LG[:, 4:8], AF.Exp, [T + "lg", T + "rc"], [T + "zz"], scale=RC[:, 641:642])
        TS("dve", ZZ, ZZ, 0.125, ALU.mult, [T + "zz"], [T + "zz"])
        if check_stop(f"rtparam_{l}"):
            A.pop(); A.pop(); A.pop(); break
        A.push()
        wrt = A.alloc([8, 1536], BF16)
        DMA("pool", wrt, winv[:, :, RT0:RT0 + 1536], [], [T + "w"], "wA")
        dests = [QT[:, 0, 0:TOK], QT[:, 1, 0:TOK], KT[:, 0, 0:TOK], KT[:, 1, 0:TOK]]
        proj_rope([wrt[:, :, c * 128:(c + 1) * 128] for c in range(4)], [wrt[:, :, 512 + c * 128:512 + (c + 1) * 128] for c in range(4)],
                  rope64[:, 0, :], rope64[:, 1, :], dests, T)
        for ci in range(4):
            dest = QT[:, ci, TOK:TOKC] if ci < 2 else KT[:, ci - 2, TOK:TOKC]
            proj_feat_plain(wrt[:, :, ci * 128:(ci + 1) * 128], dest, TOK, 256, T, f"c{ci}", ci % 2)

        def vg_post(i, pb):
            CP("act", VR[:, i, :], bank(pb, 256), [f"ps{pb}"], [T + f"v{i}"])
            ACTV(GT[:, i, :], bank(pb, 256, 256), AF.Silu, [f"ps{pb}"], [T + f"g{i}"])
        proj_tok(wrt[:, :, 1024:1536], 512, None, range(NTC), T, post=vg_post)
        A.pop()
        P.barrier()
        QK_R = [T + f"d{ci}" for ci in range(4)] + [T + f"dc{ci}" for ci in range(4)]
        if check_stop(f"rtproj_{l}"):
            A.pop(); A.pop(); A.pop(); break
        KTOK = A.alloc([NTC, 256], BF16)
        SZ = A.alloc([2, 18, 2, 64], F32)
        UCX = A.alloc([4, 2, 64], F32)
        SCX = A.alloc([2, 2, 64], F32)
        S0 = A.alloc([2, 2, 64], F32)
        SB = A.alloc([2, NTC, 2, 64], BF16)
        GR = A.alloc([4, 256], F32); EXPB = A.alloc([2, 2, 64], F32)
        for i in range(NTC):
            pb = i % 2
            for c in range(2):
                TR(bank_bf(pb)[:, c * 128:(c + 1) * 128], KT[:, c, i * 128:(i + 1) * 128], ident_bf, QK_R + ["ident_bf"], [f"ps{pb}"])
            CP("act", KTOK[:, i, :], bank_bf(pb)[:, 0:256], [f"ps{pb}"], [T + f"kt{i}"])
        vz = [A.alloc([2, 256], BF16) for _ in range(2)]
        MSET("pool", SZ[:, 0, 0], 0.0, [T + "sz"])
        MSET("pool", SZ[:, 1, 16], 0.0, [T + "sz"])

        def chunk_U(n, s2):
            for d_ in range(2):
                TT("dve" if d_ == 0 else "pool", vz[s2][:, d_].rearrange("p (h e) -> p h e", e=64), VR[:, n, :].rearrange("p (h e) -> p h e", e=64),
                   ZZ[:, d_, :].unsqueeze(2).to_broadcast([128, 4, 64]), ALU.mult, [T + f"v{n}", T + "zz"], [T + f"vz{s2}"])
            for d_ in range(2):
                for c in range(2):
                    MM(bank(2 + s2, 128, (d_ * 2 + c) * 128), KTOK[:, n, c * 128:(c + 1) * 128], vz[s2][:, d_, c * 128:(c + 1) * 128], True, True,
                       [T + f"kt{n}", T + f"vz{s2}"], [f"ps{2 + s2}"])

        udg = A.alloc([64], F32); udt = A.alloc([64], F32)

        def udiag(s2, d_, c, dst=None, dreg=None):
            blk = bank(2 + s2, 128, (d_ * 2 + c) * 128)
            o_ = udg if dst is None else dst
            TS("dve", udt, blk[:, 0:64], HM[:, 0:1], ALU.mult, [f"ps{2 + s2}", "HM"], [T + "udt"])
            STT("dve", o_, blk[:, 64:128], HM[:, 1:2], udt, ALU.mult, ALU.add, [f"ps{2 + s2}", "HM", T + "udt"], [T + "udg" if dreg is None else dreg])
            return o_

        for n in range(NT):
            chunk_U(n, n % 2)
            for c in range(2):
                u_ = udiag(n % 2, 0, c)
                STT("dve", SZ[:, 0, n + 1, c, :], SZ[:, 0, n, c, :], GC[:, 0, c:c + 1], u_, ALU.mult, ALU.add, [T + "sz", T + "gc", T + "udg"], [T + "sz"])
                udiag(n % 2, 1, c, dst=SZ[:, 1, n, c, :], dreg=T + "szb")
        for n in range(NT - 1, -1, -1):
            for c in range(2):
                STT("dve", SZ[:, 1, n, c, :], SZ[:, 1, n + 1, c, :], GC[:, 1, c:c + 1], SZ[:, 1, n, c, :], ALU.mult, ALU.add, [T + "sz", T + "szb", T + "gc"], [T + "sz", T + "szb"])
        for k_, n in enumerate((16, 17)):
            chunk_U(n, k_)
            for d_ in range(2):
                for c in range(2):
                    u_ = udiag(k_, d_, c)
                    CP("dve", UCX[:, 2 * d_ + k_, c, :], u_, [T + "udg"], [T + "ucx"])
        for c in range(2):
            STT("dve", SCX[:, 0, c, :], UCX[:, 0, c, :], GC[:, 0, c:c + 1], UCX[:, 1, c, :], ALU.mult, ALU.add, [T + "ucx", T + "gc"], [T + "scx"])
            STT("dve", SCX[:, 1, c, :], UCX[:, 3, c, :], GC[:, 1, c:c + 1], UCX[:, 2, c, :], ALU.mult, ALU.add, [T + "ucx", T + "gc"], [T + "scx"])
        CP("dve", EXPB[:, 0], SZ[:, 0, 16], [T + "sz"], [T + "expb"])
        CP("dve", EXPB[:, 1], SZ[:, 1, 0], [T + "sz"], [T + "expb"])
        e_r = dint(T + "e", [128, 256], F32); g_r = dint(T + "g", [512, 256], F32)
        DMA("sp", e_r.ap(), EXPB.rearrange("p d c e -> p (d c e)"), [T + "expb"], [T + "e"], "ex")
        AG(e_r, g_r, [T + "e"], [T + "g"], "cc")
        DMA("sp", GR, g_r.ap().rearrange("(r p) f -> p r f", p=128), [T + "g"], [T + "gr"], "kt")
        GRv = GR.rearrange("p r (d c e) -> p r d c e", d=2, c=2)
        for d_ in range(2):
            for c in range(2):
                TS("dve", S0[:, d_, c, :], SCX[:, d_, c, :], CFC[:, d_, c, 4:5], ALU.mult, [T + "scx", T + "cfc"], [T + "s0"])
                for r in range(4):
                    STT("dve", S0[:, d_, c, :], GRv[:, r, d_, c, :], CFC[:, d_, c, r:r + 1], S0[:, d_, c, :], ALU.mult, ALU.add, [T + "gr", T + "cfc", T + "s0"], [T + "s0"])
        for n in range(NT):
            for c in range(2):
                STT("dve", SB[:, 0, n, c, :], S0[:, 0, c, :], GPW[:, 0, c, n:n + 1], SZ[:, 0, n, c, :], ALU.mult, ALU.add, [T + "s0", T + "gpw", T + "sz"], [T + "sb"])
                STT("dve", SB[:, 1, n, c, :], S0[:, 1, c, :], GPW[:, 1, c, n:n + 1], SZ[:, 1, n + 1, c, :], ALU.mult, ALU.add, [T + "s0", T + "gpw", T + "sz"], [T + "sb"])
        MSET("pool", SB[:, 0, 16], 0.0, [T + "sb"])
        MSET("pool", SB[:, 1, 17], 0.0, [T + "sb"])
        CP("dve", SB[:, 0, 17], UCX[:, 0], [T + "ucx"], [T + "sb"])
        CP("dve", SB[:, 1, 16], UCX[:, 3], [T + "ucx"], [T + "sb"])
        if check_stop(f"rtA_{l}"):
            A.pop(); A.pop(); A.pop(); break
        AD = [A.alloc([4, 128], BF16) for _ in range(2)]
        QX = [A.alloc([2, 2, 2, 128], BF16) for _ in range(2)]
        qz = [A.alloc([2, 2, 128], BF16) for _ in range(2)]
        of_ = [A.alloc([4, 64], F32) for _ in range(2)]
        sq = A.alloc([4, 64], F32); ssr = A.alloc([4], F32); rsr = A.alloc([4], F32)
        yr_ = [A.alloc([4, 64], BF16) for _ in range(2)]
        def rt_A(i):
            s2 = i % 2
            ts = slice(i * 128, (i + 1) * 128)
            for hb_ in range(2):
                TS("pool" if hb_ else "dve", qz[s2][:, hb_], QT[:, :, ts], HM[:, hb_:hb_ + 1], ALU.mult, QK_R + ["HM"], [T + f"qz{s2}"])
            for h in range(4):
                c, hb_ = h // 2, h % 2
                MM(bank(s2, 128, h * 128), KT[:, c, ts], qz[s2][:, hb_, c, :], True, True, QK_R + [T + f"qz{s2}"], [f"ps{s2}"], sgc=True)

        def rt_mid(i):
            s2 = i % 2
            TT("dve", AD[s2], bank(s2).rearrange("p (h i) -> p h i", i=128), DEC, ALU.mult, [f"ps{s2}", T + "dec"], [T + f"ad{s2}"])
            for d_ in range(2):
                for hb_ in range(2):
                    TT("dve", QX[s2][:, d_, hb_], qz[s2][:, hb_], XI[:, d_], ALU.mult, [T + f"qz{s2}", T + "xi"], [T + f"qx{s2}"])

        def rt_out(i):
            s2 = i % 2
            pO = 4 + s2
            for h in range(4):
                c, hb_ = h // 2, h % 2
                o_ = bank(pO, 64, h * 64)
                MM(o_, AD[s2][:, h, :], VR[:, i, h * 64:(h + 1) * 64], h == 0, False, [T + f"ad{s2}", T + f"v{i}"], [f"ps{pO}"], sgc=True)
                MM(o_, QX[s2][:, 0, hb_, c, :], SB[:, 0, i, c, :], False, False, [T + f"qx{s2}", T + "sb"], [f"ps{pO}"], sgc=True)
                MM(o_, QX[s2][:, 1, hb_, c, :], SB[:, 1, i, c, :], False, True, [T + f"qx{s2}", T + "sb"], [f"ps{pO}"], sgc=True)

        def rt_fin(i):
            s2 = i % 2
            pO = 4 + s2
            ov = of_[s2]
            CP("act", ov, bank(pO, 256).rearrange("p (h e) -> p h e", e=64), [f"ps{pO}"], [T + f"of{s2}"])
            TT("dve", sq, ov, ov, ALU.mult, [T + f"of{s2}"], [T + "sq"])
            RED("dve", ssr, sq, [T + "sq"], [T + "ssr"])
            rstd_from_ss(ssr, 64, rsr, [T + "ssr", "epsc"], [T + "rsr"])
            TT("dve", sq, ov, rsr.unsqueeze(2).to_broadcast([128, 4, 64]), ALU.mult, [T + f"of{s2}", T + "rsr"], [T + "sq"])
            TT("pool", yr_[s2], sq, GT[:, i, :].rearrange("p (h e) -> p h e", e=64), ALU.mult, [T + "sq", T + f"g{i}"], [T + f"y{s2}"])

        def rt_tr(i):
            out_transposes(yr_[i % 2].rearrange("p h d -> p (h d)"), 6, i, T, [T + f"y{i % 2}"])

        rt_A(0)
        rt_mid(0)
        if nto > 1:
            rt_A(1)
        for i in range(nto):
            rt_out(i)
            if i + 1 < nto:
                rt_mid(i + 1)
            if i + 2 < nto:
                rt_A(i + 2)
            rt_fin(i)
            if i >= 1:
                rt_tr(i - 1)
        rt_tr(nto - 1)
        A.pop()
        A.pop()
        if "dbg_ot" in dbg and l == 0:
            DMA("sp", dbg["dbg_ot"], otd, [L + f"OT{c0}_{i}" for c0 in (0, 1, 2, 4, 6) for i in range(nto)], ["dbgot"], "dbg")
        if check_stop(f"rt_{l}") or (STOP_AFTER or "").startswith("rtB"):
            A.pop(); break

        P.barrier()
        A.push()
        h2T = hT
        wo = A.alloc([8, 1024], BF16)
        DMA("pool", wo, I[f"wout{l}"].rearrange("(kc p) n -> p kc n", p=128), [], [L + "wo"], "wA")
        if moe:
            RB = A.alloc([8, 1024], F32)
            DMA("sp", RB, pbc(I["router"]).rearrange("p (e d) -> p e d", d=1024), [], [L + "rb"], "m0")
            rj = [A.alloc([1024], F32) for _ in range(3)]; sm = A.alloc([8, 8], F32)
        xt = [A.alloc([1024], F32) for _ in range(2)]
        t1 = [A.alloc([1024], F32) for _ in range(2)]
        xm = [A.alloc([1024], F32) for _ in range(2)]
        hb = [A.alloc([1024], BF16) for _ in range(2)]
        ott = [A.alloc([8, 128], BF16) for _ in range(2)]
        ss3 = A.alloc([NTC, 2], F32); rs3 = A.alloc([NTC, 2], F32)
        xdst = (xs, xcs)

        def p3_mm(i):
            s2 = i % 2
            pb = 2 * s2
            ot_r = [L + f"OT{c0}_{i}" for c0 in (0, 1, 2, 4, 6)]
            DMA("sp", ott[s2], otd[i], ot_r, [L + f"ott{s2}"], f"ott{s2}")
            for hf in range(2):
                for kc in range(8):
                    MM(bank(pb + hf), ott[s2][:, kc, :], wo[:, kc, hf * 512:(hf + 1) * 512], kc == 0, kc == 7, [L + f"ott{s2}", L + "wo"], [f"ps{pb + hf}"])

        def p3_chain(i):
            s2 = i % 2
            v = 0 if i < NT else 1
            pb = 2 * s2
            yps = psum[:, 512 * pb:512 * pb + 1024]
            ACTV(junk, yps, AF.Square, [f"ps{pb}", f"ps{pb + 1}"], ["junk", L + f"s3_{i}"], accum=ss3[:, i, 0:1])
            rstd_from_ss(ss3[:, i, 0:1], 1024, rs3[:, i, 0:1], [L + f"s3_{i}", "epsc"], [L + f"r3_{i}"])
            DMA("sp", xt[s2], xtile_ap(xsrc, i), [], [L + f"p3xt{s2}"], f"xt{s2}")
            STT("dve", t1[s2], yps, rs3[:, i, 0:1], MOD[:, v, 2, :], ALU.mult, ALU.mult, [f"ps{pb}", f"ps{pb + 1}", L + f"r3_{i}", f"MOD{v}2"], [L + f"p3t1{s2}"])
            TT("dve", xm[s2], t1[s2], xt[s2], ALU.add, [L + f"p3t1{s2}", L + f"p3xt{s2}"], [L + f"xm{s2}"])
            DMA("sp", xtile_ap(xdst, i), xm[s2], [L + f"xm{s2}"], [L + f"xs{i}"], f"xst{s2}")
            ACTV(junk, xm[s2], AF.Square, [L + f"xm{s2}"], ["junk", L + f"s4_{i}"], accum=ss3[:, i, 1:2])
            rstd_from_ss(ss3[:, i, 1:2], 1024, rs3[:, i, 1:2], [L + f"s4_{i}", "epsc"], [L + f"r4_{i}"])
            STT("dve", t1[s2], xm[s2], rs3[:, i, 1:2], MOD[:, v, 4, :], ALU.mult, ALU.mult, [L + f"xm{s2}", L + f"r4_{i}", f"MOD{v}4"], [L + f"p3t1{s2}"])
            if moe:
                TT("dve", xt[s2], t1[s2], MOD[:, v, 3, :], ALU.add, [L + f"p3t1{s2}", f"MOD{v}3"], [L + f"p3xt{s2}"])
                CP("act", hb[s2], xt[s2], [L + f"p3xt{s2}"], [L + f"p3hb{s2}"])
                for e_ in range(8):
                    TT("dve", rj[e_ % 3], xt[s2], RB[:, e_, :], ALU.mult, [L + f"p3xt{s2}", L + "rb"], [L + f"rj{e_ % 3}"])
                    ACTV(junk, rj[e_ % 3], AF.Identity, [L + f"rj{e_ % 3}"], ["junk", L + f"logi{i}"], accum=LOGI[:, i, e_:e_ + 1])
                lg_ = LOGI[:, i, :]
                RED("dve", sm[:, 0, 0:1], lg_, [L + f"logi{i}"], [L + "sm"], mx=True)
                TS("dve", sm[:, 1, :], lg_, sm[:, 0, 0:1], ALU.is_equal, [L + f"logi{i}", L + "sm"], [L + "sm"])
                STT("dve", sm[:, 2, :], sm[:, 1, :], -1e30, lg_, ALU.mult, ALU.add, [L + "sm", L + f"logi{i}"], [L + "sm"])
                RED("dve", sm[:, 0, 1:2], sm[:, 2, :], [L + "sm"], [L + "sm"], mx=True)
                TS("dve", sm[:, 3, :], lg_, sm[:, 0, 1:2], ALU.is_ge, [L + f"logi{i}", L + "sm"], [L + "sm"])
                TS("dve", sm[:, 0, 2:3], sm[:, 0, 0:1], -1.0, ALU.mult, [L + "sm"], [L + "sm"])
                ACTV(sm[:, 4, :], lg_, AF.Exp, [L + f"logi{i}", L + "sm"], [L + "sm"], bias=sm[:, 0, 2:3])
                TT("dve", sm[:, 4, :], sm[:, 4, :], sm[:, 3, :], ALU.mult, [L + "sm"], [L + "sm"])
                RED("dve", sm[:, 0, 3:4], sm[:, 4, :], [L + "sm"], [L + "sm"])
                RECIP(sm[:, 0, 3:4], sm[:, 0, 3:4], [L + "sm"], [L + "sm"])
                TS("dve", GATES[:, i, :], sm[:, 4, :], sm[:, 0, 3:4], ALU.mult, [L + "sm"], [L + f"gates{i}"])
            else:
                TT("dve", hb[s2], t1[s2], MOD[:, v, 3, :], ALU.add, [L + f"p3t1{s2}", f"MOD{v}3"], [L + f"p3hb{s2}"])

        def p3_tr(i):
            s2 = i % 2
            ts = slice(i * 128, (i + 1) * 128)
            pt_ = 4 + s2
            for kc in range(8):
                TR(bank_bf(pt_)[:, kc * 128:(kc + 1) * 128], hb[s2][:, kc * 128:(kc + 1) * 128], ident_bf, [L + f"p3hb{s2}", "ident_bf"], [f"ps{pt_}"])
            CP("act", h2T[:, :, ts], bank_bf(pt_).rearrange("p (k t) -> p k t", t=128), [f"ps{pt_}"], [L + f"h2T{i}"])

        p3_mm(0)
        for i in range(nto):
            p3_chain(i)
            if i + 1 < nto:
                p3_mm(i + 1)
            p3_tr(i)
        H2_ALL = [L + f"h2T{i}" for i in range(nto)]
        A.pop()
        if check_stop(f"p3_{l}"):
            A.pop(); break

        P.barrier()
        Y = A.alloc([nto, 1024], F32)
        A.push()
        wg = [A.alloc([8, 256], BF16) for _ in range(2)]
        wu = [A.alloc([8, 256], BF16) for _ in range(2)]
        wd = [A.alloc([2, 1024], BF16) for _ in range(2)]
        sg = [A.alloc([512], BF16) for _ in range(2)]
        AT = [A.alloc([2, 512], BF16) for _ in range(2)]
        ntok = nto * 128
        tblocks = [(t0, min(512, ntok - t0)) for t0 in range(0, ntok, 512)]
        if moe:
            slabs = [(e_, s_) for e_ in range(8) for s_ in range(14)]
        else:
            slabs = [(None, s_) for s_ in range(11)]
        nmm = 0
        for si, (e_, s_) in enumerate(slabs):
            sl = si % 2
            if moe:
                gsrc = I["mwg"][e_].rearrange("(kc p) f -> p kc f", p=128)[:, :, s_ * 256:(s_ + 1) * 256]
                usrc = I["mwu"][e_].rearrange("(kc p) f -> p kc f", p=128)[:, :, s_ * 256:(s_ + 1) * 256]
                dsrc = I["mwd"][e_][s_ * 256:(s_ + 1) * 256, :].rearrange("(c p) n -> p c n", p=128)
            else:
                gsrc = I["fwg"].rearrange("(kc p) f -> p kc f", p=128)[:, :, s_ * 256:(s_ + 1) * 256]
                usrc = I["fwu"].rearrange("(kc p) f -> p kc f", p=128)[:, :, s_ * 256:(s_ + 1) * 256]
                dsrc = I["fwd"][s_ * 256:(s_ + 1) * 256, :].rearrange("(c p) n -> p c n", p=128)
            DMA("pool", wg[sl], gsrc, [], [L + f"wg{sl}"], f"fw{sl}")
            DMA("pool", wu[sl], usrc, [], [L + f"wu{sl}"], f"fw{sl}")
            DMA("pool", wd[sl], dsrc, [], [L + f"wd{sl}"], f"fw{sl}")
            for bi, (t0, nt_) in enumerate(tblocks):
                a2 = bi % 2
                for fcl in range(2):
                    pg = nmm % 2; nmm += 1
                    for kc in range(8):
                        MM(bank(pg, nt_), wg[sl][:, kc, fcl * 128:(fcl + 1) * 128], h2T[:, kc, t0:t0 + nt_], kc == 0, kc == 7, H2_ALL + [L + f"wg{sl}"], [f"ps{pg}"])
                    for kc in range(8):
                        MM(bank(2 + pg, nt_), wu[sl][:, kc, fcl * 128:(fcl + 1) * 128], h2T[:, kc, t0:t0 + nt_], kc == 0, kc == 7, H2_ALL + [L + f"wu{sl}"], [f"ps{2 + pg}"])
                    ACTV(sg[pg][:, 0:nt_], bank(pg, nt_), AF.Silu, [f"ps{pg}"], [L + f"sg{pg}"])
                    TT("dve", AT[a2][:, fcl, 0:nt_], sg[pg][:, 0:nt_], bank(2 + pg, nt_), ALU.mult, [L + f"sg{pg}", f"ps{2 + pg}"], [L + f"at{a2}_{fcl}"])
                for tt in range(nt_ // 128):
                    ti = t0 // 128 + tt
                    py = 4 + 2 * (ti % 2)
                    for hf in range(2):
                        for fcl in range(2):
                            MM(bank(py + hf), AT[a2][:, fcl, tt * 128:(tt + 1) * 128], wd[sl][:, fcl, hf * 512:(hf + 1) * 512], fcl == 0, fcl == 1,
                               [L + f"at{a2}_0", L + f"at{a2}_1", L + f"wd{sl}"], [f"ps{py + hf}"])
                    yps = psum[:, 512 * py:512 * py + 1024]
                    rr = [f"ps{py}", f"ps{py + 1}"]
                    if moe:
                        gsc = GATES[:, ti, e_:e_ + 1]
                        if si == 0:
                            TS("dve", Y[:, ti, :], yps, gsc, ALU.mult, rr + [L + f"gates{ti}"], [L + f"Y{ti}"])
                        else:
                            STT("dve", Y[:, ti, :], yps, gsc, Y[:, ti, :], ALU.mult, ALU.add, rr + [L + f"gates{ti}", L + f"Y{ti}"], [L + f"Y{ti}"])
                    else:
                        if si == 0:
                            CP("dve", Y[:, ti, :], yps, rr, [L + f"Y{ti}"])
                        else:
                            TT("dve", Y[:, ti, :], yps, Y[:, ti, :], ALU.add, rr + [L + f"Y{ti}"], [L + f"Y{ti}"])
        A.pop()
        if check_stop(f"p4_{l}"):
            A.pop(); break

        A.push()
        ss5 = A.alloc([NTC], F32); rs5 = A.alloc([NTC], F32)
        xt = [A.alloc([1024], F32) for _ in range(2)]
        t1 = [A.alloc([1024], F32) for _ in range(2)]
        xo = [A.alloc([1024], F32) for _ in range(2)]
        for i in range(nto):
            s2 = i % 2
            v = 0 if i < NT else 1
            ACTV(junk, Y[:, i, :], AF.Square, [L + f"Y{i}"], ["junk", L + f"s5_{i}"], accum=ss5[:, i:i + 1])
            rstd_from_ss(ss5[:, i:i + 1], 1024, rs5[:, i:i + 1], [L + f"s5_{i}", "epsc"], [L + f"r5_{i}"])
            DMA("sp", xt[s2], xtile_ap(xdst, i), [L + f"xs{i}"], [L + f"p5xt{s2}"], f"xt{s2}")
            STT("dve", t1[s2], Y[:, i, :], rs5[:, i:i + 1], MOD[:, v, 5, :], ALU.mult, ALU.mult, [L + f"Y{i}", L + f"r5_{i}", f"MOD{v}5"], [L + f"p5t1{s2}"])
            TT("dve", xo[s2], t1[s2], xt[s2], ALU.add, [L + f"p5t1{s2}", L + f"p5xt{s2}"], [L + f"xo{s2}"])
            if l == 0:
                DMA("sp", xtile_ap(xdst, i), xo[s2], [L + f"xo{s2}"], [L + f"xs{i}"], f"xst{s2}")
                if "dbg_x" in dbg:
                    dd = dbg["dbg_x"][i * 128:(i + 1) * 128, :] if i < NT else dbg["dbg_xc"][(i - NT) * 128:(i - NT + 1) * 128, :]
                    DMA("sp", dd, xo[s2], [L + f"xo{s2}"], [L + f"dbgx{i}"], "dbg")
            else:
                DMA("sp", out[i * 128:(i + 1) * 128, :], xo[s2], [L + f"xo{s2}"], [f"out{i}"], f"xst{s2}")
        A.pop()
        A.pop()
        if check_stop(f"l{l}"):
            break
    return nc, P, es, A, I


def _emit(nc, P, es):
    tls = P.finalize()
    sems = {tl: es.enter_context(nc.semaphore("s_" + str(tl))) for tl in tls}
    with nc.Block() as block:
        block.sync(P.engine_body("sp", sems, final=True))
        block.tensor(P.engine_body("pe", sems))
        block.vector(P.engine_body("dve", sems))
        block.scalar(P.engine_body("act", sems))
        block.gpsimd(P.engine_body("pool", sems))


_CACHE = {}


def _get_program():
    if "nc" not in _CACHE:
        nc, P, es, A, I = build_program()
        _CACHE["inputs"] = list(I.keys())
        with es:
            _emit(nc, P, es)
        _CACHE["nc"] = nc
        _CACHE["peak"] = A.peak
        _CACHE["nops"] = len(P.ops)
    return _CACHE["nc"]


def _host_inputs(inp):
    f = lambda a: np.ascontiguousarray(np.asarray(a, dtype=np.float32))
    shared = {}
    for l in range(2):
        shared[f"wmod{l}"] = f(inp["w_mod"][l])
        shared[f"bmod{l}"] = f(inp["b_mod"][l]).reshape(1, 6144)
        shared[f"gvec{l}"] = f(np.concatenate([inp["g_attn_pre"][l], inp["g_attn_post"][l], inp["g_ffn_pre"][l], inp["g_ffn_post"][l]])).reshape(1, 4096)
        shared[f"win{l}"] = f(np.asarray(inp["w_in"][l])[:, WIN_PERM])
        shared[f"wout{l}"] = f(inp["w_out"][l])
        shared[f"dal{l}"] = f(np.concatenate([inp["da_lambda_q1"][l], inp["da_lambda_k1"][l], inp["da_lambda_q2"][l], inp["da_lambda_k2"][l]])).reshape(1, 128)
        shared[f"subln{l}"] = f(inp["da_subln"][l]).reshape(1, 64)
        shared[f"sink{l}"] = f(inp["swa_sink"][l]).reshape(1, 4)
        shared[f"gam{l}"] = f(np.concatenate([inp["ret_gamma_fwd"][l], inp["ret_gamma_bwd"][l]])).reshape(1, 8)
        shared[f"nabias{l}"] = _na_bias_layout(np.asarray(inp["na_rpb"][l], dtype=np.float32))
    shared["fwg"] = f(inp["ffn_w_gate"][0]); shared["fwu"] = f(inp["ffn_w_up"][0]); shared["fwd"] = f(inp["ffn_w_down"][0])
    shared["router"] = f(np.asarray(inp["moe_router"][0]).T).reshape(1, 8 * 1024)
    shared["mwg"] = f(inp["moe_w_gate"][0]); shared["mwu"] = f(inp["moe_w_up"][0]); shared["mwd"] = f(inp["moe_w_down"][0])
    shared["idxb"] = _idxb_table()
    x = np.asarray(inp["x"], dtype=np.float32); ctx = np.asarray(inp["ctx"], dtype=np.float32)
    c = np.asarray(inp["c"], dtype=np.float32); c_ctx = np.asarray(inp["c_ctx"], dtype=np.float32)
    maps = []
    for core in range(8):
        b, j = core // 4, core % 4
        m = dict(shared)
        m["xin"] = np.ascontiguousarray(x[b, TOK * j:TOK * (j + 1)])
        m["xcin"] = np.ascontiguousarray(ctx[b])
        m["cvec"] = np.ascontiguousarray(np.concatenate([c[b].reshape(8, 128).T, c_ctx.reshape(8, 128).T], axis=1))
        C64, S64 = _rope_tables(j, 64)
        C32, S32 = _rope_tables(j, 32)
        m["rope64"] = np.ascontiguousarray(np.stack([C64, S64], axis=1))
        m["rope32"] = np.ascontiguousarray(np.stack([C32, S32], axis=1))
        m["swamask"] = _swa_masks(j)
        m["namask"] = _na_masks(j)
        m["retc"] = _ret_consts(j)
        if "inputs" in _CACHE:
            m = {k: v for k, v in m.items() if k in _CACHE["inputs"]}
        maps.append(m)
    return maps


def kernel(**inputs):
    nc = _get_program()
    maps = _host_inputs(inputs)
    res = run_bass_kernel_spmd(nc, maps, core_ids=list(range(8)))
    _CACHE["last"] = res
    outp = np.empty((2, 8192, 1024), np.float32)
    for core in range(8):
        b, j = core // 4, core % 4
        outp[b, TOK * j:TOK * (j + 1)] = res.results[core]["out"]
    return outp
```

```python
import contextlib
import os
import math
import numpy as np
import concourse.bass as bass
import concourse.mybir as mybir
from concourse.bass_utils import run_bass_kernel_spmd

F32 = mybir.dt.float32
BF16 = mybir.dt.bfloat16
AF = mybir.ActivationFunctionType
ALU = mybir.AluOpType
AX = mybir.AxisListType
ENGS = ("pe", "act", "dve", "pool", "sp")
EPS = 1e-6
NT = 16
NTC = 18
TOK = 2048
TOKC = 2304
DEBUG = []
STOP_AFTER = None


class Op:
    __slots__ = ("eng", "fn", "tl", "deps", "awaited", "count", "inc", "idx")


class Prog:
    def __init__(self):
        self.ops = []
        self.last_w = {}
        self.readers = {}
        self.tl_last = {}
        self.bar = set()
        self.bar_done = set(ENGS)

    def op(self, eng, fn, reads=(), writes=(), tl=None, inc=1):
        o = Op()
        o.eng = eng
        o.fn = fn
        o.tl = tl if tl is not None else eng
        o.inc = inc
        o.awaited = o.tl not in ENGS
        o.count = None
        o.idx = len(self.ops)
        deps = set()
        for r in reads:
            w = self.last_w.get(r)
            if w is not None:
                deps.add(w)
        for w_ in writes:
            w = self.last_w.get(w_)
            if w is not None:
                deps.add(w)
            rl = self.readers.get(w_)
            if rl:
                deps.update(rl)
        if eng not in self.bar_done:
            deps |= self.bar
            self.bar_done.add(eng)
        o.deps = deps
        self.ops.append(o)
        for r in reads:
            self.readers.setdefault(r, []).append(o.idx)
        for w_ in writes:
            self.last_w[w_] = o.idx
            self.readers[w_] = []
        self.tl_last[o.tl] = o.idx
        return o

    def barrier(self):
        self.bar = set(self.tl_last.values())
        self.bar_done = set()

    def finalize(self):
        ops = self.ops
        for i in self.tl_last.values():
            ops[i].awaited = True
        for o in ops:
            for d in o.deps:
                od = ops[d]
                if od.tl == "pe" and o.tl == "pe":
                    continue
                od.awaited = True
        cnt = {}
        for o in ops:
            if o.awaited:
                cnt[o.tl] = cnt.get(o.tl, 0) + o.inc
                o.count = cnt[o.tl]
        self.totals = cnt
        run_latest = {}
        self.need = [None] * len(ops)
        for o in ops:
            need = {}
            for d in o.deps:
                od = ops[d]
                if od.tl == "pe" and o.tl == "pe":
                    continue
                v = od.count if od.tl in ENGS else run_latest[od.tl]
                if need.get(od.tl, 0) < v:
                    need[od.tl] = v
            self.need[o.idx] = need
            if o.awaited:
                run_latest[o.tl] = o.count
        return sorted(cnt.keys(), key=str)

    def engine_body(self, ename, sems, final=False):
        mine = [o for o in self.ops if o.eng == ename]

        def body(e):
            waited = {}
            for o in mine:
                for tl, v in self.need[o.idx].items():
                    if waited.get(tl, 0) < v:
                        e.wait_ge(sems[tl], v)
                        waited[tl] = v
                ins = o.fn(e)
                if o.awaited:
                    ins.then_inc(sems[o.tl], o.inc)
            if final:
                for tl, v in self.totals.items():
                    if waited.get(tl, 0) < v:
                        e.wait_ge(sems[tl], v)
        return body


class Arena:
    def __init__(self, ap, nbytes, prog=None):
        self.prog = prog
        self.ap = ap
        self.cap = nbytes
        self.off = 0
        self.stack = []
        self.peak = 0

    def alloc(self, shape, dt):
        shape = list(shape)
        n = int(np.prod(shape))
        nb = n * (4 if dt == F32 else 2)
        nb = (nb + 63) // 64 * 64
        assert self.off + nb <= self.cap, f"SBUF arena overflow {self.off}+{nb}>{self.cap}"
        v = self.ap[:, self.off // 2:(self.off + nb) // 2]
        if dt == F32:
            v = v.bitcast(F32)
        v = v[:, 0:n]
        self.off += nb
        self.peak = max(self.peak, self.off)
        if len(shape) == 2:
            v = v.rearrange("p (a b) -> p a b", b=shape[1])
        elif len(shape) == 3:
            v = v.rearrange("p (a b c) -> p a b c", b=shape[1], c=shape[2])
        elif len(shape) == 4:
            v = v.rearrange("p (a b c d) -> p a b c d", b=shape[1], c=shape[2], d=shape[3])
        return v

    def push(self):
        self.stack.append(self.off)

    def pop(self):
        self.off = self.stack.pop()
        if self.prog is not None:
            self.prog.barrier()


def _swap_idx(dh):
    q = dh // 4
    return np.concatenate([np.arange(q, 2 * q), np.arange(0, q), np.arange(3 * q, 4 * q), np.arange(2 * q, 3 * q)])


def _win_perm():
    cols = []
    base = 0
    q = np.arange(base, base + 256)
    k = np.arange(base + 256, base + 512)
    v = np.arange(base + 512, base + 768)
    sw32 = np.concatenate([_swap_idx(32) + 32 * i for i in range(8)])
    cols += [q, k, q[sw32], k[sw32], v]
    base = 768
    qn = np.arange(base, base + 256).reshape(2, 2, 64)
    qperm = np.transpose(qn, (1, 0, 2)).reshape(256)
    kk = np.arange(base + 256, base + 384)
    vv = np.arange(base + 384, base + 512)
    sw64_4 = np.concatenate([_swap_idx(64) + 64 * i for i in range(4)])
    sw64_2 = np.concatenate([_swap_idx(64) + 64 * i for i in range(2)])
    cols += [qperm, kk, qperm[sw64_4], kk[sw64_2], vv]
    base = 1280
    cols += [np.arange(base, base + 768)]
    base = 2048
    q = np.arange(base, base + 256)
    k = np.arange(base + 256, base + 512)
    vg = np.arange(base + 512, base + 1024)
    cols += [q, k, q[sw64_4], k[sw64_4], vg]
    return np.concatenate(cols)


WIN_PERM = _win_perm()
NWIN = len(WIN_PERM)
DA0, SW0, NA0, RT0 = 0, 1280, 2176, 2944


def _rope_tables(j, dh):
    t = 2048 * j + np.arange(2048)
    row = (t // 64).astype(np.float64)
    col = (t % 64).astype(np.float64)
    half = dh // 2
    qd = dh // 4
    inv = 10000.0 ** (-np.arange(qd, dtype=np.float64) * 2.0 / half)
    C = np.zeros((128, 2048), np.float32)
    S = np.zeros((128, 2048), np.float32)
    for p in range(128):
        d = p % dh
        pos = row if d < half else col
        dd = d % half
        i = dd % qd
        ang = pos * inv[i]
        C[p] = np.cos(ang)
        S[p] = -np.sin(ang) if dd < qd else np.sin(ang)
    return C, S


def _swa_masks(j):
    kk = np.arange(128)[:, None]
    qq = np.arange(128)[None, :]
    mprev = (qq <= kk).astype(np.float32)
    mnext = (kk <= qq).astype(np.float32)
    m = np.zeros((10, 128, 128), np.float32)
    m[0] = mprev
    m[1] = mnext
    for r in range(4):
        if r == j - 1:
            m[2 + r] = mprev
        if r == j + 1:
            m[6 + r] = mnext
    return m


def _na_mask(Tq, Tk, flag=True):
    if (not flag) or Tk < 0 or Tk > 63:
        return np.zeros((128, 128), np.float32)
    p = np.arange(128)
    Rk = (2 * Tk + p // 64)[:, None]
    kc = (p % 64)[:, None]
    Rq = (2 * Tq + p // 64)[None, :]
    qc = (p % 64)[None, :]
    start = np.clip(Rq - 4, 0, 120)
    cs = np.clip(qc - 8, 0, 48)
    ok = (Rk >= start) & (Rk < start + 8) & (kc >= cs) & (kc < cs + 16)
    return ok.astype(np.float32)


def _na_masks(j):
    m = np.zeros((45, 128, 128), np.float32)
    for d in range(-2, 3):
        m[d + 2] = _na_mask(10, 10 + d)
    T0 = 16 * j
    idx = 5
    for d in (0, 1, 2, 3):
        m[idx] = _na_mask(T0, T0 + d); idx += 1
    for r in range(4):
        m[idx] = _na_mask(T0, T0 - 2, r == j - 1); idx += 1
    for r in range(4):
        m[idx] = _na_mask(T0, T0 - 1, r == j - 1); idx += 1
    for d in (-1, 0, 1, 2):
        m[idx] = _na_mask(T0 + 1, T0 + 1 + d); idx += 1
    for r in range(4):
        m[idx] = _na_mask(T0 + 1, T0 - 1, r == j - 1); idx += 1
    for d in (-2, -1, 0, 1):
        m[idx] = _na_mask(T0 + 14, T0 + 14 + d); idx += 1
    for r in range(4):
        m[idx] = _na_mask(T0 + 14, T0 + 16, r == j + 1); idx += 1
    for d in (-3, -2, -1, 0):
        m[idx] = _na_mask(T0 + 15, T0 + 15 + d); idx += 1
    for r in range(4):
        m[idx] = _na_mask(T0 + 15, T0 + 16, r == j + 1); idx += 1
    for r in range(4):
        m[idx] = _na_mask(T0 + 15, T0 + 17, r == j + 1); idx += 1
    assert idx == 45
    return m


def _na_bias_layout(rpb):
    p = np.arange(128)
    kr = (p // 64)[:, None]; kc = (p % 64)[:, None]
    qr = (p // 64)[None, :]; qc = (p % 64)[None, :]
    out = np.empty((7, 4, 128, 128), np.float32)
    dc = np.clip(kc - qc, -15, 15) + 15
    for di, d in enumerate(range(-3, 4)):
        dr = np.clip(2 * d + kr - qr, -7, 7) + 7
        out[di] = rpb[:, dr, dc]
    return out


def _ret_consts(j):
    c = np.zeros((128, 700), np.float32)
    i = np.arange(128)
    o = 0
    dif = i[None, :] - i[:, None]
    c[:, 0:128] = np.maximum(dif, 0)
    c[:, 128:256] = (dif >= 0) * 0.125
    c[:, 256:384] = np.maximum(-dif, 0)
    c[:, 384:512] = (dif < 0) * 0.125
    c[:, 512:640] = (i + 1)[None, :]
    c[:, 640] = 127 - i
    c[:, 641] = i
    c[:, 642:660] = (128.0 * np.arange(18))[None, :]
    c[:, 660:678] = (128.0 * (15 - np.arange(18)))[None, :]
    for r in range(4):
        if r < j:
            c[:, 678 + r] = 2048.0 * (j - 1 - r); c[:, 688 + r] = 1.0
        if r > j:
            c[:, 683 + r] = 2048.0 * (r - j - 1); c[:, 693 + r] = 1.0
    c[:, 682] = 2048.0 * j; c[:, 692] = 1.0
    c[:, 687] = 2048.0 * (3 - j); c[:, 697] = 1.0
    return c


def _idxb_table():
    i = np.arange(128)
    return np.broadcast_to((128 - i)[None, :], (128, 128)).astype(np.float32).copy()


def build_program():
    nc = bass.Bass("TRN2", target_bir_lowering=False)
    P = Prog()
    es = contextlib.ExitStack()

    def din(name, shape, dt=F32):
        return nc.dram_tensor(name, list(shape), dt, kind="ExternalInput").ap()

    def dint(name, shape, dt):
        return nc.dram_tensor(name, list(shape), dt)

    SHAPES = {"xin": [TOK, 1024], "xcin": [256, 1024], "cvec": [128, 16], "fwg": [1024, 2816], "fwu": [1024, 2816], "fwd": [2816, 1024],
              "router": [1, 8 * 1024], "mwg": [8, 1024, 3584], "mwu": [8, 1024, 3584], "mwd": [8, 3584, 1024],
              "rope64": [128, 2, 2048], "rope32": [128, 2, 2048], "swamask": [10, 128, 128], "namask": [45, 128, 128],
              "retc": [128, 700], "idxb": [128, 128]}
    for l_ in range(2):
        SHAPES.update({f"wmod{l_}": [1024, 6144], f"bmod{l_}": [1, 6144], f"gvec{l_}": [1, 4096], f"win{l_}": [1024, NWIN],
                       f"wout{l_}": [1024, 1024], f"dal{l_}": [1, 128], f"subln{l_}": [1, 64], f"sink{l_}": [1, 4],
                       f"gam{l_}": [1, 8], f"nabias{l_}": [7, 4, 128, 128]})

    class LazyIn(dict):
        def __missing__(self, k):
            self[k] = din(k, SHAPES[k])
            return self[k]
    I = LazyIn()
    USED_INPUTS = I
    out = nc.dram_tensor("out", [TOK, 1024], F32, kind="ExternalOutput").ap()
    dbg = {}
    for name, shape, dt in (("dbg_ot", [NTC, 128, 8, 128], BF16), ("dbg_x", [TOK, 1024], F32), ("dbg_xc", [256, 1024], F32),
                            ("dbg_misc", [128, 4096], F32)):
        if name in DEBUG:
            dbg[name] = nc.dram_tensor(name, shape, dt, kind="ExternalOutput").ap()

    xs = dint("xs", [TOK, 1024], F32).ap(); xcs = dint("xcs", [256, 1024], F32).ap()
    GROUPS = [[0, 1, 2, 3], [4, 5, 6, 7]]

    arena_t = es.enter_context(nc.sbuf_tensor("arena", [128, 94 * 1024], BF16))
    A = Arena(arena_t, 188 * 1024, P)
    psum = es.enter_context(nc.psum_tensor("psum", [128, 4096], F32))

    def bank(i, n=512, off=0):
        return psum[:, 512 * i + off:512 * i + off + n]

    def bank_bf(i):
        return psum[:, 512 * i:512 * (i + 1)].bitcast(BF16)

    def MM(o, lhsT, rhs, st, sp_, r, w, tp=None, sgc=False):
        kw = {}
        if tp is not None:
            kw["tile_position"] = tp
        if sgc:
            kw["skip_group_check"] = True
        P.op("pe", lambda e: e.matmul(o, lhsT=lhsT, rhs=rhs, start=st, stop=sp_, **kw), r, w)

    def MM64(o, lhsT, rhs, base, st, sp_, r, w):
        if base == 0:
            MM(o, lhsT[0:64], rhs[0:64], st, sp_, r, w, sgc=True)
        else:
            MM(o, lhsT[64:96], rhs[64:96], st, False, r, w, tp=(64, 0), sgc=True)
            MM(o, lhsT[96:128], rhs[96:128], False, sp_, r, w, tp=(96, 0), sgc=True)

    def TR(o, i, ident, r, w):
        P.op("pe", lambda e: e.transpose(o, i, ident), r, w)

    def ACTV(o, i, func, r, w, bias=None, scale=None, accum=None):
        kw = {}
        if bias is not None:
            kw["bias"] = bias
        if scale is not None:
            kw["scale"] = scale
        if accum is not None:
            kw["accum_out"] = accum
        P.op("act", lambda e: e.activation(out=o, in_=i, func=func, **kw), r, w)

    def TT(eng, o, a, b, op, r, w):
        P.op(eng, lambda e: e.tensor_tensor(out=o, in0=a, in1=b, op=op), r, w)

    def TS(eng, o, a, s1, op0, r, w, s2=None, op1=None):
        if op1 is None:
            P.op(eng, lambda e: e.tensor_scalar(out=o, in0=a, scalar1=s1, scalar2=None, op0=op0), r, w)
        else:
            P.op(eng, lambda e: e.tensor_scalar(out=o, in0=a, scalar1=s1, scalar2=s2, op0=op0, op1=op1), r, w)

    def STT(eng, o, a, s, b, op0, op1, r, w):
        P.op(eng, lambda e: e.scalar_tensor_tensor(out=o, in0=a, scalar=s, in1=b, op0=op0, op1=op1), r, w)

    def CP(eng, o, i, r, w):
        if eng == "act":
            P.op("act", lambda e: e.copy(out=o, in_=i), r, w)
        else:
            P.op(eng, lambda e: e.tensor_copy(out=o, in_=i), r, w)

    def MSET(eng, o, val, w):
        P.op(eng, lambda e: e.memset(o, val), (), w)

    def RED(eng, o, i, r, w, mx=False):
        if mx:
            P.op(eng, lambda e: e.reduce_max(out=o, in_=i, axis=AX.X), r, w)
        else:
            P.op(eng, lambda e: e.reduce_sum(out=o, in_=i, axis=AX.X), r, w)

    def RECIP(o, i, r, w):
        P.op("dve", lambda e: e.reciprocal(out=o, in_=i), r, w)

    def DMA(q, o, i, r, w, tl):
        P.op(q, lambda e: e.dma_start(out=o, in_=i), r, w, tl=tl, inc=16)

    def AG(src, dst, r, w, tl):
        P.op("pool", lambda e: e.collective_compute("AllGather", ALU.bypass, replica_groups=GROUPS,
                                                    ins=[src.ap().opt()], outs=[dst.ap().opt()]), r, w, tl=tl, inc=1)

    def rstd_from_ss(ss, n, rstd, r, w):
        ACTV(rstd, ss, AF.Sqrt, r, w, bias=epsc[:, 0:1], scale=1.0 / n)
        RECIP(rstd, rstd, w, w)

    ident_bf = A.alloc([128], BF16); ident_f = A.alloc([128], F32); zeros = A.alloc([128], BF16)
    epsc = A.alloc([1], F32)
    junk = A.alloc([1024], BF16)
    MOD = A.alloc([2, 6, 1024], BF16)
    MSET("pool", ident_f, 0.0, ["ident_f"])
    P.op("pool", lambda e: e.affine_select(out=ident_f, in_=ident_f, pattern=[[-1, 128]], compare_op=ALU.not_equal,
                                           fill=1.0, base=0, channel_multiplier=1), ["ident_f"], ["ident_f"])
    CP("pool", ident_bf, ident_f, ["ident_f"], ["ident_bf"])
    HM = A.alloc([2], F32)
    RED("dve", HM[:, 0:1], ident_f[:, 0:64], ["ident_f"], ["HM"])
    RED("dve", HM[:, 1:2], ident_f[:, 64:128], ["ident_f"], ["HM"])
    MSET("pool", zeros, 0.0, ["zeros"])
    MSET("pool", epsc, EPS, ["epsc"])

    def pbc(ap):
        b = ap.partition_broadcast(128)
        if len(b.shape) == 3 and b.shape[1] == 1:
            b = b[:, 0]
        return b

    stop = [False]

    def tap(name, ap, reads, flat):
        if name in DEBUG:
            shp = [128, int(np.prod(ap.shape[1:]))]
            d = nc.dram_tensor(name, shp, ap.dtype, kind="ExternalOutput").ap()
            DMA("sp", d, ap.rearrange(flat) if flat else ap, reads, [name], "dbg")

    def check_stop(name):
        if STOP_AFTER == name:
            stop[0] = True
        return stop[0]

    for l in range(2):
        if stop[0]:
            break
        with_ctx = (l == 0)
        lam_init = 0.8 - 0.6 * math.exp(-0.3 * l)
        ntl = NTC
        nto = NTC if with_ctx else NT
        xsrc = (I["xin"], I["xcin"]) if l == 0 else (xs, xcs)
        L = f"L{l}"

        def xtile_ap(src2, i):
            return src2[0][i * 128:(i + 1) * 128, :] if i < NT else src2[1][(i - NT) * 128:(i - NT + 1) * 128, :]

        P.barrier()
        A.push()
        cv = A.alloc([16], F32); sil = A.alloc([16], F32); sbc = A.alloc([2, 8, 128], BF16)
        gv = A.alloc([4, 1024], F32); tmpm = A.alloc([1024], F32)
        wm = [A.alloc([8, 1024], BF16) for _ in range(2)]
        bs = [A.alloc([1024], F32) for _ in range(2)]
        DMA("sp", cv, I["cvec"], [], [L + "cv"], "m0")
        DMA("sp", gv, pbc(I[f"gvec{l}"]).rearrange("p (a b) -> p a b", b=1024), [], [L + "gv"], "m0")
        ACTV(sil, cv, AF.Silu, [L + "cv"], [L + "sil"])
        for v in range(2):
            for kc in range(8):
                ACTV(sbc[:, v, kc, :], zeros, AF.Identity, [L + "sil", "zeros"], [L + "sbc"], bias=sil[:, v * 8 + kc:v * 8 + kc + 1])
        wmv = I[f"wmod{l}"].rearrange("(kc p) n -> p kc n", p=128)
        for s in range(6):
            sl = s % 2
            DMA("pool", wm[sl], wmv[:, :, s * 1024:(s + 1) * 1024], [], [L + f"wm{sl}"], f"wm{sl}")
            DMA("sp", bs[sl], pbc(I[f"bmod{l}"][0:1, s * 1024:(s + 1) * 1024]), [], [L + f"bs{sl}"], f"bs{sl}")
            for v in range(2):
                pb = 4 * (s % 2) + 2 * v
                for hf in range(2):
                    for kc in range(8):
                        MM(bank(pb + hf), sbc[:, v, kc, :], wm[sl][:, kc, hf * 512:(hf + 1) * 512], kc == 0, kc == 7,
                           [L + "sbc", L + f"wm{sl}"], [f"ps{pb + hf}"])
                TT("dve", tmpm, psum[:, 512 * pb:512 * pb + 1024], bs[sl], ALU.add, [f"ps{pb}", f"ps{pb + 1}", L + f"bs{sl}"], [L + "tmpm"])
                dst = MOD[:, v, s, :]
                if s in (0, 3):
                    CP("dve", dst, tmpm, [L + "tmpm"], [f"MOD{v}{s}"])
                elif s in (1, 4):
                    STT("dve", dst, tmpm, 1.0, gv[:, 0 if s == 1 else 2, :], ALU.add, ALU.mult, [L + "tmpm", L + "gv"], [f"MOD{v}{s}"])
                else:
                    TT("dve", dst, tmpm, gv[:, 1 if s == 2 else 3, :], ALU.mult, [L + "tmpm", L + "gv"], [f"MOD{v}{s}"])
        if l == 0:
            tap("t_mod", MOD, [f"MOD{v}{s}" for v in range(2) for s in range(6)], "p a b c -> p (a b c)")
        A.pop()
        if check_stop(f"p0_{l}"):
            break

        P.barrier()
        A.push()
        moe = (l == 1)
        if moe:
            LOGI = A.alloc([NT, 8], F32); GATES = A.alloc([NT, 8], F32)
        hT = A.alloc([8, TOKC], BF16)
        otd = dint(L + "otd", [NTC, 128, 8, 128], BF16).ap()
        A.push()
        otst = [A.alloc([2, 128], BF16) for _ in range(2)]

        A.push()
        xt = [A.alloc([1024], F32) for _ in range(2)]
        t1 = [A.alloc([1024], F32) for _ in range(2)]
        hb = [A.alloc([1024], BF16) for _ in range(2)]
        ssb = A.alloc([NTC], F32); rsb = A.alloc([NTC], F32)
        for i in range(ntl):
            s2 = i % 2
            v = 0 if i < NT else 1
            DMA("sp", xt[s2], xtile_ap(xsrc, i), [], [L + f"xt{s2}"], f"xt{s2}")
            ACTV(junk, xt[s2], AF.Square, [L + f"xt{s2}"], ["junk", L + f"ss{i}"], accum=ssb[:, i:i + 1])
            rstd_from_ss(ssb[:, i:i + 1], 1024, rsb[:, i:i + 1], [L + f"ss{i}", "epsc"], [L + f"rs{i}"])
            STT("dve", t1[s2], xt[s2], rsb[:, i:i + 1], MOD[:, v, 1, :], ALU.mult, ALU.mult, [L + f"xt{s2}", L + f"rs{i}", f"MOD{v}1"], [L + f"t1{s2}"])
            TT("dve", hb[s2], t1[s2], MOD[:, v, 0, :], ALU.add, [L + f"t1{s2}", f"MOD{v}0"], [L + f"hb{s2}"])
            for kc in range(8):
                TR(bank_bf(s2)[:, kc * 128:(kc + 1) * 128], hb[s2][:, kc * 128:(kc + 1) * 128], ident_bf, [L + f"hb{s2}", "ident_bf"], [f"ps{s2}"])
            CP("act", hT[:, :, i * 128:(i + 1) * 128], bank_bf(s2).rearrange("p (k t) -> p k t", t=128), [f"ps{s2}"], [L + f"hT{i}"])
        A.pop()
        HT_ALL = [L + f"hT{i}" for i in range(ntl)]
        if l == 0:
            tap("t_hT", hT, HT_ALL, "p a b -> p (a b)")
        if check_stop(f"p1a_{l}"):
            A.pop(); A.pop(); break

        winv = I[f"win{l}"].rearrange("(kc p) n -> p kc n", p=128)

        def proj_rope(wq, wqs, Ctab, Stab, dests, tag, name0=0):
            tA = [A.alloc([512], F32) for _ in range(2)]
            tB = [A.alloc([512], F32) for _ in range(2)]
            n = 0
            for ci in range(len(wq)):
                for tb in range(4):
                    s2 = n % 2; n += 1
                    ts = slice(tb * 512, (tb + 1) * 512)
                    for kc in range(8):
                        MM(bank(s2), wq[ci][:, kc, :], hT[:, kc, ts], kc == 0, kc == 7, HT_ALL[4 * tb:4 * tb + 4] + [tag + "w"], [f"ps{s2}"])
                    for kc in range(8):
                        MM(bank(2 + s2), wqs[ci][:, kc, :], hT[:, kc, ts], kc == 0, kc == 7, HT_ALL[4 * tb:4 * tb + 4] + [tag + "w"], [f"ps{2 + s2}"])
                    TT("dve", tA[s2], bank(s2), Ctab[:, ts], ALU.mult, [f"ps{s2}", L + "rope"], [tag + f"tA{s2}"])
                    TT("dve", tB[s2], bank(2 + s2), Stab[:, ts], ALU.mult, [f"ps{2 + s2}", L + "rope"], [tag + f"tB{s2}"])
                    TT("dve", dests[ci][:, ts], tA[s2], tB[s2], ALU.add, [tag + f"tA{s2}", tag + f"tB{s2}"], [tag + f"d{ci + name0}"])

        def proj_feat_plain(wq, dest, t0, nt, tag, ci, pb):
            for kc in range(8):
                MM(bank(pb, nt), wq[:, kc, :], hT[:, kc, t0:t0 + nt], kc == 0, kc == 7, HT_ALL + [tag + "w"], [f"ps{pb}"])
            CP("act", dest, bank(pb, nt), [f"ps{pb}"], [tag + f"d{ci}"])

        def proj_tok(wv, ncols, dest_fn, tiles, tag, post=None, view=None):
            for n, i in enumerate(tiles):
                pb = 4 + n % 2
                for kc in range(8):
                    MM(bank(pb, ncols), hT[:, kc, i * 128:(i + 1) * 128], wv[:, kc, :], kc == 0, kc == 7, [L + f"hT{i}", tag + "w"], [f"ps{pb}"])
                if post is None:
                    src_ = bank(pb, ncols)
                    if view is not None:
                        src_ = view(src_)
                    CP("act", dest_fn(i), src_, [f"ps{pb}"], [tag + f"v{i}"])
                else:
                    post(i, pb)

        def out_transposes(ytok, chunk0, i, tag, rd):
            pb = 6 + (i % 2)
            st = otst[i % 2]
            for cc in range(2):
                TR(bank_bf(pb)[:, cc * 128:(cc + 1) * 128], ytok[:, cc * 128:(cc + 1) * 128], ident_bf, rd + ["ident_bf"], [f"ps{pb}"])
            CP("act", st, bank_bf(pb)[:, 0:256].rearrange("p (c t) -> p c t", t=128), [f"ps{pb}"], [L + f"otst{i % 2}"])
            DMA("sp", otd[i, :, chunk0:chunk0 + 2, :], st, [L + f"otst{i % 2}"], [L + f"OT{chunk0}_{i}"], f"ot{i % 2}")

        def attn_pipeline(tiles_slots, S_fn, E_fn, AV_fn, FIN_fn):
            units = []
            for (i, slots) in tiles_slots:
                ngr = (len(slots) + 1) // 2
                for gi in range(ngr):
                    units.append((i, gi, ngr, slots[2 * gi:2 * gi + 2]))
            pending = None
            for k, u in enumerate(units):
                if k == 0:
                    S_fn(0, u)
                E_fn(k, u)
                if k + 1 < len(units):
                    S_fn(k + 1, units[k + 1])
                AV_fn(k, u)
                if pending is not None:
                    FIN_fn(pending); pending = None
                if u[1] == u[2] - 1:
                    pending = u[0]
            if pending is not None:
                FIN_fn(pending)

        rope64 = A.alloc([2, 2048], BF16); rope32 = A.alloc([2, 2048], BF16)
        DMA("pool", rope64, I["rope64"], [], [L + "rope"], "rp")
        DMA("pool", rope32, I["rope32"], [], [L + "rope"], "rp")

        T = L + "da"
        A.push()
        QT = A.alloc([2, TOKC], BF16); KTc = A.alloc([2, 256], BF16)
        Vc = A.alloc([2, 256], BF16)
        nlam = A.alloc([1], F32); gsub = A.alloc([64], F32)
        A.push()
        dl = A.alloc([4, 32], F32); pr = A.alloc([2, 32], F32); s12 = A.alloc([2], F32)
        DMA("sp", dl, pbc(I[f"dal{l}"]).rearrange("p (a b) -> p a b", b=32), [], [T + "dl"], "m0")
        DMA("sp", gsub, pbc(I[f"subln{l}"]), [], [T + "gsub"], "m0")
        TT("dve", pr[:, 0, :], dl[:, 0, :], dl[:, 1, :], ALU.mult, [T + "dl"], [T + "pr"])
        TT("dve", pr[:, 1, :], dl[:, 2, :], dl[:, 3, :], ALU.mult, [T + "dl"], [T + "pr"])
        RED("dve", s12, pr, [T + "pr"], [T + "s12"])
        ACTV(s12, s12, AF.Exp, [T + "s12"], [T + "s12"])
        TT("dve", nlam, s12[:, 1:2], s12[:, 0:1], ALU.subtract, [T + "s12"], [T + "nlam"])
        TS("dve", nlam, nlam, -lam_init, ALU.add, [T + "nlam"], [T + "nlam"])
        TS("dve", gsub, gsub, 1.0 - lam_init, ALU.mult, [T + "gsub"], [T + "gsub"])
        A.pop()
        A.push()
        wda = A.alloc([8, 1280], BF16)
        KTo = A.alloc([2, TOK], BF16); Vo = A.alloc([NT, 256], BF16)
        DMA("pool", wda, winv[:, :, DA0:DA0 + 1280], [], [T + "w"], "wA")
        qk_dest = [QT[:, 0, 0:TOK], QT[:, 1, 0:TOK], KTo[:, 0, :], KTo[:, 1, :]]
        wq_l = [wda[:, :, c * 128:(c + 1) * 128] for c in range(4)]
        wqs_l = [wda[:, :, 512 + c * 128:512 + (c + 1) * 128] for c in range(4)]
        proj_rope(wq_l[2:4], wqs_l[2:4], rope32[:, 0, :], rope32[:, 1, :], qk_dest[2:4], T, name0=2)
        proj_tok(wda[:, :, 1024:1280], 256, lambda i: Vo[:, i, :] if i < NT else Vc[:, i - NT, :], range(NTC), T)
        V_R = [T + f"v{i}" for i in range(NTC)]
        e_k = dint(T + "ek", [256, TOK], BF16); e_v = dint(T + "ev", [TOK, 256], BF16)
        g_k = dint(T + "gk", [1024, TOK], BF16); g_v = dint(T + "gv", [4 * TOK, 256], BF16)
        DMA("sp", e_k.ap().rearrange("(c p) t -> p c t", p=128), KTo, [T + "d2", T + "d3"], [T + "ek"], "ex")
        DMA("sp", e_v.ap().rearrange("(i p) f -> p i f", p=128), Vo, V_R, [T + "ev"], "ex")
        AG(e_k, g_k, [T + "ek"], [T + "gk"], "cc")
        AG(e_v, g_v, [T + "ev"], [T + "gv"], "cc")
        proj_rope(wq_l[0:2], wqs_l[0:2], rope32[:, 0, :], rope32[:, 1, :], qk_dest[0:2], T, name0=0)
        for ci in range(4):
            dest = QT[:, ci, TOK:TOKC] if ci < 2 else KTc[:, ci - 2, :]
            proj_feat_plain(wda[:, :, ci * 128:(ci + 1) * 128], dest, TOK, 256, T, f"c{ci}", ci % 2)
        QK_R = [T + f"d{ci}" for ci in range(4)] + [T + f"dc{ci}" for ci in range(4)]
        if check_stop(f"daproj_{l}"):
            tap("t_qt", QT, QK_R, "p a b -> p (a b)")
            tap("t_kto", KTo, QK_R, "p a b -> p (a b)")
            tap("t_vo", Vo, V_R, "p a b -> p (a b)")
            tap("t_ktc", KTc, QK_R, "p a b -> p (a b)")
            tap("t_vc", Vc, V_R, "p a b -> p (a b)")
            A.pop(); A.pop(); A.pop(); A.pop(); break
        A.pop()
        P.barrier()
        if check_stop(f"daag_{l}"):
            A.pop(); A.pop(); A.pop(); break
        KT = A.alloc([8448], BF16); V1 = A.alloc([66, 2, 65], BF16)
        ptH = [[A.alloc([2, 512], BF16) for _ in range(2)] for _ in range(2)]
        o_f = A.alloc([2, 64], F32); o1 = A.alloc([64], F32)
        rec = A.alloc([4], F32); rn = A.alloc([2], F32); ssd = A.alloc([2], F32); rsd = A.alloc([2], F32)
        yda = [A.alloc([2, 64], BF16) for _ in range(2)]
        MSET("pool", V1[:, :, :, 64:65], 1.0, [T + "V1ones"])
        g_kv = g_k.ap().rearrange("(r c p) t -> p r c t", r=4, c=2)
        g_vv = g_v.ap().rearrange("(r i p) f -> p r i f", r=4, i=NT)
        nblk = 0
        for c in range(2):
            CP("pool", KT[:, 0:256], KTc[:, c, :], QK_R, [T + "KT"])
            for r in range(4):
                DMA("sp", KT[:, 256 + r * TOK:256 + (r + 1) * TOK], g_kv[:, r, c, :], [T + "gk"], [T + "KT"], "kt")
                for hh in range(2):
                    DMA("sp", V1[:, 2 + r * NT:2 + (r + 1) * NT, hh, 0:64], g_vv[:, r, :, c * 128 + hh * 64:c * 128 + hh * 64 + 64],
                        [T + "gv"], [T + "V1"], "kt")
            CP("pool", V1[:, 0:2, :, 0:64], Vc[:, :, c * 128:(c + 1) * 128].rearrange("p i (h d) -> p i h d", d=64), V_R, [T + "V1"])
            if STOP_AFTER == f"daload_{l}":
                continue
            qblocks = [(qb * 512, 512, 0, 66) for qb in range(4)]
            if STOP_AFTER == f"daq1_{l}":
                qblocks = [(0, 512, 0, 66)] if c == 0 else []
            if STOP_AFTER == f"daq1nf_{l}":
                qblocks = [(0, 512, 0, 66)] if c == 0 else []
            if with_ctx:
                qblocks.append((TOK, 256, 0, 2))
            for (q0, nq, k0, k1) in qblocks:
                sc_ = 1.0 / math.sqrt(32.0)

                def S_half(kt, hf):
                    for g in (2 * hf, 2 * hf + 1):
                        MM(bank(g, nq), KT[32 * g:32 * g + 32, kt * 128:(kt + 1) * 128], QT[32 * g:32 * g + 32, c, q0:q0 + nq], True, True,
                           [T + "KT"] + QK_R, [f"psS{hf}"], tp=(32 * g, 0))

                def E_half(kt, hf):
                    s2 = (kt - k0) % 2
                    ACTV(ptH[hf][s2][:, :, 0:nq], psum[:, 1024 * hf:1024 * hf + 1024].rearrange("p (g n) -> p g n", n=512)[:, :, 0:nq], AF.Exp,
                         [f"psS{hf}"], [T + f"pt{hf}{s2}"], scale=sc_)

                def AV_half(kt, hf):
                    s2 = (kt - k0) % 2
                    for sb in range(nq // 128):
                        for gg in range(2):
                            g = 2 * hf + gg
                            MM(bank(4 + sb, 65, g * 65), ptH[hf][s2][:, gg, sb * 128:(sb + 1) * 128], V1[:, kt, hf, :], kt == k0 and g == 0, kt == k1 - 1,
                               [T + f"pt{hf}{s2}", T + "V1", T + "V1ones"], [f"psO{sb}"], sgc=True)

                S_half(k0, 0); S_half(k0, 1)
                for kt in range(k0, k1):
                    E_half(kt, 0); E_half(kt, 1)
                    AV_half(kt, 0)
                    if kt + 1 < k1:
                        S_half(kt + 1, 0)
                    AV_half(kt, 1)
                    if kt + 1 < k1:
                        S_half(kt + 1, 1)
                if STOP_AFTER == f"daq1nf_{l}":
                    continue
                for sb in range(nq // 128):
                    tile_i = (q0 + sb * 128) // 128
                    yb = yda[nblk % 2]; ybn = T + f"yda{nblk % 2}"; nblk += 1
                    bk = 4 + sb
                    pr_ = f"psO{sb}"
                    Tv = bank(bk, 260).rearrange("p (g e) -> p g e", e=65)
                    RECIP(rec, Tv[:, :, 64], [pr_], [T + "rec"])
                    TS("dve", rn, rec.rearrange("p (h m) -> p h m", m=2)[:, :, 1], nlam[:, 0:1], ALU.mult, [T + "rec", T + "nlam"], [T + "rn"])
                    for hh in range(2):
                        TS("dve", o1, Tv[:, 2 * hh, 0:64], rec[:, 2 * hh:2 * hh + 1], ALU.mult, [pr_, T + "rec"], [T + "o1"])
                        STT("dve", o_f[:, hh, :], Tv[:, 2 * hh + 1, 0:64], rn[:, hh:hh + 1], o1, ALU.mult, ALU.add, [pr_, T + "rn", T + "o1"], [T + "o_f"])
                        ACTV(junk[:, 0:64], o_f[:, hh, :], AF.Square, [T + "o_f"], ["junk", T + "ssd"], accum=ssd[:, hh:hh + 1])
                    rstd_from_ss(ssd, 64, rsd, [T + "ssd", "epsc"], [T + "rsd"])
                    for hh in range(2):
                        STT("dve", yb[:, hh, :], o_f[:, hh, :], rsd[:, hh:hh + 1], gsub, ALU.mult, ALU.mult, [T + "o_f", T + "rsd", T + "gsub"], [ybn])
                    TR(bank_bf(bk)[:, 640:768], yb.rearrange("p h d -> p (h d)"), ident_bf, [ybn, "ident_bf"], [pr_])
                    CP("act", otst[sb % 2][:, 0, :], bank_bf(bk)[:, 640:768], [pr_], [L + f"otst{sb % 2}"])
                    DMA("sp", otd[tile_i, :, c, :], otst[sb % 2][:, 0, :], [L + f"otst{sb % 2}"], [L + f"OT{c}_{tile_i}"], f"ot{sb % 2}")
        A.pop()
        if check_stop(f"da_{l}") or STOP_AFTER in (f"daload_{l}", f"daq1_{l}", f"daq1nf_{l}"):
            P.barrier()
            tap("t_nlam", nlam, [], None)
            tap("t_gsub", gsub, [], None)
            if "dbg_ot" in dbg:
                P.barrier()
                DMA("sp", dbg["dbg_ot"], otd, [], ["dbgot"], "dbg")
            A.pop(); A.pop(); break

        P.barrier()
        T = L + "sw"
        A.push()
        QT = A.alloc([2, TOKC], BF16)
        KT = A.alloc([3328], BF16)
        V1 = A.alloc([26, 2, 65], BF16)
        MSW = A.alloc([10, 128], BF16)
        esink = A.alloc([4], F32)
        DMA("pool", MSW, I["swamask"].rearrange("m k q -> k m q"), [], [T + "msw"], "wB")
        DMA("sp", esink, pbc(I[f"sink{l}"]), [], [T + "esink"], "m0")
        ACTV(esink, esink, AF.Exp, [T + "esink"], [T + "esink"])
        MSET("pool", V1[:, :, :, 64:65], 1.0, [T + "V1ones"])
        A.push()
        wsw = A.alloc([8, 896], BF16)
        DMA("pool", wsw, winv[:, :, SW0:SW0 + 896], [], [T + "w"], "wA")
        dests = [QT[:, 0, 0:TOK], QT[:, 1, 0:TOK], KT[:, 0:TOK]]
        wq_l = [wsw[:, :, c * 128:(c + 1) * 128] for c in range(3)]
        wqs_l = [wsw[:, :, 384 + c * 128:384 + (c + 1) * 128] for c in range(3)]
        proj_rope(wq_l[2:3], wqs_l[2:3], rope64[:, 0, :], rope64[:, 1, :], dests[2:3], T, name0=2)
        proj_tok(wsw[:, :, 768:896], 128, lambda i: V1[:, i, :, 0:64], range(NTC), T, view=lambda a: a.rearrange("p (h d) -> p h d", d=64))
        V_R = [T + f"v{i}" for i in range(NTC)]
        e_s = dint(T + "e", [128, 512], BF16); g_s = dint(T + "g", [512, 512], BF16)
        DMA("sp", e_s.ap()[:, 0:128], KT[:, 0:128], [T + "d2"], [T + "e"], "ex")
        DMA("sp", e_s.ap()[:, 128:256], KT[:, TOK - 128:TOK], [T + "d2"], [T + "e"], "ex")
        DMA("sp", e_s.ap()[:, 256:384].rearrange("p (h d) -> p h d", d=64), V1[:, 0, :, 0:64], V_R, [T + "e"], "ex")
        DMA("sp", e_s.ap()[:, 384:512].rearrange("p (h d) -> p h d", d=64), V1[:, 15, :, 0:64], V_R, [T + "e"], "ex")
        AG(e_s, g_s, [T + "e"], [T + "g"], "cc")
        proj_rope(wq_l[0:2], wqs_l[0:2], rope64[:, 0, :], rope64[:, 1, :], dests[0:2], T, name0=0)
        for ci in range(3):
            dest = QT[:, ci, TOK:TOKC] if ci < 2 else KT[:, TOK:TOKC]
            proj_feat_plain(wsw[:, :, ci * 128:(ci + 1) * 128], dest, TOK, 256, T, f"c{ci}", ci % 2)
        A.pop()
        QK_R = [T + f"d{ci}" for ci in range(3)] + [T + f"dc{ci}" for ci in range(3)]
        if check_stop(f"swproj_{l}"):
            A.pop(); A.pop(); A.pop(); break
        g_sv = g_s.ap().rearrange("(r p) f -> p r f", p=128)
        DMA("sp", KT[:, TOKC:TOKC + 512].rearrange("p (r t) -> p r t", t=128), g_sv[:, :, 128:256], [T + "g"], [T + "halo"], "kt")
        DMA("sp", KT[:, TOKC + 512:TOKC + 1024].rearrange("p (r t) -> p r t", t=128), g_sv[:, :, 0:128], [T + "g"], [T + "halo"], "kt")
        for kv in range(2):
            DMA("sp", V1[:, 18:22, kv, 0:64], g_sv[:, :, 384 + kv * 64:448 + kv * 64], [T + "g"], [T + "halo"], "kt")
            DMA("sp", V1[:, 22:26, kv, 0:64], g_sv[:, :, 256 + kv * 64:320 + kv * 64], [T + "g"], [T + "halo"], "kt")
        if check_stop(f"swag_{l}"):
            A.pop(); A.pop(); A.pop(); break
        ptw = [A.alloc([2, 4, 128], BF16) for _ in range(2)]
        qz = [A.alloc([2, 2, 128], BF16) for _ in range(2)]
        den = [A.alloc([4], F32) for _ in range(2)]; ysw = [A.alloc([4, 64], BF16) for _ in range(2)]
        ALLR = QK_R + V_R + [T + "halo", T + "V1ones"]
        tiles_slots = []
        for i in range(nto):
            if i < NT:
                slots = []
                if i > 0:
                    slots.append((128 * (i - 1), i - 1, 0))
                slots.append((128 * i, i, None))
                if i < NT - 1:
                    slots.append((128 * (i + 1), i + 1, 1))
                slots += [(TOK, 16, None), (TOK + 128, 17, None)]
                if i == 0:
                    slots += [(TOKC + 128 * r, 18 + r, 2 + r) for r in range(4)]
                if i == NT - 1:
                    slots += [(TOKC + 512 + 128 * r, 22 + r, 6 + r) for r in range(4)]
            else:
                slots = [(TOK, 16, None), (TOK + 128, 17, None)]
            tiles_slots.append((i, slots))

        def sw_S(k, u):
            i, gi, ngr, grp = u
            s2 = k % 2
            ts = slice(i * 128, (i + 1) * 128)
            if gi == 0:
                for kv in range(2):
                    TS("pool" if kv else "dve", qz[i % 2][:, kv], QT[:, :, ts], HM[:, kv:kv + 1], ALU.mult, QK_R + ["HM"], [T + f"qz{i % 2}"])
            for si, (kc0, vt, mi) in enumerate(grp):
                for kv in range(2):
                    for g in range(2):
                        MM(bank(2 * s2 + si, 128, (kv * 2 + g) * 128), KT[:, kc0:kc0 + 128], qz[i % 2][:, kv, g, :], True, True,
                           ALLR + [T + f"qz{i % 2}"], [f"psS{s2}"], sgc=True)

        def sw_E(k, u):
            i, gi, ngr, grp = u
            s2 = k % 2
            ns = len(grp)
            ACTV(ptw[s2][:, 0:ns].rearrange("p s h q -> p (s h q)"), psum[:, 1024 * s2:1024 * s2 + 512 * ns], AF.Exp, [f"psS{s2}"], [T + f"pt{s2}"], scale=0.125)
            for si, (kc0, vt, mi) in enumerate(grp):
                if mi is not None:
                    TT("dve", ptw[s2][:, si], ptw[s2][:, si], MSW[:, mi, :].unsqueeze(1).to_broadcast([128, 4, 128]), ALU.mult, [T + f"pt{s2}", T + "msw"], [T + f"pt{s2}"])

        def sw_AV(k, u):
            i, gi, ngr, grp = u
            s2 = k % 2
            ns = len(grp)
            for si, (kc0, vt, mi) in enumerate(grp):
                first = (gi == 0 and si == 0); last = (gi == ngr - 1 and si == ns - 1)
                for kv in range(2):
                    for g in range(2):
                        h = kv * 2 + g
                        MM(bank(4 + i % 2, 65, h * 65), ptw[s2][:, si, h, :], V1[:, vt, kv, :], first and h == 0, last, [T + f"pt{s2}"] + ALLR, [f"psO{i % 2}"], sgc=True)

        def sw_FIN(i):
            Ov = bank(4 + i % 2, 260).rearrange("p (h e) -> p h e", e=65)
            dn = den[i % 2]
            TT("dve", dn, Ov[:, :, 64], esink, ALU.add, [f"psO{i % 2}", T + "esink"], [T + f"den{i % 2}"])
            RECIP(dn, dn, [T + f"den{i % 2}"], [T + f"den{i % 2}"])
            yb = ysw[i % 2]
            TT("dve", yb, Ov[:, :, 0:64], dn.unsqueeze(2).to_broadcast([128, 4, 64]), ALU.mult, [f"psO{i % 2}", T + f"den{i % 2}"], [T + f"y{i % 2}"])
            out_transposes(yb.rearrange("p h d -> p (h d)"), 2, i, T, [T + f"y{i % 2}"])

        attn_pipeline(tiles_slots, sw_S, sw_E, sw_AV, sw_FIN)
        A.pop()
        if check_stop(f"sw_{l}") or (STOP_AFTER or "").startswith("swq1"):
            if "dbg_ot" in dbg:
                P.barrier()
                DMA("sp", dbg["dbg_ot"], otd, [], ["dbgot"], "dbg")
            A.pop(); A.pop(); break

        P.barrier()
        T = L + "na"
        A.push()
        QT = A.alloc([2, TOKC], BF16)
        KT = A.alloc([2, 4352], BF16)
        V1 = A.alloc([34, 4, 65], BF16)
        MBK = A.alloc([45, 128], BF16)
        BEX = A.alloc([7, 4, 128], BF16)
        EIN = A.alloc([5, 4, 128], BF16)
        for m0 in range(0, 45, 9):
            DMA("pool", MBK[:, m0:m0 + 9, :], I["namask"][m0:m0 + 9].rearrange("m k q -> k m q"), [], [T + "mbk"], "wB")
        A.push()
        bfl = A.alloc([7, 4, 128], F32)
        for d7 in range(7):
            DMA("sp", bfl[:, d7], I[f"nabias{l}"][d7].rearrange("h k q -> k h q"), [], [T + "bfl"], "m0")
        ACTV(BEX, bfl, AF.Exp, [T + "bfl"], [T + "bex"])
        A.pop()
        for d in range(5):
            TT("dve", EIN[:, d], BEX[:, d + 1], MBK[:, d, :].unsqueeze(1).to_broadcast([128, 4, 128]), ALU.mult, [T + "bex", T + "mbk"], [T + "ein"])
        MSET("pool", V1[:, :, :, 64:65], 1.0, [T + "V1ones"])
        A.push()
        wna = A.alloc([8, 768], BF16)
        DMA("pool", wna, winv[:, :, NA0:NA0 + 768], [], [T + "w"], "wA")
        n = 0
        for ci in (2, 3):
            for (t0, nt_) in ((0, 512), (512, 512), (1024, 512), (1536, 512), (TOK, 256)):
                dest = KT[:, ci - 2, t0:t0 + nt_]
                proj_feat_plain(wna[:, :, ci * 128:(ci + 1) * 128], dest, t0, nt_, T, f"c{ci}", n % 4); n += 1
        proj_tok(wna[:, :, 512:768], 256, lambda i: V1[:, i, :, 0:64], range(NTC), T, view=lambda a: a.rearrange("p (h d) -> p h d", d=64))
        V_R = [T + f"v{i}" for i in range(NTC)]
        e_n = dint(T + "e", [128, 2048], BF16); g_n = dint(T + "g", [512, 2048], BF16)
        env = e_n.ap()
        for c in range(2):
            DMA("sp", env[:, c * 512:c * 512 + 256], KT[:, c, 0:256], [T + "dc2", T + "dc3"], [T + "e"], "ex")
            DMA("sp", env[:, c * 512 + 256:c * 512 + 512], KT[:, c, TOK - 256:TOK], [T + "dc2", T + "dc3"], [T + "e"], "ex")
        DMA("sp", env[:, 1024:1536].rearrange("p (i h d) -> p i h d", h=4, d=64), V1[:, 0:2, :, 0:64], V_R, [T + "e"], "ex")
        DMA("sp", env[:, 1536:2048].rearrange("p (i h d) -> p i h d", h=4, d=64), V1[:, 14:16, :, 0:64], V_R, [T + "e"], "ex")
        AG(e_n, g_n, [T + "e"], [T + "g"], "cc")
        for ci in (0, 1):
            for (t0, nt_) in ((0, 512), (512, 512), (1024, 512), (1536, 512), (TOK, 256)):
                dest = QT[:, ci, t0:t0 + nt_]
                proj_feat_plain(wna[:, :, ci * 128:(ci + 1) * 128], dest, t0, nt_, T, f"c{ci}", n % 4); n += 1
        A.pop()
        P.barrier()
        QK_R = [T + f"dc{ci}" for ci in range(4)]
        g_nv = g_n.ap().rearrange("(r p) f -> p r f", p=128)
        for c in range(2):
            DMA("sp", KT[:, c, TOKC:TOKC + 1024].rearrange("p (r t) -> p r t", t=256), g_nv[:, :, c * 512 + 256:c * 512 + 512], [T + "g"], [T + "halo"], "kt")
            DMA("sp", KT[:, c, TOKC + 1024:TOKC + 2048].rearrange("p (r t) -> p r t", t=256), g_nv[:, :, c * 512:c * 512 + 256], [T + "g"], [T + "halo"], "kt")
        for r in range(4):
            DMA("sp", V1[:, 18 + 2 * r:20 + 2 * r, :, 0:64], g_nv[:, r, 1536:2048].rearrange("p (i h d) -> p i h d", h=4, d=64), [T + "g"], [T + "halo"], "kt")
            DMA("sp", V1[:, 26 + 2 * r:28 + 2 * r, :, 0:64], g_nv[:, r, 1024:1536].rearrange("p (i h d) -> p i h d", h=4, d=64), [T + "g"], [T + "halo"], "kt")
        ptn = [A.alloc([2, 4, 128], BF16) for _ in range(2)]
        qz = [A.alloc([2, 2, 128], BF16) for _ in range(2)]
        den = [A.alloc([4], F32) for _ in range(2)]; yna = [A.alloc([4, 64], BF16) for _ in range(2)]
        ALLR = QK_R + V_R + [T + "halo", T + "V1ones"]
        PC0 = TOKC; NC0 = TOKC + 1024
        tiles_slots = []
        for i in range(nto):
            if i >= NT:
                slots = [(TOK, 16, 0, 0, 0), (TOK + 128, 17, 0, 0, 0)]
            elif 2 <= i <= 13:
                slots = [(128 * (i + d), i + d, 1, d + 2, 0) for d in range(-2, 3)]
            elif i == 0:
                slots = [(128 * d, d, 2, d + 3, 5 + d) for d in (0, 1, 2, 3)]
                slots += [(PC0 + 256 * r, 18 + 2 * r, 2, 1, 9 + r) for r in range(4)]
                slots += [(PC0 + 256 * r + 128, 19 + 2 * r, 2, 2, 13 + r) for r in range(4)]
            elif i == 1:
                slots = [(128 * (1 + d), 1 + d, 2, d + 3, 17 + (d + 1)) for d in (-1, 0, 1, 2)]
                slots += [(PC0 + 256 * r + 128, 19 + 2 * r, 2, 1, 21 + r) for r in range(4)]
            elif i == 14:
                slots = [(128 * (14 + d), 14 + d, 2, d + 3, 25 + (d + 2)) for d in (-2, -1, 0, 1)]
                slots += [(NC0 + 256 * r, 26 + 2 * r, 2, 5, 29 + r) for r in range(4)]
            else:
                slots = [(128 * (15 + d), 15 + d, 2, d + 3, 33 + (d + 3)) for d in (-3, -2, -1, 0)]
                slots += [(NC0 + 256 * r, 26 + 2 * r, 2, 4, 37 + r) for r in range(4)]
                slots += [(NC0 + 256 * r + 128, 27 + 2 * r, 2, 5, 41 + r) for r in range(4)]
            if i < NT:
                slots += [(TOK, 16, 0, 0, 0), (TOK + 128, 17, 0, 0, 0)]
            tiles_slots.append((i, slots))

        def na_S(k, u):
            i, gi, ngr, grp = u
            s2 = k % 2
            ts = slice(i * 128, (i + 1) * 128)
            if gi == 0:
                for hb_ in range(2):
                    TS("pool" if hb_ else "dve", qz[i % 2][:, hb_], QT[:, :, ts], HM[:, hb_:hb_ + 1], ALU.mult, QK_R + ["HM"], [T + f"qz{i % 2}"])
            for si, (kc0, vt, kind, bd, mi) in enumerate(grp):
                for h in range(4):
                    c, hb_ = h // 2, h % 2
                    MM(bank(2 * s2 + si, 128, h * 128), KT[:, c, kc0:kc0 + 128], qz[i % 2][:, hb_, c, :], True, True, ALLR + [T + f"qz{i % 2}"], [f"psS{s2}"], sgc=True)

        def na_E(k, u):
            i, gi, ngr, grp = u
            s2 = k % 2
            ns = len(grp)
            ACTV(ptn[s2][:, 0:ns].rearrange("p s h q -> p (s h q)"), psum[:, 1024 * s2:1024 * s2 + 512 * ns], AF.Exp, [f"psS{s2}"], [T + f"pt{s2}"], scale=0.125)
            for si, (kc0, vt, kind, bd, mi) in enumerate(grp):
                if kind == 1:
                    TT("dve", ptn[s2][:, si], ptn[s2][:, si], EIN[:, bd], ALU.mult, [T + f"pt{s2}", T + "ein"], [T + f"pt{s2}"])
                elif kind == 2:
                    TT("dve", ptn[s2][:, si], ptn[s2][:, si], BEX[:, bd], ALU.mult, [T + f"pt{s2}", T + "bex"], [T + f"pt{s2}"])
                    TT("pool", ptn[s2][:, si], ptn[s2][:, si], MBK[:, mi, :].unsqueeze(1).to_broadcast([128, 4, 128]), ALU.mult, [T + f"pt{s2}", T + "mbk"], [T + f"pt{s2}"])

        def na_AV(k, u):
            i, gi, ngr, grp = u
            s2 = k % 2
            ns = len(grp)
            for si, (kc0, vt, kind, bd, mi) in enumerate(grp):
                first = (gi == 0 and si == 0); last = (gi == ngr - 1 and si == ns - 1)
                for h in range(4):
                    MM(bank(4 + i % 2, 65, h * 65), ptn[s2][:, si, h, :], V1[:, vt, h, :], first and h == 0, last, [T + f"pt{s2}"] + ALLR, [f"psO{i % 2}"], sgc=True)

        def na_FIN(i):
            Ov = bank(4 + i % 2, 260).rearrange("p (h e) -> p h e", e=65)
            dn = den[i % 2]
            RECIP(dn, Ov[:, :, 64], [f"psO{i % 2}"], [T + f"den{i % 2}"])
            yb = yna[i % 2]
            TT("dve", yb, Ov[:, :, 0:64], dn.unsqueeze(2).to_broadcast([128, 4, 64]), ALU.mult, [f"psO{i % 2}", T + f"den{i % 2}"], [T + f"y{i % 2}"])
            out_transposes(yb.rearrange("p h d -> p (h d)"), 4, i, T, [T + f"y{i % 2}"])

        attn_pipeline(tiles_slots, na_S, na_E, na_AV, na_FIN)
        A.pop()
        if check_stop(f"na_{l}"):
            if "dbg_ot" in dbg:
                P.barrier()
                DMA("sp", dbg["dbg_ot"], otd, [], ["dbgot"], "dbg")
            A.pop(); A.pop(); break

        P.barrier()
        T = L + "rt"
        A.push()
        QT = A.alloc([2, TOKC], BF16); KT = A.alloc([2, TOKC], BF16)
        VR = A.alloc([NTC, 256], BF16); GT = A.alloc([NTC, 256], BF16)
        RC = A.alloc([700], F32); IDXB = A.alloc([128], F32)
        LG = A.alloc([8], F32); LGS = A.alloc([2, 2], F32)
        DEC = A.alloc([4, 128], BF16); XI = A.alloc([2, 2, 128], BF16)
        ZZ = A.alloc([2, 4], F32); GC = A.alloc([2, 2], F32); GPW = A.alloc([2, 2, 18], F32); CFC = A.alloc([2, 2, 5], F32)
        DMA("sp", RC, I["retc"], [], [T + "rc"], "m0")
        DMA("sp", IDXB, I["idxb"], [], [T + "rc"], "m0")
        DMA("sp", LG, pbc(I[f"gam{l}"]), [], [T + "lg"], "m0")
        ACTV(LG, LG, AF.Exp, [T + "lg"], [T + "lg"], scale=-1.0)
        TS("dve", LG, LG, 1.0, ALU.add, [T + "lg"], [T + "lg"])
        ACTV(LG, LG, AF.Ln, [T + "lg"], [T + "lg"])
        TS("dve", LG, LG, -1.0, ALU.mult, [T + "lg"], [T + "lg"])
        for d_ in range(2):
            for c in range(2):
                k0_ = 4 * d_ + 2 * c
                TS("dve", LGS[:, d_, c:c + 1], LG[:, k0_:k0_ + 1], HM[:, 0:1], ALU.mult, [T + "lg", "HM"], [T + "lgs"])
                STT("dve", LGS[:, d_, c:c + 1], LG[:, k0_ + 1:k0_ + 2], HM[:, 1:2], LGS[:, d_, c:c + 1], ALU.mult, ALU.add, [T + "lg", "HM", T + "lgs"], [T + "lgs"])
        A.push()
        tf = A.alloc([128], F32); tb_ = A.alloc([128], F32)
        for h in range(4):
            ACTV(tf, RC[:, 0:128], AF.Exp, [T + "rc", T + "lg"], [T + "tf"], scale=LG[:, h:h + 1])
            TT("dve", tf, tf, RC[:, 128:256], ALU.mult, [T + "tf", T + "rc"], [T + "tf"])
            ACTV(tb_, RC[:, 256:384], AF.Exp, [T + "rc", T + "lg"], [T + "tb"], scale=LG[:, 4 + h:5 + h])
            TT("dve", tb_, tb_, RC[:, 384:512], ALU.mult, [T + "tb", T + "rc"], [T + "tb"])
            TT("dve", DEC[:, h, :], tf, tb_, ALU.add, [T + "tf", T + "tb"], [T + "dec"])
        A.pop()
        for c in range(2):
            ACTV(XI[:, 0, c, :], RC[:, 512:640], AF.Exp, [T + "rc", T + "lgs"], [T + "xi"], scale=LGS[:, 0, c:c + 1])
            ACTV(XI[:, 1, c, :], IDXB, AF.Exp, [T + "rc", T + "lgs"], [T + "xi"], scale=LGS[:, 1, c:c + 1])
            for d_ in range(2):
                ACTV(GC[:, d_, c:c + 1], LGS[:, d_, c:c + 1], AF.Exp, [T + "lgs"], [T + "gc"], scale=128.0)
                ACTV(GPW[:, d_, c, :], RC[:, 642 + 18 * d_:660 + 18 * d_], AF.Exp, [T + "rc", T + "lgs"], [T + "gpw"], scale=LGS[:, d_, c:c + 1])
                ACTV(CFC[:, d_, c, :], RC[:, 678 + 5 * d_:683 + 5 * d_], AF.Exp, [T + "rc", T + "lgs"], [T + "cfc"], scale=LGS[:, d_, c:c + 1])
                TT("dve", CFC[:, d_, c, :], CFC[:, d_, c, :], RC[:, 688 + 5 * d_:693 + 5 * d_], ALU.mult, [T + "cfc", T + "rc"], [T + "cfc"])
        ACTV(ZZ[:, 0, :], LG[:, 0:4], AF.Exp, [T + "lg", T + "rc"], [T + "zz"], scale=RC[:, 640:641])
        ACTV(ZZ[:, 1, :], LG[:, 4:8], AF.Exp, [T + "lg", T + "rc"], [T + "zz"], scale=RC[:, 641:642])
        TS("dve", ZZ, ZZ, 0.125, ALU.mult, [T + "zz"], [T + "zz"])
        if check_stop(f"rtparam_{l}"):
            A.pop(); A.pop(); A.pop(); break
        A.push()
        wrt = A.alloc([8, 1536], BF16)
        DMA("pool", wrt, winv[:, :, RT0:RT0 + 1536], [], [T + "w"], "wA")
        dests = [QT[:, 0, 0:TOK], QT[:, 1, 0:TOK], KT[:, 0, 0:TOK], KT[:, 1, 0:TOK]]
        proj_rope([wrt[:, :, c * 128:(c + 1) * 128] for c in range(4)], [wrt[:, :, 512 + c * 128:512 + (c + 1) * 128] for c in range(4)],
                  rope64[:, 0, :], rope64[:, 1, :], dests, T)
        for ci in range(4):
            dest = QT[:, ci, TOK:TOKC] if ci < 2 else KT[:, ci - 2, TOK:TOKC]
            proj_feat_plain(wrt[:, :, ci * 128:(ci + 1) * 128], dest, TOK, 256, T, f"c{ci}", ci % 2)

        def vg_post(i, pb):
            CP("act", VR[:, i, :], bank(pb, 256), [f"ps{pb}"], [T + f"v{i}"])
            ACTV(GT[:, i, :], bank(pb, 256, 256), AF.Silu, [f"ps{pb}"], [T + f"g{i}"])
        proj_tok(wrt[:, :, 1024:1536], 512, None, range(NTC), T, post=vg_post)
        A.pop()
        P.barrier()
        QK_R = [T + f"d{ci}" for ci in range(4)] + [T + f"dc{ci}" for ci in range(4)]
        if check_stop(f"rtproj_{l}"):
            A.pop(); A.pop(); A.pop(); break
        KTOK = A.alloc([NTC, 256], BF16)
        SZ = A.alloc([2, 18, 2, 64], F32)
        UCX = A.alloc([4, 2, 64], F32)
        SCX = A.alloc([2, 2, 64], F32)
        S0 = A.alloc([2, 2, 64], F32)
        SB = A.alloc([2, NTC, 2, 64], BF16)
        GR = A.alloc([4, 256], F32); EXPB = A.alloc([2, 2, 64], F32)
        for i in range(NTC):
            pb = i % 2
            for c in range(2):
                TR(bank_bf(pb)[:, c * 128:(c + 1) * 128], KT[:, c, i * 128:(i + 1) * 128], ident_bf, QK_R + ["ident_bf"], [f"ps{pb}"])
            CP("act", KTOK[:, i, :], bank_bf(pb)[:, 0:256], [f"ps{pb}"], [T + f"kt{i}"])
        vz = [A.alloc([2, 256], BF16) for _ in range(2)]
        MSET("pool", SZ[:, 0, 0], 0.0, [T + "sz"])
        MSET("pool", SZ[:, 1, 16], 0.0, [T + "sz"])

        def chunk_U(n, s2):
            for d_ in range(2):
                TT("dve" if d_ == 0 else "pool", vz[s2][:, d_].rearrange("p (h e) -> p h e", e=64), VR[:, n, :].rearrange("p (h e) -> p h e", e=64),
                   ZZ[:, d_, :].unsqueeze(2).to_broadcast([128, 4, 64]), ALU.mult, [T + f"v{n}", T + "zz"], [T + f"vz{s2}"])
            for d_ in range(2):
                for c in range(2):
                    MM(bank(2 + s2, 128, (d_ * 2 + c) * 128), KTOK[:, n, c * 128:(c + 1) * 128], vz[s2][:, d_, c * 128:(c + 1) * 128], True, True,
                       [T + f"kt{n}", T + f"vz{s2}"], [f"ps{2 + s2}"])

        udg = A.alloc([64], F32); udt = A.alloc([64], F32)

        def udiag(s2, d_, c, dst=None, dreg=None):
            blk = bank(2 + s2, 128, (d_ * 2 + c) * 128)
            o_ = udg if dst is None else dst
            TS("dve", udt, blk[:, 0:64], HM[:, 0:1], ALU.mult, [f"ps{2 + s2}", "HM"], [T + "udt"])
            STT("dve", o_, blk[:, 64:128], HM[:, 1:2], udt, ALU.mult, ALU.add, [f"ps{2 + s2}", "HM", T + "udt"], [T + "udg" if dreg is None else dreg])
            return o_

        for n in range(NT):
            chunk_U(n, n % 2)
            for c in range(2):
                u_ = udiag(n % 2, 0, c)
                STT("dve", SZ[:, 0, n + 1, c, :], SZ[:, 0, n, c, :], GC[:, 0, c:c + 1], u_, ALU.mult, ALU.add, [T + "sz", T + "gc", T + "udg"], [T + "sz"])
                udiag(n % 2, 1, c, dst=SZ[:, 1, n, c, :], dreg=T + "szb")
        for n in range(NT - 1, -1, -1):
            for c in range(2):
                STT("dve", SZ[:, 1, n, c, :], SZ[:, 1, n + 1, c, :], GC[:, 1, c:c + 1], SZ[:, 1, n, c, :], ALU.mult, ALU.add, [T + "sz", T + "szb", T + "gc"], [T + "sz", T + "szb"])
        for k_, n in enumerate((16, 17)):
            chunk_U(n, k_)
            for d_ in range(2):
                for c in range(2):
                    u_ = udiag(k_, d_, c)
                    CP("dve", UCX[:, 2 * d_ + k_, c, :], u_, [T + "udg"], [T + "ucx"])
        for c in range(2):
            STT("dve", SCX[:, 0, c, :], UCX[:, 0, c, :], GC[:, 0, c:c + 1], UCX[:, 1, c, :], ALU.mult, ALU.add, [T + "ucx", T + "gc"], [T + "scx"])
            STT("dve", SCX[:, 1, c, :], UCX[:, 3, c, :], GC[:, 1, c:c + 1], UCX[:, 2, c, :], ALU.mult, ALU.add, [T + "ucx", T + "gc"], [T + "scx"])
        CP("dve", EXPB[:, 0], SZ[:, 0, 16], [T + "sz"], [T + "expb"])
        CP("dve", EXPB[:, 1], SZ[:, 1, 0], [T + "sz"], [T + "expb"])
        e_r = dint(T + "e", [128, 256], F32); g_r = dint(T + "g", [512, 256], F32)
        DMA("sp", e_r.ap(), EXPB.rearrange("p d c e -> p (d c e)"), [T + "expb"], [T + "e"], "ex")
        AG(e_r, g_r, [T + "e"], [T + "g"], "cc")
        DMA("sp", GR, g_r.ap().rearrange("(r p) f -> p r f", p=128), [T + "g"], [T + "gr"], "kt")
        GRv = GR.rearrange("p r (d c e) -> p r d c e", d=2, c=2)
        for d_ in range(2):
            for c in range(2):
                TS("dve", S0[:, d_, c, :], SCX[:, d_, c, :], CFC[:, d_, c, 4:5], ALU.mult, [T + "scx", T + "cfc"], [T + "s0"])
                for r in range(4):
                    STT("dve", S0[:, d_, c, :], GRv[:, r, d_, c, :], CFC[:, d_, c, r:r + 1], S0[:, d_, c, :], ALU.mult, ALU.add, [T + "gr", T + "cfc", T + "s0"], [T + "s0"])
        for n in range(NT):
            for c in range(2):
                STT("dve", SB[:, 0, n, c, :], S0[:, 0, c, :], GPW[:, 0, c, n:n + 1], SZ[:, 0, n, c, :], ALU.mult, ALU.add, [T + "s0", T + "gpw", T + "sz"], [T + "sb"])
                STT("dve", SB[:, 1, n, c, :], S0[:, 1, c, :], GPW[:, 1, c, n:n + 1], SZ[:, 1, n + 1, c, :], ALU.mult, ALU.add, [T + "s0", T + "gpw", T + "sz"], [T + "sb"])
        MSET("pool", SB[:, 0, 16], 0.0, [T + "sb"])
        MSET("pool", SB[:, 1, 17], 0.0, [T + "sb"])
        CP("dve", SB[:, 0, 17], UCX[:, 0], [T + "ucx"], [T + "sb"])
        CP("dve", SB[:, 1, 16], UCX[:, 3], [T + "ucx"], [T + "sb"])
        if check_stop(f"rtA_{l}"):
            A.pop(); A.pop(); A.pop(); break
        AD = [A.alloc([4, 128], BF16) for _ in range(2)]
        QX = [A.alloc([2, 2, 2, 128], BF16) for _ in range(2)]
        qz = [A.alloc([2, 2, 128], BF16) for _ in range(2)]
        of_ = [A.alloc([4, 64], F32) for _ in range(2)]
        sq = A.alloc([4, 64], F32); ssr = A.alloc([4], F32); rsr = A.alloc([4], F32)
        yr_ = [A.alloc([4, 64], BF16) for _ in range(2)]
        def rt_A(i):
            s2 = i % 2
            ts = slice(i * 128, (i + 1) * 128)
            for hb_ in range(2):
                TS("pool" if hb_ else "dve", qz[s2][:, hb_], QT[:, :, ts], HM[:, hb_:hb_ + 1], ALU.mult, QK_R + ["HM"], [T + f"qz{s2}"])
            for h in range(4):
                c, hb_ = h // 2, h % 2
                MM(bank(s2, 128, h * 128), KT[:, c, ts], qz[s2][:, hb_, c, :], True, True, QK_R + [T + f"qz{s2}"], [f"ps{s2}"], sgc=True)

        def rt_mid(i):
            s2 = i % 2
            TT("dve", AD[s2], bank(s2).rearrange("p (h i) -> p h i", i=128), DEC, ALU.mult, [f"ps{s2}", T + "dec"], [T + f"ad{s2}"])
            for d_ in range(2):
                for hb_ in range(2):
                    TT("dve", QX[s2][:, d_, hb_], qz[s2][:, hb_], XI[:, d_], ALU.mult, [T + f"qz{s2}", T + "xi"], [T + f"qx{s2}"])

        def rt_out(i):
            s2 = i % 2
            pO = 4 + s2
            for h in range(4):
                c, hb_ = h // 2, h % 2
                o_ = bank(pO, 64, h * 64)
                MM(o_, AD[s2][:, h, :], VR[:, i, h * 64:(h + 1) * 64], h == 0, False, [T + f"ad{s2}", T + f"v{i}"], [f"ps{pO}"], sgc=True)
                MM(o_, QX[s2][:, 0, hb_, c, :], SB[:, 0, i, c, :], False, False, [T + f"qx{s2}", T + "sb"], [f"ps{pO}"], sgc=True)
                MM(o_, QX[s2][:, 1, hb_, c, :], SB[:, 1, i, c, :], False, True, [T + f"qx{s2}", T + "sb"], [f"ps{pO}"], sgc=True)

        def rt_fin(i):
            s2 = i % 2
            pO = 4 + s2
            ov = of_[s2]
            CP("act", ov, bank(pO, 256).rearrange("p (h e) -> p h e", e=64), [f"ps{pO}"], [T + f"of{s2}"])
            TT("dve", sq, ov, ov, ALU.mult, [T + f"of{s2}"], [T + "sq"])
            RED("dve", ssr, sq, [T + "sq"], [T + "ssr"])
            rstd_from_ss(ssr, 64, rsr, [T + "ssr", "epsc"], [T + "rsr"])
            TT("dve", sq, ov, rsr.unsqueeze(2).to_broadcast([128, 4, 64]), ALU.mult, [T + f"of{s2}", T + "rsr"], [T + "sq"])
            TT("pool", yr_[s2], sq, GT[:, i, :].rearrange("p (h e) -> p h e", e=64), ALU.mult, [T + "sq", T + f"g{i}"], [T + f"y{s2}"])

        def rt_tr(i):
            out_transposes(yr_[i % 2].rearrange("p h d -> p (h d)"), 6, i, T, [T + f"y{i % 2}"])

        rt_A(0)
        rt_mid(0)
        if nto > 1:
            rt_A(1)
        for i in range(nto):
            rt_out(i)
            if i + 1 < nto:
                rt_mid(i + 1)
            if i + 2 < nto:
                rt_A(i + 2)
            rt_fin(i)
            if i >= 1:
                rt_tr(i - 1)
        rt_tr(nto - 1)
        A.pop()
        A.pop()
        if "dbg_ot" in dbg and l == 0:
            DMA("sp", dbg["dbg_ot"], otd, [L + f"OT{c0}_{i}" for c0 in (0, 1, 2, 4, 6) for i in range(nto)], ["dbgot"], "dbg")
        if check_stop(f"rt_{l}") or (STOP_AFTER or "").startswith("rtB"):
            A.pop(); break

        P.barrier()
        A.push()
        h2T = hT
        wo = A.alloc([8, 1024], BF16)
        DMA("pool", wo, I[f"wout{l}"].rearrange("(kc p) n -> p kc n", p=128), [], [L + "wo"], "wA")
        if moe:
            RB = A.alloc([8, 1024], F32)
            DMA("sp", RB, pbc(I["router"]).rearrange("p (e d) -> p e d", d=1024), [], [L + "rb"], "m0")
            rj = [A.alloc([1024], F32) for _ in range(3)]; sm = A.alloc([8, 8], F32)
        xt = [A.alloc([1024], F32) for _ in range(2)]
        t1 = [A.alloc([1024], F32) for _ in range(2)]
        xm = [A.alloc([1024], F32) for _ in range(2)]
        hb = [A.alloc([1024], BF16) for _ in range(2)]
        ott = [A.alloc([8, 128], BF16) for _ in range(2)]
        ss3 = A.alloc([NTC, 2], F32); rs3 = A.alloc([NTC, 2], F32)
        xdst = (xs, xcs)

        def p3_mm(i):
            s2 = i % 2
            pb = 2 * s2
            ot_r = [L + f"OT{c0}_{i}" for c0 in (0, 1, 2, 4, 6)]
            DMA("sp", ott[s2], otd[i], ot_r, [L + f"ott{s2}"], f"ott{s2}")
            for hf in range(2):
                for kc in range(8):
                    MM(bank(pb + hf), ott[s2][:, kc, :], wo[:, kc, hf * 512:(hf + 1) * 512], kc == 0, kc == 7, [L + f"ott{s2}", L + "wo"], [f"ps{pb + hf}"])

        def p3_chain(i):
            s2 = i % 2
            v = 0 if i < NT else 1
            pb = 2 * s2
            yps = psum[:, 512 * pb:512 * pb + 1024]
            ACTV(junk, yps, AF.Square, [f"ps{pb}", f"ps{pb + 1}"], ["junk", L + f"s3_{i}"], accum=ss3[:, i, 0:1])
            rstd_from_ss(ss3[:, i, 0:1], 1024, rs3[:, i, 0:1], [L + f"s3_{i}", "epsc"], [L + f"r3_{i}"])
            DMA("sp", xt[s2], xtile_ap(xsrc, i), [], [L + f"p3xt{s2}"], f"xt{s2}")
            STT("dve", t1[s2], yps, rs3[:, i, 0:1], MOD[:, v, 2, :], ALU.mult, ALU.mult, [f"ps{pb}", f"ps{pb + 1}", L + f"r3_{i}", f"MOD{v}2"], [L + f"p3t1{s2}"])
            TT("dve", xm[s2], t1[s2], xt[s2], ALU.add, [L + f"p3t1{s2}", L + f"p3xt{s2}"], [L + f"xm{s2}"])
            DMA("sp", xtile_ap(xdst, i), xm[s2], [L + f"xm{s2}"], [L + f"xs{i}"], f"xst{s2}")
            ACTV(junk, xm[s2], AF.Square, [L + f"xm{s2}"], ["junk", L + f"s4_{i}"], accum=ss3[:, i, 1:2])
            rstd_from_ss(ss3[:, i, 1:2], 1024, rs3[:, i, 1:2], [L + f"s4_{i}", "epsc"], [L + f"r4_{i}"])
            STT("dve", t1[s2], xm[s2], rs3[:, i, 1:2], MOD[:, v, 4, :], ALU.mult, ALU.mult, [L + f"xm{s2}", L + f"r4_{i}", f"MOD{v}4"], [L + f"p3t1{s2}"])
            if moe:
                TT("dve", xt[s2], t1[s2], MOD[:, v, 3, :], ALU.add, [L + f"p3t1{s2}", f"MOD{v}3"], [L + f"p3xt{s2}"])
                CP("act", hb[s2], xt[s2], [L + f"p3xt{s2}"], [L + f"p3hb{s2}"])
                for e_ in range(8):
                    TT("dve", rj[e_ % 3], xt[s2], RB[:, e_, :], ALU.mult, [L + f"p3xt{s2}", L + "rb"], [L + f"rj{e_ % 3}"])
                    ACTV(junk, rj[e_ % 3], AF.Identity, [L + f"rj{e_ % 3}"], ["junk", L + f"logi{i}"], accum=LOGI[:, i, e_:e_ + 1])
                lg_ = LOGI[:, i, :]
                RED("dve", sm[:, 0, 0:1], lg_, [L + f"logi{i}"], [L + "sm"], mx=True)
                TS("dve", sm[:, 1, :], lg_, sm[:, 0, 0:1], ALU.is_equal, [L + f"logi{i}", L + "sm"], [L + "sm"])
                STT("dve", sm[:, 2, :], sm[:, 1, :], -1e30, lg_, ALU.mult, ALU.add, [L + "sm", L + f"logi{i}"], [L + "sm"])
                RED("dve", sm[:, 0, 1:2], sm[:, 2, :], [L + "sm"], [L + "sm"], mx=True)
                TS("dve", sm[:, 3, :], lg_, sm[:, 0, 1:2], ALU.is_ge, [L + f"logi{i}", L + "sm"], [L + "sm"])
                TS("dve", sm[:, 0, 2:3], sm[:, 0, 0:1], -1.0, ALU.mult, [L + "sm"], [L + "sm"])
                ACTV(sm[:, 4, :], lg_, AF.Exp, [L + f"logi{i}", L + "sm"], [L + "sm"], bias=sm[:, 0, 2:3])
                TT("dve", sm[:, 4, :], sm[:, 4, :], sm[:, 3, :], ALU.mult, [L + "sm"], [L + "sm"])
                RED("dve", sm[:, 0, 3:4], sm[:, 4, :], [L + "sm"], [L + "sm"])
                RECIP(sm[:, 0, 3:4], sm[:, 0, 3:4], [L + "sm"], [L + "sm"])
                TS("dve", GATES[:, i, :], sm[:, 4, :], sm[:, 0, 3:4], ALU.mult, [L + "sm"], [L + f"gates{i}"])
            else:
                TT("dve", hb[s2], t1[s2], MOD[:, v, 3, :], ALU.add, [L + f"p3t1{s2}", f"MOD{v}3"], [L + f"p3hb{s2}"])

        def p3_tr(i):
            s2 = i % 2
            ts = slice(i * 128, (i + 1) * 128)
            pt_ = 4 + s2
            for kc in range(8):
                TR(bank_bf(pt_)[:, kc * 128:(kc + 1) * 128], hb[s2][:, kc * 128:(kc + 1) * 128], ident_bf, [L + f"p3hb{s2}", "ident_bf"], [f"ps{pt_}"])
            CP("act", h2T[:, :, ts], bank_bf(pt_).rearrange("p (k t) -> p k t", t=128), [f"ps{pt_}"], [L + f"h2T{i}"])

        p3_mm(0)
        for i in range(nto):
            p3_chain(i)
            if i + 1 < nto:
                p3_mm(i + 1)
            p3_tr(i)
        H2_ALL = [L + f"h2T{i}" for i in range(nto)]
        A.pop()
        if check_stop(f"p3_{l}"):
            A.pop(); break

        P.barrier()
        Y = A.alloc([nto, 1024], F32)
        A.push()
        wg = [A.alloc([8, 256], BF16) for _ in range(2)]
        wu = [A.alloc([8, 256], BF16) for _ in range(2)]
        wd = [A.alloc([2, 1024], BF16) for _ in range(2)]
        sg = [A.alloc([512], BF16) for _ in range(2)]
        AT = [A.alloc([2, 512], BF16) for _ in range(2)]
        ntok = nto * 128
        tblocks = [(t0, min(512, ntok - t0)) for t0 in range(0, ntok, 512)]
        if moe:
            slabs = [(e_, s_) for e_ in range(8) for s_ in range(14)]
        else:
            slabs = [(None, s_) for s_ in range(11)]
        nmm = 0
        for si, (e_, s_) in enumerate(slabs):
            sl = si % 2
            if moe:
                gsrc = I["mwg"][e_].rearrange("(kc p) f -> p kc f", p=128)[:, :, s_ * 256:(s_ + 1) * 256]
                usrc = I["mwu"][e_].rearrange("(kc p) f -> p kc f", p=128)[:, :, s_ * 256:(s_ + 1) * 256]
                dsrc = I["mwd"][e_][s_ * 256:(s_ + 1) * 256, :].rearrange("(c p) n -> p c n", p=128)
            else:
                gsrc = I["fwg"].rearrange("(kc p) f -> p kc f", p=128)[:, :, s_ * 256:(s_ + 1) * 256]
                usrc = I["fwu"].rearrange("(kc p) f -> p kc f", p=128)[:, :, s_ * 256:(s_ + 1) * 256]
                dsrc = I["fwd"][s_ * 256:(s_ + 1) * 256, :].rearrange("(c p) n -> p c n", p=128)
            DMA("pool", wg[sl], gsrc, [], [L + f"wg{sl}"], f"fw{sl}")
            DMA("pool", wu[sl], usrc, [], [L + f"wu{sl}"], f"fw{sl}")
            DMA("pool", wd[sl], dsrc, [], [L + f"wd{sl}"], f"fw{sl}")
            for bi, (t0, nt_) in enumerate(tblocks):
                a2 = bi % 2
                for fcl in range(2):
                    pg = nmm % 2; nmm += 1
                    for kc in range(8):
                        MM(bank(pg, nt_), wg[sl][:, kc, fcl * 128:(fcl + 1) * 128], h2T[:, kc, t0:t0 + nt_], kc == 0, kc == 7, H2_ALL + [L + f"wg{sl}"], [f"ps{pg}"])
                    for kc in range(8):
                        MM(bank(2 + pg, nt_), wu[sl][:, kc, fcl * 128:(fcl + 1) * 128], h2T[:, kc, t0:t0 + nt_], kc == 0, kc == 7, H2_ALL + [L + f"wu{sl}"], [f"ps{2 + pg}"])
                    ACTV(sg[pg][:, 0:nt_], bank(pg, nt_), AF.Silu, [f"ps{pg}"], [L + f"sg{pg}"])
                    TT("dve", AT[a2][:, fcl, 0:nt_], sg[pg][:, 0:nt_], bank(2 + pg, nt_), ALU.mult, [L + f"sg{pg}", f"ps{2 + pg}"], [L + f"at{a2}_{fcl}"])
                for tt in range(nt_ // 128):
                    ti = t0 // 128 + tt
                    py = 4 + 2 * (ti % 2)
                    for hf in range(2):
                        for fcl in range(2):
                            MM(bank(py + hf), AT[a2][:, fcl, tt * 128:(tt + 1) * 128], wd[sl][:, fcl, hf * 512:(hf + 1) * 512], fcl == 0, fcl == 1,
                               [L + f"at{a2}_0", L + f"at{a2}_1", L + f"wd{sl}"], [f"ps{py + hf}"])
                    yps = psum[:, 512 * py:512 * py + 1024]
                    rr = [f"ps{py}", f"ps{py + 1}"]
                    if moe:
                        gsc = GATES[:, ti, e_:e_ + 1]
                        if si == 0:
                            TS("dve", Y[:, ti, :], yps, gsc, ALU.mult, rr + [L + f"gates{ti}"], [L + f"Y{ti}"])
                        else:
                            STT("dve", Y[:, ti, :], yps, gsc, Y[:, ti, :], ALU.mult, ALU.add, rr + [L + f"gates{ti}", L + f"Y{ti}"], [L + f"Y{ti}"])
                    else:
                        if si == 0:
                            CP("dve", Y[:, ti, :], yps, rr, [L + f"Y{ti}"])
                        else:
                            TT("dve", Y[:, ti, :], yps, Y[:, ti, :], ALU.add, rr + [L + f"Y{ti}"], [L + f"Y{ti}"])
        A.pop()
        if check_stop(f"p4_{l}"):
            A.pop(); break

        A.push()
        ss5 = A.alloc([NTC], F32); rs5 = A.alloc([NTC], F32)
        xt = [A.alloc([1024], F32) for _ in range(2)]
        t1 = [A.alloc([1024], F32) for _ in range(2)]
        xo = [A.alloc([1024], F32) for _ in range(2)]
        for i in range(nto):
            s2 = i % 2
            v = 0 if i < NT else 1
            ACTV(junk, Y[:, i, :], AF.Square, [L + f"Y{i}"], ["junk", L + f"s5_{i}"], accum=ss5[:, i:i + 1])
            rstd_from_ss(ss5[:, i:i + 1], 1024, rs5[:, i:i + 1], [L + f"s5_{i}", "epsc"], [L + f"r5_{i}"])
            DMA("sp", xt[s2], xtile_ap(xdst, i), [L + f"xs{i}"], [L + f"p5xt{s2}"], f"xt{s2}")
            STT("dve", t1[s2], Y[:, i, :], rs5[:, i:i + 1], MOD[:, v, 5, :], ALU.mult, ALU.mult, [L + f"Y{i}", L + f"r5_{i}", f"MOD{v}5"], [L + f"p5t1{s2}"])
            TT("dve", xo[s2], t1[s2], xt[s2], ALU.add, [L + f"p5t1{s2}", L + f"p5xt{s2}"], [L + f"xo{s2}"])
            if l == 0:
                DMA("sp", xtile_ap(xdst, i), xo[s2], [L + f"xo{s2}"], [L + f"xs{i}"], f"xst{s2}")
                if "dbg_x" in dbg:
                    dd = dbg["dbg_x"][i * 128:(i + 1) * 128, :] if i < NT else dbg["dbg_xc"][(i - NT) * 128:(i - NT + 1) * 128, :]
                    DMA("sp", dd, xo[s2], [L + f"xo{s2}"], [L + f"dbgx{i}"], "dbg")
            else:
                DMA("sp", out[i * 128:(i + 1) * 128, :], xo[s2], [L + f"xo{s2}"], [f"out{i}"], f"xst{s2}")
        A.pop()
        A.pop()
        if check_stop(f"l{l}"):
            break
    return nc, P, es, A, I


def _emit(nc, P, es):
    tls = P.finalize()
    sems = {tl: es.enter_context(nc.semaphore("s_" + str(tl))) for tl in tls}
    with nc.Block() as block:
        block.sync(P.engine_body("sp", sems, final=True))
        block.tensor(P.engine_body("pe", sems))
        block.vector(P.engine_body("dve", sems))
        block.scalar(P.engine_body("act", sems))
        block.gpsimd(P.engine_body("pool", sems))


_CACHE = {}


def _get_program():
    if "nc" not in _CACHE:
        nc, P, es, A, I = build_program()
        _CACHE["inputs"] = list(I.keys())
        with es:
            _emit(nc, P, es)
        _CACHE["nc"] = nc
        _CACHE["peak"] = A.peak
        _CACHE["nops"] = len(P.ops)
    return _CACHE["nc"]


def _host_inputs(inp):
    f = lambda a: np.ascontiguousarray(np.asarray(a, dtype=np.float32))
    shared = {}
    for l in range(2):
        shared[f"wmod{l}"] = f(inp["w_mod"][l])
        shared[f"bmod{l}"] = f(inp["b_mod"][l]).reshape(1, 6144)
        shared[f"gvec{l}"] = f(np.concatenate([inp["g_attn_pre"][l], inp["g_attn_post"][l], inp["g_ffn_pre"][l], inp["g_ffn_post"][l]])).reshape(1, 4096)
        shared[f"win{l}"] = f(np.asarray(inp["w_in"][l])[:, WIN_PERM])
        shared[f"wout{l}"] = f(inp["w_out"][l])
        shared[f"dal{l}"] = f(np.concatenate([inp["da_lambda_q1"][l], inp["da_lambda_k1"][l], inp["da_lambda_q2"][l], inp["da_lambda_k2"][l]])).reshape(1, 128)
        shared[f"subln{l}"] = f(inp["da_subln"][l]).reshape(1, 64)
        shared[f"sink{l}"] = f(inp["swa_sink"][l]).reshape(1, 4)
        shared[f"gam{l}"] = f(np.concatenate([inp["ret_gamma_fwd"][l], inp["ret_gamma_bwd"][l]])).reshape(1, 8)
        shared[f"nabias{l}"] = _na_bias_layout(np.asarray(inp["na_rpb"][l], dtype=np.float32))
    shared["fwg"] = f(inp["ffn_w_gate"][0]); shared["fwu"] = f(inp["ffn_w_up"][0]); shared["fwd"] = f(inp["ffn_w_down"][0])
    shared["router"] = f(np.asarray(inp["moe_router"][0]).T).reshape(1, 8 * 1024)
    shared["mwg"] = f(inp["moe_w_gate"][0]); shared["mwu"] = f(inp["moe_w_up"][0]); shared["mwd"] = f(inp["moe_w_down"][0])
    shared["idxb"] = _idxb_table()
    x = np.asarray(inp["x"], dtype=np.float32); ctx = np.asarray(inp["ctx"], dtype=np.float32)
    c = np.asarray(inp["c"], dtype=np.float32); c_ctx = np.asarray(inp["c_ctx"], dtype=np.float32)
    maps = []
    for core in range(8):
        b, j = core // 4, core % 4
        m = dict(shared)
        m["xin"] = np.ascontiguousarray(x[b, TOK * j:TOK * (j + 1)])
        m["xcin"] = np.ascontiguousarray(ctx[b])
        m["cvec"] = np.ascontiguousarray(np.concatenate([c[b].reshape(8, 128).T, c_ctx.reshape(8, 128).T], axis=1))
        C64, S64 = _rope_tables(j, 64)
        C32, S32 = _rope_tables(j, 32)
        m["rope64"] = np.ascontiguousarray(np.stack([C64, S64], axis=1))
        m["rope32"] = np.ascontiguousarray(np.stack([C32, S32], axis=1))
        m["swamask"] = _swa_masks(j)
        m["namask"] = _na_masks(j)
        m["retc"] = _ret_consts(j)
        if "inputs" in _CACHE:
            m = {k: v for k, v in m.items() if k in _CACHE["inputs"]}
        maps.append(m)
    return maps


def kernel(**inputs):
    nc = _get_program()
    maps = _host_inputs(inputs)
    res = run_bass_kernel_spmd(nc, maps, core_ids=list(range(8)))
    _CACHE["last"] = res
    outp = np.empty((2, 8192, 1024), np.float32)
    for core in range(8):
        b, j = core // 4, core % 4
        outp[b, TOK * j:TOK * (j + 1)] = res.results[core]["out"]
    return outp
```

```python
import contextlib
import os
import math
import numpy as np
import concourse.bass as bass
import concourse.mybir as mybir
from concourse.bass_utils import run_bass_kernel_spmd

F32 = mybir.dt.float32
BF16 = mybir.dt.bfloat16
AF = mybir.ActivationFunctionType
ALU = mybir.AluOpType
AX = mybir.AxisListType
ENGS = ("pe", "act", "dve", "pool", "sp")
EPS = 1e-6
NT = 16
NTC = 18
TOK = 2048
TOKC = 2304
DEBUG = []
STOP_AFTER = None


class Op:
    __slots__ = ("eng", "fn", "tl", "deps", "awaited", "count", "inc", "idx")


class Prog:
    def __init__(self):
        self.ops = []
        self.last_w = {}
        self.readers = {}
        self.tl_last = {}
        self.bar = set()
        self.bar_done = set(ENGS)

    def op(self, eng, fn, reads=(), writes=(), tl=None, inc=1):
        o = Op()
        o.eng = eng
        o.fn = fn
        o.tl = tl if tl is not None else eng
        o.inc = inc
        o.awaited = o.tl not in ENGS
        o.count = None
        o.idx = len(self.ops)
        deps = set()
        for r in reads:
            w = self.last_w.get(r)
            if w is not None:
                deps.add(w)
        for w_ in writes:
            w = self.last_w.get(w_)
            if w is not None:
                deps.add(w)
            rl = self.readers.get(w_)
            if rl:
                deps.update(rl)
        if eng not in self.bar_done:
            deps |= self.bar
            self.bar_done.add(eng)
        o.deps = deps
        self.ops.append(o)
        for r in reads:
            self.readers.setdefault(r, []).append(o.idx)
        for w_ in writes:
            self.last_w[w_] = o.idx
            self.readers[w_] = []
        self.tl_last[o.tl] = o.idx
        return o

    def barrier(self):
        self.bar = set(self.tl_last.values())
        self.bar_done = set()

    def finalize(self):
        ops = self.ops
        for i in self.tl_last.values():
            ops[i].awaited = True
        for o in ops:
            for d in o.deps:
                od = ops[d]
                if od.tl == "pe" and o.tl == "pe":
                    continue
                od.awaited = True
        cnt = {}
        for o in ops:
            if o.awaited:
                cnt[o.tl] = cnt.get(o.tl, 0) + o.inc
                o.count = cnt[o.tl]
        self.totals = cnt
        run_latest = {}
        self.need = [None] * len(ops)
        for o in ops:
            need = {}
            for d in o.deps:
                od = ops[d]
                if od.tl == "pe" and o.tl == "pe":
                    continue
                v = od.count if od.tl in ENGS else run_latest[od.tl]
                if need.get(od.tl, 0) < v:
                    need[od.tl] = v
            self.need[o.idx] = need
            if o.awaited:
                run_latest[o.tl] = o.count
        return sorted(cnt.keys(), key=str)

    def engine_body(self, ename, sems, final=False):
        mine = [o for o in self.ops if o.eng == ename]

        def body(e):
            waited = {}
            for o in mine:
                for tl, v in self.need[o.idx].items():
                    if waited.get(tl, 0) < v:
                        e.wait_ge(sems[tl], v)
                        waited[tl] = v
                ins = o.fn(e)
                if o.awaited:
                    ins.then_inc(sems[o.tl], o.inc)
            if final:
                for tl, v in self.totals.items():
                    if waited.get(tl, 0) < v:
                        e.wait_ge(sems[tl], v)
        return body


class Arena:
    def __init__(self, ap, nbytes, prog=None):
        self.prog = prog
        self.ap = ap
        self.cap = nbytes
        self.off = 0
        self.stack = []
        self.peak = 0

    def alloc(self, shape, dt):
        shape = list(shape)
        n = int(np.prod(shape))
        nb = n * (4 if dt == F32 else 2)
        nb = (nb + 63) // 64 * 64
        assert self.off + nb <= self.cap, f"SBUF arena overflow {self.off}+{nb}>{self.cap}"
        v = self.ap[:, self.off // 2:(self.off + nb) // 2]
        if dt == F32:
            v = v.bitcast(F32)
        v = v[:, 0:n]
        self.off += nb
        self.peak = max(self.peak, self.off)
        if len(shape) == 2:
            v = v.rearrange("p (a b) -> p a b", b=shape[1])
        elif len(shape) == 3:
            v = v.rearrange("p (a b c) -> p a b c", b=shape[1], c=shape[2])
        elif len(shape) == 4:
            v = v.rearrange("p (a b c d) -> p a b c d", b=shape[1], c=shape[2], d=shape[3])
        return v

    def push(self):
        self.stack.append(self.off)

    def pop(self):
        self.off = self.stack.pop()
        if self.prog is not None:
            self.prog.barrier()


def _swap_idx(dh):
    q = dh // 4
    return np.concatenate([np.arange(q, 2 * q), np.arange(0, q), np.arange(3 * q, 4 * q), np.arange(2 * q, 3 * q)])


def _win_perm():
    cols = []
    base = 0
    q = np.arange(base, base + 256)
    k = np.arange(base + 256, base + 512)
    v = np.arange(base + 512, base + 768)
    sw32 = np.concatenate([_swap_idx(32) + 32 * i for i in range(8)])
    cols += [q, k, q[sw32], k[sw32], v]
    base = 768
    qn = np.arange(base, base + 256).reshape(2, 2, 64)
    qperm = np.transpose(qn, (1, 0, 2)).reshape(256)
    kk = np.arange(base + 256, base + 384)
    vv = np.arange(base + 384, base + 512)
    sw64_4 = np.concatenate([_swap_idx(64) + 64 * i for i in range(4)])
    sw64_2 = np.concatenate([_swap_idx(64) + 64 * i for i in range(2)])
    cols += [qperm, kk, qperm[sw64_4], kk[sw64_2], vv]
    base = 1280
    cols += [np.arange(base, base + 768)]
    base = 2048
    q = np.arange(base, base + 256)
    k = np.arange(base + 256, base + 512)
    vg = np.arange(base + 512, base + 1024)
    cols += [q, k, q[sw64_4], k[sw64_4], vg]
    return np.concatenate(cols)


WIN_PERM = _win_perm()
NWIN = len(WIN_PERM)
DA0, SW0, NA0, RT0 = 0, 1280, 2176, 2944


def _rope_tables(j, dh):
    t = 2048 * j + np.arange(2048)
    row = (t // 64).astype(np.float64)
    col = (t % 64).astype(np.float64)
    half = dh // 2
    qd = dh // 4
    inv = 10000.0 ** (-np.arange(qd, dtype=np.float64) * 2.0 / half)
    C = np.zeros((128, 2048), np.float32)
    S = np.zeros((128, 2048), np.float32)
    for p in range(128):
        d = p % dh
        pos = row if d < half else col
        dd = d % half
        i = dd % qd
        ang = pos * inv[i]
        C[p] = np.cos(ang)
        S[p] = -np.sin(ang) if dd < qd else np.sin(ang)
    return C, S


def _swa_masks(j):
    kk = np.arange(128)[:, None]
    qq = np.arange(128)[None, :]
    mprev = (qq <= kk).astype(np.float32)
    mnext = (kk <= qq).astype(np.float32)
    m = np.zeros((10, 128, 128), np.float32)
    m[0] = mprev
    m[1] = mnext
    for r in range(4):
        if r == j - 1:
            m[2 + r] = mprev
        if r == j + 1:
            m[6 + r] = mnext
    return m


def _na_mask(Tq, Tk, flag=True):
    if (not flag) or Tk < 0 or Tk > 63:
        return np.zeros((128, 128), np.float32)
    p = np.arange(128)
    Rk = (2 * Tk + p // 64)[:, None]
    kc = (p % 64)[:, None]
    Rq = (2 * Tq + p // 64)[None, :]
    qc = (p % 64)[None, :]
    start = np.clip(Rq - 4, 0, 120)
    cs = np.clip(qc - 8, 0, 48)
    ok = (Rk >= start) & (Rk < start + 8) & (kc >= cs) & (kc < cs + 16)
    return ok.astype(np.float32)


def _na_masks(j):
    m = np.zeros((45, 128, 128), np.float32)
    for d in range(-2, 3):
        m[d + 2] = _na_mask(10, 10 + d)
    T0 = 16 * j
    idx = 5
    for d in (0, 1, 2, 3):
        m[idx] = _na_mask(T0, T0 + d); idx += 1
    for r in range(4):
        m[idx] = _na_mask(T0, T0 - 2, r == j - 1); idx += 1
    for r in range(4):
        m[idx] = _na_mask(T0, T0 - 1, r == j - 1); idx += 1
    for d in (-1, 0, 1, 2):
        m[idx] = _na_mask(T0 + 1, T0 + 1 + d); idx += 1
    for r in range(4):
        m[idx] = _na_mask(T0 + 1, T0 - 1, r == j - 1); idx += 1
    for d in (-2, -1, 0, 1):
        m[idx] = _na_mask(T0 + 14, T0 + 14 + d); idx += 1
    for r in range(4):
        m[idx] = _na_mask(T0 + 14, T0 + 16, r == j + 1); idx += 1
    for d in (-3, -2, -1, 0):
        m[idx] = _na_mask(T0 + 15, T0 + 15 + d); idx += 1
    for r in range(4):
        m[idx] = _na_mask(T0 + 15, T0 + 16, r == j + 1); idx += 1
    for r in range(4):
        m[idx] = _na_mask(T0 + 15, T0 + 17, r == j + 1); idx += 1
    assert idx == 45
    return m


def _na_bias_layout(rpb):
    p = np.arange(128)
    kr = (p // 64)[:, None]; kc = (p % 64)[:, None]
    qr = (p // 64)[None, :]; qc = (p % 64)[None, :]
    out = np.empty((7, 4, 128, 128), np.float32)
    dc = np.clip(kc - qc, -15, 15) + 15
    for di, d in enumerate(range(-3, 4)):
        dr = np.clip(2 * d + kr - qr, -7, 7) + 7
        out[di] = rpb[:, dr, dc]
    return out


def _ret_consts(j):
    c = np.zeros((128, 700), np.float32)
    i = np.arange(128)
    o = 0
    dif = i[None, :] - i[:, None]
    c[:, 0:128] = np.maximum(dif, 0)
    c[:, 128:256] = (dif >= 0) * 0.125
    c[:, 256:384] = np.maximum(-dif, 0)
    c[:, 384:512] = (dif < 0) * 0.125
    c[:, 512:640] = (i + 1)[None, :]
    c[:, 640] = 127 - i
    c[:, 641] = i
    c[:, 642:660] = (128.0 * np.arange(18))[None, :]
    c[:, 660:678] = (128.0 * (15 - np.arange(18)))[None, :]
    for r in range(4):
        if r < j:
            c[:, 678 + r] = 2048.0 * (j - 1 - r); c[:, 688 + r] = 1.0
        if r > j:
            c[:, 683 + r] = 2048.0 * (r - j - 1); c[:, 693 + r] = 1.0
    c[:, 682] = 2048.0 * j; c[:, 692] = 1.0
    c[:, 687] = 2048.0 * (3 - j); c[:, 697] = 1.0
    return c


def _idxb_table():
    i = np.arange(128)
    return np.broadcast_to((128 - i)[None, :], (128, 128)).astype(np.float32).copy()


def build_program():
    nc = bass.Bass("TRN2", target_bir_lowering=False)
    P = Prog()
    es = contextlib.ExitStack()

    def din(name, shape, dt=F32):
        return nc.dram_tensor(name, list(shape), dt, kind="ExternalInput").ap()

    def dint(name, shape, dt):
        return nc.dram_tensor(name, list(shape), dt)

    SHAPES = {"xin": [TOK, 1024], "xcin": [256, 1024], "cvec": [128, 16], "fwg": [1024, 2816], "fwu": [1024, 2816], "fwd": [2816, 1024],
              "router": [1, 8 * 1024], "mwg": [8, 1024, 3584], "mwu": [8, 1024, 3584], "mwd": [8, 3584, 1024],
              "rope64": [128, 2, 2048], "rope32": [128, 2, 2048], "swamask": [10, 128, 128], "namask": [45, 128, 128],
              "retc": [128, 700], "idxb": [128, 128]}
    for l_ in range(2):
        SHAPES.update({f"wmod{l_}": [1024, 6144], f"bmod{l_}": [1, 6144], f"gvec{l_}": [1, 4096], f"win{l_}": [1024, NWIN],
                       f"wout{l_}": [1024, 1024], f"dal{l_}": [1, 128], f"subln{l_}": [1, 64], f"sink{l_}": [1, 4],
                       f"gam{l_}": [1, 8], f"nabias{l_}": [7, 4, 128, 128]})

    class LazyIn(dict):
        def __missing__(self, k):
            self[k] = din(k, SHAPES[k])
            return self[k]
    I = LazyIn()
    USED_INPUTS = I
    out = nc.dram_tensor("out", [TOK, 1024], F32, kind="ExternalOutput").ap()
    dbg = {}
    for name, shape, dt in (("dbg_ot", [NTC, 128, 8, 128], BF16), ("dbg_x", [TOK, 1024], F32), ("dbg_xc", [256, 1024], F32),
                            ("dbg_misc", [128, 4096], F32)):
        if name in DEBUG:
            dbg[name] = nc.dram_tensor(name, shape, dt, kind="ExternalOutput").ap()

    xs = dint("xs", [TOK, 1024], F32).ap(); xcs = dint("xcs", [256, 1024], F32).ap()
    GROUPS = [[0, 1, 2, 3], [4, 5, 6, 7]]

    arena_t = es.enter_context(nc.sbuf_tensor("arena", [128, 94 * 1024], BF16))
    A = Arena(arena_t, 188 * 1024, P)
    psum = es.enter_context(nc.psum_tensor("psum", [128, 4096], F32))

    def bank(i, n=512, off=0):
        return psum[:, 512 * i + off:512 * i + off + n]

    def bank_bf(i):
        return psum[:, 512 * i:512 * (i + 1)].bitcast(BF16)

    def MM(o, lhsT, rhs, st, sp_, r, w, tp=None, sgc=False):
        kw = {}
        if tp is not None:
            kw["tile_position"] = tp
        if sgc:
            kw["skip_group_check"] = True
        P.op("pe", lambda e: e.matmul(o, lhsT=lhsT, rhs=rhs, start=st, stop=sp_, **kw), r, w)

    def MM64(o, lhsT, rhs, base, st, sp_, r, w):
        if base == 0:
            MM(o, lhsT[0:64], rhs[0:64], st, sp_, r, w, sgc=True)
        else:
            MM(o, lhsT[64:96], rhs[64:96], st, False, r, w, tp=(64, 0), sgc=True)
            MM(o, lhsT[96:128], rhs[96:128], False, sp_, r, w, tp=(96, 0), sgc=True)

    def TR(o, i, ident, r, w):
        P.op("pe", lambda e: e.transpose(o, i, ident), r, w)

    def ACTV(o, i, func, r, w, bias=None, scale=None, accum=None):
        kw = {}
        if bias is not None:
            kw["bias"] = bias
        if scale is not None:
            kw["scale"] = scale
        if accum is not None:
            kw["accum_out"] = accum
        P.op("act", lambda e: e.activation(out=o, in_=i, func=func, **kw), r, w)

    def TT(eng, o, a, b, op, r, w):
        P.op(eng, lambda e: e.tensor_tensor(out=o, in0=a, in1=b, op=op), r, w)

    def TS(eng, o, a, s1, op0, r, w, s2=None, op1=None):
        if op1 is None:
            P.op(eng, lambda e: e.tensor_scalar(out=o, in0=a, scalar1=s1, scalar2=None, op0=op0), r, w)
        else:
            P.op(eng, lambda e: e.tensor_scalar(out=o, in0=a, scalar1=s1, scalar2=s2, op0=op0, op1=op1), r, w)

    def STT(eng, o, a, s, b, op0, op1, r, w):
        P.op(eng, lambda e: e.scalar_tensor_tensor(out=o, in0=a, scalar=s, in1=b, op0=op0, op1=op1), r, w)

    def CP(eng, o, i, r, w):
        if eng == "act":
            P.op("act", lambda e: e.copy(out=o, in_=i), r, w)
        else:
            P.op(eng, lambda e: e.tensor_copy(out=o, in_=i), r, w)

    def MSET(eng, o, val, w):
        P.op(eng, lambda e: e.memset(o, val), (), w)

    def RED(eng, o, i, r, w, mx=False):
        if mx:
            P.op(eng, lambda e: e.reduce_max(out=o, in_=i, axis=AX.X), r, w)
        else:
            P.op(eng, lambda e: e.reduce_sum(out=o, in_=i, axis=AX.X), r, w)

    def RECIP(o, i, r, w):
        P.op("dve", lambda e: e.reciprocal(out=o, in_=i), r, w)

    def DMA(q, o, i, r, w, tl):
        P.op(q, lambda e: e.dma_start(out=o, in_=i), r, w, tl=tl, inc=16)

    def AG(src, dst, r, w, tl):
        P.op("pool", lambda e: e.collective_compute("AllGather", ALU.bypass, replica_groups=GROUPS,
                                                    ins=[src.ap().opt()], outs=[dst.ap().opt()]), r, w, tl=tl, inc=1)

    def rstd_from_ss(ss, n, rstd, r, w):
        ACTV(rstd, ss, AF.Sqrt, r, w, bias=epsc[:, 0:1], scale=1.0 / n)
        RECIP(rstd, rstd, w, w)

    ident_bf = A.alloc([128], BF16); ident_f = A.alloc([128], F32); zeros = A.alloc([128], BF16)
    epsc = A.alloc([1], F32)
    junk = A.alloc([1024], BF16)
    MOD = A.alloc([2, 6, 1024], BF16)
    MSET("pool", ident_f, 0.0, ["ident_f"])
    P.op("pool", lambda e: e.affine_select(out=ident_f, in_=ident_f, pattern=[[-1, 128]], compare_op=ALU.not_equal,
                                           fill=1.0, base=0, channel_multiplier=1), ["ident_f"], ["ident_f"])
    CP("pool", ident_bf, ident_f, ["ident_f"], ["ident_bf"])
    HM = A.alloc([2], F32)
    RED("dve", HM[:, 0:1], ident_f[:, 0:64], ["ident_f"], ["HM"])
    RED("dve", HM[:, 1:2], ident_f[:, 64:128], ["ident_f"], ["HM"])
    MSET("pool", zeros, 0.0, ["zeros"])
    MSET("pool", epsc, EPS, ["epsc"])

    def pbc(ap):
        b = ap.partition_broadcast(128)
        if len(b.shape) == 3 and b.shape[1] == 1:
            b = b[:, 0]
        return b

    stop = [False]

    def tap(name, ap, reads, flat):
        if name in DEBUG:
            shp = [128, int(np.prod(ap.shape[1:]))]
            d = nc.dram_tensor(name, shp, ap.dtype, kind="ExternalOutput").ap()
            DMA("sp", d, ap.rearrange(flat) if flat else ap, reads, [name], "dbg")

    def check_stop(name):
        if STOP_AFTER == name:
            stop[0] = True
        return stop[0]

    for l in range(2):
        if stop[0]:
            break
        with_ctx = (l == 0)
        lam_init = 0.8 - 0.6 * math.exp(-0.3 * l)
        ntl = NTC
        nto = NTC if with_ctx else NT
        xsrc = (I["xin"], I["xcin"]) if l == 0 else (xs, xcs)
        L = f"L{l}"

        def xtile_ap(src2, i):
            return src2[0][i * 128:(i + 1) * 128, :] if i < NT else src2[1][(i - NT) * 128:(i - NT + 1) * 128, :]

        P.barrier()
        A.push()
        cv = A.alloc([16], F32); sil = A.alloc([16], F32); sbc = A.alloc([2, 8, 128], BF16)
        gv = A.alloc([4, 1024], F32); tmpm = A.alloc([1024], F32)
        wm = [A.alloc([8, 1024], BF16) for _ in range(2)]
        bs = [A.alloc([1024], F32) for _ in range(2)]
        DMA("sp", cv, I["cvec"], [], [L + "cv"], "m0")
        DMA("sp", gv, pbc(I[f"gvec{l}"]).rearrange("p (a b) -> p a b", b=1024), [], [L + "gv"], "m0")
        ACTV(sil, cv, AF.Silu, [L + "cv"], [L + "sil"])
        for v in range(2):
            for kc in range(8):
                ACTV(sbc[:, v, kc, :], zeros, AF.Identity, [L + "sil", "zeros"], [L + "sbc"], bias=sil[:, v * 8 + kc:v * 8 + kc + 1])
        wmv = I[f"wmod{l}"].rearrange("(kc p) n -> p kc n", p=128)
        for s in range(6):
            sl = s % 2
            DMA("pool", wm[sl], wmv[:, :, s * 1024:(s + 1) * 1024], [], [L + f"wm{sl}"], f"wm{sl}")
            DMA("sp", bs[sl], pbc(I[f"bmod{l}"][0:1, s * 1024:(s + 1) * 1024]), [], [L + f"bs{sl}"], f"bs{sl}")
            for v in range(2):
                pb = 4 * (s % 2) + 2 * v
                for hf in range(2):
                    for kc in range(8):
                        MM(bank(pb + hf), sbc[:, v, kc, :], wm[sl][:, kc, hf * 512:(hf + 1) * 512], kc == 0, kc == 7,
                           [L + "sbc", L + f"wm{sl}"], [f"ps{pb + hf}"])
                TT("dve", tmpm, psum[:, 512 * pb:512 * pb + 1024], bs[sl], ALU.add, [f"ps{pb}", f"ps{pb + 1}", L + f"bs{sl}"], [L + "tmpm"])
                dst = MOD[:, v, s, :]
                if s in (0, 3):
                    CP("dve", dst, tmpm, [L + "tmpm"], [f"MOD{v}{s}"])
                elif s in (1, 4):
                    STT("dve", dst, tmpm, 1.0, gv[:, 0 if s == 1 else 2, :], ALU.add, ALU.mult, [L + "tmpm", L + "gv"], [f"MOD{v}{s}"])
                else:
                    TT("dve", dst, tmpm, gv[:, 1 if s == 2 else 3, :], ALU.mult, [L + "tmpm", L + "gv"], [f"MOD{v}{s}"])
        if l == 0:
            tap("t_mod", MOD, [f"MOD{v}{s}" for v in range(2) for s in range(6)], "p a b c -> p (a b c)")
        A.pop()
        if check_stop(f"p0_{l}"):
            break

        P.barrier()
        A.push()
        moe = (l == 1)
        if moe:
            LOGI = A.alloc([NT, 8], F32); GATES = A.alloc([NT, 8], F32)
        hT = A.alloc([8, TOKC], BF16)
        otd = dint(L + "otd", [NTC, 128, 8, 128], BF16).ap()
        A.push()
        otst = [A.alloc([2, 128], BF16) for _ in range(2)]

        A.push()
        xt = [A.alloc([1024], F32) for _ in range(2)]
        t1 = [A.alloc([1024], F32) for _ in range(2)]
        hb = [A.alloc([1024], BF16) for _ in range(2)]
        ssb = A.alloc([NTC], F32); rsb = A.alloc([NTC], F32)
        def p1a_A(i):
            s2 = i % 2
            DMA("sp", xt[s2], xtile_ap(xsrc, i), [], [L + f"xt{s2}"], f"xt{s2}")
            ACTV(junk, xt[s2], AF.Square, [L + f"xt{s2}"], ["junk", L + f"ss{i}"], accum=ssb[:, i:i + 1])
            rstd_from_ss(ssb[:, i:i + 1], 1024, rsb[:, i:i + 1], [L + f"ss{i}", "epsc"], [L + f"rs{i}"])

        def p1a_B(i):
            s2 = i % 2
            v = 0 if i < NT else 1
            STT("dve", t1[s2], xt[s2], rsb[:, i:i + 1], MOD[:, v, 1, :], ALU.mult, ALU.mult, [L + f"xt{s2}", L + f"rs{i}", f"MOD{v}1"], [L + f"t1{s2}"])
            TT("dve", hb[s2], t1[s2], MOD[:, v, 0, :], ALU.add, [L + f"t1{s2}", f"MOD{v}0"], [L + f"hb{s2}"])
            for kc in range(8):
                TR(bank_bf(s2)[:, kc * 128:(kc + 1) * 128], hb[s2][:, kc * 128:(kc + 1) * 128], ident_bf, [L + f"hb{s2}", "ident_bf"], [f"ps{s2}"])

        def p1a_C(i):
            s2 = i % 2
            CP("act", hT[:, :, i * 128:(i + 1) * 128], bank_bf(s2).rearrange("p (k t) -> p k t", t=128), [f"ps{s2}"], [L + f"hT{i}"])

        p1a_A(0)
        for i in range(ntl):
            p1a_B(i)
            if i + 1 < ntl:
                p1a_A(i + 1)
            p1a_C(i)
        A.pop()
        HT_ALL = [L + f"hT{i}" for i in range(ntl)]
        if l == 0:
            tap("t_hT", hT, HT_ALL, "p a b -> p (a b)")
        if check_stop(f"p1a_{l}"):
            A.pop(); A.pop(); break

        winv = I[f"win{l}"].rearrange("(kc p) n -> p kc n", p=128)

        def proj_rope(wq, wqs, Ctab, Stab, dests, tag, name0=0):
            tA = [A.alloc([512], F32) for _ in range(2)]
            tB = [A.alloc([512], F32) for _ in range(2)]
            n = 0
            for ci in range(len(wq)):
                for tb in range(4):
                    s2 = n % 2; n += 1
                    ts = slice(tb * 512, (tb + 1) * 512)
                    for kc in range(8):
                        MM(bank(s2), wq[ci][:, kc, :], hT[:, kc, ts], kc == 0, kc == 7, HT_ALL[4 * tb:4 * tb + 4] + [tag + "w"], [f"ps{s2}"])
                    for kc in range(8):
                        MM(bank(2 + s2), wqs[ci][:, kc, :], hT[:, kc, ts], kc == 0, kc == 7, HT_ALL[4 * tb:4 * tb + 4] + [tag + "w"], [f"ps{2 + s2}"])
                    TT("dve", tA[s2], bank(s2), Ctab[:, ts], ALU.mult, [f"ps{s2}", L + "rope"], [tag + f"tA{s2}"])
                    TT("dve", tB[s2], bank(2 + s2), Stab[:, ts], ALU.mult, [f"ps{2 + s2}", L + "rope"], [tag + f"tB{s2}"])
                    TT("dve", dests[ci][:, ts], tA[s2], tB[s2], ALU.add, [tag + f"tA{s2}", tag + f"tB{s2}"], [tag + f"d{ci + name0}"])

        def proj_feat_plain(wq, dest, t0, nt, tag, ci, pb):
            for kc in range(8):
                MM(bank(pb, nt), wq[:, kc, :], hT[:, kc, t0:t0 + nt], kc == 0, kc == 7, HT_ALL + [tag + "w"], [f"ps{pb}"])
            CP("act", dest, bank(pb, nt), [f"ps{pb}"], [tag + f"d{ci}"])

        def proj_tok(wv, ncols, dest_fn, tiles, tag, post=None, view=None):
            for n, i in enumerate(tiles):
                pb = 4 + n % 2
                for kc in range(8):
                    MM(bank(pb, ncols), hT[:, kc, i * 128:(i + 1) * 128], wv[:, kc, :], kc == 0, kc == 7, [L + f"hT{i}", tag + "w"], [f"ps{pb}"])
                if post is None:
                    src_ = bank(pb, ncols)
                    if view is not None:
                        src_ = view(src_)
                    CP("act", dest_fn(i), src_, [f"ps{pb}"], [tag + f"v{i}"])
                else:
                    post(i, pb)

        def out_transposes(ytok, chunk0, i, tag, rd):
            pb = 6 + (i % 2)
            st = otst[i % 2]
            for cc in range(2):
                TR(bank_bf(pb)[:, cc * 128:(cc + 1) * 128], ytok[:, cc * 128:(cc + 1) * 128], ident_bf, rd + ["ident_bf"], [f"ps{pb}"])
            CP("act", st, bank_bf(pb)[:, 0:256].rearrange("p (c t) -> p c t", t=128), [f"ps{pb}"], [L + f"otst{i % 2}"])
            DMA("sp", otd[i, :, chunk0:chunk0 + 2, :], st, [L + f"otst{i % 2}"], [L + f"OT{chunk0}_{i}"], f"ot{i % 2}")

        def attn_pipeline(tiles_slots, S_fn, E_fn, AV_fn, FIN_fn):
            units = []
            for (i, slots) in tiles_slots:
                ngr = (len(slots) + 1) // 2
                for gi in range(ngr):
                    units.append((i, gi, ngr, slots[2 * gi:2 * gi + 2]))
            pending = None
            for k, u in enumerate(units):
                if k == 0:
                    S_fn(0, u)
                E_fn(k, u)
                if k + 1 < len(units):
                    S_fn(k + 1, units[k + 1])
                AV_fn(k, u)
                if pending is not None:
                    FIN_fn(pending); pending = None
                if u[1] == u[2] - 1:
                    pending = u[0]
            if pending is not None:
                FIN_fn(pending)

        rope64 = A.alloc([2, 2048], BF16); rope32 = A.alloc([2, 2048], BF16)
        DMA("pool", rope64, I["rope64"], [], [L + "rope"], "rp")
        DMA("pool", rope32, I["rope32"], [], [L + "rope"], "rp")

        T = L + "da"
        A.push()
        QT = A.alloc([2, TOKC], BF16); KTc = A.alloc([2, 256], BF16)
        Vc = A.alloc([2, 256], BF16)
        nlam = A.alloc([1], F32); gsub = A.alloc([64], F32)
        A.push()
        dl = A.alloc([4, 32], F32); pr = A.alloc([2, 32], F32); s12 = A.alloc([2], F32)
        DMA("sp", dl, pbc(I[f"dal{l}"]).rearrange("p (a b) -> p a b", b=32), [], [T + "dl"], "m0")
        DMA("sp", gsub, pbc(I[f"subln{l}"]), [], [T + "gsub"], "m0")
        TT("dve", pr[:, 0, :], dl[:, 0, :], dl[:, 1, :], ALU.mult, [T + "dl"], [T + "pr"])
        TT("dve", pr[:, 1, :], dl[:, 2, :], dl[:, 3, :], ALU.mult, [T + "dl"], [T + "pr"])
        RED("dve", s12, pr, [T + "pr"], [T + "s12"])
        ACTV(s12, s12, AF.Exp, [T + "s12"], [T + "s12"])
        TT("dve", nlam, s12[:, 1:2], s12[:, 0:1], ALU.subtract, [T + "s12"], [T + "nlam"])
        TS("dve", nlam, nlam, -lam_init, ALU.add, [T + "nlam"], [T + "nlam"])
        TS("dve", gsub, gsub, 1.0 - lam_init, ALU.mult, [T + "gsub"], [T + "gsub"])
        A.pop()
        A.push()
        wda = A.alloc([8, 1280], BF16)
        KTo = A.alloc([2, TOK], BF16); Vo = A.alloc([NT, 256], BF16)
        DMA("pool", wda, winv[:, :, DA0:DA0 + 1280], [], [T + "w"], "wA")
        qk_dest = [QT[:, 0, 0:TOK], QT[:, 1, 0:TOK], KTo[:, 0, :], KTo[:, 1, :]]
        wq_l = [wda[:, :, c * 128:(c + 1) * 128] for c in range(4)]
        wqs_l = [wda[:, :, 512 + c * 128:512 + (c + 1) * 128] for c in range(4)]
        proj_rope(wq_l[2:4], wqs_l[2:4], rope32[:, 0, :], rope32[:, 1, :], qk_dest[2:4], T, name0=2)
        proj_tok(wda[:, :, 1024:1280], 256, lambda i: Vo[:, i, :] if i < NT else Vc[:, i - NT, :], range(NTC), T)
        V_R = [T + f"v{i}" for i in range(NTC)]
        e_k = dint(T + "ek", [256, TOK], BF16); e_v = dint(T + "ev", [TOK, 256], BF16)
        g_k = dint(T + "gk", [1024, TOK], BF16); g_v = dint(T + "gv", [4 * TOK, 256], BF16)
        DMA("sp", e_k.ap().rearrange("(c p) t -> p c t", p=128), KTo, [T + "d2", T + "d3"], [T + "ek"], "ex")
        DMA("sp", e_v.ap().rearrange("(i p) f -> p i f", p=128), Vo, V_R, [T + "ev"], "ex")
        AG(e_k, g_k, [T + "ek"], [T + "gk"], "cc")
        AG(e_v, g_v, [T + "ev"], [T + "gv"], "cc")
        proj_rope(wq_l[0:2], wqs_l[0:2], rope32[:, 0, :], rope32[:, 1, :], qk_dest[0:2], T, name0=0)
        for ci in range(4):
            dest = QT[:, ci, TOK:TOKC] if ci < 2 else KTc[:, ci - 2, :]
            proj_feat_plain(wda[:, :, ci * 128:(ci + 1) * 128], dest, TOK, 256, T, f"c{ci}", ci % 2)
        QK_R = [T + f"d{ci}" for ci in range(4)] + [T + f"dc{ci}" for ci in range(4)]
        if check_stop(f"daproj_{l}"):
            tap("t_qt", QT, QK_R, "p a b -> p (a b)")
            tap("t_kto", KTo, QK_R, "p a b -> p (a b)")
            tap("t_vo", Vo, V_R, "p a b -> p (a b)")
            tap("t_ktc", KTc, QK_R, "p a b -> p (a b)")
            tap("t_vc", Vc, V_R, "p a b -> p (a b)")
            A.pop(); A.pop(); A.pop(); A.pop(); break
        A.pop()
        P.barrier()
        if check_stop(f"daag_{l}"):
            A.pop(); A.pop(); A.pop(); break
        KT = A.alloc([8448], BF16); V1 = A.alloc([66, 2, 65], BF16)
        ptH = [[A.alloc([2, 512], BF16) for _ in range(2)] for _ in range(2)]
        o_f = A.alloc([2, 64], F32); o1 = A.alloc([64], F32)
        rec = A.alloc([4], F32); rn = A.alloc([2], F32); ssd = A.alloc([2], F32); rsd = A.alloc([2], F32)
        yda = [A.alloc([2, 64], BF16) for _ in range(2)]
        MSET("pool", V1[:, :, :, 64:65], 1.0, [T + "V1ones"])
        g_kv = g_k.ap().rearrange("(r c p) t -> p r c t", r=4, c=2)
        g_vv = g_v.ap().rearrange("(r i p) f -> p r i f", r=4, i=NT)
        nblk = 0
        for c in range(2):
            CP("pool", KT[:, 0:256], KTc[:, c, :], QK_R, [T + "KT"])
            for r in range(4):
                DMA("sp", KT[:, 256 + r * TOK:256 + (r + 1) * TOK], g_kv[:, r, c, :], [T + "gk"], [T + "KT"], "kt")
                for hh in range(2):
                    DMA("sp", V1[:, 2 + r * NT:2 + (r + 1) * NT, hh, 0:64], g_vv[:, r, :, c * 128 + hh * 64:c * 128 + hh * 64 + 64],
                        [T + "gv"], [T + "V1"], "kt")
            CP("pool", V1[:, 0:2, :, 0:64], Vc[:, :, c * 128:(c + 1) * 128].rearrange("p i (h d) -> p i h d", d=64), V_R, [T + "V1"])
            if STOP_AFTER == f"daload_{l}":
                continue
            qblocks = [(qb * 512, 512, 0, 66) for qb in range(4)]
            if STOP_AFTER == f"daq1_{l}":
                qblocks = [(0, 512, 0, 66)] if c == 0 else []
            if STOP_AFTER == f"daq1nf_{l}":
                qblocks = [(0, 512, 0, 66)] if c == 0 else []
            if with_ctx:
                qblocks.append((TOK, 256, 0, 2))
            for (q0, nq, k0, k1) in qblocks:
                sc_ = 1.0 / math.sqrt(32.0)

                def S_half(kt, hf):
                    for g in (2 * hf, 2 * hf + 1):
                        MM(bank(g, nq), KT[32 * g:32 * g + 32, kt * 128:(kt + 1) * 128], QT[32 * g:32 * g + 32, c, q0:q0 + nq], True, True,
                           [T + "KT"] + QK_R, [f"psS{hf}"], tp=(32 * g, 0))

                def E_half(kt, hf):
                    s2 = (kt - k0) % 2
                    ACTV(ptH[hf][s2][:, :, 0:nq], psum[:, 1024 * hf:1024 * hf + 1024].rearrange("p (g n) -> p g n", n=512)[:, :, 0:nq], AF.Exp,
                         [f"psS{hf}"], [T + f"pt{hf}{s2}"], scale=sc_)

                def AV_half(kt, hf):
                    s2 = (kt - k0) % 2
                    for sb in range(nq // 128):
                        for gg in range(2):
                            g = 2 * hf + gg
                            MM(bank(4 + sb, 65, g * 65), ptH[hf][s2][:, gg, sb * 128:(sb + 1) * 128], V1[:, kt, hf, :], kt == k0 and g == 0, kt == k1 - 1,
                               [T + f"pt{hf}{s2}", T + "V1", T + "V1ones"], [f"psO{sb}"], sgc=True)

                S_half(k0, 0); S_half(k0, 1)
                for kt in range(k0, k1):
                    E_half(kt, 0); E_half(kt, 1)
                    AV_half(kt, 0)
                    if kt + 1 < k1:
                        S_half(kt + 1, 0)
                    AV_half(kt, 1)
                    if kt + 1 < k1:
                        S_half(kt + 1, 1)
                if STOP_AFTER == f"daq1nf_{l}":
                    continue
                for sb in range(nq // 128):
                    tile_i = (q0 + sb * 128) // 128
                    yb = yda[nblk % 2]; ybn = T + f"yda{nblk % 2}"; nblk += 1
                    bk = 4 + sb
                    pr_ = f"psO{sb}"
                    Tv = bank(bk, 260).rearrange("p (g e) -> p g e", e=65)
                    RECIP(rec, Tv[:, :, 64], [pr_], [T + "rec"])
                    TS("dve", rn, rec.rearrange("p (h m) -> p h m", m=2)[:, :, 1], nlam[:, 0:1], ALU.mult, [T + "rec", T + "nlam"], [T + "rn"])
                    for hh in range(2):
                        TS("dve", o1, Tv[:, 2 * hh, 0:64], rec[:, 2 * hh:2 * hh + 1], ALU.mult, [pr_, T + "rec"], [T + "o1"])
                        STT("dve", o_f[:, hh, :], Tv[:, 2 * hh + 1, 0:64], rn[:, hh:hh + 1], o1, ALU.mult, ALU.add, [pr_, T + "rn", T + "o1"], [T + "o_f"])
                        ACTV(junk[:, 0:64], o_f[:, hh, :], AF.Square, [T + "o_f"], ["junk", T + "ssd"], accum=ssd[:, hh:hh + 1])
                    rstd_from_ss(ssd, 64, rsd, [T + "ssd", "epsc"], [T + "rsd"])
                    for hh in range(2):
                        STT("dve", yb[:, hh, :], o_f[:, hh, :], rsd[:, hh:hh + 1], gsub, ALU.mult, ALU.mult, [T + "o_f", T + "rsd", T + "gsub"], [ybn])
                    TR(bank_bf(bk)[:, 640:768], yb.rearrange("p h d -> p (h d)"), ident_bf, [ybn, "ident_bf"], [pr_])
                    CP("act", otst[sb % 2][:, 0, :], bank_bf(bk)[:, 640:768], [pr_], [L + f"otst{sb % 2}"])
                    DMA("sp", otd[tile_i, :, c, :], otst[sb % 2][:, 0, :], [L + f"otst{sb % 2}"], [L + f"OT{c}_{tile_i}"], f"ot{sb % 2}")
        A.pop()
        if check_stop(f"da_{l}") or STOP_AFTER in (f"daload_{l}", f"daq1_{l}", f"daq1nf_{l}"):
            P.barrier()
            tap("t_nlam", nlam, [], None)
            tap("t_gsub", gsub, [], None)
            if "dbg_ot" in dbg:
                P.barrier()
                DMA("sp", dbg["dbg_ot"], otd, [], ["dbgot"], "dbg")
            A.pop(); A.pop(); break

        P.barrier()
        T = L + "sw"
        A.push()
        QT = A.alloc([2, TOKC], BF16)
        KT = A.alloc([3328], BF16)
        V1 = A.alloc([26, 2, 65], BF16)
        MSW = A.alloc([10, 128], BF16)
        esink = A.alloc([4], F32)
        DMA("pool", MSW, I["swamask"].rearrange("m k q -> k m q"), [], [T + "msw"], "wB")
        DMA("sp", esink, pbc(I[f"sink{l}"]), [], [T + "esink"], "m0")
        ACTV(esink, esink, AF.Exp, [T + "esink"], [T + "esink"])
        MSET("pool", V1[:, :, :, 64:65], 1.0, [T + "V1ones"])
        A.push()
        wsw = A.alloc([8, 896], BF16)
        DMA("pool", wsw, winv[:, :, SW0:SW0 + 896], [], [T + "w"], "wA")
        dests = [QT[:, 0, 0:TOK], QT[:, 1, 0:TOK], KT[:, 0:TOK]]
        wq_l = [wsw[:, :, c * 128:(c + 1) * 128] for c in range(3)]
        wqs_l = [wsw[:, :, 384 + c * 128:384 + (c + 1) * 128] for c in range(3)]
        proj_rope(wq_l[2:3], wqs_l[2:3], rope64[:, 0, :], rope64[:, 1, :], dests[2:3], T, name0=2)
        proj_tok(wsw[:, :, 768:896], 128, lambda i: V1[:, i, :, 0:64], range(NTC), T, view=lambda a: a.rearrange("p (h d) -> p h d", d=64))
        V_R = [T + f"v{i}" for i in range(NTC)]
        e_s = dint(T + "e", [128, 512], BF16); g_s = dint(T + "g", [512, 512], BF16)
        DMA("sp", e_s.ap()[:, 0:128], KT[:, 0:128], [T + "d2"], [T + "e"], "ex")
        DMA("sp", e_s.ap()[:, 128:256], KT[:, TOK - 128:TOK], [T + "d2"], [T + "e"], "ex")
        DMA("sp", e_s.ap()[:, 256:384].rearrange("p (h d) -> p h d", d=64), V1[:, 0, :, 0:64], V_R, [T + "e"], "ex")
        DMA("sp", e_s.ap()[:, 384:512].rearrange("p (h d) -> p h d", d=64), V1[:, 15, :, 0:64], V_R, [T + "e"], "ex")
        AG(e_s, g_s, [T + "e"], [T + "g"], "cc")
        proj_rope(wq_l[0:2], wqs_l[0:2], rope64[:, 0, :], rope64[:, 1, :], dests[0:2], T, name0=0)
        for ci in range(3):
            dest = QT[:, ci, TOK:TOKC] if ci < 2 else KT[:, TOK:TOKC]
            proj_feat_plain(wsw[:, :, ci * 128:(ci + 1) * 128], dest, TOK, 256, T, f"c{ci}", ci % 2)
        A.pop()
        QK_R = [T + f"d{ci}" for ci in range(3)] + [T + f"dc{ci}" for ci in range(3)]
        if check_stop(f"swproj_{l}"):
            A.pop(); A.pop(); A.pop(); break
        g_sv = g_s.ap().rearrange("(r p) f -> p r f", p=128)
        DMA("sp", KT[:, TOKC:TOKC + 512].rearrange("p (r t) -> p r t", t=128), g_sv[:, :, 128:256], [T + "g"], [T + "halo"], "kt")
        DMA("sp", KT[:, TOKC + 512:TOKC + 1024].rearrange("p (r t) -> p r t", t=128), g_sv[:, :, 0:128], [T + "g"], [T + "halo"], "kt")
        for kv in range(2):
            DMA("sp", V1[:, 18:22, kv, 0:64], g_sv[:, :, 384 + kv * 64:448 + kv * 64], [T + "g"], [T + "halo"], "kt")
            DMA("sp", V1[:, 22:26, kv, 0:64], g_sv[:, :, 256 + kv * 64:320 + kv * 64], [T + "g"], [T + "halo"], "kt")
        if check_stop(f"swag_{l}"):
            A.pop(); A.pop(); A.pop(); break
        ptw = [A.alloc([2, 4, 128], BF16) for _ in range(2)]
        qz = [A.alloc([2, 2, 128], BF16) for _ in range(2)]
        den = [A.alloc([4], F32) for _ in range(2)]; ysw = [A.alloc([4, 64], BF16) for _ in range(2)]
        ALLR = QK_R + V_R + [T + "halo", T + "V1ones"]
        tiles_slots = []
        for i in range(nto):
            if i < NT:
                slots = []
                if i > 0:
                    slots.append((128 * (i - 1), i - 1, 0))
                slots.append((128 * i, i, None))
                if i < NT - 1:
                    slots.append((128 * (i + 1), i + 1, 1))
                slots += [(TOK, 16, None), (TOK + 128, 17, None)]
                if i == 0:
                    slots += [(TOKC + 128 * r, 18 + r, 2 + r) for r in range(4)]
                if i == NT - 1:
                    slots += [(TOKC + 512 + 128 * r, 22 + r, 6 + r) for r in range(4)]
            else:
                slots = [(TOK, 16, None), (TOK + 128, 17, None)]
            tiles_slots.append((i, slots))

        def sw_S(k, u):
            i, gi, ngr, grp = u
            s2 = k % 2
            ts = slice(i * 128, (i + 1) * 128)
            if gi == 0:
                for kv in range(2):
                    TS("pool" if kv else "dve", qz[i % 2][:, kv], QT[:, :, ts], HM[:, kv:kv + 1], ALU.mult, QK_R + ["HM"], [T + f"qz{i % 2}"])
            for si, (kc0, vt, mi) in enumerate(grp):
                for kv in range(2):
                    for g in range(2):
                        MM(bank(2 * s2 + si, 128, (kv * 2 + g) * 128), KT[:, kc0:kc0 + 128], qz[i % 2][:, kv, g, :], True, True,
                           ALLR + [T + f"qz{i % 2}"], [f"psS{s2}"], sgc=True)

        def sw_E(k, u):
            i, gi, ngr, grp = u
            s2 = k % 2
            ns = len(grp)
            ACTV(ptw[s2][:, 0:ns].rearrange("p s h q -> p (s h q)"), psum[:, 1024 * s2:1024 * s2 + 512 * ns], AF.Exp, [f"psS{s2}"], [T + f"pt{s2}"], scale=0.125)
            for si, (kc0, vt, mi) in enumerate(grp):
                if mi is not None:
                    TT("dve", ptw[s2][:, si], ptw[s2][:, si], MSW[:, mi, :].unsqueeze(1).to_broadcast([128, 4, 128]), ALU.mult, [T + f"pt{s2}", T + "msw"], [T + f"pt{s2}"])

        def sw_AV(k, u):
            i, gi, ngr, grp = u
            s2 = k % 2
            ns = len(grp)
            for si, (kc0, vt, mi) in enumerate(grp):
                first = (gi == 0 and si == 0); last = (gi == ngr - 1 and si == ns - 1)
                for kv in range(2):
                    for g in range(2):
                        h = kv * 2 + g
                        MM(bank(4 + i % 2, 65, h * 65), ptw[s2][:, si, h, :], V1[:, vt, kv, :], first and h == 0, last, [T + f"pt{s2}"] + ALLR, [f"psO{i % 2}"], sgc=True)

        def sw_FIN(i):
            Ov = bank(4 + i % 2, 260).rearrange("p (h e) -> p h e", e=65)
            dn = den[i % 2]
            TT("dve", dn, Ov[:, :, 64], esink, ALU.add, [f"psO{i % 2}", T + "esink"], [T + f"den{i % 2}"])
            RECIP(dn, dn, [T + f"den{i % 2}"], [T + f"den{i % 2}"])
            yb = ysw[i % 2]
            TT("dve", yb, Ov[:, :, 0:64], dn.unsqueeze(2).to_broadcast([128, 4, 64]), ALU.mult, [f"psO{i % 2}", T + f"den{i % 2}"], [T + f"y{i % 2}"])
            out_transposes(yb.rearrange("p h d -> p (h d)"), 2, i, T, [T + f"y{i % 2}"])

        attn_pipeline(tiles_slots, sw_S, sw_E, sw_AV, sw_FIN)
        A.pop()
        if check_stop(f"sw_{l}") or (STOP_AFTER or "").startswith("swq1"):
            if "dbg_ot" in dbg:
                P.barrier()
                DMA("sp", dbg["dbg_ot"], otd, [], ["dbgot"], "dbg")
            A.pop(); A.pop(); break

        P.barrier()
        T = L + "na"
        A.push()
        QT = A.alloc([2, TOKC], BF16)
        KT = A.alloc([2, 4352], BF16)
        V1 = A.alloc([34, 4, 65], BF16)
        MBK = A.alloc([45, 128], BF16)
        BEX = A.alloc([7, 4, 128], BF16)
        EIN = A.alloc([5, 4, 128], BF16)
        for m0 in range(0, 45, 9):
            DMA("pool", MBK[:, m0:m0 + 9, :], I["namask"][m0:m0 + 9].rearrange("m k q -> k m q"), [], [T + "mbk"], "wB")
        A.push()
        bfl = A.alloc([7, 4, 128], F32)
        for d7 in range(7):
            DMA("sp", bfl[:, d7], I[f"nabias{l}"][d7].rearrange("h k q -> k h q"), [], [T + "bfl"], "m0")
        ACTV(BEX, bfl, AF.Exp, [T + "bfl"], [T + "bex"])
        A.pop()
        for d in range(5):
            TT("dve", EIN[:, d], BEX[:, d + 1], MBK[:, d, :].unsqueeze(1).to_broadcast([128, 4, 128]), ALU.mult, [T + "bex", T + "mbk"], [T + "ein"])
        MSET("pool", V1[:, :, :, 64:65], 1.0, [T + "V1ones"])
        A.push()
        wna = A.alloc([8, 768], BF16)
        DMA("pool", wna, winv[:, :, NA0:NA0 + 768], [], [T + "w"], "wA")
        n = 0
        for ci in (2, 3):
            for (t0, nt_) in ((0, 512), (512, 512), (1024, 512), (1536, 512), (TOK, 256)):
                dest = KT[:, ci - 2, t0:t0 + nt_]
                proj_feat_plain(wna[:, :, ci * 128:(ci + 1) * 128], dest, t0, nt_, T, f"c{ci}", n % 4); n += 1
        proj_tok(wna[:, :, 512:768], 256, lambda i: V1[:, i, :, 0:64], range(NTC), T, view=lambda a: a.rearrange("p (h d) -> p h d", d=64))
        V_R = [T + f"v{i}" for i in range(NTC)]
        e_n = dint(T + "e", [128, 2048], BF16); g_n = dint(T + "g", [512, 2048], BF16)
        env = e_n.ap()
        for c in range(2):
            DMA("sp", env[:, c * 512:c * 512 + 256], KT[:, c, 0:256], [T + "dc2", T + "dc3"], [T + "e"], "ex")
            DMA("sp", env[:, c * 512 + 256:c * 512 + 512], KT[:, c, TOK - 256:TOK], [T + "dc2", T + "dc3"], [T + "e"], "ex")
        DMA("sp", env[:, 1024:1536].rearrange("p (i h d) -> p i h d", h=4, d=64), V1[:, 0:2, :, 0:64], V_R, [T + "e"], "ex")
        DMA("sp", env[:, 1536:2048].rearrange("p (i h d) -> p i h d", h=4, d=64), V1[:, 14:16, :, 0:64], V_R, [T + "e"], "ex")
        AG(e_n, g_n, [T + "e"], [T + "g"], "cc")
        for ci in (0, 1):
            for (t0, nt_) in ((0, 512), (512, 512), (1024, 512), (1536, 512), (TOK, 256)):
                dest = QT[:, ci, t0:t0 + nt_]
                proj_feat_plain(wna[:, :, ci * 128:(ci + 1) * 128], dest, t0, nt_, T, f"c{ci}", n % 4); n += 1
        A.pop()
        P.barrier()
        QK_R = [T + f"dc{ci}" for ci in range(4)]
        g_nv = g_n.ap().rearrange("(r p) f -> p r f", p=128)
        for c in range(2):
            DMA("sp", KT[:, c, TOKC:TOKC + 1024].rearrange("p (r t) -> p r t", t=256), g_nv[:, :, c * 512 + 256:c * 512 + 512], [T + "g"], [T + "halo"], "kt")
            DMA("sp", KT[:, c, TOKC + 1024:TOKC + 2048].rearrange("p (r t) -> p r t", t=256), g_nv[:, :, c * 512:c * 512 + 256], [T + "g"], [T + "halo"], "kt")
        for r in range(4):
            DMA("sp", V1[:, 18 + 2 * r:20 + 2 * r, :, 0:64], g_nv[:, r, 1536:2048].rearrange("p (i h d) -> p i h d", h=4, d=64), [T + "g"], [T + "halo"], "kt")
            DMA("sp", V1[:, 26 + 2 * r:28 + 2 * r, :, 0:64], g_nv[:, r, 1024:1536].rearrange("p (i h d) -> p i h d", h=4, d=64), [T + "g"], [T + "halo"], "kt")
        ptn = [A.alloc([2, 4, 128], BF16) for _ in range(2)]
        qz = [A.alloc([2, 2, 128], BF16) for _ in range(2)]
        den = [A.alloc([4], F32) for _ in range(2)]; yna = [A.alloc([4, 64], BF16) for _ in range(2)]
        ALLR = QK_R + V_R + [T + "halo", T + "V1ones"]
        PC0 = TOKC; NC0 = TOKC + 1024
        tiles_slots = []
        for i in range(nto):
            if i >= NT:
                slots = [(TOK, 16, 0, 0, 0), (TOK + 128, 17, 0, 0, 0)]
            elif 2 <= i <= 13:
                slots = [(128 * (i + d), i + d, 1, d + 2, 0) for d in range(-2, 3)]
            elif i == 0:
                slots = [(128 * d, d, 2, d + 3, 5 + d) for d in (0, 1, 2, 3)]
                slots += [(PC0 + 256 * r, 18 + 2 * r, 2, 1, 9 + r) for r in range(4)]
                slots += [(PC0 + 256 * r + 128, 19 + 2 * r, 2, 2, 13 + r) for r in range(4)]
            elif i == 1:
                slots = [(128 * (1 + d), 1 + d, 2, d + 3, 17 + (d + 1)) for d in (-1, 0, 1, 2)]
                slots += [(PC0 + 256 * r + 128, 19 + 2 * r, 2, 1, 21 + r) for r in range(4)]
            elif i == 14:
                slots = [(128 * (14 + d), 14 + d, 2, d + 3, 25 + (d + 2)) for d in (-2, -1, 0, 1)]
                slots += [(NC0 + 256 * r, 26 + 2 * r, 2, 5, 29 + r) for r in range(4)]
            else:
                slots = [(128 * (15 + d), 15 + d, 2, d + 3, 33 + (d + 3)) for d in (-3, -2, -1, 0)]
                slots += [(NC0 + 256 * r, 26 + 2 * r, 2, 4, 37 + r) for r in range(4)]
                slots += [(NC0 + 256 * r + 128, 27 + 2 * r, 2, 5, 41 + r) for r in range(4)]
            if i < NT:
                slots += [(TOK, 16, 0, 0, 0), (TOK + 128, 17, 0, 0, 0)]
            tiles_slots.append((i, slots))

        def na_S(k, u):
            i, gi, ngr, grp = u
            s2 = k % 2
            ts = slice(i * 128, (i + 1) * 128)
            if gi == 0:
                for hb_ in range(2):
                    TS("pool" if hb_ else "dve", qz[i % 2][:, hb_], QT[:, :, ts], HM[:, hb_:hb_ + 1], ALU.mult, QK_R + ["HM"], [T + f"qz{i % 2}"])
            for si, (kc0, vt, kind, bd, mi) in enumerate(grp):
                for h in range(4):
                    c, hb_ = h // 2, h % 2
                    MM(bank(2 * s2 + si, 128, h * 128), KT[:, c, kc0:kc0 + 128], qz[i % 2][:, hb_, c, :], True, True, ALLR + [T + f"qz{i % 2}"], [f"psS{s2}"], sgc=True)

        def na_E(k, u):
            i, gi, ngr, grp = u
            s2 = k % 2
            ns = len(grp)
            ACTV(ptn[s2][:, 0:ns].rearrange("p s h q -> p (s h q)"), psum[:, 1024 * s2:1024 * s2 + 512 * ns], AF.Exp, [f"psS{s2}"], [T + f"pt{s2}"], scale=0.125)
            for si, (kc0, vt, kind, bd, mi) in enumerate(grp):
                if kind == 1:
                    TT("dve", ptn[s2][:, si], ptn[s2][:, si], EIN[:, bd], ALU.mult, [T + f"pt{s2}", T + "ein"], [T + f"pt{s2}"])
                elif kind == 2:
                    TT("dve", ptn[s2][:, si], ptn[s2][:, si], BEX[:, bd], ALU.mult, [T + f"pt{s2}", T + "bex"], [T + f"pt{s2}"])
                    TT("pool", ptn[s2][:, si], ptn[s2][:, si], MBK[:, mi, :].unsqueeze(1).to_broadcast([128, 4, 128]), ALU.mult, [T + f"pt{s2}", T + "mbk"], [T + f"pt{s2}"])

        def na_AV(k, u):
            i, gi, ngr, grp = u
            s2 = k % 2
            ns = len(grp)
            for si, (kc0, vt, kind, bd, mi) in enumerate(grp):
                first = (gi == 0 and si == 0); last = (gi == ngr - 1 and si == ns - 1)
                for h in range(4):
                    MM(bank(4 + i % 2, 65, h * 65), ptn[s2][:, si, h, :], V1[:, vt, h, :], first and h == 0, last, [T + f"pt{s2}"] + ALLR, [f"psO{i % 2}"], sgc=True)

        def na_FIN(i):
            Ov = bank(4 + i % 2, 260).rearrange("p (h e) -> p h e", e=65)
            dn = den[i % 2]
            RECIP(dn, Ov[:, :, 64], [f"psO{i % 2}"], [T + f"den{i % 2}"])
            yb = yna[i % 2]
            TT("dve", yb, Ov[:, :, 0:64], dn.unsqueeze(2).to_broadcast([128, 4, 64]), ALU.mult, [f"psO{i % 2}", T + f"den{i % 2}"], [T + f"y{i % 2}"])
            out_transposes(yb.rearrange("p h d -> p (h d)"), 4, i, T, [T + f"y{i % 2}"])

        attn_pipeline(tiles_slots, na_S, na_E, na_AV, na_FIN)
        A.pop()
        if check_stop(f"na_{l}"):
            if "dbg_ot" in dbg:
                P.barrier()
                DMA("sp", dbg["dbg_ot"], otd, [], ["dbgot"], "dbg")
            A.pop(); A.pop(); break

        P.barrier()
        T = L + "rt"
        A.push()
        QT = A.alloc([2, TOKC], BF16); KT = A.alloc([2, TOKC], BF16)
        VR = A.alloc([NTC, 256], BF16); GT = A.alloc([NTC, 256], BF16)
        RC = A.alloc([700], F32); IDXB = A.alloc([128], F32)
        LG = A.alloc([8], F32); LGS = A.alloc([2, 2], F32)
        DEC = A.alloc([4, 128], BF16); XI = A.alloc([2, 2, 128], BF16)
        ZZ = A.alloc([2, 4], F32); GC = A.alloc([2, 2], F32); GPW = A.alloc([2, 2, 18], F32); CFC = A.alloc([2, 2, 5], F32)
        DMA("sp", RC, I["retc"], [], [T + "rc"], "m0")
        DMA("sp", IDXB, I["idxb"], [], [T + "rc"], "m0")
        DMA("sp", LG, pbc(I[f"gam{l}"]), [], [T + "lg"], "m0")
        ACTV(LG, LG, AF.Exp, [T + "lg"], [T + "lg"], scale=-1.0)
        TS("dve", LG, LG, 1.0, ALU.add, [T + "lg"], [T + "lg"])
        ACTV(LG, LG, AF.Ln, [T + "lg"], [T + "lg"])
        TS("dve", LG, LG, -1.0, ALU.mult, [T + "lg"], [T + "lg"])
        for d_ in range(2):
            for c in range(2):
                k0_ = 4 * d_ + 2 * c
                TS("dve", LGS[:, d_, c:c + 1], LG[:, k0_:k0_ + 1], HM[:, 0:1], ALU.mult, [T + "lg", "HM"], [T + "lgs"])
                STT("dve", LGS[:, d_, c:c + 1], LG[:, k0_ + 1:k0_ + 2], HM[:, 1:2], LGS[:, d_, c:c + 1], ALU.mult, ALU.add, [T + "lg", "HM", T + "lgs"], [T + "lgs"])
        A.push()
        tf = A.alloc([128], F32); tb_ = A.alloc([128], F32)
        for h in range(4):
            ACTV(tf, RC[:, 0:128], AF.Exp, [T + "rc", T + "lg"], [T + "tf"], scale=LG[:, h:h + 1])
            TT("dve", tf, tf, RC[:, 128:256], ALU.mult, [T + "tf", T + "rc"], [T + "tf"])
            ACTV(tb_, RC[:, 256:384], AF.Exp, [T + "rc", T + "lg"], [T + "tb"], scale=LG[:, 4 + h:5 + h])
            TT("dve", tb_, tb_, RC[:, 384:512], ALU.mult, [T + "tb", T + "rc"], [T + "tb"])
            TT("dve", DEC[:, h, :], tf, tb_, ALU.add, [T + "tf", T + "tb"], [T + "dec"])
        A.pop()
        for c in range(2):
            ACTV(XI[:, 0, c, :], RC[:, 512:640], AF.Exp, [T + "rc", T + "lgs"], [T + "xi"], scale=LGS[:, 0, c:c + 1])
            ACTV(XI[:, 1, c, :], IDXB, AF.Exp, [T + "rc", T + "lgs"], [T + "xi"], scale=LGS[:, 1, c:c + 1])
            for d_ in range(2):
                ACTV(GC[:, d_, c:c + 1], LGS[:, d_, c:c + 1], AF.Exp, [T + "lgs"], [T + "gc"], scale=128.0)
                ACTV(GPW[:, d_, c, :], RC[:, 642 + 18 * d_:660 + 18 * d_], AF.Exp, [T + "rc", T + "lgs"], [T + "gpw"], scale=LGS[:, d_, c:c + 1])
                ACTV(CFC[:, d_, c, :], RC[:, 678 + 5 * d_:683 + 5 * d_], AF.Exp, [T + "rc", T + "lgs"], [T + "cfc"], scale=LGS[:, d_, c:c + 1])
                TT("dve", CFC[:, d_, c, :], CFC[:, d_, c, :], RC[:, 688 + 5 * d_:693 + 5 * d_], ALU.mult, [T + "cfc", T + "rc"], [T + "cfc"])
        ACTV(ZZ[:, 0, :], LG[:, 0:4], AF.Exp, [T + "lg", T + "rc"], [T + "zz"], scale=RC[:, 640:641])
        ACTV(ZZ[:, 1, :], LG[:, 4:8], AF.Exp, [T + "lg", T + "rc"], [T + "zz"], scale=RC[:, 641:642])
        TS("dve", ZZ, ZZ, 0.125, ALU.mult, [T + "zz"], [T + "zz"])
        if check_stop(f"rtparam_{l}"):
            A.pop(); A.pop(); A.pop(); break
        A.push()
        wrt = A.alloc([8, 1536], BF16)
        DMA("pool", wrt, winv[:, :, RT0:RT0 + 1536], [], [T + "w"], "wA")
        dests = [QT[:, 0, 0:TOK], QT[:, 1, 0:TOK], KT[:, 0, 0:TOK], KT[:, 1, 0:TOK]]
        proj_rope([wrt[:, :, c * 128:(c + 1) * 128] for c in range(4)], [wrt[:, :, 512 + c * 128:512 + (c + 1) * 128] for c in range(4)],
                  rope64[:, 0, :], rope64[:, 1, :], dests, T)
        for ci in range(4):
            dest = QT[:, ci, TOK:TOKC] if ci < 2 else KT[:, ci - 2, TOK:TOKC]
            proj_feat_plain(wrt[:, :, ci * 128:(ci + 1) * 128], dest, TOK, 256, T, f"c{ci}", ci % 2)

        def vg_post(i, pb):
            CP("act", VR[:, i, :], bank(pb, 256), [f"ps{pb}"], [T + f"v{i}"])
            ACTV(GT[:, i, :], bank(pb, 256, 256), AF.Silu, [f"ps{pb}"], [T + f"g{i}"])
        proj_tok(wrt[:, :, 1024:1536], 512, None, range(NTC), T, post=vg_post)
        A.pop()
        P.barrier()
        QK_R = [T + f"d{ci}" for ci in range(4)] + [T + f"dc{ci}" for ci in range(4)]
        if check_stop(f"rtproj_{l}"):
            A.pop(); A.pop(); A.pop(); break
        KTOK = A.alloc([NTC, 256], BF16)
        SZ = A.alloc([2, 18, 2, 64], F32)
        UCX = A.alloc([4, 2, 64], F32)
        SCX = A.alloc([2, 2, 64], F32)
        S0 = A.alloc([2, 2, 64], F32)
        SB = A.alloc([2, NTC, 2, 64], BF16)
        GR = A.alloc([4, 256], F32); EXPB = A.alloc([2, 2, 64], F32)
        for i in range(NTC):
            pb = i % 2
            for c in range(2):
                TR(bank_bf(pb)[:, c * 128:(c + 1) * 128], KT[:, c, i * 128:(i + 1) * 128], ident_bf, QK_R + ["ident_bf"], [f"ps{pb}"])
            CP("act", KTOK[:, i, :], bank_bf(pb)[:, 0:256], [f"ps{pb}"], [T + f"kt{i}"])
        vz = [A.alloc([2, 256], BF16) for _ in range(2)]
        MSET("pool", SZ[:, 0, 0], 0.0, [T + "sz"])
        MSET("pool", SZ[:, 1, 16], 0.0, [T + "sz"])

        def chunk_U(n, s2):
            for d_ in range(2):
                TT("dve" if d_ == 0 else "pool", vz[s2][:, d_].rearrange("p (h e) -> p h e", e=64), VR[:, n, :].rearrange("p (h e) -> p h e", e=64),
                   ZZ[:, d_, :].unsqueeze(2).to_broadcast([128, 4, 64]), ALU.mult, [T + f"v{n}", T + "zz"], [T + f"vz{s2}"])
            for d_ in range(2):
                for c in range(2):
                    MM(bank(2 + s2, 128, (d_ * 2 + c) * 128), KTOK[:, n, c * 128:(c + 1) * 128], vz[s2][:, d_, c * 128:(c + 1) * 128], True, True,
                       [T + f"kt{n}", T + f"vz{s2}"], [f"ps{2 + s2}"])

        udg = A.alloc([64], F32); udt = A.alloc([64], F32)

        def udiag(s2, d_, c, dst=None, dreg=None):
            blk = bank(2 + s2, 128, (d_ * 2 + c) * 128)
            o_ = udg if dst is None else dst
            TS("dve", udt, blk[:, 0:64], HM[:, 0:1], ALU.mult, [f"ps{2 + s2}", "HM"], [T + "udt"])
            STT("dve", o_, blk[:, 64:128], HM[:, 1:2], udt, ALU.mult, ALU.add, [f"ps{2 + s2}", "HM", T + "udt"], [T + "udg" if dreg is None else dreg])
            return o_

        for n in range(NT):
            chunk_U(n, n % 2)
            for c in range(2):
                u_ = udiag(n % 2, 0, c)
                STT("dve", SZ[:, 0, n + 1, c, :], SZ[:, 0, n, c, :], GC[:, 0, c:c + 1], u_, ALU.mult, ALU.add, [T + "sz", T + "gc", T + "udg"], [T + "sz"])
                udiag(n % 2, 1, c, dst=SZ[:, 1, n, c, :], dreg=T + "szb")
        for n in range(NT - 1, -1, -1):
            for c in range(2):
                STT("dve", SZ[:, 1, n, c, :], SZ[:, 1, n + 1, c, :], GC[:, 1, c:c + 1], SZ[:, 1, n, c, :], ALU.mult, ALU.add, [T + "sz", T + "szb", T + "gc"], [T + "sz", T + "szb"])
        for k_, n in enumerate((16, 17)):
            chunk_U(n, k_)
            for d_ in range(2):
                for c in range(2):
                    u_ = udiag(k_, d_, c)
                    CP("dve", UCX[:, 2 * d_ + k_, c, :], u_, [T + "udg"], [T + "ucx"])
        for c in range(2):
            STT("dve", SCX[:, 0, c, :], UCX[:, 0, c, :], GC[:, 0, c:c + 1], UCX[:, 1, c, :], ALU.mult, ALU.add, [T + "ucx", T + "gc"], [T + "scx"])
            STT("dve", SCX[:, 1, c, :], UCX[:, 3, c, :], GC[:, 1, c:c + 1], UCX[:, 2, c, :], ALU.mult, ALU.add, [T + "ucx", T + "gc"], [T + "scx"])
        CP("dve", EXPB[:, 0], SZ[:, 0, 16], [T + "sz"], [T + "expb"])
        CP("dve", EXPB[:, 1], SZ[:, 1, 0], [T + "sz"], [T + "expb"])
        e_r = dint(T + "e", [128, 256], F32); g_r = dint(T + "g", [512, 256], F32)
        DMA("sp", e_r.ap(), EXPB.rearrange("p d c e -> p (d c e)"), [T + "expb"], [T + "e"], "ex")
        AG(e_r, g_r, [T + "e"], [T + "g"], "cc")
        DMA("sp", GR, g_r.ap().rearrange("(r p) f -> p r f", p=128), [T + "g"], [T + "gr"], "kt")
        GRv = GR.rearrange("p r (d c e) -> p r d c e", d=2, c=2)
        for d_ in range(2):
            for c in range(2):
                TS("dve", S0[:, d_, c, :], SCX[:, d_, c, :], CFC[:, d_, c, 4:5], ALU.mult, [T + "scx", T + "cfc"], [T + "s0"])
                for r in range(4):
                    STT("dve", S0[:, d_, c, :], GRv[:, r, d_, c, :], CFC[:, d_, c, r:r + 1], S0[:, d_, c, :], ALU.mult, ALU.add, [T + "gr", T + "cfc", T + "s0"], [T + "s0"])
        for n in range(NT):
            for c in range(2):
                STT("dve", SB[:, 0, n, c, :], S0[:, 0, c, :], GPW[:, 0, c, n:n + 1], SZ[:, 0, n, c, :], ALU.mult, ALU.add, [T + "s0", T + "gpw", T + "sz"], [T + "sb"])
                STT("dve", SB[:, 1, n, c, :], S0[:, 1, c, :], GPW[:, 1, c, n:n + 1], SZ[:, 1, n + 1, c, :], ALU.mult, ALU.add, [T + "s0", T + "gpw", T + "sz"], [T + "sb"])
        MSET("pool", SB[:, 0, 16], 0.0, [T + "sb"])
        MSET("pool", SB[:, 1, 17], 0.0, [T + "sb"])
        CP("dve", SB[:, 0, 17], UCX[:, 0], [T + "ucx"], [T + "sb"])
        CP("dve", SB[:, 1, 16], UCX[:, 3], [T + "ucx"], [T + "sb"])
        if check_stop(f"rtA_{l}"):
            A.pop(); A.pop(); A.pop(); break
        AD = [A.alloc([4, 128], BF16) for _ in range(2)]
        QX = [A.alloc([2, 2, 2, 128], BF16) for _ in range(2)]
        qz = [A.alloc([2, 2, 128], BF16) for _ in range(2)]
        of_ = [A.alloc([4, 64], F32) for _ in range(2)]
        sq = A.alloc([4, 64], F32); ssr = A.alloc([4], F32); rsr = A.alloc([4], F32)
        yr_ = [A.alloc([4, 64], BF16) for _ in range(2)]
        def rt_A(i):
            s2 = i % 2
            ts = slice(i * 128, (i + 1) * 128)
            for hb_ in range(2):
                TS("pool" if hb_ else "dve", qz[s2][:, hb_], QT[:, :, ts], HM[:, hb_:hb_ + 1], ALU.mult, QK_R + ["HM"], [T + f"qz{s2}"])
            for h in range(4):
                c, hb_ = h // 2, h % 2
                MM(bank(s2, 128, h * 128), KT[:, c, ts], qz[s2][:, hb_, c, :], True, True, QK_R + [T + f"qz{s2}"], [f"ps{s2}"], sgc=True)

        def rt_mid(i):
            s2 = i % 2
            TT("dve", AD[s2], bank(s2).rearrange("p (h i) -> p h i", i=128), DEC, ALU.mult, [f"ps{s2}", T + "dec"], [T + f"ad{s2}"])
            for d_ in range(2):
                for hb_ in range(2):
                    TT("dve", QX[s2][:, d_, hb_], qz[s2][:, hb_], XI[:, d_], ALU.mult, [T + f"qz{s2}", T + "xi"], [T + f"qx{s2}"])

        def rt_out(i):
            s2 = i % 2
            pO = 4 + s2
            for h in range(4):
                c, hb_ = h // 2, h % 2
                o_ = bank(pO, 64, h * 64)
                MM(o_, AD[s2][:, h, :], VR[:, i, h * 64:(h + 1) * 64], h == 0, False, [T + f"ad{s2}", T + f"v{i}"], [f"ps{pO}"], sgc=True)
                MM(o_, QX[s2][:, 0, hb_, c, :], SB[:, 0, i, c, :], False, False, [T + f"qx{s2}", T + "sb"], [f"ps{pO}"], sgc=True)
                MM(o_, QX[s2][:, 1, hb_, c, :], SB[:, 1, i, c, :], False, True, [T + f"qx{s2}", T + "sb"], [f"ps{pO}"], sgc=True)

        def rt_fin(i):
            s2 = i % 2
            pO = 4 + s2
            ov = of_[s2]
            CP("act", ov, bank(pO, 256).rearrange("p (h e) -> p h e", e=64), [f"ps{pO}"], [T + f"of{s2}"])
            TT("dve", sq, ov, ov, ALU.mult, [T + f"of{s2}"], [T + "sq"])
            RED("dve", ssr, sq, [T + "sq"], [T + "ssr"])
            rstd_from_ss(ssr, 64, rsr, [T + "ssr", "epsc"], [T + "rsr"])
            TT("dve", sq, ov, rsr.unsqueeze(2).to_broadcast([128, 4, 64]), ALU.mult, [T + f"of{s2}", T + "rsr"], [T + "sq"])
            TT("pool", yr_[s2], sq, GT[:, i, :].rearrange("p (h e) -> p h e", e=64), ALU.mult, [T + "sq", T + f"g{i}"], [T + f"y{s2}"])

        def rt_tr(i):
            out_transposes(yr_[i % 2].rearrange("p h d -> p (h d)"), 6, i, T, [T + f"y{i % 2}"])

        rt_A(0)
        rt_mid(0)
        if nto > 1:
            rt_A(1)
        for i in range(nto):
            rt_out(i)
            if i + 1 < nto:
                rt_mid(i + 1)
            if i + 2 < nto:
                rt_A(i + 2)
            rt_fin(i)
            if i >= 1:
                rt_tr(i - 1)
        rt_tr(nto - 1)
        A.pop()
        A.pop()
        if "dbg_ot" in dbg and l == 0:
            DMA("sp", dbg["dbg_ot"], otd, [L + f"OT{c0}_{i}" for c0 in (0, 1, 2, 4, 6) for i in range(nto)], ["dbgot"], "dbg")
        if check_stop(f"rt_{l}") or (STOP_AFTER or "").startswith("rtB"):
            A.pop(); break

        P.barrier()
        A.push()
        h2T = hT
        wo = A.alloc([8, 1024], BF16)
        DMA("pool", wo, I[f"wout{l}"].rearrange("(kc p) n -> p kc n", p=128), [], [L + "wo"], "wA")
        if moe:
            RB = A.alloc([8, 1024], F32)
            DMA("sp", RB, pbc(I["router"]).rearrange("p (e d) -> p e d", d=1024), [], [L + "rb"], "m0")
            rj = [A.alloc([1024], F32) for _ in range(3)]; sm = A.alloc([8, 8], F32)
        xt = [A.alloc([1024], F32) for _ in range(2)]
        t1 = [A.alloc([1024], F32) for _ in range(2)]
        xm = [A.alloc([1024], F32) for _ in range(2)]
        hb = [A.alloc([1024], BF16) for _ in range(2)]
        ott = [A.alloc([8, 128], BF16) for _ in range(2)]
        ss3 = A.alloc([NTC, 2], F32); rs3 = A.alloc([NTC, 2], F32)
        xdst = (xs, xcs)

        def p3_mm(i):
            s2 = i % 2
            pb = 2 * s2
            ot_r = [L + f"OT{c0}_{i}" for c0 in (0, 1, 2, 4, 6)]
            DMA("sp", ott[s2], otd[i], ot_r, [L + f"ott{s2}"], f"ott{s2}")
            for hf in range(2):
                for kc in range(8):
                    MM(bank(pb + hf), ott[s2][:, kc, :], wo[:, kc, hf * 512:(hf + 1) * 512], kc == 0, kc == 7, [L + f"ott{s2}", L + "wo"], [f"ps{pb + hf}"])

        def p3_s1(i):
            s2 = i % 2
            pb = 2 * s2
            yps = psum[:, 512 * pb:512 * pb + 1024]
            ACTV(junk, yps, AF.Square, [f"ps{pb}", f"ps{pb + 1}"], ["junk", L + f"s3_{i}"], accum=ss3[:, i, 0:1])
            rstd_from_ss(ss3[:, i, 0:1], 1024, rs3[:, i, 0:1], [L + f"s3_{i}", "epsc"], [L + f"r3_{i}"])
            DMA("sp", xt[s2], xtile_ap(xsrc, i), [], [L + f"p3xt{s2}"], f"xt{s2}")

        def p3_s2(i):
            s2 = i % 2
            v = 0 if i < NT else 1
            pb = 2 * s2
            yps = psum[:, 512 * pb:512 * pb + 1024]
            STT("dve", t1[s2], yps, rs3[:, i, 0:1], MOD[:, v, 2, :], ALU.mult, ALU.mult, [f"ps{pb}", f"ps{pb + 1}", L + f"r3_{i}", f"MOD{v}2"], [L + f"p3t1{s2}"])
            TT("dve", xm[s2], t1[s2], xt[s2], ALU.add, [L + f"p3t1{s2}", L + f"p3xt{s2}"], [L + f"xm{s2}"])
            DMA("sp", xtile_ap(xdst, i), xm[s2], [L + f"xm{s2}"], [L + f"xs{i}"], f"xst{s2}")
            ACTV(junk, xm[s2], AF.Square, [L + f"xm{s2}"], ["junk", L + f"s4_{i}"], accum=ss3[:, i, 1:2])
            rstd_from_ss(ss3[:, i, 1:2], 1024, rs3[:, i, 1:2], [L + f"s4_{i}", "epsc"], [L + f"r4_{i}"])

        def p3_s3(i):
            s2 = i % 2
            v = 0 if i < NT else 1
            STT("dve", t1[s2], xm[s2], rs3[:, i, 1:2], MOD[:, v, 4, :], ALU.mult, ALU.mult, [L + f"xm{s2}", L + f"r4_{i}", f"MOD{v}4"], [L + f"p3t1{s2}"])
            if moe:
                TT("dve", xt[s2], t1[s2], MOD[:, v, 3, :], ALU.add, [L + f"p3t1{s2}", f"MOD{v}3"], [L + f"p3xt{s2}"])
                CP("act", hb[s2], xt[s2], [L + f"p3xt{s2}"], [L + f"p3hb{s2}"])
                for e_ in range(8):
                    TT("dve", rj[e_ % 3], xt[s2], RB[:, e_, :], ALU.mult, [L + f"p3xt{s2}", L + "rb"], [L + f"rj{e_ % 3}"])
                    ACTV(junk, rj[e_ % 3], AF.Identity, [L + f"rj{e_ % 3}"], ["junk", L + f"logi{i}"], accum=LOGI[:, i, e_:e_ + 1])
                lg_ = LOGI[:, i, :]
                RED("dve", sm[:, 0, 0:1], lg_, [L + f"logi{i}"], [L + "sm"], mx=True)
                TS("dve", sm[:, 1, :], lg_, sm[:, 0, 0:1], ALU.is_equal, [L + f"logi{i}", L + "sm"], [L + "sm"])
                STT("dve", sm[:, 2, :], sm[:, 1, :], -1e30, lg_, ALU.mult, ALU.add, [L + "sm", L + f"logi{i}"], [L + "sm"])
                RED("dve", sm[:, 0, 1:2], sm[:, 2, :], [L + "sm"], [L + "sm"], mx=True)
                TS("dve", sm[:, 3, :], lg_, sm[:, 0, 1:2], ALU.is_ge, [L + f"logi{i}", L + "sm"], [L + "sm"])
                TS("dve", sm[:, 0, 2:3], sm[:, 0, 0:1], -1.0, ALU.mult, [L + "sm"], [L + "sm"])
                ACTV(sm[:, 4, :], lg_, AF.Exp, [L + f"logi{i}", L + "sm"], [L + "sm"], bias=sm[:, 0, 2:3])
                TT("dve", sm[:, 4, :], sm[:, 4, :], sm[:, 3, :], ALU.mult, [L + "sm"], [L + "sm"])
                RED("dve", sm[:, 0, 3:4], sm[:, 4, :], [L + "sm"], [L + "sm"])
                RECIP(sm[:, 0, 3:4], sm[:, 0, 3:4], [L + "sm"], [L + "sm"])
                TS("dve", GATES[:, i, :], sm[:, 4, :], sm[:, 0, 3:4], ALU.mult, [L + "sm"], [L + f"gates{i}"])
            else:
                TT("dve", hb[s2], t1[s2], MOD[:, v, 3, :], ALU.add, [L + f"p3t1{s2}", f"MOD{v}3"], [L + f"p3hb{s2}"])

        def p3_tr(i):
            s2 = i % 2
            ts = slice(i * 128, (i + 1) * 128)
            pt_ = 4 + s2
            for kc in range(8):
                TR(bank_bf(pt_)[:, kc * 128:(kc + 1) * 128], hb[s2][:, kc * 128:(kc + 1) * 128], ident_bf, [L + f"p3hb{s2}", "ident_bf"], [f"ps{pt_}"])
            CP("act", h2T[:, :, ts], bank_bf(pt_).rearrange("p (k t) -> p k t", t=128), [f"ps{pt_}"], [L + f"h2T{i}"])

        p3_mm(0)
        p3_s1(0)
        if nto > 1:
            p3_mm(1)
        for i in range(nto):
            p3_s2(i)
            if i + 1 < nto:
                p3_s1(i + 1)
            p3_s3(i)
            if i + 2 < nto:
                p3_mm(i + 2)
            p3_tr(i)
        H2_ALL = [L + f"h2T{i}" for i in range(nto)]
        A.pop()
        if check_stop(f"p3_{l}"):
            A.pop(); break

        P.barrier()
        Y = A.alloc([nto, 1024], F32)
        A.push()
        wg = [A.alloc([8, 256], BF16) for _ in range(2)]
        wu = [A.alloc([8, 256], BF16) for _ in range(2)]
        wd = [A.alloc([2, 1024], BF16) for _ in range(2)]
        sg = [A.alloc([512], BF16) for _ in range(2)]
        AT = [A.alloc([2, 512], BF16) for _ in range(2)]
        ntok = nto * 128
        tblocks = [(t0, min(512, ntok - t0)) for t0 in range(0, ntok, 512)]
        if moe:
            slabs = [(e_, s_) for e_ in range(8) for s_ in range(14)]
        else:
            slabs = [(None, s_) for s_ in range(11)]
        nmm = 0
        for si, (e_, s_) in enumerate(slabs):
            sl = si % 2
            if moe:
                gsrc = I["mwg"][e_].rearrange("(kc p) f -> p kc f", p=128)[:, :, s_ * 256:(s_ + 1) * 256]
                usrc = I["mwu"][e_].rearrange("(kc p) f -> p kc f", p=128)[:, :, s_ * 256:(s_ + 1) * 256]
                dsrc = I["mwd"][e_][s_ * 256:(s_ + 1) * 256, :].rearrange("(c p) n -> p c n", p=128)
            else:
                gsrc = I["fwg"].rearrange("(kc p) f -> p kc f", p=128)[:, :, s_ * 256:(s_ + 1) * 256]
                usrc = I["fwu"].rearrange("(kc p) f -> p kc f", p=128)[:, :, s_ * 256:(s_ + 1) * 256]
                dsrc = I["fwd"][s_ * 256:(s_ + 1) * 256, :].rearrange("(c p) n -> p c n", p=128)
            DMA("pool", wg[sl], gsrc, [], [L + f"wg{sl}"], f"fw{sl}")
            DMA("pool", wu[sl], usrc, [], [L + f"wu{sl}"], f"fw{sl}")
            DMA("pool", wd[sl], dsrc, [], [L + f"wd{sl}"], f"fw{sl}")
            for bi, (t0, nt_) in enumerate(tblocks):
                a2 = bi % 2
                for fcl in range(2):
                    pg = nmm % 2; nmm += 1
                    for kc in range(8):
                        MM(bank(pg, nt_), wg[sl][:, kc, fcl * 128:(fcl + 1) * 128], h2T[:, kc, t0:t0 + nt_], kc == 0, kc == 7, H2_ALL + [L + f"wg{sl}"], [f"ps{pg}"])
                    for kc in range(8):
                        MM(bank(2 + pg, nt_), wu[sl][:, kc, fcl * 128:(fcl + 1) * 128], h2T[:, kc, t0:t0 + nt_], kc == 0, kc == 7, H2_ALL + [L + f"wu{sl}"], [f"ps{2 + pg}"])
                    ACTV(sg[pg][:, 0:nt_], bank(pg, nt_), AF.Silu, [f"ps{pg}"], [L + f"sg{pg}"])
                    TT("dve", AT[a2][:, fcl, 0:nt_], sg[pg][:, 0:nt_], bank(2 + pg, nt_), ALU.mult, [L + f"sg{pg}", f"ps{2 + pg}"], [L + f"at{a2}_{fcl}"])
                for tt in range(nt_ // 128):
                    ti = t0 // 128 + tt
                    py = 4 + 2 * (ti % 2)
                    for hf in range(2):
                        for fcl in range(2):
                            MM(bank(py + hf), AT[a2][:, fcl, tt * 128:(tt + 1) * 128], wd[sl][:, fcl, hf * 512:(hf + 1) * 512], fcl == 0, fcl == 1,
                               [L + f"at{a2}_0", L + f"at{a2}_1", L + f"wd{sl}"], [f"ps{py + hf}"])
                    yps = psum[:, 512 * py:512 * py + 1024]
                    rr = [f"ps{py}", f"ps{py + 1}"]
                    if moe:
                        gsc = GATES[:, ti, e_:e_ + 1]
                        if si == 0:
                            TS("dve", Y[:, ti, :], yps, gsc, ALU.mult, rr + [L + f"gates{ti}"], [L + f"Y{ti}"])
                        else:
                            STT("dve", Y[:, ti, :], yps, gsc, Y[:, ti, :], ALU.mult, ALU.add, rr + [L + f"gates{ti}", L + f"Y{ti}"], [L + f"Y{ti}"])
                    else:
                        if si == 0:
                            CP("dve", Y[:, ti, :], yps, rr, [L + f"Y{ti}"])
                        else:
                            TT("dve", Y[:, ti, :], yps, Y[:, ti, :], ALU.add, rr + [L + f"Y{ti}"], [L + f"Y{ti}"])
        A.pop()
        if check_stop(f"p4_{l}"):
            A.pop(); break

        A.push()
        ss5 = A.alloc([NTC], F32); rs5 = A.alloc([NTC], F32)
        xt = [A.alloc([1024], F32) for _ in range(2)]
        t1 = [A.alloc([1024], F32) for _ in range(2)]
        xo = [A.alloc([1024], F32) for _ in range(2)]
        def p5_load(i):
            DMA("sp", xt[i % 2], xtile_ap(xdst, i), [L + f"xs{i}"], [L + f"p5xt{i % 2}"], f"xt{i % 2}")

        p5_load(0)
        for i in range(nto):
            s2 = i % 2
            v = 0 if i < NT else 1
            ACTV(junk, Y[:, i, :], AF.Square, [L + f"Y{i}"], ["junk", L + f"s5_{i}"], accum=ss5[:, i:i + 1])
            rstd_from_ss(ss5[:, i:i + 1], 1024, rs5[:, i:i + 1], [L + f"s5_{i}", "epsc"], [L + f"r5_{i}"])
            if i + 1 < nto:
                p5_load(i + 1)
            STT("dve", t1[s2], Y[:, i, :], rs5[:, i:i + 1], MOD[:, v, 5, :], ALU.mult, ALU.mult, [L + f"Y{i}", L + f"r5_{i}", f"MOD{v}5"], [L + f"p5t1{s2}"])
            TT("dve", xo[s2], t1[s2], xt[s2], ALU.add, [L + f"p5t1{s2}", L + f"p5xt{s2}"], [L + f"xo{s2}"])
            if l == 0:
                DMA("sp", xtile_ap(xdst, i), xo[s2], [L + f"xo{s2}"], [L + f"xs{i}"], f"xst{s2}")
                if "dbg_x" in dbg:
                    dd = dbg["dbg_x"][i * 128:(i + 1) * 128, :] if i < NT else dbg["dbg_xc"][(i - NT) * 128:(i - NT + 1) * 128, :]
                    DMA("sp", dd, xo[s2], [L + f"xo{s2}"], [L + f"dbgx{i}"], "dbg")
            else:
                DMA("sp", out[i * 128:(i + 1) * 128, :], xo[s2], [L + f"xo{s2}"], [f"out{i}"], f"xst{s2}")
        A.pop()
        A.pop()
        if check_stop(f"l{l}"):
            break
    return nc, P, es, A, I


def _emit(nc, P, es):
    tls = P.finalize()
    sems = {tl: es.enter_context(nc.semaphore("s_" + str(tl))) for tl in tls}
    with nc.Block() as block:
        block.sync(P.engine_body("sp", sems, final=True))
        block.tensor(P.engine_body("pe", sems))
        block.vector(P.engine_body("dve", sems))
        block.scalar(P.engine_body("act", sems))
        block.gpsimd(P.engine_body("pool", sems))


_CACHE = {}


def _get_program():
    if "nc" not in _CACHE:
        nc, P, es, A, I = build_program()
        _CACHE["inputs"] = list(I.keys())
        with es:
            _emit(nc, P, es)
        _CACHE["nc"] = nc
        _CACHE["peak"] = A.peak
        _CACHE["nops"] = len(P.ops)
    return _CACHE["nc"]


def _host_inputs(inp):
    f = lambda a: np.ascontiguousarray(np.asarray(a, dtype=np.float32))
    shared = {}
    for l in range(2):
        shared[f"wmod{l}"] = f(inp["w_mod"][l])
        shared[f"bmod{l}"] = f(inp["b_mod"][l]).reshape(1, 6144)
        shared[f"gvec{l}"] = f(np.concatenate([inp["g_attn_pre"][l], inp["g_attn_post"][l], inp["g_ffn_pre"][l], inp["g_ffn_post"][l]])).reshape(1, 4096)
        shared[f"win{l}"] = f(np.asarray(inp["w_in"][l])[:, WIN_PERM])
        shared[f"wout{l}"] = f(inp["w_out"][l])
        shared[f"dal{l}"] = f(np.concatenate([inp["da_lambda_q1"][l], inp["da_lambda_k1"][l], inp["da_lambda_q2"][l], inp["da_lambda_k2"][l]])).reshape(1, 128)
        shared[f"subln{l}"] = f(inp["da_subln"][l]).reshape(1, 64)
        shared[f"sink{l}"] = f(inp["swa_sink"][l]).reshape(1, 4)
        shared[f"gam{l}"] = f(np.concatenate([inp["ret_gamma_fwd"][l], inp["ret_gamma_bwd"][l]])).reshape(1, 8)
        shared[f"nabias{l}"] = _na_bias_layout(np.asarray(inp["na_rpb"][l], dtype=np.float32))
    shared["fwg"] = f(inp["ffn_w_gate"][0]); shared["fwu"] = f(inp["ffn_w_up"][0]); shared["fwd"] = f(inp["ffn_w_down"][0])
    shared["router"] = f(np.asarray(inp["moe_router"][0]).T).reshape(1, 8 * 1024)
    shared["mwg"] = f(inp["moe_w_gate"][0]); shared["mwu"] = f(inp["moe_w_up"][0]); shared["mwd"] = f(inp["moe_w_down"][0])
    shared["idxb"] = _idxb_table()
    x = np.asarray(inp["x"], dtype=np.float32); ctx = np.asarray(inp["ctx"], dtype=np.float32)
    c = np.asarray(inp["c"], dtype=np.float32); c_ctx = np.asarray(inp["c_ctx"], dtype=np.float32)
    maps = []
    for core in range(8):
        b, j = core // 4, core % 4
        m = dict(shared)
        m["xin"] = np.ascontiguousarray(x[b, TOK * j:TOK * (j + 1)])
        m["xcin"] = np.ascontiguousarray(ctx[b])
        m["cvec"] = np.ascontiguousarray(np.concatenate([c[b].reshape(8, 128).T, c_ctx.reshape(8, 128).T], axis=1))
        C64, S64 = _rope_tables(j, 64)
        C32, S32 = _rope_tables(j, 32)
        m["rope64"] = np.ascontiguousarray(np.stack([C64, S64], axis=1))
        m["rope32"] = np.ascontiguousarray(np.stack([C32, S32], axis=1))
        m["swamask"] = _swa_masks(j)
        m["namask"] = _na_masks(j)
        m["retc"] = _ret_consts(j)
        if "inputs" in _CACHE:
            m = {k: v for k, v in m.items() if k in _CACHE["inputs"]}
        maps.append(m)
    return maps


def kernel(**inputs):
    nc = _get_program()
    maps = _host_inputs(inputs)
    res = run_bass_kernel_spmd(nc, maps, core_ids=list(range(8)))
    _CACHE["last"] = res
    outp = np.empty((2, 8192, 1024), np.float32)
    for core in range(8):
        b, j = core // 4, core % 4
        outp[b, TOK * j:TOK * (j + 1)] = res.results[core]["out"]
    return outp
```

```python
import contextlib
import os
import math
import numpy as np
import concourse.bass as bass
import concourse.mybir as mybir
from concourse.bass_utils import run_bass_kernel_spmd

F32 = mybir.dt.float32
BF16 = mybir.dt.bfloat16
AF = mybir.ActivationFunctionType
ALU = mybir.AluOpType
AX = mybir.AxisListType
ENGS = ("pe", "act", "dve", "pool", "sp")
EPS = 1e-6
NT = 16
NTC = 18
TOK = 2048
TOKC = 2304
DEBUG = []
STOP_AFTER = None


class Op:
    __slots__ = ("eng", "fn", "tl", "deps", "awaited", "count", "inc", "idx")


class Prog:
    def __init__(self):
        self.ops = []
        self.last_w = {}
        self.readers = {}
        self.tl_last = {}
        self.bar = set()
        self.bar_done = set(ENGS)

    def op(self, eng, fn, reads=(), writes=(), tl=None, inc=1):
        o = Op()
        o.eng = eng
        o.fn = fn
        o.tl = tl if tl is not None else eng
        o.inc = inc
        o.awaited = o.tl not in ENGS
        o.count = None
        o.idx = len(self.ops)
        deps = set()
        for r in reads:
            w = self.last_w.get(r)
            if w is not None:
                deps.add(w)
        for w_ in writes:
            w = self.last_w.get(w_)
            if w is not None:
                deps.add(w)
            rl = self.readers.get(w_)
            if rl:
                deps.update(rl)
        if eng not in self.bar_done:
            deps |= self.bar
            self.bar_done.add(eng)
        o.deps = deps
        self.ops.append(o)
        for r in reads:
            self.readers.setdefault(r, []).append(o.idx)
        for w_ in writes:
            self.last_w[w_] = o.idx
            self.readers[w_] = []
        self.tl_last[o.tl] = o.idx
        return o

    def barrier(self):
        self.bar = set(self.tl_last.values())
        self.bar_done = set()

    def finalize(self):
        ops = self.ops
        for i in self.tl_last.values():
            ops[i].awaited = True
        for o in ops:
            for d in o.deps:
                od = ops[d]
                if od.tl == "pe" and o.tl == "pe":
                    continue
                od.awaited = True
        cnt = {}
        for o in ops:
            if o.awaited:
                cnt[o.tl] = cnt.get(o.tl, 0) + o.inc
                o.count = cnt[o.tl]
        self.totals = cnt
        run_latest = {}
        self.need = [None] * len(ops)
        for o in ops:
            need = {}
            for d in o.deps:
                od = ops[d]
                if od.tl == "pe" and o.tl == "pe":
                    continue
                v = od.count if od.tl in ENGS else run_latest[od.tl]
                if need.get(od.tl, 0) < v:
                    need[od.tl] = v
            self.need[o.idx] = need
            if o.awaited:
                run_latest[o.tl] = o.count
        return sorted(cnt.keys(), key=str)

    def engine_body(self, ename, sems, final=False):
        mine = [o for o in self.ops if o.eng == ename]

        def body(e):
            waited = {}
            for o in mine:
                for tl, v in self.need[o.idx].items():
                    if waited.get(tl, 0) < v:
                        e.wait_ge(sems[tl], v)
                        waited[tl] = v
                ins = o.fn(e)
                if o.awaited:
                    ins.then_inc(sems[o.tl], o.inc)
            if final:
                for tl, v in self.totals.items():
                    if waited.get(tl, 0) < v:
                        e.wait_ge(sems[tl], v)
        return body


class Arena:
    def __init__(self, ap, nbytes, prog=None):
        self.prog = prog
        self.ap = ap
        self.cap = nbytes
        self.off = 0
        self.stack = []
        self.peak = 0

    def alloc(self, shape, dt):
        shape = list(shape)
        n = int(np.prod(shape))
        nb = n * (4 if dt == F32 else 2)
        nb = (nb + 63) // 64 * 64
        assert self.off + nb <= self.cap, f"SBUF arena overflow {self.off}+{nb}>{self.cap}"
        v = self.ap[:, self.off // 2:(self.off + nb) // 2]
        if dt == F32:
            v = v.bitcast(F32)
        v = v[:, 0:n]
        self.off += nb
        self.peak = max(self.peak, self.off)
        if len(shape) == 2:
            v = v.rearrange("p (a b) -> p a b", b=shape[1])
        elif len(shape) == 3:
            v = v.rearrange("p (a b c) -> p a b c", b=shape[1], c=shape[2])
        elif len(shape) == 4:
            v = v.rearrange("p (a b c d) -> p a b c d", b=shape[1], c=shape[2], d=shape[3])
        return v

    def push(self):
        self.stack.append(self.off)

    def pop(self):
        self.off = self.stack.pop()
        if self.prog is not None:
            self.prog.barrier()


def _swap_idx(dh):
    q = dh // 4
    return np.concatenate([np.arange(q, 2 * q), np.arange(0, q), np.arange(3 * q, 4 * q), np.arange(2 * q, 3 * q)])


def _win_perm():
    cols = []
    base = 0
    q = np.arange(base, base + 256)
    k = np.arange(base + 256, base + 512)
    v = np.arange(base + 512, base + 768)
    sw32 = np.concatenate([_swap_idx(32) + 32 * i for i in range(8)])
    cols += [q, k, q[sw32], k[sw32], v]
    base = 768
    qn = np.arange(base, base + 256).reshape(2, 2, 64)
    qperm = np.transpose(qn, (1, 0, 2)).reshape(256)
    kk = np.arange(base + 256, base + 384)
    vv = np.arange(base + 384, base + 512)
    sw64_4 = np.concatenate([_swap_idx(64) + 64 * i for i in range(4)])
    sw64_2 = np.concatenate([_swap_idx(64) + 64 * i for i in range(2)])
    cols += [qperm, kk, qperm[sw64_4], kk[sw64_2], vv]
    base = 1280
    cols += [np.arange(base, base + 768)]
    base = 2048
    q = np.arange(base, base + 256)
    k = np.arange(base + 256, base + 512)
    vg = np.arange(base + 512, base + 1024)
    cols += [q, k, q[sw64_4], k[sw64_4], vg]
    return np.concatenate(cols)


WIN_PERM = _win_perm()
NWIN = len(WIN_PERM)
DA0, SW0, NA0, RT0 = 0, 1280, 2176, 2944


def _rope_tables(j, dh):
    t = 2048 * j + np.arange(2048)
    row = (t // 64).astype(np.float64)
    col = (t % 64).astype(np.float64)
    half = dh // 2
    qd = dh // 4
    inv = 10000.0 ** (-np.arange(qd, dtype=np.float64) * 2.0 / half)
    C = np.zeros((128, 2048), np.float32)
    S = np.zeros((128, 2048), np.float32)
    for p in range(128):
        d = p % dh
        pos = row if d < half else col
        dd = d % half
        i = dd % qd
        ang = pos * inv[i]
        C[p] = np.cos(ang)
        S[p] = -np.sin(ang) if dd < qd else np.sin(ang)
    return C, S


def _swa_masks(j):
    kk = np.arange(128)[:, None]
    qq = np.arange(128)[None, :]
    mprev = (qq <= kk).astype(np.float32)
    mnext = (kk <= qq).astype(np.float32)
    m = np.zeros((10, 128, 128), np.float32)
    m[0] = mprev
    m[1] = mnext
    for r in range(4):
        if r == j - 1:
            m[2 + r] = mprev
        if r == j + 1:
            m[6 + r] = mnext
    return m


def _na_mask(Tq, Tk, flag=True):
    if (not flag) or Tk < 0 or Tk > 63:
        return np.zeros((128, 128), np.float32)
    p = np.arange(128)
    Rk = (2 * Tk + p // 64)[:, None]
    kc = (p % 64)[:, None]
    Rq = (2 * Tq + p // 64)[None, :]
    qc = (p % 64)[None, :]
    start = np.clip(Rq - 4, 0, 120)
    cs = np.clip(qc - 8, 0, 48)
    ok = (Rk >= start) & (Rk < start + 8) & (kc >= cs) & (kc < cs + 16)
    return ok.astype(np.float32)


def _na_masks(j):
    m = np.zeros((45, 128, 128), np.float32)
    for d in range(-2, 3):
        m[d + 2] = _na_mask(10, 10 + d)
    T0 = 16 * j
    idx = 5
    for d in (0, 1, 2, 3):
        m[idx] = _na_mask(T0, T0 + d); idx += 1
    for r in range(4):
        m[idx] = _na_mask(T0, T0 - 2, r == j - 1); idx += 1
    for r in range(4):
        m[idx] = _na_mask(T0, T0 - 1, r == j - 1); idx += 1
    for d in (-1, 0, 1, 2):
        m[idx] = _na_mask(T0 + 1, T0 + 1 + d); idx += 1
    for r in range(4):
        m[idx] = _na_mask(T0 + 1, T0 - 1, r == j - 1); idx += 1
    for d in (-2, -1, 0, 1):
        m[idx] = _na_mask(T0 + 14, T0 + 14 + d); idx += 1
    for r in range(4):
        m[idx] = _na_mask(T0 + 14, T0 + 16, r == j + 1); idx += 1
    for d in (-3, -2, -1, 0):
        m[idx] = _na_mask(T0 + 15, T0 + 15 + d); idx += 1
    for r in range(4):
        m[idx] = _na_mask(T0 + 15, T0 + 16, r == j + 1); idx += 1
    for r in range(4):
        m[idx] = _na_mask(T0 + 15, T0 + 17, r == j + 1); idx += 1
    assert idx == 45
    return m


def _na_bias_layout(rpb):
    p = np.arange(128)
    kr = (p // 64)[:, None]; kc = (p % 64)[:, None]
    qr = (p // 64)[None, :]; qc = (p % 64)[None, :]
    out = np.empty((7, 4, 128, 128), np.float32)
    dc = np.clip(kc - qc, -15, 15) + 15
    for di, d in enumerate(range(-3, 4)):
        dr = np.clip(2 * d + kr - qr, -7, 7) + 7
        out[di] = rpb[:, dr, dc]
    return out


def _ret_consts(j):
    c = np.zeros((128, 700), np.float32)
    i = np.arange(128)
    o = 0
    dif = i[None, :] - i[:, None]
    c[:, 0:128] = np.maximum(dif, 0)
    c[:, 128:256] = (dif >= 0) * 0.125
    c[:, 256:384] = np.maximum(-dif, 0)
    c[:, 384:512] = (dif < 0) * 0.125
    c[:, 512:640] = (i + 1)[None, :]
    c[:, 640] = 127 - i
    c[:, 641] = i
    c[:, 642:660] = (128.0 * np.arange(18))[None, :]
    c[:, 660:678] = (128.0 * (15 - np.arange(18)))[None, :]
    for r in range(4):
        if r < j:
            c[:, 678 + r] = 2048.0 * (j - 1 - r); c[:, 688 + r] = 1.0
        if r > j:
            c[:, 683 + r] = 2048.0 * (r - j - 1); c[:, 693 + r] = 1.0
    c[:, 682] = 2048.0 * j; c[:, 692] = 1.0
    c[:, 687] = 2048.0 * (3 - j); c[:, 697] = 1.0
    return c


def _idxb_table():
    i = np.arange(128)
    return np.broadcast_to((128 - i)[None, :], (128, 128)).astype(np.float32).copy()


def build_program():
    nc = bass.Bass("TRN2", target_bir_lowering=False)
    P = Prog()
    es = contextlib.ExitStack()

    def din(name, shape, dt=F32):
        return nc.dram_tensor(name, list(shape), dt, kind="ExternalInput").ap()

    def dint(name, shape, dt):
        return nc.dram_tensor(name, list(shape), dt)

    SHAPES = {"xin": [TOK, 1024], "xcin": [256, 1024], "cvec": [128, 16], "fwg": [1024, 2816], "fwu": [1024, 2816], "fwd": [2816, 1024],
              "router": [1, 8 * 1024], "mwg": [8, 1024, 3584], "mwu": [8, 1024, 3584], "mwd": [8, 3584, 1024],
              "rope64": [128, 2, 2048], "rope32": [128, 2, 2048], "swamask": [10, 128, 128], "namask": [45, 128, 128],
              "retc": [128, 700], "idxb": [128, 128]}
    for l_ in range(2):
        SHAPES.update({f"wmod{l_}": [1024, 6144], f"bmod{l_}": [1, 6144], f"gvec{l_}": [1, 4096], f"win{l_}": [1024, NWIN],
                       f"wout{l_}": [1024, 1024], f"dal{l_}": [1, 128], f"subln{l_}": [1, 64], f"sink{l_}": [1, 4],
                       f"gam{l_}": [1, 8], f"nabias{l_}": [7, 4, 128, 128]})

    class LazyIn(dict):
        def __missing__(self, k):
            self[k] = din(k, SHAPES[k])
            return self[k]
    I = LazyIn()
    USED_INPUTS = I
    out = nc.dram_tensor("out", [TOK, 1024], F32, kind="ExternalOutput").ap()
    dbg = {}
    for name, shape, dt in (("dbg_ot", [NTC, 128, 8, 128], BF16), ("dbg_x", [TOK, 1024], F32), ("dbg_xc", [256, 1024], F32),
                            ("dbg_misc", [128, 4096], F32)):
        if name in DEBUG:
            dbg[name] = nc.dram_tensor(name, shape, dt, kind="ExternalOutput").ap()

    xs = dint("xs", [TOK, 1024], F32).ap(); xcs = dint("xcs", [256, 1024], F32).ap()
    GROUPS = [[0, 1, 2, 3], [4, 5, 6, 7]]

    arena_t = es.enter_context(nc.sbuf_tensor("arena", [128, 94 * 1024], BF16))
    A = Arena(arena_t, 188 * 1024, P)
    psum = es.enter_context(nc.psum_tensor("psum", [128, 4096], F32))

    def bank(i, n=512, off=0):
        return psum[:, 512 * i + off:512 * i + off + n]

    def bank_bf(i):
        return psum[:, 512 * i:512 * (i + 1)].bitcast(BF16)

    def MM(o, lhsT, rhs, st, sp_, r, w, tp=None, sgc=False):
        kw = {}
        if tp is not None:
            kw["tile_position"] = tp
        if sgc:
            kw["skip_group_check"] = True
        P.op("pe", lambda e: e.matmul(o, lhsT=lhsT, rhs=rhs, start=st, stop=sp_, **kw), r, w)

    def MM64(o, lhsT, rhs, base, st, sp_, r, w):
        if base == 0:
            MM(o, lhsT[0:64], rhs[0:64], st, sp_, r, w, sgc=True)
        else:
            MM(o, lhsT[64:96], rhs[64:96], st, False, r, w, tp=(64, 0), sgc=True)
            MM(o, lhsT[96:128], rhs[96:128], False, sp_, r, w, tp=(96, 0), sgc=True)

    def TR(o, i, ident, r, w):
        P.op("pe", lambda e: e.transpose(o, i, ident), r, w)

    def ACTV(o, i, func, r, w, bias=None, scale=None, accum=None):
        kw = {}
        if bias is not None:
            kw["bias"] = bias
        if scale is not None:
            kw["scale"] = scale
        if accum is not None:
            kw["accum_out"] = accum
        P.op("act", lambda e: e.activation(out=o, in_=i, func=func, **kw), r, w)

    def TT(eng, o, a, b, op, r, w):
        P.op(eng, lambda e: e.tensor_tensor(out=o, in0=a, in1=b, op=op), r, w)

    def TS(eng, o, a, s1, op0, r, w, s2=None, op1=None):
        if op1 is None:
            P.op(eng, lambda e: e.tensor_scalar(out=o, in0=a, scalar1=s1, scalar2=None, op0=op0), r, w)
        else:
            P.op(eng, lambda e: e.tensor_scalar(out=o, in0=a, scalar1=s1, scalar2=s2, op0=op0, op1=op1), r, w)

    def STT(eng, o, a, s, b, op0, op1, r, w):
        P.op(eng, lambda e: e.scalar_tensor_tensor(out=o, in0=a, scalar=s, in1=b, op0=op0, op1=op1), r, w)

    def CP(eng, o, i, r, w):
        if eng == "act":
            P.op("act", lambda e: e.copy(out=o, in_=i), r, w)
        else:
            P.op(eng, lambda e: e.tensor_copy(out=o, in_=i), r, w)

    def MSET(eng, o, val, w):
        P.op(eng, lambda e: e.memset(o, val), (), w)

    def RED(eng, o, i, r, w, mx=False):
        if mx:
            P.op(eng, lambda e: e.reduce_max(out=o, in_=i, axis=AX.X), r, w)
        else:
            P.op(eng, lambda e: e.reduce_sum(out=o, in_=i, axis=AX.X), r, w)

    def RECIP(o, i, r, w):
        P.op("dve", lambda e: e.reciprocal(out=o, in_=i), r, w)

    def DMA(q, o, i, r, w, tl):
        P.op(q, lambda e: e.dma_start(out=o, in_=i), r, w, tl=tl, inc=16)

    def AG(src, dst, r, w, tl):
        P.op("pool", lambda e: e.collective_compute("AllGather", ALU.bypass, replica_groups=GROUPS,
                                                    ins=[src.ap().opt()], outs=[dst.ap().opt()]), r, w, tl=tl, inc=1)

    def rstd_from_ss(ss, n, rstd, r, w):
        ACTV(rstd, ss, AF.Sqrt, r, w, bias=epsc[:, 0:1], scale=1.0 / n)
        RECIP(rstd, rstd, w, w)

    ident_bf = A.alloc([128], BF16); ident_f = A.alloc([128], F32); zeros = A.alloc([128], BF16)
    epsc = A.alloc([1], F32)
    junk = A.alloc([1024], BF16)
    MOD = A.alloc([2, 6, 1024], BF16)
    MSET("pool", ident_f, 0.0, ["ident_f"])
    P.op("pool", lambda e: e.affine_select(out=ident_f, in_=ident_f, pattern=[[-1, 128]], compare_op=ALU.not_equal,
                                           fill=1.0, base=0, channel_multiplier=1), ["ident_f"], ["ident_f"])
    CP("pool", ident_bf, ident_f, ["ident_f"], ["ident_bf"])
    HM = A.alloc([2], F32)
    RED("dve", HM[:, 0:1], ident_f[:, 0:64], ["ident_f"], ["HM"])
    RED("dve", HM[:, 1:2], ident_f[:, 64:128], ["ident_f"], ["HM"])
    MSET("pool", zeros, 0.0, ["zeros"])
    MSET("pool", epsc, EPS, ["epsc"])

    def pbc(ap):
        b = ap.partition_broadcast(128)
        if len(b.shape) == 3 and b.shape[1] == 1:
            b = b[:, 0]
        return b

    stop = [False]

    def tap(name, ap, reads, flat):
        if name in DEBUG:
            shp = [128, int(np.prod(ap.shape[1:]))]
            d = nc.dram_tensor(name, shp, ap.dtype, kind="ExternalOutput").ap()
            DMA("sp", d, ap.rearrange(flat) if flat else ap, reads, [name], "dbg")

    def check_stop(name):
        if STOP_AFTER == name:
            stop[0] = True
        return stop[0]

    for l in range(2):
        if stop[0]:
            break
        with_ctx = (l == 0)
        lam_init = 0.8 - 0.6 * math.exp(-0.3 * l)
        ntl = NTC
        nto = NTC if with_ctx else NT
        xsrc = (I["xin"], I["xcin"]) if l == 0 else (xs, xcs)
        L = f"L{l}"

        def xtile_ap(src2, i):
            return src2[0][i * 128:(i + 1) * 128, :] if i < NT else src2[1][(i - NT) * 128:(i - NT + 1) * 128, :]

        P.barrier()
        A.push()
        cv = A.alloc([16], F32); sil = A.alloc([16], F32); sbc = A.alloc([2, 8, 128], BF16)
        gv = A.alloc([4, 1024], F32); tmpm = A.alloc([1024], F32)
        wm = [A.alloc([8, 1024], BF16) for _ in range(2)]
        bs = [A.alloc([1024], F32) for _ in range(2)]
        DMA("sp", cv, I["cvec"], [], [L + "cv"], "m0")
        DMA("sp", gv, pbc(I[f"gvec{l}"]).rearrange("p (a b) -> p a b", b=1024), [], [L + "gv"], "m0")
        ACTV(sil, cv, AF.Silu, [L + "cv"], [L + "sil"])
        for v in range(2):
            for kc in range(8):
                ACTV(sbc[:, v, kc, :], zeros, AF.Identity, [L + "sil", "zeros"], [L + "sbc"], bias=sil[:, v * 8 + kc:v * 8 + kc + 1])
        wmv = I[f"wmod{l}"].rearrange("(kc p) n -> p kc n", p=128)
        for s in range(6):
            sl = s % 2
            DMA("pool", wm[sl], wmv[:, :, s * 1024:(s + 1) * 1024], [], [L + f"wm{sl}"], f"wm{sl}")
            DMA("sp", bs[sl], pbc(I[f"bmod{l}"][0:1, s * 1024:(s + 1) * 1024]), [], [L + f"bs{sl}"], f"bs{sl}")
            for v in range(2):
                pb = 4 * (s % 2) + 2 * v
                for hf in range(2):
                    for kc in range(8):
                        MM(bank(pb + hf), sbc[:, v, kc, :], wm[sl][:, kc, hf * 512:(hf + 1) * 512], kc == 0, kc == 7,
                           [L + "sbc", L + f"wm{sl}"], [f"ps{pb + hf}"])
                TT("dve", tmpm, psum[:, 512 * pb:512 * pb + 1024], bs[sl], ALU.add, [f"ps{pb}", f"ps{pb + 1}", L + f"bs{sl}"], [L + "tmpm"])
                dst = MOD[:, v, s, :]
                if s in (0, 3):
                    CP("dve", dst, tmpm, [L + "tmpm"], [f"MOD{v}{s}"])
                elif s in (1, 4):
                    STT("dve", dst, tmpm, 1.0, gv[:, 0 if s == 1 else 2, :], ALU.add, ALU.mult, [L + "tmpm", L + "gv"], [f"MOD{v}{s}"])
                else:
                    TT("dve", dst, tmpm, gv[:, 1 if s == 2 else 3, :], ALU.mult, [L + "tmpm", L + "gv"], [f"MOD{v}{s}"])
        if l == 0:
            tap("t_mod", MOD, [f"MOD{v}{s}" for v in range(2) for s in range(6)], "p a b c -> p (a b c)")
        A.pop()
        if check_stop(f"p0_{l}"):
            break

        P.barrier()
        A.push()
        moe = (l == 1)
        if moe:
            LOGI = A.alloc([NT, 8], F32); GATES = A.alloc([NT, 8], F32)
        hT = A.alloc([8, TOKC], BF16)
        otd = dint(L + "otd", [NTC, 128, 8, 128], BF16).ap()
        A.push()
        otst = [A.alloc([2, 128], BF16) for _ in range(2)]

        A.push()
        xt = [A.alloc([1024], F32) for _ in range(2)]
        t1 = [A.alloc([1024], F32) for _ in range(2)]
        hb = [A.alloc([1024], BF16) for _ in range(2)]
        ssb = A.alloc([NTC], F32); rsb = A.alloc([NTC], F32)
        def p1a_A(i):
            s2 = i % 2
            DMA("sp", xt[s2], xtile_ap(xsrc, i), [], [L + f"xt{s2}"], f"xt{s2}")
            ACTV(junk, xt[s2], AF.Square, [L + f"xt{s2}"], ["junk", L + f"ss{i}"], accum=ssb[:, i:i + 1])
            rstd_from_ss(ssb[:, i:i + 1], 1024, rsb[:, i:i + 1], [L + f"ss{i}", "epsc"], [L + f"rs{i}"])

        def p1a_B(i):
            s2 = i % 2
            v = 0 if i < NT else 1
            STT("dve", t1[s2], xt[s2], rsb[:, i:i + 1], MOD[:, v, 1, :], ALU.mult, ALU.mult, [L + f"xt{s2}", L + f"rs{i}", f"MOD{v}1"], [L + f"t1{s2}"])
            TT("dve", hb[s2], t1[s2], MOD[:, v, 0, :], ALU.add, [L + f"t1{s2}", f"MOD{v}0"], [L + f"hb{s2}"])
            for kc in range(8):
                TR(bank_bf(s2)[:, kc * 128:(kc + 1) * 128], hb[s2][:, kc * 128:(kc + 1) * 128], ident_bf, [L + f"hb{s2}", "ident_bf"], [f"ps{s2}"])

        def p1a_C(i):
            s2 = i % 2
            CP("act", hT[:, :, i * 128:(i + 1) * 128], bank_bf(s2).rearrange("p (k t) -> p k t", t=128), [f"ps{s2}"], [L + f"hT{i}"])

        p1a_A(0)
        for i in range(ntl):
            p1a_B(i)
            if i + 1 < ntl:
                p1a_A(i + 1)
            p1a_C(i)
        A.pop()
        HT_ALL = [L + f"hT{i}" for i in range(ntl)]
        if l == 0:
            tap("t_hT", hT, HT_ALL, "p a b -> p (a b)")
        if check_stop(f"p1a_{l}"):
            A.pop(); A.pop(); break

        winv = I[f"win{l}"].rearrange("(kc p) n -> p kc n", p=128)

        def proj_rope(wq, wqs, Ctab, Stab, dests, tag, name0=0):
            tA = [A.alloc([512], F32) for _ in range(2)]
            tB = [A.alloc([512], F32) for _ in range(2)]
            n = 0
            for ci in range(len(wq)):
                for tb in range(4):
                    s2 = n % 2; n += 1
                    ts = slice(tb * 512, (tb + 1) * 512)
                    for kc in range(8):
                        MM(bank(s2), wq[ci][:, kc, :], hT[:, kc, ts], kc == 0, kc == 7, HT_ALL[4 * tb:4 * tb + 4] + [tag + "w"], [f"ps{s2}"])
                    for kc in range(8):
                        MM(bank(2 + s2), wqs[ci][:, kc, :], hT[:, kc, ts], kc == 0, kc == 7, HT_ALL[4 * tb:4 * tb + 4] + [tag + "w"], [f"ps{2 + s2}"])
                    TT("dve", tA[s2], bank(s2), Ctab[:, ts], ALU.mult, [f"ps{s2}", L + "rope"], [tag + f"tA{s2}"])
                    TT("dve", tB[s2], bank(2 + s2), Stab[:, ts], ALU.mult, [f"ps{2 + s2}", L + "rope"], [tag + f"tB{s2}"])
                    TT("dve", dests[ci][:, ts], tA[s2], tB[s2], ALU.add, [tag + f"tA{s2}", tag + f"tB{s2}"], [tag + f"d{ci + name0}"])

        def proj_feat_plain(wq, dest, t0, nt, tag, ci, pb):
            for kc in range(8):
                MM(bank(pb, nt), wq[:, kc, :], hT[:, kc, t0:t0 + nt], kc == 0, kc == 7, HT_ALL + [tag + "w"], [f"ps{pb}"])
            CP("act", dest, bank(pb, nt), [f"ps{pb}"], [tag + f"d{ci}"])

        def proj_tok(wv, ncols, dest_fn, tiles, tag, post=None, view=None):
            for n, i in enumerate(tiles):
                pb = 4 + n % 2
                for kc in range(8):
                    MM(bank(pb, ncols), hT[:, kc, i * 128:(i + 1) * 128], wv[:, kc, :], kc == 0, kc == 7, [L + f"hT{i}", tag + "w"], [f"ps{pb}"])
                if post is None:
                    src_ = bank(pb, ncols)
                    if view is not None:
                        src_ = view(src_)
                    CP("act", dest_fn(i), src_, [f"ps{pb}"], [tag + f"v{i}"])
                else:
                    post(i, pb)

        def out_transposes(ytok, chunk0, i, tag, rd):
            pb = 6 + (i % 2)
            st = otst[i % 2]
            for cc in range(2):
                TR(bank_bf(pb)[:, cc * 128:(cc + 1) * 128], ytok[:, cc * 128:(cc + 1) * 128], ident_bf, rd + ["ident_bf"], [f"ps{pb}"])
            CP("act", st, bank_bf(pb)[:, 0:256].rearrange("p (c t) -> p c t", t=128), [f"ps{pb}"], [L + f"otst{i % 2}"])
            DMA("sp", otd[i, :, chunk0:chunk0 + 2, :], st, [L + f"otst{i % 2}"], [L + f"OT{chunk0}_{i}"], f"ot{i % 2}")

        def attn_pipeline(tiles_slots, S_fn, E_fn, AV_fn, FIN_fn):
            units = []
            for (i, slots) in tiles_slots:
                ngr = (len(slots) + 1) // 2
                for gi in range(ngr):
                    units.append((i, gi, ngr, slots[2 * gi:2 * gi + 2]))
            pending = None
            for k, u in enumerate(units):
                if k == 0:
                    S_fn(0, u)
                E_fn(k, u)
                if k + 1 < len(units):
                    S_fn(k + 1, units[k + 1])
                AV_fn(k, u)
                if pending is not None:
                    FIN_fn(pending); pending = None
                if u[1] == u[2] - 1:
                    pending = u[0]
            if pending is not None:
                FIN_fn(pending)

        rope64 = A.alloc([2, 2048], BF16); rope32 = A.alloc([2, 2048], BF16)
        DMA("pool", rope64, I["rope64"], [], [L + "rope"], "rp")
        DMA("pool", rope32, I["rope32"], [], [L + "rope"], "rp")

        T = L + "da"
        A.push()
        QT = A.alloc([2, TOKC], BF16); KTc = A.alloc([2, 256], BF16)
        Vc = A.alloc([2, 256], BF16)
        nlam = A.alloc([1], F32); gsub = A.alloc([64], F32)
        A.push()
        dl = A.alloc([4, 32], F32); pr = A.alloc([2, 32], F32); s12 = A.alloc([2], F32)
        DMA("sp", dl, pbc(I[f"dal{l}"]).rearrange("p (a b) -> p a b", b=32), [], [T + "dl"], "m0")
        DMA("sp", gsub, pbc(I[f"subln{l}"]), [], [T + "gsub"], "m0")
        TT("dve", pr[:, 0, :], dl[:, 0, :], dl[:, 1, :], ALU.mult, [T + "dl"], [T + "pr"])
        TT("dve", pr[:, 1, :], dl[:, 2, :], dl[:, 3, :], ALU.mult, [T + "dl"], [T + "pr"])
        RED("dve", s12, pr, [T + "pr"], [T + "s12"])
        ACTV(s12, s12, AF.Exp, [T + "s12"], [T + "s12"])
        TT("dve", nlam, s12[:, 1:2], s12[:, 0:1], ALU.subtract, [T + "s12"], [T + "nlam"])
        TS("dve", nlam, nlam, -lam_init, ALU.add, [T + "nlam"], [T + "nlam"])
        TS("dve", gsub, gsub, 1.0 - lam_init, ALU.mult, [T + "gsub"], [T + "gsub"])
        A.pop()
        A.push()
        wda = A.alloc([8, 1280], BF16)
        KTo = A.alloc([2, TOK], BF16); Vo = A.alloc([NT, 256], BF16)
        DMA("pool", wda, winv[:, :, DA0:DA0 + 1280], [], [T + "w"], "wA")
        qk_dest = [QT[:, 0, 0:TOK], QT[:, 1, 0:TOK], KTo[:, 0, :], KTo[:, 1, :]]
        wq_l = [wda[:, :, c * 128:(c + 1) * 128] for c in range(4)]
        wqs_l = [wda[:, :, 512 + c * 128:512 + (c + 1) * 128] for c in range(4)]
        proj_rope(wq_l[2:4], wqs_l[2:4], rope32[:, 0, :], rope32[:, 1, :], qk_dest[2:4], T, name0=2)
        e_k = dint(T + "ek", [256, TOK], BF16); e_v = dint(T + "ev", [TOK, 256], BF16)
        g_k = dint(T + "gk", [1024, TOK], BF16); g_v = dint(T + "gv", [4 * TOK, 256], BF16)
        DMA("sp", e_k.ap().rearrange("(c p) t -> p c t", p=128), KTo, [T + "d2", T + "d3"], [T + "ek"], "ex")
        AG(e_k, g_k, [T + "ek"], [T + "gk"], "cc")
        proj_tok(wda[:, :, 1024:1280], 256, lambda i: Vo[:, i, :] if i < NT else Vc[:, i - NT, :], range(NTC), T)
        V_R = [T + f"v{i}" for i in range(NTC)]
        DMA("sp", e_v.ap().rearrange("(i p) f -> p i f", p=128), Vo, V_R, [T + "ev"], "ex")
        AG(e_v, g_v, [T + "ev"], [T + "gv"], "cc")
        proj_rope(wq_l[0:2], wqs_l[0:2], rope32[:, 0, :], rope32[:, 1, :], qk_dest[0:2], T, name0=0)
        for ci in range(4):
            dest = QT[:, ci, TOK:TOKC] if ci < 2 else KTc[:, ci - 2, :]
            proj_feat_plain(wda[:, :, ci * 128:(ci + 1) * 128], dest, TOK, 256, T, f"c{ci}", ci % 2)
        QK_R = [T + f"d{ci}" for ci in range(4)] + [T + f"dc{ci}" for ci in range(4)]
        if check_stop(f"daproj_{l}"):
            tap("t_qt", QT, QK_R, "p a b -> p (a b)")
            tap("t_kto", KTo, QK_R, "p a b -> p (a b)")
            tap("t_vo", Vo, V_R, "p a b -> p (a b)")
            tap("t_ktc", KTc, QK_R, "p a b -> p (a b)")
            tap("t_vc", Vc, V_R, "p a b -> p (a b)")
            A.pop(); A.pop(); A.pop(); A.pop(); break
        A.pop()
        P.barrier()
        if check_stop(f"daag_{l}"):
            A.pop(); A.pop(); A.pop(); break
        KT = A.alloc([8448], BF16); V1 = A.alloc([66, 2, 65], BF16)
        ptH = [[A.alloc([2, 512], BF16) for _ in range(2)] for _ in range(2)]
        o_f = A.alloc([2, 64], F32); o1 = A.alloc([64], F32)
        rec = A.alloc([4], F32); rn = A.alloc([2], F32); ssd = A.alloc([2], F32); rsd = A.alloc([2], F32)
        yda = [A.alloc([2, 64], BF16) for _ in range(2)]
        MSET("pool", V1[:, :, :, 64:65], 1.0, [T + "V1ones"])
        g_kv = g_k.ap().rearrange("(r c p) t -> p r c t", r=4, c=2)
        g_vv = g_v.ap().rearrange("(r i p) f -> p r i f", r=4, i=NT)
        nblk = 0
        for c in range(2):
            CP("pool", KT[:, 0:256], KTc[:, c, :], QK_R, [T + "KT"])
            for r in range(4):
                DMA("sp", KT[:, 256 + r * TOK:256 + (r + 1) * TOK], g_kv[:, r, c, :], [T + "gk"], [T + "KT"], "kt")
                for hh in range(2):
                    DMA("sp", V1[:, 2 + r * NT:2 + (r + 1) * NT, hh, 0:64], g_vv[:, r, :, c * 128 + hh * 64:c * 128 + hh * 64 + 64],
                        [T + "gv"], [T + "V1"], "kt")
            CP("pool", V1[:, 0:2, :, 0:64], Vc[:, :, c * 128:(c + 1) * 128].rearrange("p i (h d) -> p i h d", d=64), V_R, [T + "V1"])
            if STOP_AFTER == f"daload_{l}":
                continue
            qblocks = [(qb * 512, 512, 0, 66) for qb in range(4)]
            if STOP_AFTER == f"daq1_{l}":
                qblocks = [(0, 512, 0, 66)] if c == 0 else []
            if STOP_AFTER == f"daq1nf_{l}":
                qblocks = [(0, 512, 0, 66)] if c == 0 else []
            if with_ctx:
                qblocks.append((TOK, 256, 0, 2))
            for (q0, nq, k0, k1) in qblocks:
                sc_ = 1.0 / math.sqrt(32.0)

                def S_half(kt, hf):
                    for g in (2 * hf, 2 * hf + 1):
                        MM(bank(g, nq), KT[32 * g:32 * g + 32, kt * 128:(kt + 1) * 128], QT[32 * g:32 * g + 32, c, q0:q0 + nq], True, True,
                           [T + "KT"] + QK_R, [f"psS{hf}"], tp=(32 * g, 0))

                def E_half(kt, hf):
                    s2 = (kt - k0) % 2
                    ACTV(ptH[hf][s2][:, :, 0:nq], psum[:, 1024 * hf:1024 * hf + 1024].rearrange("p (g n) -> p g n", n=512)[:, :, 0:nq], AF.Exp,
                         [f"psS{hf}"], [T + f"pt{hf}{s2}"], scale=sc_)

                def AV_half(kt, hf):
                    s2 = (kt - k0) % 2
                    for sb in range(nq // 128):
                        for gg in range(2):
                            g = 2 * hf + gg
                            MM(bank(4 + sb, 65, g * 65), ptH[hf][s2][:, gg, sb * 128:(sb + 1) * 128], V1[:, kt, hf, :], kt == k0 and g == 0, kt == k1 - 1,
                               [T + f"pt{hf}{s2}", T + "V1", T + "V1ones"], [f"psO{sb}"], sgc=True)

                S_half(k0, 0); S_half(k0, 1)
                for kt in range(k0, k1):
                    E_half(kt, 0); E_half(kt, 1)
                    AV_half(kt, 0)
                    if kt + 1 < k1:
                        S_half(kt + 1, 0)
                    AV_half(kt, 1)
                    if kt + 1 < k1:
                        S_half(kt + 1, 1)
                if STOP_AFTER == f"daq1nf_{l}":
                    continue
                for sb in range(nq // 128):
                    tile_i = (q0 + sb * 128) // 128
                    yb = yda[nblk % 2]; ybn = T + f"yda{nblk % 2}"; nblk += 1
                    bk = 4 + sb
                    pr_ = f"psO{sb}"
                    Tv = bank(bk, 260).rearrange("p (g e) -> p g e", e=65)
                    RECIP(rec, Tv[:, :, 64], [pr_], [T + "rec"])
                    TS("dve", rn, rec.rearrange("p (h m) -> p h m", m=2)[:, :, 1], nlam[:, 0:1], ALU.mult, [T + "rec", T + "nlam"], [T + "rn"])
                    for hh in range(2):
                        TS("dve", o1, Tv[:, 2 * hh, 0:64], rec[:, 2 * hh:2 * hh + 1], ALU.mult, [pr_, T + "rec"], [T + "o1"])
                        STT("dve", o_f[:, hh, :], Tv[:, 2 * hh + 1, 0:64], rn[:, hh:hh + 1], o1, ALU.mult, ALU.add, [pr_, T + "rn", T + "o1"], [T + "o_f"])
                        ACTV(junk[:, 0:64], o_f[:, hh, :], AF.Square, [T + "o_f"], ["junk", T + "ssd"], accum=ssd[:, hh:hh + 1])
                    rstd_from_ss(ssd, 64, rsd, [T + "ssd", "epsc"], [T + "rsd"])
                    for hh in range(2):
                        STT("dve", yb[:, hh, :], o_f[:, hh, :], rsd[:, hh:hh + 1], gsub, ALU.mult, ALU.mult, [T + "o_f", T + "rsd", T + "gsub"], [ybn])
                    TR(bank_bf(bk)[:, 640:768], yb.rearrange("p h d -> p (h d)"), ident_bf, [ybn, "ident_bf"], [pr_])
                    CP("act", otst[sb % 2][:, 0, :], bank_bf(bk)[:, 640:768], [pr_], [L + f"otst{sb % 2}"])
                    DMA("sp", otd[tile_i, :, c, :], otst[sb % 2][:, 0, :], [L + f"otst{sb % 2}"], [L + f"OT{c}_{tile_i}"], f"ot{sb % 2}")
        A.pop()
        if check_stop(f"da_{l}") or STOP_AFTER in (f"daload_{l}", f"daq1_{l}", f"daq1nf_{l}"):
            P.barrier()
            tap("t_nlam", nlam, [], None)
            tap("t_gsub", gsub, [], None)
            if "dbg_ot" in dbg:
                P.barrier()
                DMA("sp", dbg["dbg_ot"], otd, [], ["dbgot"], "dbg")
            A.pop(); A.pop(); break

        P.barrier()
        T = L + "sw"
        A.push()
        QT = A.alloc([2, TOKC], BF16)
        KT = A.alloc([3328], BF16)
        V1 = A.alloc([26, 2, 65], BF16)
        MSW = A.alloc([10, 128], BF16)
        esink = A.alloc([4], F32)
        DMA("pool", MSW, I["swamask"].rearrange("m k q -> k m q"), [], [T + "msw"], "wB")
        DMA("sp", esink, pbc(I[f"sink{l}"]), [], [T + "esink"], "m0")
        ACTV(esink, esink, AF.Exp, [T + "esink"], [T + "esink"])
        MSET("pool", V1[:, :, :, 64:65], 1.0, [T + "V1ones"])
        A.push()
        wsw = A.alloc([8, 896], BF16)
        DMA("pool", wsw, winv[:, :, SW0:SW0 + 896], [], [T + "w"], "wA")
        dests = [QT[:, 0, 0:TOK], QT[:, 1, 0:TOK], KT[:, 0:TOK]]
        wq_l = [wsw[:, :, c * 128:(c + 1) * 128] for c in range(3)]
        wqs_l = [wsw[:, :, 384 + c * 128:384 + (c + 1) * 128] for c in range(3)]
        proj_rope(wq_l[2:3], wqs_l[2:3], rope64[:, 0, :], rope64[:, 1, :], dests[2:3], T, name0=2)
        proj_tok(wsw[:, :, 768:896], 128, lambda i: V1[:, i, :, 0:64], range(NTC), T, view=lambda a: a.rearrange("p (h d) -> p h d", d=64))
        V_R = [T + f"v{i}" for i in range(NTC)]
        e_s = dint(T + "e", [128, 512], BF16); g_s = dint(T + "g", [512, 512], BF16)
        DMA("sp", e_s.ap()[:, 0:128], KT[:, 0:128], [T + "d2"], [T + "e"], "ex")
        DMA("sp", e_s.ap()[:, 128:256], KT[:, TOK - 128:TOK], [T + "d2"], [T + "e"], "ex")
        DMA("sp", e_s.ap()[:, 256:384].rearrange("p (h d) -> p h d", d=64), V1[:, 0, :, 0:64], V_R, [T + "e"], "ex")
        DMA("sp", e_s.ap()[:, 384:512].rearrange("p (h d) -> p h d", d=64), V1[:, 15, :, 0:64], V_R, [T + "e"], "ex")
        AG(e_s, g_s, [T + "e"], [T + "g"], "cc")
        proj_rope(wq_l[0:2], wqs_l[0:2], rope64[:, 0, :], rope64[:, 1, :], dests[0:2], T, name0=0)
        for ci in range(3):
            dest = QT[:, ci, TOK:TOKC] if ci < 2 else KT[:, TOK:TOKC]
            proj_feat_plain(wsw[:, :, ci * 128:(ci + 1) * 128], dest, TOK, 256, T, f"c{ci}", ci % 2)
        A.pop()
        QK_R = [T + f"d{ci}" for ci in range(3)] + [T + f"dc{ci}" for ci in range(3)]
        if check_stop(f"swproj_{l}"):
            A.pop(); A.pop(); A.pop(); break
        g_sv = g_s.ap().rearrange("(r p) f -> p r f", p=128)
        DMA("sp", KT[:, TOKC:TOKC + 512].rearrange("p (r t) -> p r t", t=128), g_sv[:, :, 128:256], [T + "g"], [T + "halo"], "kt")
        DMA("sp", KT[:, TOKC + 512:TOKC + 1024].rearrange("p (r t) -> p r t", t=128), g_sv[:, :, 0:128], [T + "g"], [T + "halo"], "kt")
        for kv in range(2):
            DMA("sp", V1[:, 18:22, kv, 0:64], g_sv[:, :, 384 + kv * 64:448 + kv * 64], [T + "g"], [T + "halo"], "kt")
            DMA("sp", V1[:, 22:26, kv, 0:64], g_sv[:, :, 256 + kv * 64:320 + kv * 64], [T + "g"], [T + "halo"], "kt")
        if check_stop(f"swag_{l}"):
            A.pop(); A.pop(); A.pop(); break
        ptw = [A.alloc([2, 4, 128], BF16) for _ in range(2)]
        qz = [A.alloc([2, 2, 128], BF16) for _ in range(2)]
        den = [A.alloc([4], F32) for _ in range(2)]; ysw = [A.alloc([4, 64], BF16) for _ in range(2)]
        ALLR = QK_R + V_R + [T + "halo", T + "V1ones"]
        tiles_slots = []
        for i in range(nto):
            if i < NT:
                slots = []
                if i > 0:
                    slots.append((128 * (i - 1), i - 1, 0))
                slots.append((128 * i, i, None))
                if i < NT - 1:
                    slots.append((128 * (i + 1), i + 1, 1))
                slots += [(TOK, 16, None), (TOK + 128, 17, None)]
                if i == 0:
                    slots += [(TOKC + 128 * r, 18 + r, 2 + r) for r in range(4)]
                if i == NT - 1:
                    slots += [(TOKC + 512 + 128 * r, 22 + r, 6 + r) for r in range(4)]
            else:
                slots = [(TOK, 16, None), (TOK + 128, 17, None)]
            tiles_slots.append((i, slots))

        def sw_S(k, u):
            i, gi, ngr, grp = u
            s2 = k % 2
            ts = slice(i * 128, (i + 1) * 128)
            if gi == 0:
                for kv in range(2):
                    TS("dve", qz[i % 2][:, kv], QT[:, :, ts], HM[:, kv:kv + 1], ALU.mult, QK_R + ["HM"], [T + f"qz{i % 2}"])
            for si, (kc0, vt, mi) in enumerate(grp):
                for kv in range(2):
                    for g in range(2):
                        MM(bank(2 * s2 + si, 128, (kv * 2 + g) * 128), KT[:, kc0:kc0 + 128], qz[i % 2][:, kv, g, :], True, True,
                           ALLR + [T + f"qz{i % 2}"], [f"psS{s2}"], sgc=True)

        def sw_E(k, u):
            i, gi, ngr, grp = u
            s2 = k % 2
            ns = len(grp)
            ACTV(ptw[s2][:, 0:ns].rearrange("p s h q -> p (s h q)"), psum[:, 1024 * s2:1024 * s2 + 512 * ns], AF.Exp, [f"psS{s2}"], [T + f"pt{s2}"], scale=0.125)
            for si, (kc0, vt, mi) in enumerate(grp):
                if mi is not None:
                    TT("dve", ptw[s2][:, si], ptw[s2][:, si], MSW[:, mi, :].unsqueeze(1).to_broadcast([128, 4, 128]), ALU.mult, [T + f"pt{s2}", T + "msw"], [T + f"pt{s2}"])

        def sw_AV(k, u):
            i, gi, ngr, grp = u
            s2 = k % 2
            ns = len(grp)
            for si, (kc0, vt, mi) in enumerate(grp):
                first = (gi == 0 and si == 0); last = (gi == ngr - 1 and si == ns - 1)
                for kv in range(2):
                    for g in range(2):
                        h = kv * 2 + g
                        MM(bank(4 + i % 2, 65, h * 65), ptw[s2][:, si, h, :], V1[:, vt, kv, :], first and h == 0, last, [T + f"pt{s2}"] + ALLR, [f"psO{i % 2}"], sgc=True)

        def sw_FIN(i):
            Ov = bank(4 + i % 2, 260).rearrange("p (h e) -> p h e", e=65)
            dn = den[i % 2]
            TT("dve", dn, Ov[:, :, 64], esink, ALU.add, [f"psO{i % 2}", T + "esink"], [T + f"den{i % 2}"])
            RECIP(dn, dn, [T + f"den{i % 2}"], [T + f"den{i % 2}"])
            yb = ysw[i % 2]
            TT("dve", yb, Ov[:, :, 0:64], dn.unsqueeze(2).to_broadcast([128, 4, 64]), ALU.mult, [f"psO{i % 2}", T + f"den{i % 2}"], [T + f"y{i % 2}"])
            out_transposes(yb.rearrange("p h d -> p (h d)"), 2, i, T, [T + f"y{i % 2}"])

        attn_pipeline(tiles_slots, sw_S, sw_E, sw_AV, sw_FIN)
        A.pop()
        if check_stop(f"sw_{l}") or (STOP_AFTER or "").startswith("swq1"):
            if "dbg_ot" in dbg:
                P.barrier()
                DMA("sp", dbg["dbg_ot"], otd, [], ["dbgot"], "dbg")
            A.pop(); A.pop(); break

        P.barrier()
        T = L + "na"
        A.push()
        QT = A.alloc([2, TOKC], BF16)
        KT = A.alloc([2, 4352], BF16)
        V1 = A.alloc([34, 4, 65], BF16)
        MBK = A.alloc([45, 128], BF16)
        BEX = A.alloc([7, 4, 128], BF16)
        EIN = A.alloc([5, 4, 128], BF16)
        for m0 in range(0, 45, 9):
            DMA("pool", MBK[:, m0:m0 + 9, :], I["namask"][m0:m0 + 9].rearrange("m k q -> k m q"), [], [T + "mbk"], "wB")
        A.push()
        bfl = A.alloc([7, 4, 128], F32)
        for d7 in range(7):
            DMA("sp", bfl[:, d7], I[f"nabias{l}"][d7].rearrange("h k q -> k h q"), [], [T + "bfl"], "m0")
        ACTV(BEX, bfl, AF.Exp, [T + "bfl"], [T + "bex"])
        A.pop()
        for d in range(5):
            TT("dve", EIN[:, d], BEX[:, d + 1], MBK[:, d, :].unsqueeze(1).to_broadcast([128, 4, 128]), ALU.mult, [T + "bex", T + "mbk"], [T + "ein"])
        MSET("pool", V1[:, :, :, 64:65], 1.0, [T + "V1ones"])
        A.push()
        wna = A.alloc([8, 768], BF16)
        DMA("pool", wna, winv[:, :, NA0:NA0 + 768], [], [T + "w"], "wA")
        n = 0
        for ci in (2, 3):
            for (t0, nt_) in ((0, 512), (512, 512), (1024, 512), (1536, 512), (TOK, 256)):
                dest = KT[:, ci - 2, t0:t0 + nt_]
                proj_feat_plain(wna[:, :, ci * 128:(ci + 1) * 128], dest, t0, nt_, T, f"c{ci}", n % 4); n += 1
        proj_tok(wna[:, :, 512:768], 256, lambda i: V1[:, i, :, 0:64], range(NTC), T, view=lambda a: a.rearrange("p (h d) -> p h d", d=64))
        V_R = [T + f"v{i}" for i in range(NTC)]
        e_n = dint(T + "e", [128, 2048], BF16); g_n = dint(T + "g", [512, 2048], BF16)
        env = e_n.ap()
        for c in range(2):
            DMA("sp", env[:, c * 512:c * 512 + 256], KT[:, c, 0:256], [T + "dc2", T + "dc3"], [T + "e"], "ex")
            DMA("sp", env[:, c * 512 + 256:c * 512 + 512], KT[:, c, TOK - 256:TOK], [T + "dc2", T + "dc3"], [T + "e"], "ex")
        DMA("sp", env[:, 1024:1536].rearrange("p (i h d) -> p i h d", h=4, d=64), V1[:, 0:2, :, 0:64], V_R, [T + "e"], "ex")
        DMA("sp", env[:, 1536:2048].rearrange("p (i h d) -> p i h d", h=4, d=64), V1[:, 14:16, :, 0:64], V_R, [T + "e"], "ex")
        AG(e_n, g_n, [T + "e"], [T + "g"], "cc")
        for ci in (0, 1):
            for (t0, nt_) in ((0, 512), (512, 512), (1024, 512), (1536, 512), (TOK, 256)):
                dest = QT[:, ci, t0:t0 + nt_]
                proj_feat_plain(wna[:, :, ci * 128:(ci + 1) * 128], dest, t0, nt_, T, f"c{ci}", n % 4); n += 1
        A.pop()
        P.barrier()
        QK_R = [T + f"dc{ci}" for ci in range(4)]
        g_nv = g_n.ap().rearrange("(r p) f -> p r f", p=128)
        for c in range(2):
            DMA("sp", KT[:, c, TOKC:TOKC + 1024].rearrange("p (r t) -> p r t", t=256), g_nv[:, :, c * 512 + 256:c * 512 + 512], [T + "g"], [T + "halo"], "kt")
            DMA("sp", KT[:, c, TOKC + 1024:TOKC + 2048].rearrange("p (r t) -> p r t", t=256), g_nv[:, :, c * 512:c * 512 + 256], [T + "g"], [T + "halo"], "kt")
        for r in range(4):
            DMA("sp", V1[:, 18 + 2 * r:20 + 2 * r, :, 0:64], g_nv[:, r, 1536:2048].rearrange("p (i h d) -> p i h d", h=4, d=64), [T + "g"], [T + "halo"], "kt")
            DMA("sp", V1[:, 26 + 2 * r:28 + 2 * r, :, 0:64], g_nv[:, r, 1024:1536].rearrange("p (i h d) -> p i h d", h=4, d=64), [T + "g"], [T + "halo"], "kt")
        ptn = [A.alloc([2, 4, 128], BF16) for _ in range(2)]
        qz = [A.alloc([2, 2, 128], BF16) for _ in range(2)]
        den = [A.alloc([4], F32) for _ in range(2)]; yna = [A.alloc([4, 64], BF16) for _ in range(2)]
        ALLR = QK_R + V_R + [T + "halo", T + "V1ones"]
        PC0 = TOKC; NC0 = TOKC + 1024
        tiles_slots = []
        for i in range(nto):
            if i >= NT:
                slots = [(TOK, 16, 0, 0, 0), (TOK + 128, 17, 0, 0, 0)]
            elif 2 <= i <= 13:
                slots = [(128 * (i + d), i + d, 1, d + 2, 0) for d in range(-2, 3)]
            elif i == 0:
                slots = [(128 * d, d, 2, d + 3, 5 + d) for d in (0, 1, 2, 3)]
                slots += [(PC0 + 256 * r, 18 + 2 * r, 2, 1, 9 + r) for r in range(4)]
                slots += [(PC0 + 256 * r + 128, 19 + 2 * r, 2, 2, 13 + r) for r in range(4)]
            elif i == 1:
                slots = [(128 * (1 + d), 1 + d, 2, d + 3, 17 + (d + 1)) for d in (-1, 0, 1, 2)]
                slots += [(PC0 + 256 * r + 128, 19 + 2 * r, 2, 1, 21 + r) for r in range(4)]
            elif i == 14:
                slots = [(128 * (14 + d), 14 + d, 2, d + 3, 25 + (d + 2)) for d in (-2, -1, 0, 1)]
                slots += [(NC0 + 256 * r, 26 + 2 * r, 2, 5, 29 + r) for r in range(4)]
            else:
                slots = [(128 * (15 + d), 15 + d, 2, d + 3, 33 + (d + 3)) for d in (-3, -2, -1, 0)]
                slots += [(NC0 + 256 * r, 26 + 2 * r, 2, 4, 37 + r) for r in range(4)]
                slots += [(NC0 + 256 * r + 128, 27 + 2 * r, 2, 5, 41 + r) for r in range(4)]
            if i < NT:
                slots += [(TOK, 16, 0, 0, 0), (TOK + 128, 17, 0, 0, 0)]
            tiles_slots.append((i, slots))

        def na_S(k, u):
            i, gi, ngr, grp = u
            s2 = k % 2
            ts = slice(i * 128, (i + 1) * 128)
            if gi == 0:
                for hb_ in range(2):
                    TS("dve", qz[i % 2][:, hb_], QT[:, :, ts], HM[:, hb_:hb_ + 1], ALU.mult, QK_R + ["HM"], [T + f"qz{i % 2}"])
            for si, (kc0, vt, kind, bd, mi) in enumerate(grp):
                for h in range(4):
                    c, hb_ = h // 2, h % 2
                    MM(bank(2 * s2 + si, 128, h * 128), KT[:, c, kc0:kc0 + 128], qz[i % 2][:, hb_, c, :], True, True, ALLR + [T + f"qz{i % 2}"], [f"psS{s2}"], sgc=True)

        def na_E(k, u):
            i, gi, ngr, grp = u
            s2 = k % 2
            ns = len(grp)
            ACTV(ptn[s2][:, 0:ns].rearrange("p s h q -> p (s h q)"), psum[:, 1024 * s2:1024 * s2 + 512 * ns], AF.Exp, [f"psS{s2}"], [T + f"pt{s2}"], scale=0.125)
            for si, (kc0, vt, kind, bd, mi) in enumerate(grp):
                if kind == 1:
                    TT("dve", ptn[s2][:, si], ptn[s2][:, si], EIN[:, bd], ALU.mult, [T + f"pt{s2}", T + "ein"], [T + f"pt{s2}"])
                elif kind == 2:
                    TT("dve", ptn[s2][:, si], ptn[s2][:, si], BEX[:, bd], ALU.mult, [T + f"pt{s2}", T + "bex"], [T + f"pt{s2}"])
                    TT("dve", ptn[s2][:, si], ptn[s2][:, si], MBK[:, mi, :].unsqueeze(1).to_broadcast([128, 4, 128]), ALU.mult, [T + f"pt{s2}", T + "mbk"], [T + f"pt{s2}"])

        def na_AV(k, u):
            i, gi, ngr, grp = u
            s2 = k % 2
            ns = len(grp)
            for si, (kc0, vt, kind, bd, mi) in enumerate(grp):
                first = (gi == 0 and si == 0); last = (gi == ngr - 1 and si == ns - 1)
                for h in range(4):
                    MM(bank(4 + i % 2, 65, h * 65), ptn[s2][:, si, h, :], V1[:, vt, h, :], first and h == 0, last, [T + f"pt{s2}"] + ALLR, [f"psO{i % 2}"], sgc=True)

        def na_FIN(i):
            Ov = bank(4 + i % 2, 260).rearrange("p (h e) -> p h e", e=65)
            dn = den[i % 2]
            RECIP(dn, Ov[:, :, 64], [f"psO{i % 2}"], [T + f"den{i % 2}"])
            yb = yna[i % 2]
            TT("dve", yb, Ov[:, :, 0:64], dn.unsqueeze(2).to_broadcast([128, 4, 64]), ALU.mult, [f"psO{i % 2}", T + f"den{i % 2}"], [T + f"y{i % 2}"])
            out_transposes(yb.rearrange("p h d -> p (h d)"), 4, i, T, [T + f"y{i % 2}"])

        attn_pipeline(tiles_slots, na_S, na_E, na_AV, na_FIN)
        A.pop()
        if check_stop(f"na_{l}"):
            if "dbg_ot" in dbg:
                P.barrier()
                DMA("sp", dbg["dbg_ot"], otd, [], ["dbgot"], "dbg")
            A.pop(); A.pop(); break

        P.barrier()
        T = L + "rt"
        A.push()
        QT = A.alloc([2, TOKC], BF16); KT = A.alloc([2, TOKC], BF16)
        VR = A.alloc([NTC, 256], BF16); GT = A.alloc([NTC, 256], BF16)
        RC = A.alloc([700], F32); IDXB = A.alloc([128], F32)
        LG = A.alloc([8], F32); LGS = A.alloc([2, 2], F32)
        DEC = A.alloc([4, 128], BF16); XI = A.alloc([2, 2, 128], BF16)
        ZZ = A.alloc([2, 4], F32); GC = A.alloc([2, 2], F32); GPW = A.alloc([2, 2, 18], F32); CFC = A.alloc([2, 2, 5], F32)
        DMA("sp", RC, I["retc"], [], [T + "rc"], "m0")
        DMA("sp", IDXB, I["idxb"], [], [T + "rc"], "m0")
        DMA("sp", LG, pbc(I[f"gam{l}"]), [], [T + "lg"], "m0")
        ACTV(LG, LG, AF.Exp, [T + "lg"], [T + "lg"], scale=-1.0)
        TS("dve", LG, LG, 1.0, ALU.add, [T + "lg"], [T + "lg"])
        ACTV(LG, LG, AF.Ln, [T + "lg"], [T + "lg"])
        TS("dve", LG, LG, -1.0, ALU.mult, [T + "lg"], [T + "lg"])
        for d_ in range(2):
            for c in range(2):
                k0_ = 4 * d_ + 2 * c
                TS("dve", LGS[:, d_, c:c + 1], LG[:, k0_:k0_ + 1], HM[:, 0:1], ALU.mult, [T + "lg", "HM"], [T + "lgs"])
                STT("dve", LGS[:, d_, c:c + 1], LG[:, k0_ + 1:k0_ + 2], HM[:, 1:2], LGS[:, d_, c:c + 1], ALU.mult, ALU.add, [T + "lg", "HM", T + "lgs"], [T + "lgs"])
        A.push()
        tf = A.alloc([128], F32); tb_ = A.alloc([128], F32)
        for h in range(4):
            ACTV(tf, RC[:, 0:128], AF.Exp, [T + "rc", T + "lg"], [T + "tf"], scale=LG[:, h:h + 1])
            TT("dve", tf, tf, RC[:, 128:256], ALU.mult, [T + "tf", T + "rc"], [T + "tf"])
            ACTV(tb_, RC[:, 256:384], AF.Exp, [T + "rc", T + "lg"], [T + "tb"], scale=LG[:, 4 + h:5 + h])
            TT("dve", tb_, tb_, RC[:, 384:512], ALU.mult, [T + "tb", T + "rc"], [T + "tb"])
            TT("dve", DEC[:, h, :], tf, tb_, ALU.add, [T + "tf", T + "tb"], [T + "dec"])
        A.pop()
        for c in range(2):
            ACTV(XI[:, 0, c, :], RC[:, 512:640], AF.Exp, [T + "rc", T + "lgs"], [T + "xi"], scale=LGS[:, 0, c:c + 1])
            ACTV(XI[:, 1, c, :], IDXB, AF.Exp, [T + "rc", T + "lgs"], [T + "xi"], scale=LGS[:, 1, c:c + 1])
            for d_ in range(2):
                ACTV(GC[:, d_, c:c + 1], LGS[:, d_, c:c + 1], AF.Exp, [T + "lgs"], [T + "gc"], scale=128.0)
                ACTV(GPW[:, d_, c, :], RC[:, 642 + 18 * d_:660 + 18 * d_], AF.Exp, [T + "rc", T + "lgs"], [T + "gpw"], scale=LGS[:, d_, c:c + 1])
                ACTV(CFC[:, d_, c, :], RC[:, 678 + 5 * d_:683 + 5 * d_], AF.Exp, [T + "rc", T + "lgs"], [T + "cfc"], scale=LGS[:, d_, c:c + 1])
                TT("dve", CFC[:, d_, c, :], CFC[:, d_, c, :], RC[:, 688 + 5 * d_:693 + 5 * d_], ALU.mult, [T + "cfc", T + "rc"], [T + "cfc"])
        ACTV(ZZ[:, 0, :], LG[:, 0:4], AF.Exp, [T + "lg", T + "rc"], [T + "zz"], scale=RC[:, 640:641])
        ACTV(ZZ[:, 1, :], LG[:, 4:8], AF.Exp, [T + "lg", T + "rc"], [T + "zz"], scale=RC[:, 641:642])
        TS("dve", ZZ, ZZ, 0.125, ALU.mult, [T + "zz"], [T + "zz"])
        if check_stop(f"rtparam_{l}"):
            A.pop(); A.pop(); A.pop(); break
        A.push()
        wrt = A.alloc([8, 1536], BF16)
        DMA("pool", wrt, winv[:, :, RT0:RT0 + 1536], [], [T + "w"], "wA")
        dests = [QT[:, 0, 0:TOK], QT[:, 1, 0:TOK], KT[:, 0, 0:TOK], KT[:, 1, 0:TOK]]
        proj_rope([wrt[:, :, c * 128:(c + 1) * 128] for c in range(4)], [wrt[:, :, 512 + c * 128:512 + (c + 1) * 128] for c in range(4)],
                  rope64[:, 0, :], rope64[:, 1, :], dests, T)
        for ci in range(4):
            dest = QT[:, ci, TOK:TOKC] if ci < 2 else KT[:, ci - 2, TOK:TOKC]
            proj_feat_plain(wrt[:, :, ci * 128:(ci + 1) * 128], dest, TOK, 256, T, f"c{ci}", ci % 2)

        def vg_post(i, pb):
            CP("act", VR[:, i, :], bank(pb, 256), [f"ps{pb}"], [T + f"v{i}"])
            ACTV(GT[:, i, :], bank(pb, 256, 256), AF.Silu, [f"ps{pb}"], [T + f"g{i}"])
        proj_tok(wrt[:, :, 1024:1536], 512, None, range(NTC), T, post=vg_post)
        A.pop()
        P.barrier()
        QK_R = [T + f"d{ci}" for ci in range(4)] + [T + f"dc{ci}" for ci in range(4)]
        if check_stop(f"rtproj_{l}"):
            A.pop(); A.pop(); A.pop(); break
        KTOK = A.alloc([NTC, 256], BF16)
        SZ = A.alloc([2, 18, 2, 64], F32)
        UCX = A.alloc([4, 2, 64], F32)
        SCX = A.alloc([2, 2, 64], F32)
        S0 = A.alloc([2, 2, 64], F32)
        SB = A.alloc([2, NTC, 2, 64], BF16)
        GR = A.alloc([4, 256], F32); EXPB = A.alloc([2, 2, 64], F32)
        for i in range(NTC):
            pb = i % 2
            for c in range(2):
                TR(bank_bf(pb)[:, c * 128:(c + 1) * 128], KT[:, c, i * 128:(i + 1) * 128], ident_bf, QK_R + ["ident_bf"], [f"ps{pb}"])
            CP("act", KTOK[:, i, :], bank_bf(pb)[:, 0:256], [f"ps{pb}"], [T + f"kt{i}"])
        vz = [A.alloc([2, 256], BF16) for _ in range(2)]
        MSET("pool", SZ[:, 0, 0], 0.0, [T + "sz"])
        MSET("pool", SZ[:, 1, 16], 0.0, [T + "sz"])

        def chunk_U(n, s2):
            for d_ in range(2):
                TT("dve" if d_ == 0 else "pool", vz[s2][:, d_].rearrange("p (h e) -> p h e", e=64), VR[:, n, :].rearrange("p (h e) -> p h e", e=64),
                   ZZ[:, d_, :].unsqueeze(2).to_broadcast([128, 4, 64]), ALU.mult, [T + f"v{n}", T + "zz"], [T + f"vz{s2}"])
            for d_ in range(2):
                for c in range(2):
                    MM(bank(2 + s2, 128, (d_ * 2 + c) * 128), KTOK[:, n, c * 128:(c + 1) * 128], vz[s2][:, d_, c * 128:(c + 1) * 128], True, True,
                       [T + f"kt{n}", T + f"vz{s2}"], [f"ps{2 + s2}"])

        udg = A.alloc([64], F32); udt = A.alloc([64], F32)

        def udiag(s2, d_, c, dst=None, dreg=None):
            blk = bank(2 + s2, 128, (d_ * 2 + c) * 128)
            o_ = udg if dst is None else dst
            TS("dve", udt, blk[:, 0:64], HM[:, 0:1], ALU.mult, [f"ps{2 + s2}", "HM"], [T + "udt"])
            STT("dve", o_, blk[:, 64:128], HM[:, 1:2], udt, ALU.mult, ALU.add, [f"ps{2 + s2}", "HM", T + "udt"], [T + "udg" if dreg is None else dreg])
            return o_

        for n in range(NT):
            chunk_U(n, n % 2)
            for c in range(2):
                u_ = udiag(n % 2, 0, c)
                STT("dve", SZ[:, 0, n + 1, c, :], SZ[:, 0, n, c, :], GC[:, 0, c:c + 1], u_, ALU.mult, ALU.add, [T + "sz", T + "gc", T + "udg"], [T + "sz"])
                udiag(n % 2, 1, c, dst=SZ[:, 1, n, c, :], dreg=T + "szb")
        for n in range(NT - 1, -1, -1):
            for c in range(2):
                STT("dve", SZ[:, 1, n, c, :], SZ[:, 1, n + 1, c, :], GC[:, 1, c:c + 1], SZ[:, 1, n, c, :], ALU.mult, ALU.add, [T + "sz", T + "szb", T + "gc"], [T + "sz", T + "szb"])
        for k_, n in enumerate((16, 17)):
            chunk_U(n, k_)
            for d_ in range(2):
                for c in range(2):
                    u_ = udiag(k_, d_, c)
                    CP("dve", UCX[:, 2 * d_ + k_, c, :], u_, [T + "udg"], [T + "ucx"])
        for c in range(2):
            STT("dve", SCX[:, 0, c, :], UCX[:, 0, c, :], GC[:, 0, c:c + 1], UCX[:, 1, c, :], ALU.mult, ALU.add, [T + "ucx", T + "gc"], [T + "scx"])
            STT("dve", SCX[:, 1, c, :], UCX[:, 3, c, :], GC[:, 1, c:c + 1], UCX[:, 2, c, :], ALU.mult, ALU.add, [T + "ucx", T + "gc"], [T + "scx"])
        CP("dve", EXPB[:, 0], SZ[:, 0, 16], [T + "sz"], [T + "expb"])
        CP("dve", EXPB[:, 1], SZ[:, 1, 0], [T + "sz"], [T + "expb"])
        e_r = dint(T + "e", [128, 256], F32); g_r = dint(T + "g", [512, 256], F32)
        DMA("sp", e_r.ap(), EXPB.rearrange("p d c e -> p (d c e)"), [T + "expb"], [T + "e"], "ex")
        AG(e_r, g_r, [T + "e"], [T + "g"], "cc")
        DMA("sp", GR, g_r.ap().rearrange("(r p) f -> p r f", p=128), [T + "g"], [T + "gr"], "kt")
        GRv = GR.rearrange("p r (d c e) -> p r d c e", d=2, c=2)
        for d_ in range(2):
            for c in range(2):
                TS("dve", S0[:, d_, c, :], SCX[:, d_, c, :], CFC[:, d_, c, 4:5], ALU.mult, [T + "scx", T + "cfc"], [T + "s0"])
                for r in range(4):
                    STT("dve", S0[:, d_, c, :], GRv[:, r, d_, c, :], CFC[:, d_, c, r:r + 1], S0[:, d_, c, :], ALU.mult, ALU.add, [T + "gr", T + "cfc", T + "s0"], [T + "s0"])
        for n in range(NT):
            for c in range(2):
                STT("dve", SB[:, 0, n, c, :], S0[:, 0, c, :], GPW[:, 0, c, n:n + 1], SZ[:, 0, n, c, :], ALU.mult, ALU.add, [T + "s0", T + "gpw", T + "sz"], [T + "sb"])
                STT("dve", SB[:, 1, n, c, :], S0[:, 1, c, :], GPW[:, 1, c, n:n + 1], SZ[:, 1, n + 1, c, :], ALU.mult, ALU.add, [T + "s0", T + "gpw", T + "sz"], [T + "sb"])
        MSET("pool", SB[:, 0, 16], 0.0, [T + "sb"])
        MSET("pool", SB[:, 1, 17], 0.0, [T + "sb"])
        CP("dve", SB[:, 0, 17], UCX[:, 0], [T + "ucx"], [T + "sb"])
        CP("dve", SB[:, 1, 16], UCX[:, 3], [T + "ucx"], [T + "sb"])
        if check_stop(f"rtA_{l}"):
            A.pop(); A.pop(); A.pop(); break
        AD = [A.alloc([4, 128], BF16) for _ in range(2)]
        QX = [A.alloc([2, 2, 2, 128], BF16) for _ in range(2)]
        qz = [A.alloc([2, 2, 128], BF16) for _ in range(2)]
        of_ = [A.alloc([4, 64], F32) for _ in range(2)]
        sq = A.alloc([4, 64], F32); ssr = A.alloc([4], F32); rsr = A.alloc([4], F32)
        yr_ = [A.alloc([4, 64], BF16) for _ in range(2)]
        def rt_A(i):
            s2 = i % 2
            ts = slice(i * 128, (i + 1) * 128)
            for hb_ in range(2):
                TS("pool" if hb_ else "dve", qz[s2][:, hb_], QT[:, :, ts], HM[:, hb_:hb_ + 1], ALU.mult, QK_R + ["HM"], [T + f"qz{s2}"])
            for h in range(4):
                c, hb_ = h // 2, h % 2
                MM(bank(s2, 128, h * 128), KT[:, c, ts], qz[s2][:, hb_, c, :], True, True, QK_R + [T + f"qz{s2}"], [f"ps{s2}"], sgc=True)

        def rt_mid(i):
            s2 = i % 2
            TT("dve", AD[s2], bank(s2).rearrange("p (h i) -> p h i", i=128), DEC, ALU.mult, [f"ps{s2}", T + "dec"], [T + f"ad{s2}"])
            for d_ in range(2):
                for hb_ in range(2):
                    TT("dve", QX[s2][:, d_, hb_], qz[s2][:, hb_], XI[:, d_], ALU.mult, [T + f"qz{s2}", T + "xi"], [T + f"qx{s2}"])

        def rt_out(i):
            s2 = i % 2
            pO = 4 + s2
            for h in range(4):
                c, hb_ = h // 2, h % 2
                o_ = bank(pO, 64, h * 64)
                MM(o_, AD[s2][:, h, :], VR[:, i, h * 64:(h + 1) * 64], h == 0, False, [T + f"ad{s2}", T + f"v{i}"], [f"ps{pO}"], sgc=True)
                MM(o_, QX[s2][:, 0, hb_, c, :], SB[:, 0, i, c, :], False, False, [T + f"qx{s2}", T + "sb"], [f"ps{pO}"], sgc=True)
                MM(o_, QX[s2][:, 1, hb_, c, :], SB[:, 1, i, c, :], False, True, [T + f"qx{s2}", T + "sb"], [f"ps{pO}"], sgc=True)

        def rt_fin(i):
            s2 = i % 2
            pO = 4 + s2
            ov = of_[s2]
            CP("act", ov, bank(pO, 256).rearrange("p (h e) -> p h e", e=64), [f"ps{pO}"], [T + f"of{s2}"])
            TT("dve", sq, ov, ov, ALU.mult, [T + f"of{s2}"], [T + "sq"])
            RED("dve", ssr, sq, [T + "sq"], [T + "ssr"])
            rstd_from_ss(ssr, 64, rsr, [T + "ssr", "epsc"], [T + "rsr"])
            TT("dve", sq, ov, rsr.unsqueeze(2).to_broadcast([128, 4, 64]), ALU.mult, [T + f"of{s2}", T + "rsr"], [T + "sq"])
            TT("dve", yr_[s2], sq, GT[:, i, :].rearrange("p (h e) -> p h e", e=64), ALU.mult, [T + "sq", T + f"g{i}"], [T + f"y{s2}"])

        def rt_tr(i):
            out_transposes(yr_[i % 2].rearrange("p h d -> p (h d)"), 6, i, T, [T + f"y{i % 2}"])

        rt_A(0)
        rt_mid(0)
        if nto > 1:
            rt_A(1)
        for i in range(nto):
            rt_out(i)
            if i + 1 < nto:
                rt_mid(i + 1)
            if i + 2 < nto:
                rt_A(i + 2)
            rt_fin(i)
            if i >= 1:
                rt_tr(i - 1)
        rt_tr(nto - 1)
        A.pop()
        A.pop()
        if "dbg_ot" in dbg and l == 0:
            DMA("sp", dbg["dbg_ot"], otd, [L + f"OT{c0}_{i}" for c0 in (0, 1, 2, 4, 6) for i in range(nto)], ["dbgot"], "dbg")
        if check_stop(f"rt_{l}") or (STOP_AFTER or "").startswith("rtB"):
            A.pop(); break

        P.barrier()
        A.push()
        h2T = hT
        wo = A.alloc([8, 1024], BF16)
        DMA("pool", wo, I[f"wout{l}"].rearrange("(kc p) n -> p kc n", p=128), [], [L + "wo"], "wA")
        if moe:
            RB = A.alloc([8, 1024], F32)
            DMA("sp", RB, pbc(I["router"]).rearrange("p (e d) -> p e d", d=1024), [], [L + "rb"], "m0")
            rj = [A.alloc([1024], F32) for _ in range(3)]; sm = A.alloc([8, 8], F32)
        xt = [A.alloc([1024], F32) for _ in range(2)]
        t1 = [A.alloc([1024], F32) for _ in range(2)]
        xm = [A.alloc([1024], F32) for _ in range(2)]
        hb = [A.alloc([1024], BF16) for _ in range(2)]
        ott = [A.alloc([8, 128], BF16) for _ in range(2)]
        ss3 = A.alloc([NTC, 2], F32); rs3 = A.alloc([NTC, 2], F32)
        xdst = (xs, xcs)

        def p3_mm(i):
            s2 = i % 2
            pb = 2 * s2
            ot_r = [L + f"OT{c0}_{i}" for c0 in (0, 1, 2, 4, 6)]
            DMA("sp", ott[s2], otd[i], ot_r, [L + f"ott{s2}"], f"ott{s2}")
            for hf in range(2):
                for kc in range(8):
                    MM(bank(pb + hf), ott[s2][:, kc, :], wo[:, kc, hf * 512:(hf + 1) * 512], kc == 0, kc == 7, [L + f"ott{s2}", L + "wo"], [f"ps{pb + hf}"])

        def p3_s1(i):
            s2 = i % 2
            pb = 2 * s2
            yps = psum[:, 512 * pb:512 * pb + 1024]
            ACTV(junk, yps, AF.Square, [f"ps{pb}", f"ps{pb + 1}"], ["junk", L + f"s3_{i}"], accum=ss3[:, i, 0:1])
            rstd_from_ss(ss3[:, i, 0:1], 1024, rs3[:, i, 0:1], [L + f"s3_{i}", "epsc"], [L + f"r3_{i}"])
            DMA("sp", xt[s2], xtile_ap(xsrc, i), [], [L + f"p3xt{s2}"], f"xt{s2}")

        def p3_s2(i):
            s2 = i % 2
            v = 0 if i < NT else 1
            pb = 2 * s2
            yps = psum[:, 512 * pb:512 * pb + 1024]
            STT("dve", t1[s2], yps, rs3[:, i, 0:1], MOD[:, v, 2, :], ALU.mult, ALU.mult, [f"ps{pb}", f"ps{pb + 1}", L + f"r3_{i}", f"MOD{v}2"], [L + f"p3t1{s2}"])
            TT("dve", xm[s2], t1[s2], xt[s2], ALU.add, [L + f"p3t1{s2}", L + f"p3xt{s2}"], [L + f"xm{s2}"])
            DMA("sp", xtile_ap(xdst, i), xm[s2], [L + f"xm{s2}"], [L + f"xs{i}"], f"xst{s2}")
            ACTV(junk, xm[s2], AF.Square, [L + f"xm{s2}"], ["junk", L + f"s4_{i}"], accum=ss3[:, i, 1:2])
            rstd_from_ss(ss3[:, i, 1:2], 1024, rs3[:, i, 1:2], [L + f"s4_{i}", "epsc"], [L + f"r4_{i}"])

        def p3_s3(i):
            s2 = i % 2
            v = 0 if i < NT else 1
            STT("dve", t1[s2], xm[s2], rs3[:, i, 1:2], MOD[:, v, 4, :], ALU.mult, ALU.mult, [L + f"xm{s2}", L + f"r4_{i}", f"MOD{v}4"], [L + f"p3t1{s2}"])
            if moe:
                TT("dve", xt[s2], t1[s2], MOD[:, v, 3, :], ALU.add, [L + f"p3t1{s2}", f"MOD{v}3"], [L + f"p3xt{s2}"])
                CP("act", hb[s2], xt[s2], [L + f"p3xt{s2}"], [L + f"p3hb{s2}"])
                for e_ in range(8):
                    TT("dve", rj[e_ % 3], xt[s2], RB[:, e_, :], ALU.mult, [L + f"p3xt{s2}", L + "rb"], [L + f"rj{e_ % 3}"])
                    ACTV(junk, rj[e_ % 3], AF.Identity, [L + f"rj{e_ % 3}"], ["junk", L + f"logi{i}"], accum=LOGI[:, i, e_:e_ + 1])
                lg_ = LOGI[:, i, :]
                RED("dve", sm[:, 0, 0:1], lg_, [L + f"logi{i}"], [L + "sm"], mx=True)
                TS("dve", sm[:, 1, :], lg_, sm[:, 0, 0:1], ALU.is_equal, [L + f"logi{i}", L + "sm"], [L + "sm"])
                STT("dve", sm[:, 2, :], sm[:, 1, :], -1e30, lg_, ALU.mult, ALU.add, [L + "sm", L + f"logi{i}"], [L + "sm"])
                RED("dve", sm[:, 0, 1:2], sm[:, 2, :], [L + "sm"], [L + "sm"], mx=True)
                TS("dve", sm[:, 3, :], lg_, sm[:, 0, 1:2], ALU.is_ge, [L + f"logi{i}", L + "sm"], [L + "sm"])
                TS("dve", sm[:, 0, 2:3], sm[:, 0, 0:1], -1.0, ALU.mult, [L + "sm"], [L + "sm"])
                ACTV(sm[:, 4, :], lg_, AF.Exp, [L + f"logi{i}", L + "sm"], [L + "sm"], bias=sm[:, 0, 2:3])
                TT("dve", sm[:, 4, :], sm[:, 4, :], sm[:, 3, :], ALU.mult, [L + "sm"], [L + "sm"])
                RED("dve", sm[:, 0, 3:4], sm[:, 4, :], [L + "sm"], [L + "sm"])
                RECIP(sm[:, 0, 3:4], sm[:, 0, 3:4], [L + "sm"], [L + "sm"])
                TS("dve", GATES[:, i, :], sm[:, 4, :], sm[:, 0, 3:4], ALU.mult, [L + "sm"], [L + f"gates{i}"])
            else:
                TT("dve", hb[s2], t1[s2], MOD[:, v, 3, :], ALU.add, [L + f"p3t1{s2}", f"MOD{v}3"], [L + f"p3hb{s2}"])

        def p3_tr(i):
            s2 = i % 2
            ts = slice(i * 128, (i + 1) * 128)
            pt_ = 4 + s2
            for kc in range(8):
                TR(bank_bf(pt_)[:, kc * 128:(kc + 1) * 128], hb[s2][:, kc * 128:(kc + 1) * 128], ident_bf, [L + f"p3hb{s2}", "ident_bf"], [f"ps{pt_}"])
            CP("act", h2T[:, :, ts], bank_bf(pt_).rearrange("p (k t) -> p k t", t=128), [f"ps{pt_}"], [L + f"h2T{i}"])

        p3_mm(0)
        p3_s1(0)
        if nto > 1:
            p3_mm(1)
        for i in range(nto):
            p3_s2(i)
            if i + 1 < nto:
                p3_s1(i + 1)
            p3_s3(i)
            if i + 2 < nto:
                p3_mm(i + 2)
            p3_tr(i)
        H2_ALL = [L + f"h2T{i}" for i in range(nto)]
        A.pop()
        if check_stop(f"p3_{l}"):
            A.pop(); break

        P.barrier()
        Y = A.alloc([nto, 1024], F32)
        A.push()
        wg = [A.alloc([8, 256], BF16) for _ in range(2)]
        wu = [A.alloc([8, 256], BF16) for _ in range(2)]
        wd = [A.alloc([2, 1024], BF16) for _ in range(2)]
        sg = [A.alloc([512], BF16) for _ in range(2)]
        AT = [A.alloc([2, 512], BF16) for _ in range(2)]
        ntok = nto * 128
        tblocks = [(t0, min(512, ntok - t0)) for t0 in range(0, ntok, 512)]
        if moe:
            slabs = [(e_, s_) for e_ in range(8) for s_ in range(14)]
        else:
            slabs = [(None, s_) for s_ in range(11)]
        nmm = 0
        for si, (e_, s_) in enumerate(slabs):
            sl = si % 2
            if moe:
                gsrc = I["mwg"][e_].rearrange("(kc p) f -> p kc f", p=128)[:, :, s_ * 256:(s_ + 1) * 256]
                usrc = I["mwu"][e_].rearrange("(kc p) f -> p kc f", p=128)[:, :, s_ * 256:(s_ + 1) * 256]
                dsrc = I["mwd"][e_][s_ * 256:(s_ + 1) * 256, :].rearrange("(c p) n -> p c n", p=128)
            else:
                gsrc = I["fwg"].rearrange("(kc p) f -> p kc f", p=128)[:, :, s_ * 256:(s_ + 1) * 256]
                usrc = I["fwu"].rearrange("(kc p) f -> p kc f", p=128)[:, :, s_ * 256:(s_ + 1) * 256]
                dsrc = I["fwd"][s_ * 256:(s_ + 1) * 256, :].rearrange("(c p) n -> p c n", p=128)
            DMA("pool", wg[sl], gsrc, [], [L + f"wg{sl}"], f"fw{sl}")
            DMA("pool", wu[sl], usrc, [], [L + f"wu{sl}"], f"fw{sl}")
            DMA("pool", wd[sl], dsrc, [], [L + f"wd{sl}"], f"fw{sl}")
            for bi, (t0, nt_) in enumerate(tblocks):
                a2 = bi % 2
                for fcl in range(2):
                    pg = nmm % 2; nmm += 1
                    for kc in range(8):
                        MM(bank(pg, nt_), wg[sl][:, kc, fcl * 128:(fcl + 1) * 128], h2T[:, kc, t0:t0 + nt_], kc == 0, kc == 7, H2_ALL + [L + f"wg{sl}"], [f"ps{pg}"])
                    for kc in range(8):
                        MM(bank(2 + pg, nt_), wu[sl][:, kc, fcl * 128:(fcl + 1) * 128], h2T[:, kc, t0:t0 + nt_], kc == 0, kc == 7, H2_ALL + [L + f"wu{sl}"], [f"ps{2 + pg}"])
                    ACTV(sg[pg][:, 0:nt_], bank(pg, nt_), AF.Silu, [f"ps{pg}"], [L + f"sg{pg}"])
                    TT("dve", AT[a2][:, fcl, 0:nt_], sg[pg][:, 0:nt_], bank(2 + pg, nt_), ALU.mult, [L + f"sg{pg}", f"ps{2 + pg}"], [L + f"at{a2}_{fcl}"])
                for tt in range(nt_ // 128):
                    ti = t0 // 128 + tt
                    py = 4 + 2 * (ti % 2)
                    for hf in range(2):
                        for fcl in range(2):
                            MM(bank(py + hf), AT[a2][:, fcl, tt * 128:(tt + 1) * 128], wd[sl][:, fcl, hf * 512:(hf + 1) * 512], fcl == 0, fcl == 1,
                               [L + f"at{a2}_0", L + f"at{a2}_1", L + f"wd{sl}"], [f"ps{py + hf}"])
                    yps = psum[:, 512 * py:512 * py + 1024]
                    rr = [f"ps{py}", f"ps{py + 1}"]
                    if moe:
                        gsc = GATES[:, ti, e_:e_ + 1]
                        if si == 0:
                            TS("dve", Y[:, ti, :], yps, gsc, ALU.mult, rr + [L + f"gates{ti}"], [L + f"Y{ti}"])
                        else:
                            STT("dve", Y[:, ti, :], yps, gsc, Y[:, ti, :], ALU.mult, ALU.add, rr + [L + f"gates{ti}", L + f"Y{ti}"], [L + f"Y{ti}"])
                    else:
                        if si == 0:
                            CP("dve", Y[:, ti, :], yps, rr, [L + f"Y{ti}"])
                        else:
                            TT("dve", Y[:, ti, :], yps, Y[:, ti, :], ALU.add, rr + [L + f"Y{ti}"], [L + f"Y{ti}"])
        A.pop()
        if check_stop(f"p4_{l}"):
            A.pop(); break

        A.push()
        ss5 = A.alloc([NTC], F32); rs5 = A.alloc([NTC], F32)
        xt = [A.alloc([1024], F32) for _ in range(2)]
        t1 = [A.alloc([1024], F32) for _ in range(2)]
        xo = [A.alloc([1024], F32) for _ in range(2)]
        def p5_load(i):
            DMA("sp", xt[i % 2], xtile_ap(xdst, i), [L + f"xs{i}"], [L + f"p5xt{i % 2}"], f"xt{i % 2}")

        p5_load(0)
        for i in range(nto):
            s2 = i % 2
            v = 0 if i < NT else 1
            ACTV(junk, Y[:, i, :], AF.Square, [L + f"Y{i}"], ["junk", L + f"s5_{i}"], accum=ss5[:, i:i + 1])
            rstd_from_ss(ss5[:, i:i + 1], 1024, rs5[:, i:i + 1], [L + f"s5_{i}", "epsc"], [L + f"r5_{i}"])
            if i + 1 < nto:
                p5_load(i + 1)
            STT("dve", t1[s2], Y[:, i, :], rs5[:, i:i + 1], MOD[:, v, 5, :], ALU.mult, ALU.mult, [L + f"Y{i}", L + f"r5_{i}", f"MOD{v}5"], [L + f"p5t1{s2}"])
            TT("dve", xo[s2], t1[s2], xt[s2], ALU.add, [L + f"p5t1{s2}", L + f"p5xt{s2}"], [L + f"xo{s2}"])
            if l == 0:
                DMA("sp", xtile_ap(xdst, i), xo[s2], [L + f"xo{s2}"], [L + f"xs{i}"], f"xst{s2}")
                if "dbg_x" in dbg:
                    dd = dbg["dbg_x"][i * 128:(i + 1) * 128, :] if i < NT else dbg["dbg_xc"][(i - NT) * 128:(i - NT + 1) * 128, :]
                    DMA("sp", dd, xo[s2], [L + f"xo{s2}"], [L + f"dbgx{i}"], "dbg")
            else:
                DMA("sp", out[i * 128:(i + 1) * 128, :], xo[s2], [L + f"xo{s2}"], [f"out{i}"], f"xst{s2}")
        A.pop()
        A.pop()
        if check_stop(f"l{l}"):
            break
    return nc, P, es, A, I


def _emit(nc, P, es):
    tls = P.finalize()
    sems = {tl: es.enter_context(nc.semaphore("s_" + str(tl))) for tl in tls}
    with nc.Block() as block:
        block.sync(P.engine_body("sp", sems, final=True))
        block.tensor(P.engine_body("pe", sems))
        block.vector(P.engine_body("dve", sems))
        block.scalar(P.engine_body("act", sems))
        block.gpsimd(P.engine_body("pool", sems))


_CACHE = {}


def _get_program():
    if "nc" not in _CACHE:
        nc, P, es, A, I = build_program()
        _CACHE["inputs"] = list(I.keys())
        with es:
            _emit(nc, P, es)
        _CACHE["nc"] = nc
        _CACHE["peak"] = A.peak
        _CACHE["nops"] = len(P.ops)
    return _CACHE["nc"]


def _host_inputs(inp):
    f = lambda a: np.ascontiguousarray(np.asarray(a, dtype=np.float32))
    shared = {}
    for l in range(2):
        shared[f"wmod{l}"] = f(inp["w_mod"][l])
        shared[f"bmod{l}"] = f(inp["b_mod"][l]).reshape(1, 6144)
        shared[f"gvec{l}"] = f(np.concatenate([inp["g_attn_pre"][l], inp["g_attn_post"][l], inp["g_ffn_pre"][l], inp["g_ffn_post"][l]])).reshape(1, 4096)
        shared[f"win{l}"] = f(np.asarray(inp["w_in"][l])[:, WIN_PERM])
        shared[f"wout{l}"] = f(inp["w_out"][l])
        shared[f"dal{l}"] = f(np.concatenate([inp["da_lambda_q1"][l], inp["da_lambda_k1"][l], inp["da_lambda_q2"][l], inp["da_lambda_k2"][l]])).reshape(1, 128)
        shared[f"subln{l}"] = f(inp["da_subln"][l]).reshape(1, 64)
        shared[f"sink{l}"] = f(inp["swa_sink"][l]).reshape(1, 4)
        shared[f"gam{l}"] = f(np.concatenate([inp["ret_gamma_fwd"][l], inp["ret_gamma_bwd"][l]])).reshape(1, 8)
        shared[f"nabias{l}"] = _na_bias_layout(np.asarray(inp["na_rpb"][l], dtype=np.float32))
    shared["fwg"] = f(inp["ffn_w_gate"][0]); shared["fwu"] = f(inp["ffn_w_up"][0]); shared["fwd"] = f(inp["ffn_w_down"][0])
    shared["router"] = f(np.asarray(inp["moe_router"][0]).T).reshape(1, 8 * 1024)
    shared["mwg"] = f(inp["moe_w_gate"][0]); shared["mwu"] = f(inp["moe_w_up"][0]); shared["mwd"] = f(inp["moe_w_down"][0])
    shared["idxb"] = _idxb_table()
    x = np.asarray(inp["x"], dtype=np.float32); ctx = np.asarray(inp["ctx"], dtype=np.float32)
    c = np.asarray(inp["c"], dtype=np.float32); c_ctx = np.asarray(inp["c_ctx"], dtype=np.float32)
    maps = []
    for core in range(8):
        b, j = core // 4, core % 4
        m = dict(shared)
        m["xin"] = np.ascontiguousarray(x[b, TOK * j:TOK * (j + 1)])
        m["xcin"] = np.ascontiguousarray(ctx[b])
        m["cvec"] = np.ascontiguousarray(np.concatenate([c[b].reshape(8, 128).T, c_ctx.reshape(8, 128).T], axis=1))
        C64, S64 = _rope_tables(j, 64)
        C32, S32 = _rope_tables(j, 32)
        m["rope64"] = np.ascontiguousarray(np.stack([C64, S64], axis=1))
        m["rope32"] = np.ascontiguousarray(np.stack([C32, S32], axis=1))
        m["swamask"] = _swa_masks(j)
        m["namask"] = _na_masks(j)
        m["retc"] = _ret_consts(j)
        if "inputs" in _CACHE:
            m = {k: v for k, v in m.items() if k in _CACHE["inputs"]}
        maps.append(m)
    return maps


def kernel(**inputs):
    nc = _get_program()
    maps = _host_inputs(inputs)
    res = run_bass_kernel_spmd(nc, maps, core_ids=list(range(8)))
    _CACHE["last"] = res
    outp = np.empty((2, 8192, 1024), np.float32)
    for core in range(8):
        b, j = core // 4, core % 4
        outp[b, TOK * j:TOK * (j + 1)] = res.results[core]["out"]
    return outp
```

```python
import contextlib
import os
import math
import numpy as np
import concourse.bass as bass
import concourse.mybir as mybir
from concourse.bass_utils import run_bass_kernel_spmd

F32 = mybir.dt.float32
BF16 = mybir.dt.bfloat16
AF = mybir.ActivationFunctionType
ALU = mybir.AluOpType
AX = mybir.AxisListType
ENGS = ("pe", "act", "dve", "pool", "sp")
EPS = 1e-6
NT = 16
NTC = 18
TOK = 2048
TOKC = 2304
DEBUG = []
STOP_AFTER = None


class Op:
    __slots__ = ("eng", "fn", "tl", "deps", "awaited", "count", "inc", "idx")


class Prog:
    def __init__(self):
        self.ops = []
        self.last_w = {}
        self.readers = {}
        self.tl_last = {}
        self.bar = set()
        self.bar_done = set(ENGS)

    def op(self, eng, fn, reads=(), writes=(), tl=None, inc=1):
        o = Op()
        o.eng = eng
        o.fn = fn
        o.tl = tl if tl is not None else eng
        o.inc = inc
        o.awaited = o.tl not in ENGS
        o.count = None
        o.idx = len(self.ops)
        deps = set()
        for r in reads:
            w = self.last_w.get(r)
            if w is not None:
                deps.add(w)
        for w_ in writes:
            w = self.last_w.get(w_)
            if w is not None:
                deps.add(w)
            rl = self.readers.get(w_)
            if rl:
                deps.update(rl)
        if eng not in self.bar_done:
            deps |= self.bar
            self.bar_done.add(eng)
        o.deps = deps
        self.ops.append(o)
        for r in reads:
            self.readers.setdefault(r, []).append(o.idx)
        for w_ in writes:
            self.last_w[w_] = o.idx
            self.readers[w_] = []
        self.tl_last[o.tl] = o.idx
        return o

    def barrier(self):
        self.bar = set(self.tl_last.values())
        self.bar_done = set()

    def finalize(self):
        ops = self.ops
        for i in self.tl_last.values():
            ops[i].awaited = True
        for o in ops:
            for d in o.deps:
                od = ops[d]
                if od.tl == "pe" and o.tl == "pe":
                    continue
                od.awaited = True
        cnt = {}
        for o in ops:
            if o.awaited:
                cnt[o.tl] = cnt.get(o.tl, 0) + o.inc
                o.count = cnt[o.tl]
        self.totals = cnt
        run_latest = {}
        self.need = [None] * len(ops)
        for o in ops:
            need = {}
            for d in o.deps:
                od = ops[d]
                if od.tl == "pe" and o.tl == "pe":
                    continue
                v = od.count if od.tl in ENGS else run_latest[od.tl]
                if need.get(od.tl, 0) < v:
                    need[od.tl] = v
            self.need[o.idx] = need
            if o.awaited:
                run_latest[o.tl] = o.count
        return sorted(cnt.keys(), key=str)

    def engine_body(self, ename, sems, final=False):
        mine = [o for o in self.ops if o.eng == ename]

        def body(e):
            waited = {}
            for o in mine:
                for tl, v in self.need[o.idx].items():
                    if waited.get(tl, 0) < v:
                        e.wait_ge(sems[tl], v)
                        waited[tl] = v
                ins = o.fn(e)
                if o.awaited:
                    ins.then_inc(sems[o.tl], o.inc)
            if final:
                for tl, v in self.totals.items():
                    if waited.get(tl, 0) < v:
                        e.wait_ge(sems[tl], v)
        return body


class Arena:
    def __init__(self, ap, nbytes, prog=None):
        self.prog = prog
        self.ap = ap
        self.cap = nbytes
        self.off = 0
        self.stack = []
        self.peak = 0

    def alloc(self, shape, dt):
        shape = list(shape)
        n = int(np.prod(shape))
        nb = n * (4 if dt == F32 else 2)
        nb = (nb + 63) // 64 * 64
        assert self.off + nb <= self.cap, f"SBUF arena overflow {self.off}+{nb}>{self.cap}"
        v = self.ap[:, self.off // 2:(self.off + nb) // 2]
        if dt == F32:
            v = v.bitcast(F32)
        v = v[:, 0:n]
        self.off += nb
        self.peak = max(self.peak, self.off)
        if len(shape) == 2:
            v = v.rearrange("p (a b) -> p a b", b=shape[1])
        elif len(shape) == 3:
            v = v.rearrange("p (a b c) -> p a b c", b=shape[1], c=shape[2])
        elif len(shape) == 4:
            v = v.rearrange("p (a b c d) -> p a b c d", b=shape[1], c=shape[2], d=shape[3])
        return v

    def push(self):
        self.stack.append(self.off)

    def pop(self):
        self.off = self.stack.pop()
        if self.prog is not None:
            self.prog.barrier()


def _swap_idx(dh):
    q = dh // 4
    return np.concatenate([np.arange(q, 2 * q), np.arange(0, q), np.arange(3 * q, 4 * q), np.arange(2 * q, 3 * q)])


def _win_perm():
    cols = []
    base = 0
    q = np.arange(base, base + 256)
    k = np.arange(base + 256, base + 512)
    v = np.arange(base + 512, base + 768)
    sw32 = np.concatenate([_swap_idx(32) + 32 * i for i in range(8)])
    cols += [q, k, q[sw32], k[sw32], v]
    base = 768
    qn = np.arange(base, base + 256).reshape(2, 2, 64)
    qperm = np.transpose(qn, (1, 0, 2)).reshape(256)
    kk = np.arange(base + 256, base + 384)
    vv = np.arange(base + 384, base + 512)
    sw64_4 = np.concatenate([_swap_idx(64) + 64 * i for i in range(4)])
    sw64_2 = np.concatenate([_swap_idx(64) + 64 * i for i in range(2)])
    cols += [qperm, kk, qperm[sw64_4], kk[sw64_2], vv]
    base = 1280
    cols += [np.arange(base, base + 768)]
    base = 2048
    q = np.arange(base, base + 256)
    k = np.arange(base + 256, base + 512)
    vg = np.arange(base + 512, base + 1024)
    cols += [q, k, q[sw64_4], k[sw64_4], vg]
    return np.concatenate(cols)


WIN_PERM = _win_perm()
NWIN = len(WIN_PERM)
DA0, SW0, NA0, RT0 = 0, 1280, 2176, 2944


def _rope_tables(j, dh):
    t = 2048 * j + np.arange(2048)
    row = (t // 64).astype(np.float64)
    col = (t % 64).astype(np.float64)
    half = dh // 2
    qd = dh // 4
    inv = 10000.0 ** (-np.arange(qd, dtype=np.float64) * 2.0 / half)
    C = np.zeros((128, 2048), np.float32)
    S = np.zeros((128, 2048), np.float32)
    for p in range(128):
        d = p % dh
        pos = row if d < half else col
        dd = d % half
        i = dd % qd
        ang = pos * inv[i]
        C[p] = np.cos(ang)
        S[p] = -np.sin(ang) if dd < qd else np.sin(ang)
    return C, S


def _swa_masks(j):
    kk = np.arange(128)[:, None]
    qq = np.arange(128)[None, :]
    mprev = (qq <= kk).astype(np.float32)
    mnext = (kk <= qq).astype(np.float32)
    m = np.zeros((10, 128, 128), np.float32)
    m[0] = mprev
    m[1] = mnext
    for r in range(4):
        if r == j - 1:
            m[2 + r] = mprev
        if r == j + 1:
            m[6 + r] = mnext
    return m


def _na_mask(Tq, Tk, flag=True):
    if (not flag) or Tk < 0 or Tk > 63:
        return np.zeros((128, 128), np.float32)
    p = np.arange(128)
    Rk = (2 * Tk + p // 64)[:, None]
    kc = (p % 64)[:, None]
    Rq = (2 * Tq + p // 64)[None, :]
    qc = (p % 64)[None, :]
    start = np.clip(Rq - 4, 0, 120)
    cs = np.clip(qc - 8, 0, 48)
    ok = (Rk >= start) & (Rk < start + 8) & (kc >= cs) & (kc < cs + 16)
    return ok.astype(np.float32)


def _na_masks(j):
    m = np.zeros((45, 128, 128), np.float32)
    for d in range(-2, 3):
        m[d + 2] = _na_mask(10, 10 + d)
    T0 = 16 * j
    idx = 5
    for d in (0, 1, 2, 3):
        m[idx] = _na_mask(T0, T0 + d); idx += 1
    for r in range(4):
        m[idx] = _na_mask(T0, T0 - 2, r == j - 1); idx += 1
    for r in range(4):
        m[idx] = _na_mask(T0, T0 - 1, r == j - 1); idx += 1
    for d in (-1, 0, 1, 2):
        m[idx] = _na_mask(T0 + 1, T0 + 1 + d); idx += 1
    for r in range(4):
        m[idx] = _na_mask(T0 + 1, T0 - 1, r == j - 1); idx += 1
    for d in (-2, -1, 0, 1):
        m[idx] = _na_mask(T0 + 14, T0 + 14 + d); idx += 1
    for r in range(4):
        m[idx] = _na_mask(T0 + 14, T0 + 16, r == j + 1); idx += 1
    for d in (-3, -2, -1, 0):
        m[idx] = _na_mask(T0 + 15, T0 + 15 + d); idx += 1
    for r in range(4):
        m[idx] = _na_mask(T0 + 15, T0 + 16, r == j + 1); idx += 1
    for r in range(4):
        m[idx] = _na_mask(T0 + 15, T0 + 17, r == j + 1); idx += 1
    assert idx == 45
    return m


def _na_bias_layout(rpb):
    p = np.arange(128)
    kr = (p // 64)[:, None]; kc = (p % 64)[:, None]
    qr = (p // 64)[None, :]; qc = (p % 64)[None, :]
    out = np.empty((7, 4, 128, 128), np.float32)
    dc = np.clip(kc - qc, -15, 15) + 15
    for di, d in enumerate(range(-3, 4)):
        dr = np.clip(2 * d + kr - qr, -7, 7) + 7
        out[di] = rpb[:, dr, dc]
    return out


def _ret_consts(j):
    c = np.zeros((128, 700), np.float32)
    i = np.arange(128)
    o = 0
    dif = i[None, :] - i[:, None]
    c[:, 0:128] = np.maximum(dif, 0)
    c[:, 128:256] = (dif >= 0) * 0.125
    c[:, 256:384] = np.maximum(-dif, 0)
    c[:, 384:512] = (dif < 0) * 0.125
    c[:, 512:640] = (i + 1)[None, :]
    c[:, 640] = 127 - i
    c[:, 641] = i
    c[:, 642:660] = (128.0 * np.arange(18))[None, :]
    c[:, 660:678] = (128.0 * (15 - np.arange(18)))[None, :]
    for r in range(4):
        if r < j:
            c[:, 678 + r] = 2048.0 * (j - 1 - r); c[:, 688 + r] = 1.0
        if r > j:
            c[:, 683 + r] = 2048.0 * (r - j - 1); c[:, 693 + r] = 1.0
    c[:, 682] = 2048.0 * j; c[:, 692] = 1.0
    c[:, 687] = 2048.0 * (3 - j); c[:, 697] = 1.0
    return c


def _idxb_table():
    i = np.arange(128)
    return np.broadcast_to((128 - i)[None, :], (128, 128)).astype(np.float32).copy()


def build_program():
    nc = bass.Bass("TRN2", target_bir_lowering=False)
    P = Prog()
    es = contextlib.ExitStack()

    def din(name, shape, dt=F32):
        return nc.dram_tensor(name, list(shape), dt, kind="ExternalInput").ap()

    def dint(name, shape, dt):
        return nc.dram_tensor(name, list(shape), dt)

    SHAPES = {"xin": [TOK, 1024], "xcin": [256, 1024], "cvec": [128, 16], "fwg": [1024, 2816], "fwu": [1024, 2816], "fwd": [2816, 1024],
              "router": [1, 8 * 1024], "mwg": [8, 1024, 3584], "mwu": [8, 1024, 3584], "mwd": [8, 3584, 1024],
              "rope64": [128, 2, 2048], "rope32": [128, 2, 2048], "swamask": [10, 128, 128], "namask": [45, 128, 128],
              "retc": [128, 700], "idxb": [128, 128]}
    for l_ in range(2):
        SHAPES.update({f"wmod{l_}": [1024, 6144], f"bmod{l_}": [1, 6144], f"gvec{l_}": [1, 4096], f"win{l_}": [1024, NWIN],
                       f"wout{l_}": [1024, 1024], f"dal{l_}": [1, 128], f"subln{l_}": [1, 64], f"sink{l_}": [1, 4],
                       f"gam{l_}": [1, 8], f"nabias{l_}": [7, 4, 128, 128]})

    class LazyIn(dict):
        def __missing__(self, k):
            self[k] = din(k, SHAPES[k])
            return self[k]
    I = LazyIn()
    USED_INPUTS = I
    out = nc.dram_tensor("out", [TOK, 1024], F32, kind="ExternalOutput").ap()
    dbg = {}
    for name, shape, dt in (("dbg_ot", [NTC, 128, 8, 128], BF16), ("dbg_x", [TOK, 1024], F32), ("dbg_xc", [256, 1024], F32),
                            ("dbg_misc", [128, 4096], F32)):
        if name in DEBUG:
            dbg[name] = nc.dram_tensor(name, shape, dt, kind="ExternalOutput").ap()

    xs = dint("xs", [TOK, 1024], F32).ap(); xcs = dint("xcs", [256, 1024], F32).ap()
    GROUPS = [[0, 1, 2, 3], [4, 5, 6, 7]]

    arena_t = es.enter_context(nc.sbuf_tensor("arena", [128, 94 * 1024], BF16))
    A = Arena(arena_t, 188 * 1024, P)
    psum = es.enter_context(nc.psum_tensor("psum", [128, 4096], F32))

    def bank(i, n=512, off=0):
        return psum[:, 512 * i + off:512 * i + off + n]

    def bank_bf(i):
        return psum[:, 512 * i:512 * (i + 1)].bitcast(BF16)

    def MM(o, lhsT, rhs, st, sp_, r, w, tp=None, sgc=False):
        kw = {}
        if tp is not None:
            kw["tile_position"] = tp
        if sgc:
            kw["skip_group_check"] = True
        P.op("pe", lambda e: e.matmul(o, lhsT=lhsT, rhs=rhs, start=st, stop=sp_, **kw), r, w)

    def MM64(o, lhsT, rhs, base, st, sp_, r, w):
        if base == 0:
            MM(o, lhsT[0:64], rhs[0:64], st, sp_, r, w, sgc=True)
        else:
            MM(o, lhsT[64:96], rhs[64:96], st, False, r, w, tp=(64, 0), sgc=True)
            MM(o, lhsT[96:128], rhs[96:128], False, sp_, r, w, tp=(96, 0), sgc=True)

    def TR(o, i, ident, r, w):
        P.op("pe", lambda e: e.transpose(o, i, ident), r, w)

    def ACTV(o, i, func, r, w, bias=None, scale=None, accum=None):
        kw = {}
        if bias is not None:
            kw["bias"] = bias
        if scale is not None:
            kw["scale"] = scale
        if accum is not None:
            kw["accum_out"] = accum
        P.op("act", lambda e: e.activation(out=o, in_=i, func=func, **kw), r, w)

    def TT(eng, o, a, b, op, r, w):
        P.op(eng, lambda e: e.tensor_tensor(out=o, in0=a, in1=b, op=op), r, w)

    def TS(eng, o, a, s1, op0, r, w, s2=None, op1=None):
        if op1 is None:
            P.op(eng, lambda e: e.tensor_scalar(out=o, in0=a, scalar1=s1, scalar2=None, op0=op0), r, w)
        else:
            P.op(eng, lambda e: e.tensor_scalar(out=o, in0=a, scalar1=s1, scalar2=s2, op0=op0, op1=op1), r, w)

    def STT(eng, o, a, s, b, op0, op1, r, w):
        P.op(eng, lambda e: e.scalar_tensor_tensor(out=o, in0=a, scalar=s, in1=b, op0=op0, op1=op1), r, w)

    def CP(eng, o, i, r, w):
        if eng == "act":
            P.op("act", lambda e: e.copy(out=o, in_=i), r, w)
        else:
            P.op(eng, lambda e: e.tensor_copy(out=o, in_=i), r, w)

    def MSET(eng, o, val, w):
        P.op(eng, lambda e: e.memset(o, val), (), w)

    def RED(eng, o, i, r, w, mx=False):
        if mx:
            P.op(eng, lambda e: e.reduce_max(out=o, in_=i, axis=AX.X), r, w)
        else:
            P.op(eng, lambda e: e.reduce_sum(out=o, in_=i, axis=AX.X), r, w)

    def RECIP(o, i, r, w):
        P.op("dve", lambda e: e.reciprocal(out=o, in_=i), r, w)

    def DMA(q, o, i, r, w, tl):
        P.op(q, lambda e: e.dma_start(out=o, in_=i), r, w, tl=tl, inc=16)

    def AG(src, dst, r, w, tl):
        P.op("pool", lambda e: e.collective_compute("AllGather", ALU.bypass, replica_groups=GROUPS,
                                                    ins=[src.ap().opt()], outs=[dst.ap().opt()]), r, w, tl=tl, inc=1)

    def rstd_from_ss(ss, n, rstd, r, w):
        ACTV(rstd, ss, AF.Sqrt, r, w, bias=epsc[:, 0:1], scale=1.0 / n)
        RECIP(rstd, rstd, w, w)

    ident_bf = A.alloc([128], BF16); ident_f = A.alloc([128], F32); zeros = A.alloc([128], BF16)
    epsc = A.alloc([1], F32)
    junk = A.alloc([1024], BF16)
    MOD = A.alloc([2, 6, 1024], BF16)
    MSET("pool", ident_f, 0.0, ["ident_f"])
    P.op("pool", lambda e: e.affine_select(out=ident_f, in_=ident_f, pattern=[[-1, 128]], compare_op=ALU.not_equal,
                                           fill=1.0, base=0, channel_multiplier=1), ["ident_f"], ["ident_f"])
    CP("pool", ident_bf, ident_f, ["ident_f"], ["ident_bf"])
    HM = A.alloc([2], F32)
    RED("dve", HM[:, 0:1], ident_f[:, 0:64], ["ident_f"], ["HM"])
    RED("dve", HM[:, 1:2], ident_f[:, 64:128], ["ident_f"], ["HM"])
    MSET("pool", zeros, 0.0, ["zeros"])
    MSET("pool", epsc, EPS, ["epsc"])

    def pbc(ap):
        b = ap.partition_broadcast(128)
        if len(b.shape) == 3 and b.shape[1] == 1:
            b = b[:, 0]
        return b

    stop = [False]

    def tap(name, ap, reads, flat):
        if name in DEBUG:
            shp = [128, int(np.prod(ap.shape[1:]))]
            d = nc.dram_tensor(name, shp, ap.dtype, kind="ExternalOutput").ap()
            DMA("sp", d, ap.rearrange(flat) if flat else ap, reads, [name], "dbg")

    def check_stop(name):
        if STOP_AFTER == name:
            stop[0] = True
        return stop[0]

    for l in range(2):
        if stop[0]:
            break
        with_ctx = (l == 0)
        lam_init = 0.8 - 0.6 * math.exp(-0.3 * l)
        ntl = NTC
        nto = NTC if with_ctx else NT
        xsrc = (I["xin"], I["xcin"]) if l == 0 else (xs, xcs)
        L = f"L{l}"

        def xtile_ap(src2, i):
            return src2[0][i * 128:(i + 1) * 128, :] if i < NT else src2[1][(i - NT) * 128:(i - NT + 1) * 128, :]

        P.barrier()
        A.push()
        cv = A.alloc([16], F32); sil = A.alloc([16], F32); sbc = A.alloc([2, 8, 128], BF16)
        gv = A.alloc([4, 1024], F32); tmpm = A.alloc([1024], F32)
        wm = [A.alloc([8, 1024], BF16) for _ in range(2)]
        bs = [A.alloc([1024], F32) for _ in range(2)]
        DMA("sp", cv, I["cvec"], [], [L + "cv"], "m0")
        DMA("sp", gv, pbc(I[f"gvec{l}"]).rearrange("p (a b) -> p a b", b=1024), [], [L + "gv"], "m0")
        ACTV(sil, cv, AF.Silu, [L + "cv"], [L + "sil"])
        for v in range(2):
            for kc in range(8):
                ACTV(sbc[:, v, kc, :], zeros, AF.Identity, [L + "sil", "zeros"], [L + "sbc"], bias=sil[:, v * 8 + kc:v * 8 + kc + 1])
        wmv = I[f"wmod{l}"].rearrange("(kc p) n -> p kc n", p=128)
        for s in range(6):
            sl = s % 2
            DMA("pool", wm[sl], wmv[:, :, s * 1024:(s + 1) * 1024], [], [L + f"wm{sl}"], f"wm{sl}")
            DMA("sp", bs[sl], pbc(I[f"bmod{l}"][0:1, s * 1024:(s + 1) * 1024]), [], [L + f"bs{sl}"], f"bs{sl}")
            for v in range(2):
                pb = 4 * (s % 2) + 2 * v
                for hf in range(2):
                    for kc in range(8):
                        MM(bank(pb + hf), sbc[:, v, kc, :], wm[sl][:, kc, hf * 512:(hf + 1) * 512], kc == 0, kc == 7,
                           [L + "sbc", L + f"wm{sl}"], [f"ps{pb + hf}"])
                TT("dve", tmpm, psum[:, 512 * pb:512 * pb + 1024], bs[sl], ALU.add, [f"ps{pb}", f"ps{pb + 1}", L + f"bs{sl}"], [L + "tmpm"])
                dst = MOD[:, v, s, :]
                if s in (0, 3):
                    CP("dve", dst, tmpm, [L + "tmpm"], [f"MOD{v}{s}"])
                elif s in (1, 4):
                    STT("dve", dst, tmpm, 1.0, gv[:, 0 if s == 1 else 2, :], ALU.add, ALU.mult, [L + "tmpm", L + "gv"], [f"MOD{v}{s}"])
                else:
                    TT("dve", dst, tmpm, gv[:, 1 if s == 2 else 3, :], ALU.mult, [L + "tmpm", L + "gv"], [f"MOD{v}{s}"])
        if l == 0:
            tap("t_mod", MOD, [f"MOD{v}{s}" for v in range(2) for s in range(6)], "p a b c -> p (a b c)")
        A.pop()
        if check_stop(f"p0_{l}"):
            break

        P.barrier()
        A.push()
        moe = (l == 1)
        if moe:
            LOGI = A.alloc([NT, 8], F32); GATES = A.alloc([NT, 8], F32)
        hT = A.alloc([8, TOKC], BF16)
        otd = dint(L + "otd", [NTC, 128, 8, 128], BF16).ap()
        A.push()
        otst = [A.alloc([2, 128], BF16) for _ in range(2)]

        A.push()
        xt = [A.alloc([1024], F32) for _ in range(2)]
        t1 = [A.alloc([1024], F32) for _ in range(2)]
        hb = [A.alloc([1024], BF16) for _ in range(2)]
        ssb = A.alloc([NTC], F32); rsb = A.alloc([NTC], F32)
        def p1a_A(i):
            s2 = i % 2
            DMA("sp", xt[s2], xtile_ap(xsrc, i), [], [L + f"xt{s2}"], f"xt{s2}")
            ACTV(junk, xt[s2], AF.Square, [L + f"xt{s2}"], ["junk", L + f"ss{i}"], accum=ssb[:, i:i + 1])
            rstd_from_ss(ssb[:, i:i + 1], 1024, rsb[:, i:i + 1], [L + f"ss{i}", "epsc"], [L + f"rs{i}"])

        def p1a_B(i):
            s2 = i % 2
            v = 0 if i < NT else 1
            STT("dve", t1[s2], xt[s2], rsb[:, i:i + 1], MOD[:, v, 1, :], ALU.mult, ALU.mult, [L + f"xt{s2}", L + f"rs{i}", f"MOD{v}1"], [L + f"t1{s2}"])
            TT("dve", hb[s2], t1[s2], MOD[:, v, 0, :], ALU.add, [L + f"t1{s2}", f"MOD{v}0"], [L + f"hb{s2}"])
            for kc in range(8):
                TR(bank_bf(s2)[:, kc * 128:(kc + 1) * 128], hb[s2][:, kc * 128:(kc + 1) * 128], ident_bf, [L + f"hb{s2}", "ident_bf"], [f"ps{s2}"])

        def p1a_C(i):
            s2 = i % 2
            CP("act", hT[:, :, i * 128:(i + 1) * 128], bank_bf(s2).rearrange("p (k t) -> p k t", t=128), [f"ps{s2}"], [L + f"hT{i}"])

        p1a_A(0)
        for i in range(ntl):
            p1a_B(i)
            if i + 1 < ntl:
                p1a_A(i + 1)
            p1a_C(i)
        A.pop()
        HT_ALL = [L + f"hT{i}" for i in range(ntl)]
        if l == 0:
            tap("t_hT", hT, HT_ALL, "p a b -> p (a b)")
        if check_stop(f"p1a_{l}"):
            A.pop(); A.pop(); break

        winv = I[f"win{l}"].rearrange("(kc p) n -> p kc n", p=128)

        def proj_rope(wq, wqs, Ctab, Stab, dests, tag, name0=0):
            tA = [A.alloc([512], F32) for _ in range(2)]
            tB = [A.alloc([512], F32) for _ in range(2)]
            n = 0
            for ci in range(len(wq)):
                for tb in range(4):
                    s2 = n % 2; n += 1
                    ts = slice(tb * 512, (tb + 1) * 512)
                    for kc in range(8):
                        MM(bank(s2), wq[ci][:, kc, :], hT[:, kc, ts], kc == 0, kc == 7, HT_ALL[4 * tb:4 * tb + 4] + [tag + "w"], [f"ps{s2}"])
                    for kc in range(8):
                        MM(bank(2 + s2), wqs[ci][:, kc, :], hT[:, kc, ts], kc == 0, kc == 7, HT_ALL[4 * tb:4 * tb + 4] + [tag + "w"], [f"ps{2 + s2}"])
                    TT("dve", tA[s2], bank(s2), Ctab[:, ts], ALU.mult, [f"ps{s2}", L + "rope"], [tag + f"tA{s2}"])
                    TT("dve", tB[s2], bank(2 + s2), Stab[:, ts], ALU.mult, [f"ps{2 + s2}", L + "rope"], [tag + f"tB{s2}"])
                    TT("dve", dests[ci][:, ts], tA[s2], tB[s2], ALU.add, [tag + f"tA{s2}", tag + f"tB{s2}"], [tag + f"d{ci + name0}"])

        def proj_feat_plain(wq, dest, t0, nt, tag, ci, pb):
            for kc in range(8):
                MM(bank(pb, nt), wq[:, kc, :], hT[:, kc, t0:t0 + nt], kc == 0, kc == 7, HT_ALL + [tag + "w"], [f"ps{pb}"])
            CP("act", dest, bank(pb, nt), [f"ps{pb}"], [tag + f"d{ci}"])

        def proj_tok(wv, ncols, dest_fn, tiles, tag, post=None, view=None):
            for n, i in enumerate(tiles):
                pb = 4 + n % 2
                for kc in range(8):
                    MM(bank(pb, ncols), hT[:, kc, i * 128:(i + 1) * 128], wv[:, kc, :], kc == 0, kc == 7, [L + f"hT{i}", tag + "w"], [f"ps{pb}"])
                if post is None:
                    src_ = bank(pb, ncols)
                    if view is not None:
                        src_ = view(src_)
                    CP("act", dest_fn(i), src_, [f"ps{pb}"], [tag + f"v{i}"])
                else:
                    post(i, pb)

        def out_transposes(ytok, chunk0, i, tag, rd):
            pb = 6 + (i % 2)
            st = otst[i % 2]
            for cc in range(2):
                TR(bank_bf(pb)[:, cc * 128:(cc + 1) * 128], ytok[:, cc * 128:(cc + 1) * 128], ident_bf, rd + ["ident_bf"], [f"ps{pb}"])
            CP("act", st, bank_bf(pb)[:, 0:256].rearrange("p (c t) -> p c t", t=128), [f"ps{pb}"], [L + f"otst{i % 2}"])
            DMA("sp", otd[i, :, chunk0:chunk0 + 2, :], st, [L + f"otst{i % 2}"], [L + f"OT{chunk0}_{i}"], f"ot{i % 2}")

        def attn_pipeline(tiles_slots, S_fn, E_fn, AV_fn, FIN_fn):
            units = []
            for (i, slots) in tiles_slots:
                ngr = (len(slots) + 1) // 2
                for gi in range(ngr):
                    units.append((i, gi, ngr, slots[2 * gi:2 * gi + 2]))
            pending = None
            for k, u in enumerate(units):
                if k == 0:
                    S_fn(0, u)
                E_fn(k, u)
                if k + 1 < len(units):
                    S_fn(k + 1, units[k + 1])
                AV_fn(k, u)
                if pending is not None:
                    FIN_fn(pending); pending = None
                if u[1] == u[2] - 1:
                    pending = u[0]
            if pending is not None:
                FIN_fn(pending)

        rope64 = A.alloc([2, 2048], BF16); rope32 = A.alloc([2, 2048], BF16)
        DMA("pool", rope64, I["rope64"], [], [L + "rope"], "rp")
        DMA("pool", rope32, I["rope32"], [], [L + "rope"], "rp")

        T = L + "da"
        A.push()
        QT = A.alloc([2, TOKC], BF16); KTc = A.alloc([2, 256], BF16)
        Vc = A.alloc([2, 256], BF16)
        nlam = A.alloc([1], F32); gsub = A.alloc([64], F32)
        A.push()
        dl = A.alloc([4, 32], F32); pr = A.alloc([2, 32], F32); s12 = A.alloc([2], F32)
        DMA("sp", dl, pbc(I[f"dal{l}"]).rearrange("p (a b) -> p a b", b=32), [], [T + "dl"], "m0")
        DMA("sp", gsub, pbc(I[f"subln{l}"]), [], [T + "gsub"], "m0")
        TT("dve", pr[:, 0, :], dl[:, 0, :], dl[:, 1, :], ALU.mult, [T + "dl"], [T + "pr"])
        TT("dve", pr[:, 1, :], dl[:, 2, :], dl[:, 3, :], ALU.mult, [T + "dl"], [T + "pr"])
        RED("dve", s12, pr, [T + "pr"], [T + "s12"])
        ACTV(s12, s12, AF.Exp, [T + "s12"], [T + "s12"])
        TT("dve", nlam, s12[:, 1:2], s12[:, 0:1], ALU.subtract, [T + "s12"], [T + "nlam"])
        TS("dve", nlam, nlam, -lam_init, ALU.add, [T + "nlam"], [T + "nlam"])
        TS("dve", gsub, gsub, 1.0 - lam_init, ALU.mult, [T + "gsub"], [T + "gsub"])
        A.pop()
        A.push()
        wda = A.alloc([8, 1280], BF16)
        KTo = A.alloc([2, TOK], BF16); Vo = A.alloc([NT, 256], BF16)
        DMA("pool", wda, winv[:, :, DA0:DA0 + 1280], [], [T + "w"], "wA")
        qk_dest = [QT[:, 0, 0:TOK], QT[:, 1, 0:TOK], KTo[:, 0, :], KTo[:, 1, :]]
        wq_l = [wda[:, :, c * 128:(c + 1) * 128] for c in range(4)]
        wqs_l = [wda[:, :, 512 + c * 128:512 + (c + 1) * 128] for c in range(4)]
        proj_rope(wq_l[2:4], wqs_l[2:4], rope32[:, 0, :], rope32[:, 1, :], qk_dest[2:4], T, name0=2)
        e_k = dint(T + "ek", [256, TOK], BF16); e_v = dint(T + "ev", [TOK, 256], BF16)
        g_k = dint(T + "gk", [1024, TOK], BF16); g_v = dint(T + "gv", [4 * TOK, 256], BF16)
        DMA("sp", e_k.ap().rearrange("(c p) t -> p c t", p=128), KTo, [T + "d2", T + "d3"], [T + "ek"], "ex")
        AG(e_k, g_k, [T + "ek"], [T + "gk"], "cc")
        proj_tok(wda[:, :, 1024:1280], 256, lambda i: Vo[:, i, :] if i < NT else Vc[:, i - NT, :], range(NTC), T)
        V_R = [T + f"v{i}" for i in range(NTC)]
        DMA("sp", e_v.ap().rearrange("(i p) f -> p i f", p=128), Vo, V_R, [T + "ev"], "ex")
        AG(e_v, g_v, [T + "ev"], [T + "gv"], "cc")
        proj_rope(wq_l[0:2], wqs_l[0:2], rope32[:, 0, :], rope32[:, 1, :], qk_dest[0:2], T, name0=0)
        for ci in range(4):
            dest = QT[:, ci, TOK:TOKC] if ci < 2 else KTc[:, ci - 2, :]
            proj_feat_plain(wda[:, :, ci * 128:(ci + 1) * 128], dest, TOK, 256, T, f"c{ci}", ci % 2)
        QK_R = [T + f"d{ci}" for ci in range(4)] + [T + f"dc{ci}" for ci in range(4)]
        if check_stop(f"daproj_{l}"):
            tap("t_qt", QT, QK_R, "p a b -> p (a b)")
            tap("t_kto", KTo, QK_R, "p a b -> p (a b)")
            tap("t_vo", Vo, V_R, "p a b -> p (a b)")
            tap("t_ktc", KTc, QK_R, "p a b -> p (a b)")
            tap("t_vc", Vc, V_R, "p a b -> p (a b)")
            A.pop(); A.pop(); A.pop(); A.pop(); break
        A.pop()
        P.barrier()
        if check_stop(f"daag_{l}"):
            A.pop(); A.pop(); A.pop(); break
        KT = A.alloc([8448], BF16); V1 = A.alloc([66, 2, 65], BF16)
        ptH = [[A.alloc([2, 512], BF16) for _ in range(2)] for _ in range(2)]
        o_f = A.alloc([2, 64], F32); o1 = A.alloc([64], F32)
        rec = A.alloc([4], F32); rn = A.alloc([2], F32); ssd = A.alloc([2], F32); rsd = A.alloc([2], F32)
        yda = [A.alloc([2, 64], BF16) for _ in range(2)]
        MSET("pool", V1[:, :, :, 64:65], 1.0, [T + "V1ones"])
        g_kv = g_k.ap().rearrange("(r c p) t -> p r c t", r=4, c=2)
        g_vv = g_v.ap().rearrange("(r i p) f -> p r i f", r=4, i=NT)
        nblk = 0
        for c in range(2):
            CP("pool", KT[:, 0:256], KTc[:, c, :], QK_R, [T + "KT"])
            for r in range(4):
                DMA("sp", KT[:, 256 + r * TOK:256 + (r + 1) * TOK], g_kv[:, r, c, :], [T + "gk"], [T + "KT"], "kt")
                for hh in range(2):
                    DMA("sp", V1[:, 2 + r * NT:2 + (r + 1) * NT, hh, 0:64], g_vv[:, r, :, c * 128 + hh * 64:c * 128 + hh * 64 + 64],
                        [T + "gv"], [T + "V1"], "kt")
            CP("pool", V1[:, 0:2, :, 0:64], Vc[:, :, c * 128:(c + 1) * 128].rearrange("p i (h d) -> p i h d", d=64), V_R, [T + "V1"])
            if STOP_AFTER == f"daload_{l}":
                continue
            qblocks = [(qb * 512, 512, 0, 66) for qb in range(4)]
            if STOP_AFTER == f"daq1_{l}":
                qblocks = [(0, 512, 0, 66)] if c == 0 else []
            if STOP_AFTER == f"daq1nf_{l}":
                qblocks = [(0, 512, 0, 66)] if c == 0 else []
            if with_ctx:
                qblocks.append((TOK, 256, 0, 2))
            for (q0, nq, k0, k1) in qblocks:
                sc_ = 1.0 / math.sqrt(32.0)

                def S_half(kt, hf):
                    for g in (2 * hf, 2 * hf + 1):
                        MM(bank(g, nq), KT[32 * g:32 * g + 32, kt * 128:(kt + 1) * 128], QT[32 * g:32 * g + 32, c, q0:q0 + nq], True, True,
                           [T + "KT"] + QK_R, [f"psS{hf}"], tp=(32 * g, 0))

                def E_half(kt, hf):
                    s2 = (kt - k0) % 2
                    ACTV(ptH[hf][s2][:, :, 0:nq], psum[:, 1024 * hf:1024 * hf + 1024].rearrange("p (g n) -> p g n", n=512)[:, :, 0:nq], AF.Exp,
                         [f"psS{hf}"], [T + f"pt{hf}{s2}"], scale=sc_)

                def AV_half(kt, hf):
                    s2 = (kt - k0) % 2
                    for sb in range(nq // 128):
                        for gg in range(2):
                            g = 2 * hf + gg
                            MM(bank(4 + sb, 65, g * 65), ptH[hf][s2][:, gg, sb * 128:(sb + 1) * 128], V1[:, kt, hf, :], kt == k0 and g == 0, kt == k1 - 1,
                               [T + f"pt{hf}{s2}", T + "V1", T + "V1ones"], [f"psO{sb}"], sgc=True)

                S_half(k0, 0); S_half(k0, 1)
                for kt in range(k0, k1):
                    E_half(kt, 0); E_half(kt, 1)
                    AV_half(kt, 0)
                    if kt + 1 < k1:
                        S_half(kt + 1, 0)
                    AV_half(kt, 1)
                    if kt + 1 < k1:
                        S_half(kt + 1, 1)
                if STOP_AFTER == f"daq1nf_{l}":
                    continue
                for sb in range(nq // 128):
                    tile_i = (q0 + sb * 128) // 128
                    yb = yda[nblk % 2]; ybn = T + f"yda{nblk % 2}"; nblk += 1
                    bk = 4 + sb
                    pr_ = f"psO{sb}"
                    Tv = bank(bk, 260).rearrange("p (g e) -> p g e", e=65)
                    RECIP(rec, Tv[:, :, 64], [pr_], [T + "rec"])
                    TS("dve", rn, rec.rearrange("p (h m) -> p h m", m=2)[:, :, 1], nlam[:, 0:1], ALU.mult, [T + "rec", T + "nlam"], [T + "rn"])
                    for hh in range(2):
                        TS("dve", o1, Tv[:, 2 * hh, 0:64], rec[:, 2 * hh:2 * hh + 1], ALU.mult, [pr_, T + "rec"], [T + "o1"])
                        STT("dve", o_f[:, hh, :], Tv[:, 2 * hh + 1, 0:64], rn[:, hh:hh + 1], o1, ALU.mult, ALU.add, [pr_, T + "rn", T + "o1"], [T + "o_f"])
                        ACTV(junk[:, 0:64], o_f[:, hh, :], AF.Square, [T + "o_f"], ["junk", T + "ssd"], accum=ssd[:, hh:hh + 1])
                    rstd_from_ss(ssd, 64, rsd, [T + "ssd", "epsc"], [T + "rsd"])
                    for hh in range(2):
                        STT("dve", yb[:, hh, :], o_f[:, hh, :], rsd[:, hh:hh + 1], gsub, ALU.mult, ALU.mult, [T + "o_f", T + "rsd", T + "gsub"], [ybn])
                    TR(bank_bf(bk)[:, 640:768], yb.rearrange("p h d -> p (h d)"), ident_bf, [ybn, "ident_bf"], [pr_])
                    CP("act", otst[sb % 2][:, 0, :], bank_bf(bk)[:, 640:768], [pr_], [L + f"otst{sb % 2}"])
                    DMA("sp", otd[tile_i, :, c, :], otst[sb % 2][:, 0, :], [L + f"otst{sb % 2}"], [L + f"OT{c}_{tile_i}"], f"ot{sb % 2}")
        A.pop()
        if check_stop(f"da_{l}") or STOP_AFTER in (f"daload_{l}", f"daq1_{l}", f"daq1nf_{l}"):
            P.barrier()
            tap("t_nlam", nlam, [], None)
            tap("t_gsub", gsub, [], None)
            if "dbg_ot" in dbg:
                P.barrier()
                DMA("sp", dbg["dbg_ot"], otd, [], ["dbgot"], "dbg")
            A.pop(); A.pop(); break

        P.barrier()
        T = L + "sw"
        A.push()
        QT = A.alloc([2, TOKC], BF16)
        KT = A.alloc([3328], BF16)
        V1 = A.alloc([26, 2, 65], BF16)
        MSW = A.alloc([10, 128], BF16)
        esink = A.alloc([4], F32)
        DMA("pool", MSW, I["swamask"].rearrange("m k q -> k m q"), [], [T + "msw"], "wB")
        DMA("sp", esink, pbc(I[f"sink{l}"]), [], [T + "esink"], "m0")
        ACTV(esink, esink, AF.Exp, [T + "esink"], [T + "esink"])
        MSET("pool", V1[:, :, :, 64:65], 1.0, [T + "V1ones"])
        A.push()
        wsw = A.alloc([8, 896], BF16)
        DMA("pool", wsw, winv[:, :, SW0:SW0 + 896], [], [T + "w"], "wA")
        dests = [QT[:, 0, 0:TOK], QT[:, 1, 0:TOK], KT[:, 0:TOK]]
        wq_l = [wsw[:, :, c * 128:(c + 1) * 128] for c in range(3)]
        wqs_l = [wsw[:, :, 384 + c * 128:384 + (c + 1) * 128] for c in range(3)]
        proj_rope(wq_l[2:3], wqs_l[2:3], rope64[:, 0, :], rope64[:, 1, :], dests[2:3], T, name0=2)
        proj_tok(wsw[:, :, 768:896], 128, lambda i: V1[:, i, :, 0:64], range(NTC), T, view=lambda a: a.rearrange("p (h d) -> p h d", d=64))
        V_R = [T + f"v{i}" for i in range(NTC)]
        e_s = dint(T + "e", [128, 512], BF16); g_s = dint(T + "g", [512, 512], BF16)
        DMA("sp", e_s.ap()[:, 0:128], KT[:, 0:128], [T + "d2"], [T + "e"], "ex")
        DMA("sp", e_s.ap()[:, 128:256], KT[:, TOK - 128:TOK], [T + "d2"], [T + "e"], "ex")
        DMA("sp", e_s.ap()[:, 256:384].rearrange("p (h d) -> p h d", d=64), V1[:, 0, :, 0:64], V_R, [T + "e"], "ex")
        DMA("sp", e_s.ap()[:, 384:512].rearrange("p (h d) -> p h d", d=64), V1[:, 15, :, 0:64], V_R, [T + "e"], "ex")
        AG(e_s, g_s, [T + "e"], [T + "g"], "cc")
        proj_rope(wq_l[0:2], wqs_l[0:2], rope64[:, 0, :], rope64[:, 1, :], dests[0:2], T, name0=0)
        for ci in range(3):
            dest = QT[:, ci, TOK:TOKC] if ci < 2 else KT[:, TOK:TOKC]
            proj_feat_plain(wsw[:, :, ci * 128:(ci + 1) * 128], dest, TOK, 256, T, f"c{ci}", ci % 2)
        A.pop()
        QK_R = [T + f"d{ci}" for ci in range(3)] + [T + f"dc{ci}" for ci in range(3)]
        if check_stop(f"swproj_{l}"):
            A.pop(); A.pop(); A.pop(); break
        g_sv = g_s.ap().rearrange("(r p) f -> p r f", p=128)
        DMA("sp", KT[:, TOKC:TOKC + 512].rearrange("p (r t) -> p r t", t=128), g_sv[:, :, 128:256], [T + "g"], [T + "halo"], "kt")
        DMA("sp", KT[:, TOKC + 512:TOKC + 1024].rearrange("p (r t) -> p r t", t=128), g_sv[:, :, 0:128], [T + "g"], [T + "halo"], "kt")
        for kv in range(2):
            DMA("sp", V1[:, 18:22, kv, 0:64], g_sv[:, :, 384 + kv * 64:448 + kv * 64], [T + "g"], [T + "halo"], "kt")
            DMA("sp", V1[:, 22:26, kv, 0:64], g_sv[:, :, 256 + kv * 64:320 + kv * 64], [T + "g"], [T + "halo"], "kt")
        if check_stop(f"swag_{l}"):
            A.pop(); A.pop(); A.pop(); break
        ptw = [A.alloc([2, 4, 128], BF16) for _ in range(2)]
        qz = [A.alloc([2, 2, 128], BF16) for _ in range(2)]
        den = [A.alloc([4], F32) for _ in range(2)]; ysw = [A.alloc([4, 64], BF16) for _ in range(2)]
        ALLR = QK_R + V_R + [T + "halo", T + "V1ones"]
        tiles_slots = []
        for i in range(nto):
            if i < NT:
                slots = []
                if i > 0:
                    slots.append((128 * (i - 1), i - 1, 0))
                slots.append((128 * i, i, None))
                if i < NT - 1:
                    slots.append((128 * (i + 1), i + 1, 1))
                slots += [(TOK, 16, None), (TOK + 128, 17, None)]
                if i == 0:
                    slots += [(TOKC + 128 * r, 18 + r, 2 + r) for r in range(4)]
                if i == NT - 1:
                    slots += [(TOKC + 512 + 128 * r, 22 + r, 6 + r) for r in range(4)]
            else:
                slots = [(TOK, 16, None), (TOK + 128, 17, None)]
            tiles_slots.append((i, slots))

        def sw_S(k, u):
            i, gi, ngr, grp = u
            s2 = k % 2
            ts = slice(i * 128, (i + 1) * 128)
            if gi == 0:
                for kv in range(2):
                    TS("dve", qz[i % 2][:, kv], QT[:, :, ts], HM[:, kv:kv + 1], ALU.mult, QK_R + ["HM"], [T + f"qz{i % 2}"])
            for si, (kc0, vt, mi) in enumerate(grp):
                for kv in range(2):
                    for g in range(2):
                        MM(bank(2 * s2 + si, 128, (kv * 2 + g) * 128), KT[:, kc0:kc0 + 128], qz[i % 2][:, kv, g, :], True, True,
                           ALLR + [T + f"qz{i % 2}"], [f"psS{s2}"], sgc=True)

        def sw_E(k, u):
            i, gi, ngr, grp = u
            s2 = k % 2
            ns = len(grp)
            ACTV(ptw[s2][:, 0:ns].rearrange("p s h q -> p (s h q)"), psum[:, 1024 * s2:1024 * s2 + 512 * ns], AF.Exp, [f"psS{s2}"], [T + f"pt{s2}"], scale=0.125)
            for si, (kc0, vt, mi) in enumerate(grp):
                if mi is not None:
                    TT("dve", ptw[s2][:, si], ptw[s2][:, si], MSW[:, mi, :].unsqueeze(1).to_broadcast([128, 4, 128]), ALU.mult, [T + f"pt{s2}", T + "msw"], [T + f"pt{s2}"])

        def sw_AV(k, u):
            i, gi, ngr, grp = u
            s2 = k % 2
            ns = len(grp)
            for si, (kc0, vt, mi) in enumerate(grp):
                first = (gi == 0 and si == 0); last = (gi == ngr - 1 and si == ns - 1)
                for kv in range(2):
                    for g in range(2):
                        h = kv * 2 + g
                        MM(bank(4 + i % 2, 65, h * 65), ptw[s2][:, si, h, :], V1[:, vt, kv, :], first and h == 0, last, [T + f"pt{s2}"] + ALLR, [f"psO{i % 2}"], sgc=True)

        def sw_FIN(i):
            Ov = bank(4 + i % 2, 260).rearrange("p (h e) -> p h e", e=65)
            dn = den[i % 2]
            TT("dve", dn, Ov[:, :, 64], esink, ALU.add, [f"psO{i % 2}", T + "esink"], [T + f"den{i % 2}"])
            RECIP(dn, dn, [T + f"den{i % 2}"], [T + f"den{i % 2}"])
            yb = ysw[i % 2]
            TT("dve", yb, Ov[:, :, 0:64], dn.unsqueeze(2).to_broadcast([128, 4, 64]), ALU.mult, [f"psO{i % 2}", T + f"den{i % 2}"], [T + f"y{i % 2}"])
            out_transposes(yb.rearrange("p h d -> p (h d)"), 2, i, T, [T + f"y{i % 2}"])

        attn_pipeline(tiles_slots, sw_S, sw_E, sw_AV, sw_FIN)
        A.pop()
        if check_stop(f"sw_{l}") or (STOP_AFTER or "").startswith("swq1"):
            if "dbg_ot" in dbg:
                P.barrier()
                DMA("sp", dbg["dbg_ot"], otd, [], ["dbgot"], "dbg")
            A.pop(); A.pop(); break

        P.barrier()
        T = L + "na"
        A.push()
        QT = A.alloc([2, TOKC], BF16)
        KT = A.alloc([2, 4352], BF16)
        V1 = A.alloc([34, 4, 65], BF16)
        MBK = A.alloc([45, 128], BF16)
        BEX = A.alloc([7, 4, 128], BF16)
        EIN = A.alloc([5, 4, 128], BF16)
        for m0 in range(0, 45, 9):
            DMA("pool", MBK[:, m0:m0 + 9, :], I["namask"][m0:m0 + 9].rearrange("m k q -> k m q"), [], [T + "mbk"], "wB")
        A.push()
        bfl = A.alloc([7, 4, 128], F32)
        for d7 in range(7):
            DMA("sp", bfl[:, d7], I[f"nabias{l}"][d7].rearrange("h k q -> k h q"), [], [T + "bfl"], "m0")
        ACTV(BEX, bfl, AF.Exp, [T + "bfl"], [T + "bex"])
        A.pop()
        for d in range(5):
            TT("dve", EIN[:, d], BEX[:, d + 1], MBK[:, d, :].unsqueeze(1).to_broadcast([128, 4, 128]), ALU.mult, [T + "bex", T + "mbk"], [T + "ein"])
        MSET("pool", V1[:, :, :, 64:65], 1.0, [T + "V1ones"])
        A.push()
        wna = A.alloc([8, 768], BF16)
        DMA("pool", wna, winv[:, :, NA0:NA0 + 768], [], [T + "w"], "wA")
        n = 0
        for ci in (2, 3):
            for (t0, nt_) in ((0, 512), (512, 512), (1024, 512), (1536, 512), (TOK, 256)):
                dest = KT[:, ci - 2, t0:t0 + nt_]
                proj_feat_plain(wna[:, :, ci * 128:(ci + 1) * 128], dest, t0, nt_, T, f"c{ci}", n % 4); n += 1
        proj_tok(wna[:, :, 512:768], 256, lambda i: V1[:, i, :, 0:64], range(NTC), T, view=lambda a: a.rearrange("p (h d) -> p h d", d=64))
        V_R = [T + f"v{i}" for i in range(NTC)]
        e_n = dint(T + "e", [128, 2048], BF16); g_n = dint(T + "g", [512, 2048], BF16)
        env = e_n.ap()
        for c in range(2):
            DMA("sp", env[:, c * 512:c * 512 + 256], KT[:, c, 0:256], [T + "dc2", T + "dc3"], [T + "e"], "ex")
            DMA("sp", env[:, c * 512 + 256:c * 512 + 512], KT[:, c, TOK - 256:TOK], [T + "dc2", T + "dc3"], [T + "e"], "ex")
        DMA("sp", env[:, 1024:1536].rearrange("p (i h d) -> p i h d", h=4, d=64), V1[:, 0:2, :, 0:64], V_R, [T + "e"], "ex")
        DMA("sp", env[:, 1536:2048].rearrange("p (i h d) -> p i h d", h=4, d=64), V1[:, 14:16, :, 0:64], V_R, [T + "e"], "ex")
        AG(e_n, g_n, [T + "e"], [T + "g"], "cc")
        for ci in (0, 1):
            for (t0, nt_) in ((0, 512), (512, 512), (1024, 512), (1536, 512), (TOK, 256)):
                dest = QT[:, ci, t0:t0 + nt_]
                proj_feat_plain(wna[:, :, ci * 128:(ci + 1) * 128], dest, t0, nt_, T, f"c{ci}", n % 4); n += 1
        A.pop()
        P.barrier()
        QK_R = [T + f"dc{ci}" for ci in range(4)]
        g_nv = g_n.ap().rearrange("(r p) f -> p r f", p=128)
        for c in range(2):
            DMA("sp", KT[:, c, TOKC:TOKC + 1024].rearrange("p (r t) -> p r t", t=256), g_nv[:, :, c * 512 + 256:c * 512 + 512], [T + "g"], [T + "halo"], "kt")
            DMA("sp", KT[:, c, TOKC + 1024:TOKC + 2048].rearrange("p (r t) -> p r t", t=256), g_nv[:, :, c * 512:c * 512 + 256], [T + "g"], [T + "halo"], "kt")
        for r in range(4):
            DMA("sp", V1[:, 18 + 2 * r:20 + 2 * r, :, 0:64], g_nv[:, r, 1536:2048].rearrange("p (i h d) -> p i h d", h=4, d=64), [T + "g"], [T + "halo"], "kt")
            DMA("sp", V1[:, 26 + 2 * r:28 + 2 * r, :, 0:64], g_nv[:, r, 1024:1536].rearrange("p (i h d) -> p i h d", h=4, d=64), [T + "g"], [T + "halo"], "kt")
        ptn = [A.alloc([2, 4, 128], BF16) for _ in range(2)]
        qz = [A.alloc([2, 2, 128], BF16) for _ in range(2)]
        den = [A.alloc([4], F32) for _ in range(2)]; yna = [A.alloc([4, 64], BF16) for _ in range(2)]
        ALLR = QK_R + V_R + [T + "halo", T + "V1ones"]
        PC0 = TOKC; NC0 = TOKC + 1024
        tiles_slots = []
        for i in range(nto):
            if i >= NT:
                slots = [(TOK, 16, 0, 0, 0), (TOK + 128, 17, 0, 0, 0)]
            elif 2 <= i <= 13:
                slots = [(128 * (i + d), i + d, 1, d + 2, 0) for d in range(-2, 3)]
            elif i == 0:
                slots = [(128 * d, d, 2, d + 3, 5 + d) for d in (0, 1, 2, 3)]
                slots += [(PC0 + 256 * r, 18 + 2 * r, 2, 1, 9 + r) for r in range(4)]
                slots += [(PC0 + 256 * r + 128, 19 + 2 * r, 2, 2, 13 + r) for r in range(4)]
            elif i == 1:
                slots = [(128 * (1 + d), 1 + d, 2, d + 3, 17 + (d + 1)) for d in (-1, 0, 1, 2)]
                slots += [(PC0 + 256 * r + 128, 19 + 2 * r, 2, 1, 21 + r) for r in range(4)]
            elif i == 14:
                slots = [(128 * (14 + d), 14 + d, 2, d + 3, 25 + (d + 2)) for d in (-2, -1, 0, 1)]
                slots += [(NC0 + 256 * r, 26 + 2 * r, 2, 5, 29 + r) for r in range(4)]
            else:
                slots = [(128 * (15 + d), 15 + d, 2, d + 3, 33 + (d + 3)) for d in (-3, -2, -1, 0)]
                slots += [(NC0 + 256 * r, 26 + 2 * r, 2, 4, 37 + r) for r in range(4)]
                slots += [(NC0 + 256 * r + 128, 27 + 2 * r, 2, 5, 41 + r) for r in range(4)]
            if i < NT:
                slots += [(TOK, 16, 0, 0, 0), (TOK + 128, 17, 0, 0, 0)]
            tiles_slots.append((i, slots))

        def na_S(k, u):
            i, gi, ngr, grp = u
            s2 = k % 2
            ts = slice(i * 128, (i + 1) * 128)
            if gi == 0:
                for hb_ in range(2):
                    TS("dve", qz[i % 2][:, hb_], QT[:, :, ts], HM[:, hb_:hb_ + 1], ALU.mult, QK_R + ["HM"], [T + f"qz{i % 2}"])
            for si, (kc0, vt, kind, bd, mi) in enumerate(grp):
                for h in range(4):
                    c, hb_ = h // 2, h % 2
                    MM(bank(2 * s2 + si, 128, h * 128), KT[:, c, kc0:kc0 + 128], qz[i % 2][:, hb_, c, :], True, True, ALLR + [T + f"qz{i % 2}"], [f"psS{s2}"], sgc=True)

        def na_E(k, u):
            i, gi, ngr, grp = u
            s2 = k % 2
            ns = len(grp)
            ACTV(ptn[s2][:, 0:ns].rearrange("p s h q -> p (s h q)"), psum[:, 1024 * s2:1024 * s2 + 512 * ns], AF.Exp, [f"psS{s2}"], [T + f"pt{s2}"], scale=0.125)
            for si, (kc0, vt, kind, bd, mi) in enumerate(grp):
                if kind == 1:
                    TT("dve", ptn[s2][:, si], ptn[s2][:, si], EIN[:, bd], ALU.mult, [T + f"pt{s2}", T + "ein"], [T + f"pt{s2}"])
                elif kind == 2:
                    TT("dve", ptn[s2][:, si], ptn[s2][:, si], BEX[:, bd], ALU.mult, [T + f"pt{s2}", T + "bex"], [T + f"pt{s2}"])
                    TT("dve", ptn[s2][:, si], ptn[s2][:, si], MBK[:, mi, :].unsqueeze(1).to_broadcast([128, 4, 128]), ALU.mult, [T + f"pt{s2}", T + "mbk"], [T + f"pt{s2}"])

        def na_AV(k, u):
            i, gi, ngr, grp = u
            s2 = k % 2
            ns = len(grp)
            for si, (kc0, vt, kind, bd, mi) in enumerate(grp):
                first = (gi == 0 and si == 0); last = (gi == ngr - 1 and si == ns - 1)
                for h in range(4):
                    MM(bank(4 + i % 2, 65, h * 65), ptn[s2][:, si, h, :], V1[:, vt, h, :], first and h == 0, last, [T + f"pt{s2}"] + ALLR, [f"psO{i % 2}"], sgc=True)

        def na_FIN(i):
            Ov = bank(4 + i % 2, 260).rearrange("p (h e) -> p h e", e=65)
            dn = den[i % 2]
            RECIP(dn, Ov[:, :, 64], [f"psO{i % 2}"], [T + f"den{i % 2}"])
            yb = yna[i % 2]
            TT("dve", yb, Ov[:, :, 0:64], dn.unsqueeze(2).to_broadcast([128, 4, 64]), ALU.mult, [f"psO{i % 2}", T + f"den{i % 2}"], [T + f"y{i % 2}"])
            out_transposes(yb.rearrange("p h d -> p (h d)"), 4, i, T, [T + f"y{i % 2}"])

        attn_pipeline(tiles_slots, na_S, na_E, na_AV, na_FIN)
        A.pop()
        if check_stop(f"na_{l}"):
            if "dbg_ot" in dbg:
                P.barrier()
                DMA("sp", dbg["dbg_ot"], otd, [], ["dbgot"], "dbg")
            A.pop(); A.pop(); break

        P.barrier()
        T = L + "rt"
        A.push()
        QT = A.alloc([2, TOKC], BF16); KT = A.alloc([2, TOKC], BF16)
        VR = A.alloc([NTC, 256], BF16); GT = A.alloc([NTC, 256], BF16)
        RC = A.alloc([700], F32); IDXB = A.alloc([128], F32)
        LG = A.alloc([8], F32); LGS = A.alloc([2, 2], F32)
        DEC = A.alloc([4, 128], BF16); XI = A.alloc([2, 2, 128], BF16)
        ZZ = A.alloc([2, 4], F32); GC = A.alloc([2, 2], F32); GPW = A.alloc([2, 2, 18], F32); CFC = A.alloc([2, 2, 5], F32)
        DMA("sp", RC, I["retc"], [], [T + "rc"], "m0")
        DMA("sp", IDXB, I["idxb"], [], [T + "rc"], "m0")
        DMA("sp", LG, pbc(I[f"gam{l}"]), [], [T + "lg"], "m0")
        ACTV(LG, LG, AF.Exp, [T + "lg"], [T + "lg"], scale=-1.0)
        TS("dve", LG, LG, 1.0, ALU.add, [T + "lg"], [T + "lg"])
        ACTV(LG, LG, AF.Ln, [T + "lg"], [T + "lg"])
        TS("dve", LG, LG, -1.0, ALU.mult, [T + "lg"], [T + "lg"])
        for d_ in range(2):
            for c in range(2):
                k0_ = 4 * d_ + 2 * c
                TS("dve", LGS[:, d_, c:c + 1], LG[:, k0_:k0_ + 1], HM[:, 0:1], ALU.mult, [T + "lg", "HM"], [T + "lgs"])
                STT("dve", LGS[:, d_, c:c + 1], LG[:, k0_ + 1:k0_ + 2], HM[:, 1:2], LGS[:, d_, c:c + 1], ALU.mult, ALU.add, [T + "lg", "HM", T + "lgs"], [T + "lgs"])
        A.push()
        tf = A.alloc([128], F32); tb_ = A.alloc([128], F32)
        for h in range(4):
            ACTV(tf, RC[:, 0:128], AF.Exp, [T + "rc", T + "lg"], [T + "tf"], scale=LG[:, h:h + 1])
            TT("dve", tf, tf, RC[:, 128:256], ALU.mult, [T + "tf", T + "rc"], [T + "tf"])
            ACTV(tb_, RC[:, 256:384], AF.Exp, [T + "rc", T + "lg"], [T + "tb"], scale=LG[:, 4 + h:5 + h])
            TT("dve", tb_, tb_, RC[:, 384:512], ALU.mult, [T + "tb", T + "rc"], [T + "tb"])
            TT("dve", DEC[:, h, :], tf, tb_, ALU.add, [T + "tf", T + "tb"], [T + "dec"])
        A.pop()
        for c in range(2):
            ACTV(XI[:, 0, c, :], RC[:, 512:640], AF.Exp, [T + "rc", T + "lgs"], [T + "xi"], scale=LGS[:, 0, c:c + 1])
            ACTV(XI[:, 1, c, :], IDXB, AF.Exp, [T + "rc", T + "lgs"], [T + "xi"], scale=LGS[:, 1, c:c + 1])
            for d_ in range(2):
                ACTV(GC[:, d_, c:c + 1], LGS[:, d_, c:c + 1], AF.Exp, [T + "lgs"], [T + "gc"], scale=128.0)
                ACTV(GPW[:, d_, c, :], RC[:, 642 + 18 * d_:660 + 18 * d_], AF.Exp, [T + "rc", T + "lgs"], [T + "gpw"], scale=LGS[:, d_, c:c + 1])
                ACTV(CFC[:, d_, c, :], RC[:, 678 + 5 * d_:683 + 5 * d_], AF.Exp, [T + "rc", T + "lgs"], [T + "cfc"], scale=LGS[:, d_, c:c + 1])
                TT("dve", CFC[:, d_, c, :], CFC[:, d_, c, :], RC[:, 688 + 5 * d_:693 + 5 * d_], ALU.mult, [T + "cfc", T + "rc"], [T + "cfc"])
        ACTV(ZZ[:, 0, :], LG[:, 0:4], AF.Exp, [T + "lg", T + "rc"], [T + "zz"], scale=RC[:, 640:641])
        ACTV(ZZ[:, 1, :], LG[:, 4:8], AF.Exp, [T + "lg", T + "rc"], [T + "zz"], scale=RC[:, 641:642])
        TS("dve", ZZ, ZZ, 0.125, ALU.mult, [T + "zz"], [T + "zz"])
        if check_stop(f"rtparam_{l}"):
            A.pop(); A.pop(); A.pop(); break
        A.push()
        wrt = A.alloc([8, 1536], BF16)
        DMA("pool", wrt, winv[:, :, RT0:RT0 + 1536], [], [T + "w"], "wA")
        dests = [QT[:, 0, 0:TOK], QT[:, 1, 0:TOK], KT[:, 0, 0:TOK], KT[:, 1, 0:TOK]]
        proj_rope([wrt[:, :, c * 128:(c + 1) * 128] for c in range(4)], [wrt[:, :, 512 + c * 128:512 + (c + 1) * 128] for c in range(4)],
                  rope64[:, 0, :], rope64[:, 1, :], dests, T)
        for ci in range(4):
            dest = QT[:, ci, TOK:TOKC] if ci < 2 else KT[:, ci - 2, TOK:TOKC]
            proj_feat_plain(wrt[:, :, ci * 128:(ci + 1) * 128], dest, TOK, 256, T, f"c{ci}", ci % 2)

        def vg_post(i, pb):
            CP("act", VR[:, i, :], bank(pb, 256), [f"ps{pb}"], [T + f"v{i}"])
            ACTV(GT[:, i, :], bank(pb, 256, 256), AF.Silu, [f"ps{pb}"], [T + f"g{i}"])
        proj_tok(wrt[:, :, 1024:1536], 512, None, range(NTC), T, post=vg_post)
        A.pop()
        P.barrier()
        QK_R = [T + f"d{ci}" for ci in range(4)] + [T + f"dc{ci}" for ci in range(4)]
        if check_stop(f"rtproj_{l}"):
            A.pop(); A.pop(); A.pop(); break
        KTOK = A.alloc([NTC, 256], BF16)
        SZ = A.alloc([2, 18, 2, 64], F32)
        UCX = A.alloc([4, 2, 64], F32)
        SCX = A.alloc([2, 2, 64], F32)
        S0 = A.alloc([2, 2, 64], F32)
        SB = A.alloc([2, NTC, 2, 64], BF16)
        GR = A.alloc([4, 256], F32); EXPB = A.alloc([2, 2, 64], F32)
        for i in range(NTC):
            pb = i % 2
            for c in range(2):
                TR(bank_bf(pb)[:, c * 128:(c + 1) * 128], KT[:, c, i * 128:(i + 1) * 128], ident_bf, QK_R + ["ident_bf"], [f"ps{pb}"])
            CP("act", KTOK[:, i, :], bank_bf(pb)[:, 0:256], [f"ps{pb}"], [T + f"kt{i}"])
        vz = [A.alloc([2, 256], BF16) for _ in range(2)]
        MSET("pool", SZ[:, 0, 0], 0.0, [T + "sz"])
        MSET("pool", SZ[:, 1, 16], 0.0, [T + "sz"])

        def chunk_U(n, s2):
            for d_ in range(2):
                TT("dve" if d_ == 0 else "pool", vz[s2][:, d_].rearrange("p (h e) -> p h e", e=64), VR[:, n, :].rearrange("p (h e) -> p h e", e=64),
                   ZZ[:, d_, :].unsqueeze(2).to_broadcast([128, 4, 64]), ALU.mult, [T + f"v{n}", T + "zz"], [T + f"vz{s2}"])
            for d_ in range(2):
                for c in range(2):
                    MM(bank(2 + s2, 128, (d_ * 2 + c) * 128), KTOK[:, n, c * 128:(c + 1) * 128], vz[s2][:, d_, c * 128:(c + 1) * 128], True, True,
                       [T + f"kt{n}", T + f"vz{s2}"], [f"ps{2 + s2}"])

        udg = A.alloc([64], F32); udt = A.alloc([64], F32)

        def udiag(s2, d_, c, dst=None, dreg=None):
            blk = bank(2 + s2, 128, (d_ * 2 + c) * 128)
            o_ = udg if dst is None else dst
            TS("dve", udt, blk[:, 0:64], HM[:, 0:1], ALU.mult, [f"ps{2 + s2}", "HM"], [T + "udt"])
            STT("dve", o_, blk[:, 64:128], HM[:, 1:2], udt, ALU.mult, ALU.add, [f"ps{2 + s2}", "HM", T + "udt"], [T + "udg" if dreg is None else dreg])
            return o_

        chunk_U(0, 0)
        for n in range(NT):
            if n + 1 < NT:
                chunk_U(n + 1, (n + 1) % 2)
            for c in range(2):
                u_ = udiag(n % 2, 0, c)
                STT("dve", SZ[:, 0, n + 1, c, :], SZ[:, 0, n, c, :], GC[:, 0, c:c + 1], u_, ALU.mult, ALU.add, [T + "sz", T + "gc", T + "udg"], [T + "sz"])
                udiag(n % 2, 1, c, dst=SZ[:, 1, n, c, :], dreg=T + "szb")
        for n in range(NT - 1, -1, -1):
            for c in range(2):
                STT("dve", SZ[:, 1, n, c, :], SZ[:, 1, n + 1, c, :], GC[:, 1, c:c + 1], SZ[:, 1, n, c, :], ALU.mult, ALU.add, [T + "sz", T + "szb", T + "gc"], [T + "sz", T + "szb"])
        for k_, n in enumerate((16, 17)):
            chunk_U(n, k_)
            for d_ in range(2):
                for c in range(2):
                    u_ = udiag(k_, d_, c)
                    CP("dve", UCX[:, 2 * d_ + k_, c, :], u_, [T + "udg"], [T + "ucx"])
        for c in range(2):
            STT("dve", SCX[:, 0, c, :], UCX[:, 0, c, :], GC[:, 0, c:c + 1], UCX[:, 1, c, :], ALU.mult, ALU.add, [T + "ucx", T + "gc"], [T + "scx"])
            STT("dve", SCX[:, 1, c, :], UCX[:, 3, c, :], GC[:, 1, c:c + 1], UCX[:, 2, c, :], ALU.mult, ALU.add, [T + "ucx", T + "gc"], [T + "scx"])
        CP("dve", EXPB[:, 0], SZ[:, 0, 16], [T + "sz"], [T + "expb"])
        CP("dve", EXPB[:, 1], SZ[:, 1, 0], [T + "sz"], [T + "expb"])
        e_r = dint(T + "e", [128, 256], F32); g_r = dint(T + "g", [512, 256], F32)
        DMA("sp", e_r.ap(), EXPB.rearrange("p d c e -> p (d c e)"), [T + "expb"], [T + "e"], "ex")
        AG(e_r, g_r, [T + "e"], [T + "g"], "cc")
        DMA("sp", GR, g_r.ap().rearrange("(r p) f -> p r f", p=128), [T + "g"], [T + "gr"], "kt")
        GRv = GR.rearrange("p r (d c e) -> p r d c e", d=2, c=2)
        for d_ in range(2):
            for c in range(2):
                TS("dve", S0[:, d_, c, :], SCX[:, d_, c, :], CFC[:, d_, c, 4:5], ALU.mult, [T + "scx", T + "cfc"], [T + "s0"])
                for r in range(4):
                    STT("dve", S0[:, d_, c, :], GRv[:, r, d_, c, :], CFC[:, d_, c, r:r + 1], S0[:, d_, c, :], ALU.mult, ALU.add, [T + "gr", T + "cfc", T + "s0"], [T + "s0"])
        for n in range(NT):
            for c in range(2):
                STT("dve", SB[:, 0, n, c, :], S0[:, 0, c, :], GPW[:, 0, c, n:n + 1], SZ[:, 0, n, c, :], ALU.mult, ALU.add, [T + "s0", T + "gpw", T + "sz"], [T + "sb"])
                STT("dve", SB[:, 1, n, c, :], S0[:, 1, c, :], GPW[:, 1, c, n:n + 1], SZ[:, 1, n + 1, c, :], ALU.mult, ALU.add, [T + "s0", T + "gpw", T + "sz"], [T + "sb"])
        MSET("pool", SB[:, 0, 16], 0.0, [T + "sb"])
        MSET("pool", SB[:, 1, 17], 0.0, [T + "sb"])
        CP("dve", SB[:, 0, 17], UCX[:, 0], [T + "ucx"], [T + "sb"])
        CP("dve", SB[:, 1, 16], UCX[:, 3], [T + "ucx"], [T + "sb"])
        if check_stop(f"rtA_{l}"):
            A.pop(); A.pop(); A.pop(); break
        AD = [A.alloc([4, 128], BF16) for _ in range(2)]
        QX = [A.alloc([2, 2, 2, 128], BF16) for _ in range(2)]
        qz = [A.alloc([2, 2, 128], BF16) for _ in range(2)]
        of_ = [A.alloc([4, 64], F32) for _ in range(2)]
        sq = A.alloc([4, 64], F32); ssr = A.alloc([4], F32); rsr = A.alloc([4], F32)
        yr_ = [A.alloc([4, 64], BF16) for _ in range(2)]
        def rt_A(i):
            s2 = i % 2
            ts = slice(i * 128, (i + 1) * 128)
            for hb_ in range(2):
                TS("pool" if hb_ else "dve", qz[s2][:, hb_], QT[:, :, ts], HM[:, hb_:hb_ + 1], ALU.mult, QK_R + ["HM"], [T + f"qz{s2}"])
            for h in range(4):
                c, hb_ = h // 2, h % 2
                MM(bank(s2, 128, h * 128), KT[:, c, ts], qz[s2][:, hb_, c, :], True, True, QK_R + [T + f"qz{s2}"], [f"ps{s2}"], sgc=True)

        def rt_mid(i):
            s2 = i % 2
            TT("dve", AD[s2], bank(s2).rearrange("p (h i) -> p h i", i=128), DEC, ALU.mult, [f"ps{s2}", T + "dec"], [T + f"ad{s2}"])
            for d_ in range(2):
                for hb_ in range(2):
                    TT("dve", QX[s2][:, d_, hb_], qz[s2][:, hb_], XI[:, d_], ALU.mult, [T + f"qz{s2}", T + "xi"], [T + f"qx{s2}"])

        def rt_out(i):
            s2 = i % 2
            pO = 4 + s2
            for h in range(4):
                c, hb_ = h // 2, h % 2
                o_ = bank(pO, 64, h * 64)
                MM(o_, AD[s2][:, h, :], VR[:, i, h * 64:(h + 1) * 64], h == 0, False, [T + f"ad{s2}", T + f"v{i}"], [f"ps{pO}"], sgc=True)
                MM(o_, QX[s2][:, 0, hb_, c, :], SB[:, 0, i, c, :], False, False, [T + f"qx{s2}", T + "sb"], [f"ps{pO}"], sgc=True)
                MM(o_, QX[s2][:, 1, hb_, c, :], SB[:, 1, i, c, :], False, True, [T + f"qx{s2}", T + "sb"], [f"ps{pO}"], sgc=True)

        def rt_fin(i):
            s2 = i % 2
            pO = 4 + s2
            ov = of_[s2]
            CP("act", ov, bank(pO, 256).rearrange("p (h e) -> p h e", e=64), [f"ps{pO}"], [T + f"of{s2}"])
            TT("dve", sq, ov, ov, ALU.mult, [T + f"of{s2}"], [T + "sq"])
            RED("dve", ssr, sq, [T + "sq"], [T + "ssr"])
            rstd_from_ss(ssr, 64, rsr, [T + "ssr", "epsc"], [T + "rsr"])
            TT("dve", sq, ov, rsr.unsqueeze(2).to_broadcast([128, 4, 64]), ALU.mult, [T + f"of{s2}", T + "rsr"], [T + "sq"])
            TT("dve", yr_[s2], sq, GT[:, i, :].rearrange("p (h e) -> p h e", e=64), ALU.mult, [T + "sq", T + f"g{i}"], [T + f"y{s2}"])

        def rt_tr(i):
            out_transposes(yr_[i % 2].rearrange("p h d -> p (h d)"), 6, i, T, [T + f"y{i % 2}"])

        rt_A(0)
        rt_mid(0)
        if nto > 1:
            rt_A(1)
        for i in range(nto):
            rt_out(i)
            if i + 1 < nto:
                rt_mid(i + 1)
            if i + 2 < nto:
                rt_A(i + 2)
            rt_fin(i)
            if i >= 1:
                rt_tr(i - 1)
        rt_tr(nto - 1)
        A.pop()
        A.pop()
        if "dbg_ot" in dbg and l == 0:
            DMA("sp", dbg["dbg_ot"], otd, [L + f"OT{c0}_{i}" for c0 in (0, 1, 2, 4, 6) for i in range(nto)], ["dbgot"], "dbg")
        if check_stop(f"rt_{l}") or (STOP_AFTER or "").startswith("rtB"):
            A.pop(); break

        P.barrier()
        A.push()
        h2T = hT
        wo = A.alloc([8, 1024], BF16)
        DMA("pool", wo, I[f"wout{l}"].rearrange("(kc p) n -> p kc n", p=128), [], [L + "wo"], "wA")
        if moe:
            RB = A.alloc([8, 1024], F32)
            DMA("sp", RB, pbc(I["router"]).rearrange("p (e d) -> p e d", d=1024), [], [L + "rb"], "m0")
            rj = [A.alloc([1024], F32) for _ in range(3)]; sm = A.alloc([8, 8], F32)
        xt = [A.alloc([1024], F32) for _ in range(2)]
        t1 = [A.alloc([1024], F32) for _ in range(2)]
        xm = [A.alloc([1024], F32) for _ in range(2)]
        hb = [A.alloc([1024], BF16) for _ in range(2)]
        ott = [A.alloc([8, 128], BF16) for _ in range(2)]
        ss3 = A.alloc([NTC, 2], F32); rs3 = A.alloc([NTC, 2], F32)
        xdst = (xs, xcs)

        def p3_mm(i):
            s2 = i % 2
            pb = 2 * s2
            ot_r = [L + f"OT{c0}_{i}" for c0 in (0, 1, 2, 4, 6)]
            DMA("sp", ott[s2], otd[i], ot_r, [L + f"ott{s2}"], f"ott{s2}")
            for hf in range(2):
                for kc in range(8):
                    MM(bank(pb + hf), ott[s2][:, kc, :], wo[:, kc, hf * 512:(hf + 1) * 512], kc == 0, kc == 7, [L + f"ott{s2}", L + "wo"], [f"ps{pb + hf}"])

        def p3_s1(i):
            s2 = i % 2
            pb = 2 * s2
            yps = psum[:, 512 * pb:512 * pb + 1024]
            ACTV(junk, yps, AF.Square, [f"ps{pb}", f"ps{pb + 1}"], ["junk", L + f"s3_{i}"], accum=ss3[:, i, 0:1])
            rstd_from_ss(ss3[:, i, 0:1], 1024, rs3[:, i, 0:1], [L + f"s3_{i}", "epsc"], [L + f"r3_{i}"])
            DMA("sp", xt[s2], xtile_ap(xsrc, i), [], [L + f"p3xt{s2}"], f"xt{s2}")

        def p3_s2(i):
            s2 = i % 2
            v = 0 if i < NT else 1
            pb = 2 * s2
            yps = psum[:, 512 * pb:512 * pb + 1024]
            STT("dve", t1[s2], yps, rs3[:, i, 0:1], MOD[:, v, 2, :], ALU.mult, ALU.mult, [f"ps{pb}", f"ps{pb + 1}", L + f"r3_{i}", f"MOD{v}2"], [L + f"p3t1{s2}"])
            TT("dve", xm[s2], t1[s2], xt[s2], ALU.add, [L + f"p3t1{s2}", L + f"p3xt{s2}"], [L + f"xm{s2}"])
            DMA("sp", xtile_ap(xdst, i), xm[s2], [L + f"xm{s2}"], [L + f"xs{i}"], f"xst{s2}")
            ACTV(junk, xm[s2], AF.Square, [L + f"xm{s2}"], ["junk", L + f"s4_{i}"], accum=ss3[:, i, 1:2])
            rstd_from_ss(ss3[:, i, 1:2], 1024, rs3[:, i, 1:2], [L + f"s4_{i}", "epsc"], [L + f"r4_{i}"])

        def p3_s3(i):
            s2 = i % 2
            v = 0 if i < NT else 1
            STT("dve", t1[s2], xm[s2], rs3[:, i, 1:2], MOD[:, v, 4, :], ALU.mult, ALU.mult, [L + f"xm{s2}", L + f"r4_{i}", f"MOD{v}4"], [L + f"p3t1{s2}"])
            if moe:
                TT("dve", xt[s2], t1[s2], MOD[:, v, 3, :], ALU.add, [L + f"p3t1{s2}", f"MOD{v}3"], [L + f"p3xt{s2}"])
                CP("act", hb[s2], xt[s2], [L + f"p3xt{s2}"], [L + f"p3hb{s2}"])
                for e_ in range(8):
                    TT("dve", rj[e_ % 3], xt[s2], RB[:, e_, :], ALU.mult, [L + f"p3xt{s2}", L + "rb"], [L + f"rj{e_ % 3}"])
                    ACTV(junk, rj[e_ % 3], AF.Identity, [L + f"rj{e_ % 3}"], ["junk", L + f"logi{i}"], accum=LOGI[:, i, e_:e_ + 1])
                lg_ = LOGI[:, i, :]
                RED("dve", sm[:, 0, 0:1], lg_, [L + f"logi{i}"], [L + "sm"], mx=True)
                TS("dve", sm[:, 1, :], lg_, sm[:, 0, 0:1], ALU.is_equal, [L + f"logi{i}", L + "sm"], [L + "sm"])
                STT("dve", sm[:, 2, :], sm[:, 1, :], -1e30, lg_, ALU.mult, ALU.add, [L + "sm", L + f"logi{i}"], [L + "sm"])
                RED("dve", sm[:, 0, 1:2], sm[:, 2, :], [L + "sm"], [L + "sm"], mx=True)
                TS("dve", sm[:, 3, :], lg_, sm[:, 0, 1:2], ALU.is_ge, [L + f"logi{i}", L + "sm"], [L + "sm"])
                TS("dve", sm[:, 0, 2:3], sm[:, 0, 0:1], -1.0, ALU.mult, [L + "sm"], [L + "sm"])
                ACTV(sm[:, 4, :], lg_, AF.Exp, [L + f"logi{i}", L + "sm"], [L + "sm"], bias=sm[:, 0, 2:3])
                TT("dve", sm[:, 4, :], sm[:, 4, :], sm[:, 3, :], ALU.mult, [L + "sm"], [L + "sm"])
                RED("dve", sm[:, 0, 3:4], sm[:, 4, :], [L + "sm"], [L + "sm"])
                RECIP(sm[:, 0, 3:4], sm[:, 0, 3:4], [L + "sm"], [L + "sm"])
                TS("dve", GATES[:, i, :], sm[:, 4, :], sm[:, 0, 3:4], ALU.mult, [L + "sm"], [L + f"gates{i}"])
            else:
                TT("dve", hb[s2], t1[s2], MOD[:, v, 3, :], ALU.add, [L + f"p3t1{s2}", f"MOD{v}3"], [L + f"p3hb{s2}"])

        def p3_tr(i):
            s2 = i % 2
            ts = slice(i * 128, (i + 1) * 128)
            pt_ = 4 + s2
            for kc in range(8):
                TR(bank_bf(pt_)[:, kc * 128:(kc + 1) * 128], hb[s2][:, kc * 128:(kc + 1) * 128], ident_bf, [L + f"p3hb{s2}", "ident_bf"], [f"ps{pt_}"])
            CP("act", h2T[:, :, ts], bank_bf(pt_).rearrange("p (k t) -> p k t", t=128), [f"ps{pt_}"], [L + f"h2T{i}"])

        p3_mm(0)
        p3_s1(0)
        if nto > 1:
            p3_mm(1)
        for i in range(nto):
            p3_s2(i)
            if i + 1 < nto:
                p3_s1(i + 1)
            p3_s3(i)
            if i + 2 < nto:
                p3_mm(i + 2)
            p3_tr(i)
        H2_ALL = [L + f"h2T{i}" for i in range(nto)]
        A.pop()
        if check_stop(f"p3_{l}"):
            A.pop(); break

        P.barrier()
        Y = A.alloc([nto, 1024], F32)
        A.push()
        wg = [A.alloc([8, 256], BF16) for _ in range(2)]
        wu = [A.alloc([8, 256], BF16) for _ in range(2)]
        wd = [A.alloc([2, 1024], BF16) for _ in range(2)]
        sg = [A.alloc([512], BF16) for _ in range(2)]
        AT = [A.alloc([2, 512], BF16) for _ in range(2)]
        ntok = nto * 128
        tblocks = [(t0, min(512, ntok - t0)) for t0 in range(0, ntok, 512)]
        if moe:
            slabs = [(e_, s_) for e_ in range(8) for s_ in range(14)]
        else:
            slabs = [(None, s_) for s_ in range(11)]
        nmm = 0
        for si, (e_, s_) in enumerate(slabs):
            sl = si % 2
            if moe:
                gsrc = I["mwg"][e_].rearrange("(kc p) f -> p kc f", p=128)[:, :, s_ * 256:(s_ + 1) * 256]
                usrc = I["mwu"][e_].rearrange("(kc p) f -> p kc f", p=128)[:, :, s_ * 256:(s_ + 1) * 256]
                dsrc = I["mwd"][e_][s_ * 256:(s_ + 1) * 256, :].rearrange("(c p) n -> p c n", p=128)
            else:
                gsrc = I["fwg"].rearrange("(kc p) f -> p kc f", p=128)[:, :, s_ * 256:(s_ + 1) * 256]
                usrc = I["fwu"].rearrange("(kc p) f -> p kc f", p=128)[:, :, s_ * 256:(s_ + 1) * 256]
                dsrc = I["fwd"][s_ * 256:(s_ + 1) * 256, :].rearrange("(c p) n -> p c n", p=128)
            DMA("pool", wg[sl], gsrc, [], [L + f"wg{sl}"], f"fw{sl}")
            DMA("pool", wu[sl], usrc, [], [L + f"wu{sl}"], f"fw{sl}")
            DMA("pool", wd[sl], dsrc, [], [L + f"wd{sl}"], f"fw{sl}")
            for bi, (t0, nt_) in enumerate(tblocks):
                a2 = bi % 2
                for fcl in range(2):
                    pg = nmm % 2; nmm += 1
                    for kc in range(8):
                        MM(bank(pg, nt_), wg[sl][:, kc, fcl * 128:(fcl + 1) * 128], h2T[:, kc, t0:t0 + nt_], kc == 0, kc == 7, H2_ALL + [L + f"wg{sl}"], [f"ps{pg}"])
                    for kc in range(8):
                        MM(bank(2 + pg, nt_), wu[sl][:, kc, fcl * 128:(fcl + 1) * 128], h2T[:, kc, t0:t0 + nt_], kc == 0, kc == 7, H2_ALL + [L + f"wu{sl}"], [f"ps{2 + pg}"])
                    ACTV(sg[pg][:, 0:nt_], bank(pg, nt_), AF.Silu, [f"ps{pg}"], [L + f"sg{pg}"])
                    TT("dve", AT[a2][:, fcl, 0:nt_], sg[pg][:, 0:nt_], bank(2 + pg, nt_), ALU.mult, [L + f"sg{pg}", f"ps{2 + pg}"], [L + f"at{a2}_{fcl}"])
                for tt in range(nt_ // 128):
                    ti = t0 // 128 + tt
                    py = 4 + 2 * (ti % 2)
                    for hf in range(2):
                        for fcl in range(2):
                            MM(bank(py + hf), AT[a2][:, fcl, tt * 128:(tt + 1) * 128], wd[sl][:, fcl, hf * 512:(hf + 1) * 512], fcl == 0, fcl == 1,
                               [L + f"at{a2}_0", L + f"at{a2}_1", L + f"wd{sl}"], [f"ps{py + hf}"])
                    yps = psum[:, 512 * py:512 * py + 1024]
                    rr = [f"ps{py}", f"ps{py + 1}"]
                    if moe:
                        gsc = GATES[:, ti, e_:e_ + 1]
                        if si == 0:
                            TS("dve", Y[:, ti, :], yps, gsc, ALU.mult, rr + [L + f"gates{ti}"], [L + f"Y{ti}"])
                        else:
                            STT("dve", Y[:, ti, :], yps, gsc, Y[:, ti, :], ALU.mult, ALU.add, rr + [L + f"gates{ti}", L + f"Y{ti}"], [L + f"Y{ti}"])
                    else:
                        if si == 0:
                            CP("dve", Y[:, ti, :], yps, rr, [L + f"Y{ti}"])
                        else:
                            TT("dve", Y[:, ti, :], yps, Y[:, ti, :], ALU.add, rr + [L + f"Y{ti}"], [L + f"Y{ti}"])
        A.pop()
        if check_stop(f"p4_{l}"):
            A.pop(); break

        A.push()
        ss5 = A.alloc([NTC], F32); rs5 = A.alloc([NTC], F32)
        xt = [A.alloc([1024], F32) for _ in range(2)]
        t1 = [A.alloc([1024], F32) for _ in range(2)]
        xo = [A.alloc([1024], F32) for _ in range(2)]
        def p5_load(i):
            DMA("sp", xt[i % 2], xtile_ap(xdst, i), [L + f"xs{i}"], [L + f"p5xt{i % 2}"], f"xt{i % 2}")

        p5_load(0)
        for i in range(nto):
            s2 = i % 2
            v = 0 if i < NT else 1
            ACTV(junk, Y[:, i, :], AF.Square, [L + f"Y{i}"], ["junk", L + f"s5_{i}"], accum=ss5[:, i:i + 1])
            rstd_from_ss(ss5[:, i:i + 1], 1024, rs5[:, i:i + 1], [L + f"s5_{i}", "epsc"], [L + f"r5_{i}"])
            if i + 1 < nto:
                p5_load(i + 1)
            STT("dve", t1[s2], Y[:, i, :], rs5[:, i:i + 1], MOD[:, v, 5, :], ALU.mult, ALU.mult, [L + f"Y{i}", L + f"r5_{i}", f"MOD{v}5"], [L + f"p5t1{s2}"])
            TT("dve", xo[s2], t1[s2], xt[s2], ALU.add, [L + f"p5t1{s2}", L + f"p5xt{s2}"], [L + f"xo{s2}"])
            if l == 0:
                DMA("sp", xtile_ap(xdst, i), xo[s2], [L + f"xo{s2}"], [L + f"xs{i}"], f"xst{s2}")
                if "dbg_x" in dbg:
                    dd = dbg["dbg_x"][i * 128:(i + 1) * 128, :] if i < NT else dbg["dbg_xc"][(i - NT) * 128:(i - NT + 1) * 128, :]
                    DMA("sp", dd, xo[s2], [L + f"xo{s2}"], [L + f"dbgx{i}"], "dbg")
            else:
                DMA("sp", out[i * 128:(i + 1) * 128, :], xo[s2], [L + f"xo{s2}"], [f"out{i}"], f"xst{s2}")
        A.pop()
        A.pop()
        if check_stop(f"l{l}"):
            break
    return nc, P, es, A, I


def _emit(nc, P, es):
    tls = P.finalize()
    sems = {tl: es.enter_context(nc.semaphore("s_" + str(tl))) for tl in tls}
    with nc.Block() as block:
        block.sync(P.engine_body("sp", sems, final=True))
        block.tensor(P.engine_body("pe", sems))
        block.vector(P.engine_body("dve", sems))
        block.scalar(P.engine_body("act", sems))
        block.gpsimd(P.engine_body("pool", sems))


_CACHE = {}


def _get_program():
    if "nc" not in _CACHE:
        nc, P, es, A, I = build_program()
        _CACHE["inputs"] = list(I.keys())
        with es:
            _emit(nc, P, es)
        _CACHE["nc"] = nc
        _CACHE["peak"] = A.peak
        _CACHE["nops"] = len(P.ops)
    return _CACHE["nc"]


def _host_inputs(inp):
    f = lambda a: np.ascontiguousarray(np.asarray(a, dtype=np.float32))
    shared = {}
    for l in range(2):
        shared[f"wmod{l}"] = f(inp["w_mod"][l])
        shared[f"bmod{l}"] = f(inp["b_mod"][l]).reshape(1, 6144)
        shared[f"gvec{l}"] = f(np.concatenate([inp["g_attn_pre"][l], inp["g_attn_post"][l], inp["g_ffn_pre"][l], inp["g_ffn_post"][l]])).reshape(1, 4096)
        shared[f"win{l}"] = f(np.asarray(inp["w_in"][l])[:, WIN_PERM])
        shared[f"wout{l}"] = f(inp["w_out"][l])
        shared[f"dal{l}"] = f(np.concatenate([inp["da_lambda_q1"][l], inp["da_lambda_k1"][l], inp["da_lambda_q2"][l], inp["da_lambda_k2"][l]])).reshape(1, 128)
        shared[f"subln{l}"] = f(inp["da_subln"][l]).reshape(1, 64)
        shared[f"sink{l}"] = f(inp["swa_sink"][l]).reshape(1, 4)
        shared[f"gam{l}"] = f(np.concatenate([inp["ret_gamma_fwd"][l], inp["ret_gamma_bwd"][l]])).reshape(1, 8)
        shared[f"nabias{l}"] = _na_bias_layout(np.asarray(inp["na_rpb"][l], dtype=np.float32))
    shared["fwg"] = f(inp["ffn_w_gate"][0]); shared["fwu"] = f(inp["ffn_w_up"][0]); shared["fwd"] = f(inp["ffn_w_down"][0])
    shared["router"] = f(np.asarray(inp["moe_router"][0]).T).reshape(1, 8 * 1024)
    shared["mwg"] = f(inp["moe_w_gate"][0]); shared["mwu"] = f(inp["moe_w_up"][0]); shared["mwd"] = f(inp["moe_w_down"][0])
    shared["idxb"] = _idxb_table()
    x = np.asarray(inp["x"], dtype=np.float32); ctx = np.asarray(inp["ctx"], dtype=np.float32)
    c = np.asarray(inp["c"], dtype=np.float32); c_ctx = np.asarray(inp["c_ctx"], dtype=np.float32)
    maps = []
    for core in range(8):
        b, j = core // 4, core % 4
        m = dict(shared)
        m["xin"] = np.ascontiguousarray(x[b, TOK * j:TOK * (j + 1)])
        m["xcin"] = np.ascontiguousarray(ctx[b])
        m["cvec"] = np.ascontiguousarray(np.concatenate([c[b].reshape(8, 128).T, c_ctx.reshape(8, 128).T], axis=1))
        C64, S64 = _rope_tables(j, 64)
        C32, S32 = _rope_tables(j, 32)
        m["rope64"] = np.ascontiguousarray(np.stack([C64, S64], axis=1))
        m["rope32"] = np.ascontiguousarray(np.stack([C32, S32], axis=1))
        m["swamask"] = _swa_masks(j)
        m["namask"] = _na_masks(j)
        m["retc"] = _ret_consts(j)
        if "inputs" in _CACHE:
            m = {k: v for k, v in m.items() if k in _CACHE["inputs"]}
        maps.append(m)
    return maps


def kernel(**inputs):
    nc = _get_program()
    maps = _host_inputs(inputs)
    res = run_bass_kernel_spmd(nc, maps, core_ids=list(range(8)))
    _CACHE["last"] = res
    outp = np.empty((2, 8192, 1024), np.float32)
    for core in range(8):
        b, j = core // 4, core % 4
        outp[b, TOK * j:TOK * (j + 1)] = res.results[core]["out"]
    return outp
```
